# Optimizing a Trainium2 kernel written in Bass

```python
import math
import jax, jax.numpy as jnp
from jax import lax
import numpy as np

D_MODEL = 2048
BATCH = 4
SEQ = 4096
DEPTH = 4

H_A = 8
DH_A = 64
DILATED_PATTERNS = ((128, 1), (512, 4), (2048, 16))
H_K_B = 4
H_V_B = 8
DK_B = 128
DV_B = 128
CONV_K = 4
DN_CHUNK = 64
H_C = 8
HKV_C = 2
GRP_C = H_C // HKV_C
DH_C = 64
SWA_WINDOW = 128
BLOCK = 128
A_W = H_A * DH_A
BK_W = H_K_B * DK_B
BV_W = H_V_B * DV_B
C_Q_W = H_C * DH_C
C_KV_W = HKV_C * DH_C
CONV_CH = 2 * BK_W + BV_W
MIX = A_W + BV_W + C_Q_W
N_IN = 3 * A_W + 2 * BK_W + 2 * BV_W + 2 * H_V_B + C_Q_W + 2 * C_KV_W
N_GROUPS = 4
EXPERTS_PER_GROUP = 8
N_EXPERTS = N_GROUPS * EXPERTS_PER_GROUP
TOP_K = 2
D_FF = 512
MOE_BLOCK = 128
EPS = 1e-6

kernel_name = 'hybrid_parallel_heads_hier_moe'


def rmsnorm(x, g):
    xf = x.astype(jnp.float32)
    y = xf * lax.rsqrt(jnp.mean(xf * xf, axis=-1, keepdims=True) + EPS)
    return y.astype(x.dtype) * g


def l2norm(x):
    return x * lax.rsqrt(jnp.sum(x * x, axis=-1, keepdims=True) + EPS)


def alibi_slopes(n):
    return 2.0 ** (-8.0 * jnp.arange(1, n + 1, dtype=jnp.float32) / n)


def banded_attention(q, k, v, max_dist, slopes, dist_scale, sinks=None):
    N, G, R, L, dh = q.shape
    nb = -(-L // BLOCK)
    pad = nb * BLOCK - L
    qb = jnp.pad(q, ((0, 0), (0, 0), (0, 0), (0, pad), (0, 0))).reshape(N, G, R, nb, BLOCK, dh)
    kp = jnp.pad(k, ((0, 0), (0, 0), (BLOCK, pad), (0, 0))).reshape(N, G, nb + 1, BLOCK, dh)
    vp = jnp.pad(v, ((0, 0), (0, 0), (BLOCK, pad), (0, 0))).reshape(N, G, nb + 1, BLOCK, dh)
    kb = jnp.concatenate([kp[:, :, :-1], kp[:, :, 1:]], axis=3)
    vb = jnp.concatenate([vp[:, :, :-1], vp[:, :, 1:]], axis=3)
    s = jnp.einsum('ngrbqd,ngbkd->ngrbqk', qb, kb, preferred_element_type=jnp.float32) * (dh ** -0.5)
    rel = (jnp.arange(BLOCK)[:, None] + BLOCK) - jnp.arange(2 * BLOCK)[None, :]
    kabs = jnp.arange(nb)[:, None] * BLOCK - BLOCK + jnp.arange(2 * BLOCK)[None, :]
    valid = ((rel >= 0) & (rel <= max_dist))[None] & (kabs >= 0)[:, None, :]
    bias = -(slopes.astype(jnp.float32) * dist_scale)[None, :, :, None, None, None] * rel.astype(jnp.float32)
    s = jnp.where(valid, s + bias, -jnp.inf)
    m = jnp.max(s, axis=-1, keepdims=True)
    if sinks is not None:
        sk = sinks.astype(jnp.float32)[None, :, :, None, None, None]
        m = jnp.maximum(m, sk)
    p = jnp.exp(s - m)
    denom = jnp.sum(p, axis=-1, keepdims=True)
    if sinks is not None:
        denom = denom + jnp.exp(sk - m)
    o = jnp.einsum('ngrbqk,ngbkd->ngrbqd', p, vb.astype(jnp.float32)) / denom
    lse = (m + jnp.log(denom))[..., 0]
    o = o.reshape(N, G, R, nb * BLOCK, dh)[:, :, :, :L].astype(v.dtype)
    lse = lse.reshape(N, G, R, nb * BLOCK)[..., :L]
    return o, lse


def dilated_branch(q, k, v, window, dilation, slopes):
    B, S, H, dh = q.shape
    Ld = S // dilation

    def split(t):
        return t.reshape(B, Ld, dilation, H, dh).transpose(0, 2, 3, 1, 4).reshape(B * dilation, H, Ld, dh)

    o, lse = banded_attention(split(q)[:, :, None], split(k), split(v), window // dilation,
                              slopes[:, None], float(dilation))
    o = o[:, :, 0].reshape(B, dilation, H, Ld, dh).transpose(0, 3, 1, 2, 4).reshape(B, S, H, dh)
    lse = lse[:, :, 0].reshape(B, dilation, H, Ld).transpose(0, 3, 1, 2).reshape(B, S, H)
    return o, lse


def causal_depthwise_conv(x, w):
    K, C = w.shape
    return lax.conv_general_dilated(x, w[:, None, :].astype(x.dtype), window_strides=(1,),
                                    padding=[(K - 1, 0)], dimension_numbers=('NWC', 'WIO', 'NWC'),
                                    feature_group_count=C)


def gated_delta_rule(q, k, v, g, beta):
    B, H, S, dk = q.shape
    dv = v.shape[-1]
    C = DN_CHUNK
    n = S // C
    q = q * (dk ** -0.5)

    def chunks(t):
        return t.reshape(B, H, n, C, *t.shape[3:])

    q, k, v, g, beta = chunks(q), chunks(k), chunks(v), chunks(g), chunks(beta)
    gc = jnp.cumsum(g, axis=-1)
    incl = jnp.tril(jnp.ones((C, C), bool))
    strict = jnp.tril(jnp.ones((C, C), bool), -1)
    decay = jnp.exp(jnp.where(incl, gc[..., :, None] - gc[..., None, :], -jnp.inf))
    kb = k * beta[..., None]
    lmat = jnp.where(strict, jnp.einsum('bhnid,bhnjd->bhnij', kb, k) * decay, 0.0)
    a_mat = lmat + jnp.eye(C, dtype=lmat.dtype)
    rhs = jnp.concatenate([v * beta[..., None], kb * jnp.exp(gc)[..., None]], axis=-1)
    sol = lax.linalg.triangular_solve(a_mat, rhs, left_side=True, lower=True, unit_diagonal=True)
    u, w = sol[..., :dv], sol[..., dv:]
    qk = jnp.where(incl, jnp.einsum('bhnid,bhnjd->bhnij', q, k) * decay, 0.0)
    q_dec = q * jnp.exp(gc)[..., None]
    k_dec = k * jnp.exp(gc[..., -1:] - gc)[..., None]
    g_last = jnp.exp(gc[..., -1])

    def step(state, xs):
        q_i, k_i, u_i, w_i, qk_i, gl_i = xs
        v_new = u_i - jnp.einsum('bhck,bhkv->bhcv', w_i, state)
        o = jnp.einsum('bhck,bhkv->bhcv', q_i, state) + jnp.einsum('bhcj,bhjv->bhcv', qk_i, v_new)
        state = state * gl_i[..., None, None] + jnp.einsum('bhck,bhcv->bhkv', k_i, v_new)
        return state, o

    xs = tuple(jnp.moveaxis(t, 2, 0) for t in (q_dec, k_dec, u, w, qk, g_last))
    _, o = lax.scan(step, jnp.zeros((B, H, dk, dv), jnp.float32), xs)
    return jnp.moveaxis(o, 0, 2).reshape(B, H, S, dv)


def gated_deltanet(bq, bk, bv, bz, bb, ba, conv_w, a_log, dt_bias, norm_g):
    B, S, _ = bq.shape
    qkv = jax.nn.silu(causal_depthwise_conv(jnp.concatenate([bq, bk, bv], axis=-1), conv_w)).astype(jnp.float32)
    q, k, v = jnp.split(qkv, [BK_W, 2 * BK_W], axis=-1)
    rep = H_V_B // H_K_B
    q = jnp.repeat(l2norm(q.reshape(B, S, H_K_B, DK_B)), rep, axis=2)
    k = jnp.repeat(l2norm(k.reshape(B, S, H_K_B, DK_B)), rep, axis=2)
    v = v.reshape(B, S, H_V_B, DV_B)
    beta = jax.nn.sigmoid(bb.astype(jnp.float32))
    g = -jnp.exp(a_log.astype(jnp.float32)) * jax.nn.softplus(ba.astype(jnp.float32) + dt_bias.astype(jnp.float32))
    o = gated_delta_rule(jnp.moveaxis(q, 2, 1), jnp.moveaxis(k, 2, 1), jnp.moveaxis(v, 2, 1),
                         jnp.moveaxis(g, 2, 1), jnp.moveaxis(beta, 2, 1))
    o = jnp.moveaxis(o, 1, 2)
    o = rmsnorm(o, norm_g.astype(jnp.float32)) * jax.nn.silu(bz.astype(jnp.float32).reshape(B, S, H_V_B, DV_B))
    return o.reshape(B, S, BV_W).astype(bq.dtype)


def hybrid_mixer(h, w_in, conv_w, a_log, dt_bias, dn_norm_g, sinks, w_out):
    B, S, _ = h.shape
    proj = h @ w_in
    sizes = [A_W, A_W, A_W, BK_W, BK_W, BV_W, BV_W, H_V_B, H_V_B, C_Q_W, C_KV_W, C_KV_W]
    cuts = [int(i) for i in np.cumsum(sizes)[:-1]]
    aq, ak, av, bq, bk, bv, bz, bb, ba, cq, ck, cv = jnp.split(proj, cuts, axis=-1)

    aq, ak, av = (t.reshape(B, S, H_A, DH_A) for t in (aq, ak, av))
    slopes_a = alibi_slopes(H_A)
    outs, lses = zip(*[dilated_branch(aq, ak, av, wdw, dil, slopes_a) for (wdw, dil) in DILATED_PATTERNS])
    wts = jax.nn.softmax(jnp.stack(lses, axis=0), axis=0)
    o_a = jnp.einsum('pbsh,pbshd->bshd', wts, jnp.stack(outs, axis=0).astype(jnp.float32))
    o_a = o_a.astype(h.dtype).reshape(B, S, A_W)

    o_b = gated_deltanet(bq, bk, bv, bz, bb, ba, conv_w, a_log, dt_bias, dn_norm_g)

    cq = cq.reshape(B, S, HKV_C, GRP_C, DH_C).transpose(0, 2, 3, 1, 4)
    ck = ck.reshape(B, S, HKV_C, DH_C).transpose(0, 2, 1, 3)
    cv = cv.reshape(B, S, HKV_C, DH_C).transpose(0, 2, 1, 3)
    o_c, _ = banded_attention(cq, ck, cv, SWA_WINDOW - 1, alibi_slopes(H_C).reshape(HKV_C, GRP_C), 1.0,
                              sinks.reshape(HKV_C, GRP_C))
    o_c = o_c.transpose(0, 3, 1, 2, 4).reshape(B, S, C_Q_W)

    return jnp.concatenate([o_a, o_b, o_c], axis=-1) @ w_out


def grouped_expert_mlp(xt, eidx, w_gate, w_up, w_down):
    T, K = eidx.shape
    E = w_gate.shape[0]
    A = T * K
    flat_e = eidx.reshape(A)
    order = jnp.argsort(flat_e)
    sorted_e = flat_e[order]
    counts = jnp.bincount(flat_e, length=E)
    starts = jnp.cumsum(counts) - counts
    padded = (counts + MOE_BLOCK - 1) // MOE_BLOCK * MOE_BLOCK
    pad_ends = jnp.cumsum(padded)
    pad_starts = pad_ends - padded
    dest = pad_starts[sorted_e] + jnp.arange(A) - starts[sorted_e]
    nblk = -(-A // MOE_BLOCK) + E
    rows = jnp.zeros((nblk * MOE_BLOCK, xt.shape[1]), xt.dtype).at[dest].set(xt[order // K])
    blk_expert = jnp.minimum(jnp.searchsorted(pad_ends, jnp.arange(nblk) * MOE_BLOCK, side='right'), E - 1)

    def expert_block(args):
        xb, e = args
        hid = jax.nn.silu(xb @ w_gate[e]) * (xb @ w_up[e])
        return hid @ w_down[e]

    yb = lax.map(expert_block, (rows.reshape(nblk, MOE_BLOCK, -1), blk_expert))
    y_sorted = yb.reshape(nblk * MOE_BLOCK, -1)[dest]
    y = jnp.zeros((A, y_sorted.shape[-1]), y_sorted.dtype).at[order].set(y_sorted)
    return y.reshape(T, K, -1)


def hierarchical_moe(h, rg_w, rg_b, re_w, re_b, w_gate, w_up, w_down):
    B, S, D = h.shape
    xt = h.reshape(-1, D)
    T = xt.shape[0]
    tok = jnp.arange(T)
    glog = (xt @ rg_w).astype(jnp.float32) + rg_b.astype(jnp.float32)
    gsel = jnp.argmax(glog, axis=-1)
    pg = jax.nn.softmax(glog, axis=-1)[tok, gsel]
    elog = ((xt @ re_w).astype(jnp.float32) + re_b.astype(jnp.float32)).reshape(T, N_GROUPS, EXPERTS_PER_GROUP)
    top_v, top_i = lax.top_k(elog[tok, gsel], TOP_K)
    gates = pg[:, None] * jax.nn.softmax(top_v, axis=-1)
    eidx = gsel[:, None] * EXPERTS_PER_GROUP + top_i
    y = grouped_expert_mlp(xt, eidx, w_gate, w_up, w_down)
    return jnp.einsum('tk,tkd->td', gates.astype(y.dtype), y).reshape(B, S, D)


def setup_inputs(seed: int = 0) -> dict:
    key = jax.random.key(seed)
    ks = jax.random.split(key, 21)
    L, D = DEPTH, D_MODEL

    def nrm(k, shape, scale):
        return jax.random.normal(k, shape, jnp.float32) * scale

    dt = jnp.exp(jax.random.uniform(ks[9], (L, H_V_B), jnp.float32, math.log(1e-3), math.log(1e-1)))
    return {
        'x': nrm(ks[0], (BATCH, SEQ, D), 1.0),
        'c': nrm(ks[1], (BATCH, D), 1.0),
        'norm1_g': 1.0 + nrm(ks[2], (L, D), 0.02),
        'norm2_g': 1.0 + nrm(ks[3], (L, D), 0.02),
        'ada_w': nrm(ks[4], (L, D, 6 * D), 0.5 * D ** -0.5),
        'ada_b': nrm(ks[5], (L, 6 * D), 0.02),
        'w_in': nrm(ks[6], (L, D, N_IN), D ** -0.5),
        'dn_conv_w': nrm(ks[7], (L, CONV_K, CONV_CH), CONV_K ** -0.5),
        'dn_a_log': jnp.log(jax.random.uniform(ks[8], (L, H_V_B), jnp.float32, 1.0, 16.0)),
        'dn_dt_bias': dt + jnp.log(-jnp.expm1(-dt)),
        'dn_norm_g': 1.0 + nrm(ks[10], (L, DV_B), 0.02),
        'attn_sinks': nrm(ks[11], (L, H_C), 1.0),
        'w_out': nrm(ks[12], (L, MIX, D), MIX ** -0.5),
        'router_group_w': nrm(ks[13], (L, D, N_GROUPS), D ** -0.5),
        'router_group_b': nrm(ks[14], (L, N_GROUPS), 0.01),
        'router_expert_w': nrm(ks[15], (L, D, N_EXPERTS), D ** -0.5),
        'router_expert_b': nrm(ks[16], (L, N_EXPERTS), 0.01),
        'expert_w_gate': nrm(ks[17], (L, N_EXPERTS, D, D_FF), D ** -0.5),
        'expert_w_up': nrm(ks[18], (L, N_EXPERTS, D, D_FF), D ** -0.5),
        'expert_w_down': nrm(ks[19], (L, N_EXPERTS, D_FF, D), D_FF ** -0.5),
        'final_norm_g': 1.0 + nrm(ks[20], (D,), 0.02),
    }


def reference(x, c, norm1_g, norm2_g, ada_w, ada_b, w_in, dn_conv_w, dn_a_log, dn_dt_bias, dn_norm_g,
              attn_sinks, w_out, router_group_w, router_group_b, router_expert_w, router_expert_b,
              expert_w_gate, expert_w_up, expert_w_down, final_norm_g):
    c_act = jax.nn.silu(c)
    for l in range(DEPTH):
        mod = (c_act @ ada_w[l] + ada_b[l])[:, None, :]
        sh1, sc1, g1, sh2, sc2, g2 = jnp.split(mod, 6, axis=-1)
        h = rmsnorm(x, norm1_g[l]) * (1.0 + sc1) + sh1
        x = x + g1 * hybrid_mixer(h, w_in[l], dn_conv_w[l], dn_a_log[l], dn_dt_bias[l], dn_norm_g[l],
                                  attn_sinks[l], w_out[l])
        h = rmsnorm(x, norm2_g[l]) * (1.0 + sc2) + sh2
        x = x + g2 * hierarchical_moe(h, router_group_w[l], router_group_b[l], router_expert_w[l],
                                      router_expert_b[l], expert_w_gate[l], expert_w_up[l], expert_w_down[l])
    return rmsnorm(x, final_norm_g)
```

```python
import numpy as np
import concourse.bass as bass
import concourse.mybir as mybir
from concourse.bass_utils import run_bass_kernel_spmd
from contextlib import ExitStack

F32 = mybir.dt.float32
BF16 = mybir.dt.bfloat16
I32 = mybir.dt.int32
U32 = mybir.dt.uint32
AF = mybir.ActivationFunctionType
ALU = mybir.AluOpType
AX = mybir.AxisListType
ds = bass.ds if hasattr(bass, "ds") else None

D = 2048
KC = D // 128
H_A, DH_A = 8, 64
PATTERNS = ((128, 1), (512, 4), (2048, 16))
H_K_B, H_V_B, DK_B, DV_B = 4, 8, 128, 128
CONV_K = 4
H_C, HKV_C, DH_C = 8, 2, 64
A_W = 512
BK_W = 512
BV_W = 1024
C_Q_W = 512
C_KV_W = 128
N_IN = 5392
MIX = 2048
N_EXP = 32
D_FF = 512
EPS = 1e-6
O_AQ, O_AK, O_AV = 0, 512, 1024
O_BQ, O_BK, O_BV, O_BZ = 1536, 2048, 2560, 3584
O_BB, O_BA = 4608, 4616
O_CQ, O_CK, O_CV = 4624, 5136, 5264
TP = 2048
NEG = -1.0e30

ENGS = ("pe", "act", "dve", "pool", "sp")
SAME_ENGINE_SYNC = True
N_DMA_SEMS = 16
SEM_EPOCH = 30000


def sl(c0, n, step=1):
    return slice(c0, c0 + (n - 1) * step + 1, step)


class Dep:
    __slots__ = ("w", "r", "name")

    def __init__(self, name=""):
        self.w = None
        self.r = {}
        self.name = name


class Sched:
    def __init__(self, nc, stack):
        self.nc = nc
        self.stack = stack
        self.eng_obj = {"pe": nc.tensor, "act": nc.scalar, "dve": nc.vector,
                        "pool": nc.gpsimd, "sp": nc.sync}
        self.sems = {}
        self.nsem = 0
        self.ekey = {}
        self.ecount = {}
        self.allkeys = {e: [] for e in ENGS}
        for e in ENGS:
            self._new_epoch(e)
        self.waited = {e: {} for e in ENGS}
        self.dma_keys = {}
        self.dma_val = {}
        self.dma_rr = {}
        for q in ("sp", "pool"):
            ks = []
            for i in range(N_DMA_SEMS):
                k = self._newsem(f"d_{q}_{i}")
                ks.append(k)
                self.dma_val[k] = 0
            self.dma_keys[q] = ks
            self.dma_rr[q] = 0
        self.n_ins = 0

    def _newsem(self, name):
        k = self.nsem
        self.nsem += 1
        self.sems[k] = self.stack.enter_context(self.nc.semaphore(f"s{k}_{name}"))
        return k

    def _new_epoch(self, e):
        self.ekey[e] = self._newsem(f"e_{e}")
        self.ecount[e] = 0
        self.allkeys[e].append(self.ekey[e])

    def dep(self, name=""):
        return Dep(name)

    def deps(self, n, name=""):
        return [Dep(f"{name}{i}") for i in range(n)]

    def _wait(self, eng, ev):
        if ev is None:
            return
        k, v = ev
        if (not SAME_ENGINE_SYNC or eng == "pe") and k == self.ekey.get(eng):
            return
        if self.waited[eng].get(k, 0) >= v:
            return
        self.waited[eng][k] = v
        self.eng_obj[eng].wait_ge(self.sems[k], v)

    def _collect(self, eng, reads, writes):
        for d in reads:
            self._wait(eng, d.w)
        for d in writes:
            self._wait(eng, d.w)
            for k, v in d.r.items():
                self._wait(eng, (k, v))

    def _update(self, ev, reads, writes):
        k, v = ev
        for d in reads:
            if d.r.get(k, 0) < v:
                d.r[k] = v
        for d in writes:
            d.w = ev
            d.r = {}

    def op(self, eng, fn, reads=(), writes=()):
        if self.ecount[eng] >= SEM_EPOCH:
            self._new_epoch(eng)
        self._collect(eng, reads, writes)
        k = self.ekey[eng]
        self.ecount[eng] += 1
        ev = (k, self.ecount[eng])
        fn(self.eng_obj[eng]).then_inc(self.sems[k], 1)
        self._update(ev, reads, writes)
        self.n_ins += 1
        return ev

    def dma(self, q, out, in_, reads=(), writes=(), fn=None, **kw):
        ks = self.dma_keys[q]
        k = ks[self.dma_rr[q] % len(ks)]
        self.dma_rr[q] += 1
        if self.dma_val[k] > 0:
            self._wait(q, (k, self.dma_val[k]))
        self._collect(q, reads, writes)
        self.dma_val[k] += 16
        ev = (k, self.dma_val[k])
        if fn is not None:
            ins = fn(self.eng_obj[q])
        else:
            ins = self.eng_obj[q].dma_start(out=out, in_=in_, **kw)
        ins.then_inc(self.sems[k], 16)
        self._update(ev, reads, writes)
        self.n_ins += 1
        return ev

    def barrier(self):
        evs = []
        for e in ENGS:
            if self.ecount[e] > 0:
                evs.append((self.ekey[e], self.ecount[e]))
        for k, v in self.dma_val.items():
            if v > 0:
                evs.append((k, v))
        for e in ENGS:
            for ev in evs:
                if ev[0] == self.ekey[e]:
                    continue
                self._wait(e, ev)

    def finish(self, final_events=()):
        for ev in final_events:
            self._wait("sp", ev)


class Builder:
    def __init__(self, nc, T, L, n_cores=1, dbg=(), phases=("ada", "norm1", "proj"), NB=1, NSEG=1):
        self.nc = nc
        self.T = T
        self.L = L
        self.NB = NB
        self.NSEG = NSEG
        self.c0 = TP
        self.r0 = 0
        self.bi = 0
        self.seg_first = True
        self.NT = T // 128
        self.n_cores = n_cores
        self.dbg = set(dbg)
        self.phases = phases
        self.st = ExitStack()
        self.S = Sched(nc, self.st)
        self.out_events = []
        self.dn_stop = 99
        self.dn_sub = 99
        self.dn_fast = False

    def sb(self, stack, name, shape, dt):
        self._uid = getattr(self, "_uid", 0) + 1
        return stack.enter_context(self.nc.sbuf_tensor(f"{name}_u{self._uid}", list(shape), dt))

    def dram(self, name, shape, dt, kind=None):
        if kind is None:
            kind = "ExternalOutput" if name in self.dbg else "Internal"
        return self.nc.dram_tensor(name, list(shape), dt, kind=kind)

    def inp(self, name, shape, dt=F32):
        return self.nc.dram_tensor(name, list(shape), dt, kind="ExternalInput").ap()

    def declare(self):
        T, L = self.T, self.L
        I = {}
        NTOK = self.NB * self.NSEG * T
        I["x"] = self.inp("x", [NTOK, D])
        I["cT"] = self.inp("cT", [self.NB, 128, KC])
        I["norm1_g"] = self.inp("norm1_g", [L, D])
        I["norm2_g"] = self.inp("norm2_g", [L, D])
        I["ada_w"] = self.inp("ada_w", [L, D, 6 * D])
        I["ada_b"] = self.inp("ada_b", [L, 6 * D])
        I["w_in"] = self.inp("w_in", [L, D, N_IN])
        I["conv_w"] = self.inp("conv_w", [128, L, 16, CONV_K])
        I["a_log"] = self.inp("a_log", [1, L * 8])
        I["dt_bias"] = self.inp("dt_bias", [1, L * 8])
        I["dn_norm_g"] = self.inp("dn_norm_g", [L, 128, 1])
        I["sinks"] = self.inp("sinks", [1, L * 8])
        if "outproj" in self.phases:
            I["w_out"] = self.inp("w_out", [L, MIX, D])
        I["rw"] = self.inp("rw", [L, D, 36])
        I["rb"] = self.inp("rb", [L, 36])
        if "moe" in self.phases:
            I["w_gate"] = self.inp("w_gate", [L, N_EXP, D, D_FF])
            I["w_up"] = self.inp("w_up", [L, N_EXP, D, D_FF])
            I["w_down"] = self.inp("w_down", [L, N_EXP, D_FF, D])
        I["final_g"] = self.inp("final_g", [1, D])
        self.I = I
        self.y_out = self.nc.dram_tensor("y_out", [NTOK, D], F32, kind="ExternalOutput").ap()
        self.x_d = self.dram("x_d", [NTOK, D], F32).ap()
        self.mod_d = self.dram("mod_d", [self.NB, L, 6, 128, D], F32).ap()
        self.P_d = self.dram("P_d", [N_IN, TP + self.NSEG * T], F32).ap()
        self.NBLK = (2 * T) // 128 + N_EXP
        self.h2_d = self.dram("h2_d", [T, D], BF16).ap()
        self.rows_d = self.dram("rows_d", [self.NBLK * 128, D], BF16).ap()
        self.yrows_d = self.dram("yrows_d", [self.NBLK * 128, D], F32).ap()

    def consts(self):
        S, st = self.S, self.st
        self.reg_neg = self.nc.gpsimd.to_reg(NEG)
        self.reg_zero = self.nc.gpsimd.to_reg(0.0)
        self.ident = self.sb(st, "ident", [128, 128], F32)
        self.d_const = S.dep("const")
        d = self.d_const
        S.op("pool", lambda e: e.memset(self.ident[:], 1.0), writes=[d])
        S.op("pool", lambda e: e.affine_select(out=self.ident[:], in_=self.ident[:], pattern=[[-1, 128]],
                                                compare_op=ALU.is_equal, fill=self.reg_zero, base=0, channel_multiplier=1),
             reads=[d], writes=[d])
        self.ones = self.sb(st, "ones", [128, 128], F32)
        S.op("pool", lambda e: e.memset(self.ones[:], 1.0), writes=[d])
        self.epsc = self.sb(st, "epsc", [128, 1], F32)
        S.op("pool", lambda e: e.memset(self.epsc[:], EPS), writes=[d])
        self.zeros = self.sb(st, "zeros", [128, 2048], BF16)
        S.op("pool", lambda e: e.memset(self.zeros[:], 0.0), writes=[d])
        self.SCARRY = self.sb(st, "SCARRY", [128, 8, 128], F32)
        self.d_carry = S.dep("carry")
        self.d_act = S.deps(self.NT, "act")
        self.d_xt = S.deps(self.NT, "xt")
        self.d_h2 = S.dep("h2_d")
        self.d_rows = S.dep("rows_d")
        self.d_yrows = S.dep("yrows_d")
        self.ps = [st.enter_context(self.nc.psum_tensor(f"ps{i}", [128, 512], F32)) for i in range(8)]
        self.d_ps = S.deps(8, "ps")
        self.d_x = S.dep("x_d")
        self.d_mod = S.dep("mod_d")
        self.d_P = S.dep("P_d")

    def phase_init(self):
        S = self.S
        T = self.T
        S.dma("sp", self.x_d, self.I["x"], writes=[self.d_x])
        r = 0
        while r < N_IN:
            n = min(128, N_IN - r)
            S.dma("pool", self.P_d[r:r + n, 0:TP], self.zeros[:n, 0:TP], reads=[self.d_const], writes=[self.d_P])
            r += n
        for bb_ in range(self.NBLK):
            S.dma("sp", self.rows_d[bb_ * 128:(bb_ + 1) * 128, :], self.zeros[:, :], reads=[self.d_const], writes=[self.d_rows])
        S.barrier()

    def phase_ada(self):
        S, nc, I = self.S, self.nc, self.I
        with ExitStack() as ph:
            cT = self.sb(ph, "cT", [128, KC], F32)
            cact = self.sb(ph, "cact", [128, KC], F32)
            CB = self.sb(ph, "CB", [128, KC, 128], F32)
            d_c = S.dep()
            S.dma("sp", cT[:], I["cT"][self.bi], writes=[d_c])
            S.op("act", lambda e: e.activation(out=cact[:], in_=cT[:], func=AF.Silu), reads=[d_c], writes=[d_c])
            S.op("dve", lambda e: e.tensor_copy(out=CB[:], in_=cact[:].unsqueeze(2).broadcast_to([128, KC, 128])),
                 reads=[d_c], writes=[d_c])
            NB = 2
            W = [self.sb(ph, f"adaW{i}", [128, KC, 512], F32) for i in range(NB)]
            dW = S.deps(NB)
            bb = [self.sb(ph, f"adab{i}", [128, 512], F32) for i in range(NB)]
            gg = [self.sb(ph, f"adag{i}", [128, 512], F32) for i in range(NB)]
            dB = S.deps(NB)
            ot = [self.sb(ph, f"adao{i}", [128, 512], F32) for i in range(NB)]
            dO = S.deps(NB)
            it = 0
            for l in range(self.L):
                wv = I["ada_w"][l].rearrange("(kc p) n -> p kc n", p=128)
                for cg in range(24):
                    b = it % NB
                    it += 1
                    part, sub = cg // 4, cg % 4
                    cs = slice(cg * 512, (cg + 1) * 512)
                    fs = slice(sub * 512, (sub + 1) * 512)
                    S.dma("sp", W[b][:], wv[:, :, cs], writes=[dW[b]])
                    S.dma("sp", bb[b][:], I["ada_b"][l:l + 1, cs].partition_broadcast(128), writes=[dB[b]])
                    is_sc = part in (1, 4)
                    if is_sc:
                        gsrc = I["norm1_g"] if part == 1 else I["norm2_g"]
                        S.dma("sp", gg[b][:], gsrc[l:l + 1, fs].partition_broadcast(128), writes=[dB[b]])
                    pb = b
                    for kc in range(KC):
                        S.op("pe", lambda e, kc=kc, b=b, pb=pb: e.matmul(self.ps[pb][:], lhsT=CB[:, kc, :], rhs=W[b][:, kc, :],
                                                                           start=(kc == 0), stop=(kc == KC - 1)),
                             reads=[d_c, dW[b]], writes=[self.d_ps[pb]])
                    S.op("dve", lambda e, b=b, pb=pb: e.tensor_tensor(out=ot[b][:], in0=self.ps[pb][:], in1=bb[b][:], op=ALU.add),
                         reads=[self.d_ps[pb], dB[b]], writes=[dO[b]])
                    if is_sc:
                        S.op("dve", lambda e, b=b: e.scalar_tensor_tensor(out=ot[b][:], in0=ot[b][:], scalar=1.0, in1=gg[b][:],
                                                                           op0=ALU.add, op1=ALU.mult),
                             reads=[dB[b], dO[b]], writes=[dO[b]])
                    S.dma("sp", self.mod_d[self.bi, l, part, :, fs], ot[b][:], reads=[dO[b]], writes=[self.d_mod])
        S.barrier()

    def phase_norm(self, l, which, router=None):
        S, nc, I = self.S, self.nc, self.I
        T, NT = self.T, self.NT
        with ExitStack() as ph:
            Abc = self.sb(ph, "Abc", [128, D], F32)
            Bbc = self.sb(ph, "Bbc", [128, D], F32)
            d_ab = S.dep()
            if which == "final":
                S.dma("sp", Abc[:], I["final_g"][0:1, :].partition_broadcast(128), writes=[d_ab])
            else:
                pa, pb_ = (1, 0) if which == 1 else (4, 3)
                S.dma("sp", Abc[:], self.mod_d[self.bi, l, pa], reads=[self.d_mod], writes=[d_ab])
                S.dma("sp", Bbc[:], self.mod_d[self.bi, l, pb_], reads=[self.d_mod], writes=[d_ab])
            NB = 2
            xt = [self.sb(ph, f"xt{i}", [128, D], F32) for i in range(NB)]
            dx = S.deps(NB)
            hf = [self.sb(ph, f"hf{i}", [128, D], F32) for i in range(NB)]
            dh = S.deps(NB)
            junk = self.sb(ph, "junk", [128, D], BF16)
            d_junk = S.dep()
            st_ = [self.sb(ph, f"nst{i}", [128, 4], F32) for i in range(NB)]
            dst_ = S.deps(NB)
            if router is not None:
                hT32 = [self.sb(ph, f"hT32_{i}", [128, KC, 128], F32) for i in range(NB)]
                dhT = S.deps(NB)
            for i in range(NT):
                b = i % NB
                rows = slice(self.r0 + i * 128, self.r0 + (i + 1) * 128)
                S.dma("sp", xt[b][:], self.x_d[rows, :], reads=[self.d_x, self.d_xt[i]], writes=[dx[b]])
                S.op("act", lambda e, b=b: e.activation(out=junk[:], in_=xt[b][:], func=AF.Square, accum_out=st_[b][:, 0:1]),
                     reads=[dx[b]], writes=[d_junk, dst_[b]])
                S.op("act", lambda e, b=b: e.activation(out=st_[b][:, 1:2], in_=st_[b][:, 0:1], func=AF.Sqrt,
                                                        scale=1.0 / D, bias=self.epsc[:, 0:1]),
                     reads=[dst_[b], self.d_const], writes=[dst_[b]])
                S.op("dve", lambda e, b=b: e.reciprocal(out=st_[b][:, 2:3], in_=st_[b][:, 1:2]),
                     reads=[dst_[b]], writes=[dst_[b]])
                S.op("dve", lambda e, b=b: e.scalar_tensor_tensor(out=hf[b][:], in0=xt[b][:], scalar=st_[b][:, 2:3], in1=Abc[:],
                                                                   op0=ALU.mult, op1=ALU.mult),
                     reads=[dx[b], dst_[b], d_ab], writes=[dh[b]])
                if which == "final":
                    self.out_events.append(S.dma("sp", self.y_out[rows, :], hf[b][:], reads=[dh[b]]))
                    continue
                S.op("pool", lambda e, b=b: e.tensor_tensor(out=hf[b][:], in0=hf[b][:], in1=Bbc[:], op=ALU.add),
                     reads=[d_ab, dh[b]], writes=[dh[b]])
                if router is not None:
                    S.op("pool", lambda e, b=b: e.tensor_copy(out=router["hbf"][b][:], in_=hf[b][:]), reads=[dh[b]], writes=[router["d_hbf"][b]])
                    S.dma("sp", self.h2_d[i * 128:(i + 1) * 128, :], router["hbf"][b][:], reads=[router["d_hbf"][b]], writes=[self.d_h2])
                for q4 in range(4):
                    pb = (i * 4 + q4) % 4
                    for j in range(4):
                        kc = q4 * 4 + j
                        S.op("pe", lambda e, b=b, kc=kc, pb=pb, j=j: e.transpose(self.ps[pb][:, j * 128:(j + 1) * 128],
                                                                                   hf[b][:, kc * 128:(kc + 1) * 128], self.ident[:]),
                             reads=[dh[b], self.d_const], writes=[self.d_ps[pb]])
                    if router is None:
                        S.op("act", lambda e, pb=pb, q4=q4, i=i: e.activation(
                            out=self.actT[:, q4 * 4:(q4 + 1) * 4, i * 128:(i + 1) * 128],
                            in_=self.ps[pb][:].rearrange("p (j n) -> p j n", j=4), func=AF.Copy),
                            reads=[self.d_ps[pb]], writes=[self.d_act[i]])
                    if router is not None:
                        S.op("dve", lambda e, pb=pb, q4=q4, b=b: e.tensor_copy(
                            out=hT32[b][:, q4 * 4:(q4 + 1) * 4, :],
                            in_=self.ps[pb][:].rearrange("p (j n) -> p j n", j=4)),
                            reads=[self.d_ps[pb]], writes=[dhT[b]])
                if router is not None:
                    pr = 4 + (i % 2)
                    for kc in range(KC):
                        S.op("pe", lambda e, b=b, kc=kc, pr=pr: e.matmul(self.ps[pr][:, 0:36], lhsT=hT32[b][:, kc, :],
                                                                          rhs=router["RW"][:, kc, :],
                                                                          start=(kc == 0), stop=(kc == KC - 1)),
                             reads=[dhT[b], router["d_rw"]], writes=[self.d_ps[pr]])
                    S.op("dve", lambda e, pr=pr, i=i: e.tensor_tensor(out=router["LG"][:, i, :], in0=self.ps[pr][:, 0:36],
                                                                       in1=router["RB"][:], op=ALU.add),
                         reads=[self.d_ps[pr], router["d_rw"]], writes=[router["d_lg"]])
        S.barrier()

    def phase_proj(self, l):
        S, nc, I = self.S, self.nc, self.I
        T = self.T
        NTT = T // 512
        wv = I["w_in"][l].rearrange("(kc p) n -> p kc n", p=128)
        with ExitStack() as ph:
            NB = 2
            W = [self.sb(ph, f"pjW{i}", [128, KC, 512], BF16) for i in range(NB)]
            dW = S.deps(NB)
            stg = [self.sb(ph, f"pjS{i}", [128, T], F32) for i in range(NB)]
            dS_ = S.deps(NB)
            nsg = (N_IN + 511) // 512
            cnt = 0
            for sg in range(nsg):
                c0 = sg * 512
                cw = min(512, N_IN - c0)
                b = sg % NB
                S.dma("pool", W[b][:, :, 0:cw], wv[:, :, c0:c0 + cw], writes=[dW[b]])
                for j in range((cw + 127) // 128):
                    gs = min(128, cw - j * 128)
                    sb_ = cnt % NB
                    for tt in range(NTT):
                        pb = cnt % 8
                        cnt += 1
                        for kc in range(KC):
                            S.op("pe", lambda e, b=b, kc=kc, j=j, gs=gs, tt=tt, pb=pb: e.matmul(
                                self.ps[pb][0:gs, :], lhsT=W[b][:, kc, j * 128:j * 128 + gs],
                                rhs=self.actT[:, kc, tt * 512:(tt + 1) * 512], start=(kc == 0), stop=(kc == KC - 1)),
                                reads=[dW[b]] + self.d_act[tt * 4:(tt + 1) * 4], writes=[self.d_ps[pb]])
                        eng = "act" if (cnt % 2 == 0) else "dve"
                        if eng == "act":
                            S.op("act", lambda e, sb_=sb_, gs=gs, tt=tt, pb=pb: e.activation(
                                out=stg[sb_][0:gs, tt * 512:(tt + 1) * 512], in_=self.ps[pb][0:gs, :], func=AF.Copy),
                                reads=[self.d_ps[pb]], writes=[dS_[sb_]])
                        else:
                            S.op("dve", lambda e, sb_=sb_, gs=gs, tt=tt, pb=pb: e.tensor_copy(
                                out=stg[sb_][0:gs, tt * 512:(tt + 1) * 512], in_=self.ps[pb][0:gs, :]),
                                reads=[self.d_ps[pb]], writes=[dS_[sb_]])
                    r0 = c0 + j * 128
                    S.dma("sp", self.P_d[r0:r0 + gs, self.c0:self.c0 + T], stg[sb_][0:gs, :], reads=[dS_[sb_]], writes=[self.d_P])
        S.barrier()

    def attn_consts(self, ph):
        S = self.S
        d = S.dep("attnc")
        self.d_attnc = d
        reli = self.sb(ph, "reli", [128, 256], I32)
        self.REL = self.sb(ph, "REL", [128, 256], F32)
        S.op("pool", lambda e: e.iota(reli[:], pattern=[[-1, 256]], base=128, channel_multiplier=1), writes=[d])
        S.op("pool", lambda e: e.tensor_copy(out=self.REL[:], in_=reli[:]), reads=[d], writes=[d])
        self.HM = self.sb(ph, "HM", [128, 256], F32)
        S.op("pool", lambda e: e.memset(self.HM[:], 0.0), writes=[d])
        S.op("pool", lambda e: e.memset(self.HM[:, 0:128], NEG), reads=[d], writes=[d])
        self.E2 = self.sb(ph, "E2", [2, 128], F32)
        S.op("pool", lambda e: e.memset(self.E2[:], 1.0), writes=[d])
        S.op("pool", lambda e: e.affine_select(out=self.E2[:], in_=self.E2[:], pattern=[[1, 128]], compare_op=ALU.is_ge,
                                                fill=self.reg_zero, base=0, channel_multiplier=-64), reads=[d], writes=[d])
        S.op("pool", lambda e: e.affine_select(out=self.E2[:], in_=self.E2[:], pattern=[[-1, 128]], compare_op=ALU.is_ge,
                                                fill=self.reg_zero, base=63, channel_multiplier=64), reads=[d], writes=[d])

    def make_bias(self, tile, dep, coef, maxd):
        S = self.S
        S.op("dve", lambda e: e.tensor_scalar_mul(out=tile[:], in0=self.REL[:], scalar1=float(coef)),
             reads=[self.d_attnc], writes=[dep])
        S.op("pool", lambda e: e.affine_select(out=tile[:], in_=tile[:], pattern=[[-1, 256]], compare_op=ALU.is_ge,
                                                fill=self.reg_neg, base=128, channel_multiplier=1), reads=[dep], writes=[dep])
        S.op("pool", lambda e: e.affine_select(out=tile[:], in_=tile[:], pattern=[[1, 256]], compare_op=ALU.is_ge,
                                                fill=self.reg_neg, base=maxd - 128, channel_multiplier=-1), reads=[dep], writes=[dep])

    def attn_unit(self, ctx, q_ap, k_ap, bias, d_bias, first, Vb0, Vb1, d_V, half, out_ap, out_deps,
                  sink_ap=None, lse_ap=None, d_lse=None, in_deps=()):
        S = self.S
        u = ctx["u"]
        ctx["u"] += 1
        b = u % 2
        pS, dS = self.ps[b], self.d_ps[b]
        pT, dT = ctx["psT"][b], self.d_ps[2 + b]
        pO, dO = self.ps[4 + b], self.d_ps[4 + b]
        s32, d_s = ctx["s32"][b], ctx["d_s32"][b]
        pbf, d_p = ctx["pbf"][b], ctx["d_pbf"][b]
        pTs, d_pT = ctx["pTs"][b], ctx["d_pTs"][b]
        stt, d_st = ctx["stt"][b], ctx["d_stt"][b]
        S.op("pe", lambda e: e.matmul(pS[:, 0:256], lhsT=q_ap, rhs=k_ap, start=True, stop=True),
             reads=list(in_deps), writes=[dS])
        S.op("dve", lambda e: e.scalar_tensor_tensor(out=s32[:], in0=pS[:, 0:256], scalar=0.125, in1=bias[:],
                                                      op0=ALU.mult, op1=ALU.add), reads=[dS, d_bias], writes=[d_s])
        if first:
            S.op("pool", lambda e: e.tensor_tensor(out=s32[:], in0=s32[:], in1=self.HM[:], op=ALU.add),
                 reads=[d_s, self.d_attnc], writes=[d_s])
        S.op("dve", lambda e: e.tensor_reduce(out=stt[:, 0:1], in_=s32[:], axis=AX.X, op=ALU.max, negate=True),
             reads=[d_s], writes=[d_st])
        if sink_ap is not None:
            S.op("dve", lambda e: e.scalar_tensor_tensor(out=stt[:, 0:1], in0=sink_ap, scalar=-1.0, in1=stt[:, 0:1],
                                                          op0=ALU.mult, op1=ALU.min), reads=[d_st, ctx["d_sink"]], writes=[d_st])
        S.op("act", lambda e: e.activation(out=pbf[:], in_=s32[:], func=AF.Exp, bias=stt[:, 0:1], scale=1.0,
                                            accum_out=stt[:, 1:2]), reads=[d_s, d_st], writes=[d_p, d_st])
        if sink_ap is not None:
            S.op("act", lambda e: e.activation(out=stt[:, 3:4], in_=sink_ap, func=AF.Exp, bias=stt[:, 0:1], scale=1.0),
                 reads=[d_st, ctx["d_sink"]], writes=[d_st])
            S.op("dve", lambda e: e.tensor_tensor(out=stt[:, 1:2], in0=stt[:, 1:2], in1=stt[:, 3:4], op=ALU.add),
                 reads=[d_st], writes=[d_st])
        S.op("dve", lambda e: e.reciprocal(out=stt[:, 2:3], in_=stt[:, 1:2]), reads=[d_st], writes=[d_st])
        S.op("pool", lambda e: e.tensor_scalar_mul(out=pbf[:], in0=pbf[:], scalar1=stt[:, 2:3]),
             reads=[d_p, d_st], writes=[d_p])
        if lse_ap is not None:
            S.op("act", lambda e: e.activation(out=stt[:, 3:4], in_=stt[:, 1:2], func=AF.Ln), reads=[d_st], writes=[d_st])
            S.op("dve", lambda e: e.tensor_tensor(out=lse_ap, in0=stt[:, 3:4], in1=stt[:, 0:1], op=ALU.subtract),
                 reads=[d_st], writes=[d_lse])
        for j in range(2):
            S.op("pe", lambda e, j=j: e.transpose(pT[:, j * 128:(j + 1) * 128], pbf[:, j * 128:(j + 1) * 128], ctx["identb"][:]),
                 reads=[d_p, ctx["d_identb"]], writes=[dT])
        S.op("act", lambda e: e.activation(out=pTs[:], in_=pT[:, 0:256], func=AF.Copy), reads=[dT], writes=[d_pT])
        S.op("pe", lambda e: e.matmul(pO[:, 0:128], lhsT=Vb0, rhs=pTs[:, 0:128], start=True, stop=False),
             reads=[d_V, d_pT], writes=[dO])
        S.op("pe", lambda e: e.matmul(pO[:, 0:128], lhsT=Vb1, rhs=pTs[:, 128:256], start=False, stop=True),
             reads=[d_V, d_pT], writes=[dO])
        rs = slice(half * 64, half * 64 + 64)
        S.op("dve", lambda e: e.tensor_copy(out=out_ap, in_=pO[rs, 0:128]), reads=[dO], writes=list(out_deps))

    def attn_ctx(self, ph):
        S = self.S
        ctx = {"u": 0}
        ctx["psT"] = [self.ps[2].bitcast(BF16), self.ps[3].bitcast(BF16)]
        ctx["s32"] = [self.sb(ph, f"s32_{i}", [128, 256], F32) for i in range(2)]
        ctx["d_s32"] = S.deps(2)
        ctx["pbf"] = [self.sb(ph, f"pbf_{i}", [128, 256], BF16) for i in range(2)]
        ctx["d_pbf"] = S.deps(2)
        ctx["pTs"] = [self.sb(ph, f"pTs_{i}", [128, 256], BF16) for i in range(2)]
        ctx["d_pTs"] = S.deps(2)
        ctx["stt"] = [self.sb(ph, f"stt_{i}", [128, 8], F32) for i in range(2)]
        ctx["d_stt"] = S.deps(2)
        identb = self.sb(ph, "identb", [128, 128], BF16)
        ctx["identb"] = identb
        ctx["d_identb"] = S.dep()
        S.op("dve", lambda e: e.tensor_copy(out=identb[:], in_=self.ident[:]), reads=[self.d_const], writes=[ctx["d_identb"]])
        return ctx

    def build_vblocks(self, ctx, vT, d_vT, Vblk, d_Vblk, specs):
        S = self.S
        for n, (idx, c0, step) in enumerate(specs):
            b = n % 2
            pT, dT = ctx["psT"][b], self.d_ps[2 + b]
            src = vT[:, sl(c0, 128, step)]
            S.op("pe", lambda e, pT=pT, src=src: e.transpose(pT[:, 0:128], src, ctx["identb"][:]),
                 reads=[d_vT, ctx["d_identb"]], writes=[dT])
            eng = "act" if n % 2 == 0 else "dve"
            if eng == "act":
                S.op("act", lambda e, pT=pT, idx=idx: e.activation(out=Vblk[:, idx, :], in_=pT[:, 0:128], func=AF.Copy),
                     reads=[dT], writes=[d_Vblk])
            else:
                S.op("dve", lambda e, pT=pT, idx=idx: e.tensor_copy(out=Vblk[:, idx, :], in_=pT[:, 0:128]),
                     reads=[dT], writes=[d_Vblk])

    def phase_attn_c(self, l):
        S, I = self.S, self.I
        T, NT = self.T, self.NT
        with ExitStack() as ph:
            self.attn_consts(ph)
            ctx = self.attn_ctx(ph)
            sink = self.sb(ph, "sink", [128, 8], F32)
            ctx["d_sink"] = S.dep()
            S.dma("sp", sink[:], I["sinks"][0:1, l * 8:(l + 1) * 8].partition_broadcast(128), writes=[ctx["d_sink"]])
            qc = self.sb(ph, "qc", [128, 4, T], BF16)
            kc = self.sb(ph, "kc", [128, TP + T], BF16)
            vN = self.sb(ph, "vN", [128, TP + T], BF16)
            vS = self.sb(ph, "vS", [128, TP + T], BF16)
            d_q, d_k, d_vn, d_vs = S.deps(4)
            for g in range(2):
                S.dma("pool", qc[64 * g:64 * g + 64, :, :],
                      self.P_d[O_CQ + 256 * g:O_CQ + 256 * (g + 1), self.c0:self.c0 + T].rearrange("(j p) t -> p j t", p=64),
                      reads=[self.d_P], writes=[d_q])
            hs = slice(self.c0 - TP, self.c0 + T)
            S.dma("pool", kc[:], self.P_d[O_CK:O_CK + 128, hs], reads=[self.d_P], writes=[d_k])
            S.dma("pool", vN[:], self.P_d[O_CV:O_CV + 128, hs], reads=[self.d_P], writes=[d_vn])
            S.dma("pool", vS[0:64, :], self.P_d[O_CV + 64:O_CV + 128, hs], reads=[self.d_P], writes=[d_vs])
            S.dma("pool", vS[64:128, :], self.P_d[O_CV:O_CV + 64, hs], reads=[self.d_P], writes=[d_vs])
            VN = self.sb(ph, "VN", [128, NT + 1, 128], BF16)
            VS = self.sb(ph, "VS", [128, NT + 1, 128], BF16)
            d_VN, d_VS = S.deps(2)
            specs = [(j + 1, TP + 128 * j, 1) for j in range(-1, NT)]
            self.build_vblocks(ctx, vN, d_vn, VN, d_VN, specs)
            self.build_vblocks(ctx, vS, d_vs, VS, d_VS, specs)
            bias = [self.sb(ph, f"biasc{h}", [128, 256], F32) for h in range(8)]
            d_b = S.deps(8)
            for h in range(8):
                self.make_bias(bias[h], d_b[h], -(2.0 ** (-(h + 1))), 127)
            for h in range(8):
                g, hh = h // 4, h % 2
                Vb, dV = (VN, d_VN) if hh == g else (VS, d_VS)
                for j in range(NT):
                    self.attn_unit(ctx,
                                   q_ap=qc[64 * g:64 * g + 64, h % 4, 128 * j:128 * (j + 1)],
                                   k_ap=kc[64 * g:64 * g + 64, TP + 128 * (j - 1):TP + 128 * (j + 1)],
                                   bias=bias[h], d_bias=d_b[h], first=(j == 0 and self.seg_first),
                                   Vb0=Vb[:, j, :], Vb1=Vb[:, j + 1, :], d_V=dV, half=hh,
                                   out_ap=self.actT[64 * hh:64 * hh + 64, 12 + h // 2, 128 * j:128 * (j + 1)],
                                   out_deps=[self.d_act[j]], sink_ap=sink[:, h:h + 1], in_deps=[d_q, d_k])
        S.barrier()

    def phase_attn_a(self, l):
        S, I = self.S, self.I
        T, NT = self.T, self.NT
        with ExitStack() as ph:
            self.attn_consts(ph)
            ctx = self.attn_ctx(ph)
            qa = self.sb(ph, "qa", [128, T], BF16)
            ka = self.sb(ph, "ka", [128, TP + T], BF16)
            va = self.sb(ph, "va", [128, TP + T], BF16)
            d_q, d_k, d_v = S.deps(3)
            maxblk = max(d * (T // (128 * d) + 1) for _, d in PATTERNS)
            Vblk = self.sb(ph, "Vblk", [128, maxblk, 128], BF16)
            d_Vb = S.dep()
            opT = [self.sb(ph, f"opT{p}", [128, T], BF16) for p in range(3)]
            d_op = S.deps(3)
            STAT = [self.sb(ph, f"STAT{p}", [128, NT * 2], F32) for p in range(3)]
            d_stat = S.deps(3)
            R = self.sb(ph, "R", [2, 3, T], F32)
            d_R = S.dep()
            Mx = self.sb(ph, "Mx", [2, T], F32)
            d_M = S.dep()
            bias = [self.sb(ph, f"biasa{i}", [128, 256], F32) for i in range(6)]
            d_b = S.deps(6)
            acc = self.sb(ph, "acca", [128, 512], F32)
            d_acc = S.dep()
            tmp = self.sb(ph, "tmpa", [128, 512], F32)
            d_tmp = S.dep()
            for ch in range(4):
                hs = slice(self.c0 - TP, self.c0 + T)
                S.dma("pool", qa[:], self.P_d[O_AQ + 128 * ch:O_AQ + 128 * (ch + 1), self.c0:self.c0 + T], reads=[self.d_P], writes=[d_q])
                S.dma("pool", ka[:], self.P_d[O_AK + 128 * ch:O_AK + 128 * (ch + 1), hs], reads=[self.d_P], writes=[d_k])
                S.dma("pool", va[:], self.P_d[O_AV + 128 * ch:O_AV + 128 * (ch + 1), hs], reads=[self.d_P], writes=[d_v])
                for p, (w, d) in enumerate(PATTERNS):
                    for hh in range(2):
                        h = 2 * ch + hh
                        self.make_bias(bias[p * 2 + hh], d_b[p * 2 + hh], -(2.0 ** (-(h + 1))) * d, 128)
                for p, (w, d) in enumerate(PATTERNS):
                    nbq = T // (128 * d)
                    specs = []
                    for r in range(d):
                        for j in range(-1, nbq):
                            specs.append((r * (nbq + 1) + j + 1, TP + r + d * 128 * j, d))
                    self.build_vblocks(ctx, va, d_v, Vblk, d_Vb, specs)
                    for hh in range(2):
                        ps_ = slice(64 * hh, 64 * hh + 64)
                        for r in range(d):
                            for j in range(nbq):
                                blk = r * nbq + j
                                q0 = r + d * 128 * j
                                k0 = TP + r + d * 128 * (j - 1)
                                q_ap = qa[ps_, sl(q0, 128, d)]
                                k_ap = ka[ps_, sl(k0, 256, d)]
                                o_ap = opT[p][ps_, sl(q0, 128, d)]
                                vi = r * (nbq + 1) + j
                                self.attn_unit(ctx, q_ap=q_ap, k_ap=k_ap, bias=bias[p * 2 + hh], d_bias=d_b[p * 2 + hh],
                                               first=(j == 0 and self.seg_first), Vb0=Vblk[:, vi, :], Vb1=Vblk[:, vi + 1, :], d_V=d_Vb, half=hh,
                                               out_ap=o_ap, out_deps=[d_op[p]],
                                               lse_ap=STAT[p][:, blk * 2 + hh:blk * 2 + hh + 1], d_lse=d_stat[p],
                                               in_deps=[d_q, d_k])
                    for r in range(d):
                        for j in range(nbq):
                            blk = r * nbq + j
                            q0 = r + d * 128 * j
                            pb = 6 + (blk % 2)
                            S.op("pe", lambda e, pb=pb, p=p, blk=blk: e.transpose(self.ps[pb][0:2, 0:128], STAT[p][:, blk * 2:blk * 2 + 2],
                                                                                   self.ident[:]),
                                 reads=[d_stat[p], self.d_const], writes=[self.d_ps[pb]])
                            dst = R[0:2, p, sl(q0, 128, d)]
                            S.op("act", lambda e, pb=pb, dst=dst: e.activation(out=dst, in_=self.ps[pb][0:2, 0:128], func=AF.Copy),
                                 reads=[self.d_ps[pb]], writes=[d_R])
                S.op("dve", lambda e: e.tensor_tensor(out=Mx[:], in0=R[:, 0, :], in1=R[:, 1, :], op=ALU.max), reads=[d_R], writes=[d_M])
                S.op("dve", lambda e: e.tensor_tensor(out=Mx[:], in0=Mx[:], in1=R[:, 2, :], op=ALU.max), reads=[d_R, d_M], writes=[d_M])
                for p in range(3):
                    S.op("dve", lambda e, p=p: e.tensor_tensor(out=R[:, p, :], in0=R[:, p, :], in1=Mx[:], op=ALU.subtract),
                         reads=[d_M, d_R], writes=[d_R])
                S.op("act", lambda e: e.activation(out=R[:], in_=R[:], func=AF.Exp), reads=[d_R], writes=[d_R])
                S.op("dve", lambda e: e.tensor_tensor(out=Mx[:], in0=R[:, 0, :], in1=R[:, 1, :], op=ALU.add), reads=[d_R, d_M], writes=[d_M])
                S.op("dve", lambda e: e.tensor_tensor(out=Mx[:], in0=Mx[:], in1=R[:, 2, :], op=ALU.add), reads=[d_R, d_M], writes=[d_M])
                S.op("dve", lambda e: e.reciprocal(out=Mx[:], in_=Mx[:]), reads=[d_M], writes=[d_M])
                for p in range(3):
                    S.op("dve", lambda e, p=p: e.tensor_tensor(out=R[:, p, :], in0=R[:, p, :], in1=Mx[:], op=ALU.mult),
                         reads=[d_M, d_R], writes=[d_R])
                for tt in range(T // 512):
                    cs = slice(tt * 512, (tt + 1) * 512)
                    for p in range(3):
                        pb = 6 + (p % 2)
                        S.op("pe", lambda e, pb=pb, p=p, cs=cs: e.matmul(self.ps[pb][:], lhsT=self.E2[:], rhs=R[0:2, p, cs],
                                                                          start=True, stop=True),
                             reads=[d_R, self.d_attnc], writes=[self.d_ps[pb]])
                        if p == 0:
                            S.op("dve", lambda e, pb=pb, cs=cs: e.tensor_tensor(out=acc[:], in0=opT[0][:, cs], in1=self.ps[pb][:], op=ALU.mult),
                                 reads=[d_op[0], self.d_ps[pb]], writes=[d_acc])
                        else:
                            S.op("dve", lambda e, pb=pb, cs=cs, p=p: e.tensor_tensor(out=tmp[:], in0=opT[p][:, cs], in1=self.ps[pb][:], op=ALU.mult),
                                 reads=[d_op[p], self.d_ps[pb]], writes=[d_tmp])
                            if p == 1:
                                S.op("pool", lambda e: e.tensor_tensor(out=acc[:], in0=acc[:], in1=tmp[:], op=ALU.add),
                                     reads=[d_tmp, d_acc], writes=[d_acc])
                            else:
                                S.op("pool", lambda e, cs=cs, ch=ch: e.tensor_tensor(out=self.actT[:, ch, cs], in0=acc[:], in1=tmp[:], op=ALU.add),
                                     reads=[d_tmp, d_acc], writes=self.d_act[tt * 4:(tt + 1) * 4])
        S.barrier()

    def phase_dn(self, l):
        S, I = self.S, self.I
        T, NT = self.T, self.NT
        DKS = float(DK_B) ** -0.5
        with ExitStack() as ph:
            cnt = {"s": 0}

            cnt["t"] = 0
            d_bank = [S.dep() for _ in range(8)]
            d_slot = [d_bank[i // 4] for i in range(32)]

            def slot():
                bk = 2 + cnt["s"] % 6
                cnt["s"] += 1
                return self.ps[bk][:, 0:128], d_bank[bk]

            def tslot():
                bk = cnt["t"] % 2
                cnt["t"] += 1
                return self.ps[bk][:, 0:128], d_bank[bk]

            d_m = S.dep()
            ML = self.sb(ph, "ML", [128, 128], F32)
            MIT = self.sb(ph, "MIT", [128, 128], F32)
            CH0 = self.sb(ph, "CH0", [128, 128], F32)
            CH1 = self.sb(ph, "CH1", [128, 128], F32)
            S.op("pool", lambda e: e.memset(MIT[:], 1.0), writes=[d_m])
            S.op("pool", lambda e: e.affine_select(out=MIT[:], in_=MIT[:], pattern=[[1, 128]], compare_op=ALU.is_ge,
                                                    fill=self.reg_zero, base=0, channel_multiplier=-1), reads=[d_m], writes=[d_m])
            S.op("pool", lambda e: e.memset(MIT[0:64, 64:128], 0.0), reads=[d_m], writes=[d_m])
            S.op("pool", lambda e: e.memset(ML[:], 1.0), writes=[d_m])
            S.op("pool", lambda e: e.affine_select(out=ML[:], in_=ML[:], pattern=[[-1, 128]], compare_op=ALU.is_ge,
                                                    fill=self.reg_zero, base=-1, channel_multiplier=1), reads=[d_m], writes=[d_m])
            S.op("pool", lambda e: e.memset(ML[64:128, 0:64], 0.0), reads=[d_m], writes=[d_m])
            S.op("pool", lambda e: e.memset(CH0[:], 0.0), writes=[d_m])
            S.op("pool", lambda e: e.memset(CH0[0:64, :], 1.0), reads=[d_m], writes=[d_m])
            S.op("pool", lambda e: e.memset(CH1[:], 0.0), writes=[d_m])
            S.op("pool", lambda e: e.memset(CH1[64:128, :], 1.0), reads=[d_m], writes=[d_m])
            d_par = S.dep()
            cwt = self.sb(ph, "cwt", [128, 16, CONV_K], F32)
            S.dma("sp", cwt[:], I["conv_w"][:, l, :, :], writes=[d_par])
            ngc = self.sb(ph, "ngc", [128, 1], F32)
            S.dma("sp", ngc[:], I["dn_norm_g"][l], writes=[d_par])
            alog = self.sb(ph, "alog", [128, 8], F32)
            dtb = self.sb(ph, "dtb", [128, 8], F32)
            S.dma("sp", alog[:], I["a_log"][0:1, l * 8:(l + 1) * 8].partition_broadcast(128), writes=[d_par])
            S.dma("sp", dtb[:], I["dt_bias"][0:1, l * 8:(l + 1) * 8].partition_broadcast(128), writes=[d_par])
            nea = self.sb(ph, "nea", [128, 8], F32)
            S.op("act", lambda e: e.activation(out=nea[:], in_=alog[:], func=AF.Exp), reads=[d_par], writes=[d_par])
            S.op("dve", lambda e: e.tensor_scalar_mul(out=nea[:], in0=nea[:], scalar1=-1.0), reads=[d_par], writes=[d_par])
            d_g = S.dep()
            bbaT = self.sb(ph, "bbaT", [16, T], F32)
            S.dma("sp", bbaT[:], self.P_d[O_BB:O_BB + 16, self.c0:self.c0 + T], reads=[self.d_P], writes=[d_g])
            GB = self.sb(ph, "GB", [128, NT, 16], F32)
            for i in range(NT):
                p_, dp_ = tslot()
                S.op("pe", lambda e, p_=p_, i=i: e.transpose(p_[:, 0:16], bbaT[:, i * 128:(i + 1) * 128], self.ident[0:16, 0:16]),
                     reads=[d_g, self.d_const], writes=[dp_])
                S.op("act", lambda e, p_=p_, i=i: e.activation(out=GB[:, i, :], in_=p_[:, 0:16], func=AF.Copy),
                     reads=[dp_], writes=[d_g])
            BETA = self.sb(ph, "BETA", [128, NT, 8], F32)
            G = self.sb(ph, "G", [128, NT, 8], F32)
            t1 = self.sb(ph, "gt1", [128, NT, 8], F32)
            t2 = self.sb(ph, "gt2", [128, NT, 8], F32)
            S.op("act", lambda e: e.activation(out=BETA[:], in_=GB[:, :, 0:8], func=AF.Exp, scale=-1.0), reads=[d_g], writes=[d_g])
            S.op("dve", lambda e: e.tensor_scalar_add(out=BETA[:], in0=BETA[:], scalar1=1.0), reads=[d_g], writes=[d_g])
            S.op("dve", lambda e: e.reciprocal(out=BETA[:], in_=BETA[:]), reads=[d_g], writes=[d_g])
            S.op("dve", lambda e: e.tensor_tensor(out=G[:], in0=GB[:, :, 8:16], in1=dtb[:].unsqueeze(1).broadcast_to([128, NT, 8]), op=ALU.add),
                 reads=[d_g, d_par], writes=[d_g])
            S.op("dve", lambda e: e.tensor_scalar_mul(out=t1[:], in0=G[:], scalar1=-1.0), reads=[d_g], writes=[d_g])
            S.op("dve", lambda e: e.tensor_tensor(out=t1[:], in0=t1[:], in1=G[:], op=ALU.max), reads=[d_g], writes=[d_g])
            S.op("act", lambda e: e.activation(out=t1[:], in_=t1[:], func=AF.Exp, scale=-1.0), reads=[d_g], writes=[d_g])
            S.op("act", lambda e: e.activation(out=t1[:], in_=t1[:], func=AF.Ln, bias=self.ones[:, 0:1], scale=1.0),
                 reads=[d_g, self.d_const], writes=[d_g])
            S.op("dve", lambda e: e.tensor_scalar_max(out=t2[:], in0=G[:], scalar1=0.0), reads=[d_g], writes=[d_g])
            S.op("dve", lambda e: e.tensor_tensor(out=t2[:], in0=t2[:], in1=t1[:], op=ALU.add), reads=[d_g], writes=[d_g])
            S.op("dve", lambda e: e.tensor_tensor(out=G[:], in0=t2[:], in1=nea[:].unsqueeze(1).broadcast_to([128, NT, 8]), op=ALU.mult),
                 reads=[d_g, d_par], writes=[d_g])
            GC = self.sb(ph, "GC", [128, NT, 8], F32)
            GL = self.sb(ph, "GL", [128, NT, 2, 8], F32)
            for i in range(NT):
                p_, dp_ = slot()
                S.op("pe", lambda e, p_=p_, i=i: e.matmul(p_[:, 0:8], lhsT=MIT[:], rhs=G[:, i, :], start=True, stop=True),
                     reads=[d_g, d_m], writes=[dp_])
                S.op("pe", lambda e, p_=p_, i=i: e.matmul(p_[:, 8:16], lhsT=CH0[:], rhs=G[:, i, :], start=True, stop=True),
                     reads=[d_g, d_m], writes=[dp_])
                S.op("pe", lambda e, p_=p_, i=i: e.matmul(p_[:, 16:24], lhsT=CH1[:], rhs=G[:, i, :], start=True, stop=True),
                     reads=[d_g, d_m], writes=[dp_])
                S.op("act", lambda e, p_=p_, i=i: e.activation(out=GC[:, i, :], in_=p_[:, 0:8], func=AF.Copy), reads=[dp_], writes=[d_g])
                S.op("dve", lambda e, p_=p_, i=i: e.tensor_copy(out=GL[:, i, :, :], in_=p_[:, 8:24].rearrange("p (c h) -> p c h", c=2)),
                     reads=[dp_], writes=[d_g])
            EG = self.sb(ph, "EG", [128, NT, 8], F32)
            BEG = self.sb(ph, "BEG", [128, NT, 8], F32)
            KD = self.sb(ph, "KD", [128, NT, 8], F32)
            EGL = self.sb(ph, "EGL", [128, NT, 2, 8], F32)
            S.op("act", lambda e: e.activation(out=EG[:], in_=GC[:], func=AF.Exp), reads=[d_g], writes=[d_g])
            S.op("dve", lambda e: e.tensor_tensor(out=BEG[:], in0=EG[:], in1=BETA[:], op=ALU.mult), reads=[d_g], writes=[d_g])
            S.op("dve", lambda e: e.tensor_tensor(out=KD[0:64], in0=GL[0:64, :, 0, :], in1=GC[0:64], op=ALU.subtract), reads=[d_g], writes=[d_g])
            S.op("dve", lambda e: e.tensor_tensor(out=KD[64:128], in0=GL[64:128, :, 1, :], in1=GC[64:128], op=ALU.subtract), reads=[d_g], writes=[d_g])
            S.op("act", lambda e: e.activation(out=KD[:], in_=KD[:], func=AF.Exp), reads=[d_g], writes=[d_g])
            S.op("act", lambda e: e.activation(out=EGL[:], in_=GL[:], func=AF.Exp), reads=[d_g], writes=[d_g])

            stop = self.dn_stop
            qn = self.sb(ph, "qn", [128, T], F32)
            kn = self.sb(ph, "kn", [128, T], F32)
            vT = [self.sb(ph, f"vT{i}", [128, T], F32) for i in range(2)]
            zT = [self.sb(ph, f"zT{i}", [128, T], F32) for i in range(2)]
            X = self.sb(ph, "convX", [128, T + 3], F32)
            d_X = S.dep()
            d_q, d_k = S.dep(), S.dep()
            d_v = S.deps(2)
            d_z = S.deps(2)
            sq = self.sb(ph, "sqt", [128, 512], F32)
            rn = self.sb(ph, "rnt", [128, 512], F32)
            d_sq, d_rn = S.dep(), S.dep()
            NS = 2
            def mk(name):
                return [self.sb(ph, f"{name}{i}", [128, 128], F32) for i in range(NS)], S.deps(NS)
            ktok, d_ktok = mk("ktok")
            KKs, d_KKs = mk("KKs")
            QKs, d_QKs = mk("QKs")
            vtok, d_vtok = mk("vtok")
            diag, d_diag = mk("diag")
            Dm, d_Dm = mk("Dm")
            DTm, d_DTm = mk("DTm")
            EGR, d_EGR = mk("EGR")
            Lm, d_Lm = mk("Lm")
            Nm, d_Nm = mk("Nm")
            qkT, d_qkT = mk("qkT")
            AL, d_AL = mk("AL")
            AN, d_AN = mk("AN")
            PT, d_PT = mk("PT")
            PT2, d_PT2 = mk("PT2")
            vb, d_vb = mk("vb")
            kbe, d_kbe = mk("kbe")
            kdec, d_kdec = mk("kdec")
            uu, d_uu = mk("uu")
            wT, d_wT = mk("wT")
            qdT, d_qdT = mk("qdT")
            vnew, d_vnew = mk("vnew")
            otok, d_otok = mk("otok")
            ojunk, d_ojunk = mk("ojunk")
            ost = [self.sb(ph, f"ost{i}", [128, 4], F32) for i in range(NS)]
            d_ost = S.deps(NS)
            Sst = [[self.sb(ph, f"Sst{hh}_{i}", [128, 128], F32) for i in range(2)] for hh in range(2)]
            d_Sst = [S.deps(2) for _ in range(2)]

            def conv_load(dst, d_dst, row0, c16, l2=None):
                S.dma("sp", X[:], self.P_d[row0:row0 + 128, self.c0 - 3:self.c0 + T], reads=[self.d_P], writes=[d_X])
                S.op("dve", lambda e: e.tensor_scalar_mul(out=dst[:], in0=X[:, 0:T], scalar1=cwt[:, c16, 0:1]),
                     reads=[d_X, d_par], writes=[d_dst])
                for j in range(1, CONV_K):
                    S.op("dve", lambda e, j=j: e.scalar_tensor_tensor(out=dst[:], in0=X[:, j:j + T], scalar=cwt[:, c16, j:j + 1],
                                                                        in1=dst[:], op0=ALU.mult, op1=ALU.add),
                         reads=[d_X, d_par, d_dst], writes=[d_dst])
                if self.dn_sub >= 1:
                    S.op("act", lambda e: e.activation(out=dst[:], in_=dst[:], func=AF.Silu), reads=[d_dst], writes=[d_dst])
                if l2 is not None and self.dn_sub >= 2:
                    for tt in range(T // 512):
                        cs = slice(tt * 512, (tt + 1) * 512)
                        pb = 2 + tt % 2
                        S.op("act", lambda e, cs=cs: e.activation(out=sq[:], in_=dst[:, cs], func=AF.Square), reads=[d_dst], writes=[d_sq])
                        S.op("pe", lambda e, pb=pb: e.matmul(self.ps[pb][:], lhsT=self.ones[:], rhs=sq[:], start=True, stop=True),
                             reads=[d_sq, self.d_const], writes=[d_slot[pb * 4 + q] for q in range(4)])
                        if False:
                            S.op("dve", lambda e, pb=pb: e.tensor_scalar(out=rn[:], in0=self.ps[pb][:], scalar1=EPS, scalar2=-0.5, op0=ALU.add, op1=ALU.pow),
                                 reads=[d_slot[pb * 4 + q] for q in range(4)], writes=[d_rn])
                        else:
                            S.op("dve", lambda e, pb=pb: e.tensor_scalar_add(out=rn[:], in0=self.ps[pb][:], scalar1=EPS),
                                 reads=[d_slot[pb * 4 + q] for q in range(4)], writes=[d_rn])
                            S.op("act", lambda e: e.activation(out=rn[:], in_=rn[:], func=AF.Sqrt), reads=[d_rn], writes=[d_rn])
                            S.op("dve", lambda e: e.reciprocal(out=rn[:], in_=rn[:]), reads=[d_rn], writes=[d_rn])
                        S.op("dve", lambda e, cs=cs: e.scalar_tensor_tensor(out=dst[:, cs], in0=dst[:, cs], scalar=float(l2), in1=rn[:],
                                                                             op0=ALU.mult, op1=ALU.mult), reads=[d_rn, d_dst], writes=[d_dst])

            for kh in range((4 if not self.dn_fast else 1) if stop >= 2 else 0):
                conv_load(qn, d_q, O_BQ + 128 * kh, kh, l2=DKS)
                conv_load(kn, d_k, O_BK + 128 * kh, 4 + kh, l2=1.0)
                for hh in range(2):
                    h = 2 * kh + hh
                    conv_load(vT[hh], d_v[hh], O_BV + 128 * h, 8 + h)
                    S.dma("sp", zT[hh][:], self.P_d[O_BZ + 128 * h:O_BZ + 128 * (h + 1), self.c0:self.c0 + T], reads=[self.d_P], writes=[d_z[hh]])
                    S.op("act", lambda e, hh=hh: e.activation(out=zT[hh][:], in_=zT[hh][:], func=AF.Silu), reads=[d_z[hh]], writes=[d_z[hh]])
                    if self.seg_first:
                        S.op("pool", lambda e, hh=hh: e.memset(Sst[hh][0][:], 0.0), writes=[d_Sst[hh][0]])
                    else:
                        S.op("pool", lambda e, hh=hh, h=h: e.tensor_copy(out=Sst[hh][0][:], in_=self.SCARRY[:, h, :]),
                             reads=[self.d_carry], writes=[d_Sst[hh][0]])
                cur = [0, 0]
                for i in range((NT if not self.dn_fast else 2) if stop >= 3 else 0):
                    ts_ = slice(i * 128, (i + 1) * 128)
                    kb = i % NS
                    DV = 7
                    p_, dp_ = tslot()
                    if DV & 1:
                        S.op("pe", lambda e, p_=p_, ts_=ts_: e.transpose(p_, kn[:, ts_], self.ident[:]), reads=[d_k, self.d_const], writes=[dp_])
                        S.op("act", lambda e, p_=p_, kb=kb: e.activation(out=ktok[kb][:], in_=p_, func=AF.Copy), reads=[dp_], writes=[d_ktok[kb]])
                    pKK_, dKK_ = slot()
                    S.op("pe", lambda e, pKK_=pKK_, ts_=ts_: e.matmul(pKK_, lhsT=kn[:, ts_], rhs=kn[:, ts_], start=True, stop=True),
                         reads=[d_k], writes=[dKK_])
                    S.op("act", lambda e, pKK_=pKK_, kb=kb: e.activation(out=KKs[kb][:], in_=pKK_, func=AF.Copy), reads=[dKK_], writes=[d_KKs[kb]])
                    pQK_, dQK_ = slot()
                    S.op("pe", lambda e, pQK_=pQK_, ts_=ts_: e.matmul(pQK_, lhsT=kn[:, ts_], rhs=qn[:, ts_], start=True, stop=True),
                         reads=[d_k, d_q], writes=[dQK_])
                    S.op("dve", lambda e, pQK_=pQK_, kb=kb: e.tensor_copy(out=QKs[kb][:], in_=pQK_), reads=[dQK_], writes=[d_QKs[kb]])
                    pKK, dKK, pQK, dQK = KKs[kb][:], d_KKs[kb], QKs[kb][:], d_QKs[kb]
                    for hh in range(2 if self.dn_sub >= 11 else 0):
                        h = 2 * kh + hh
                        b = (i * 2 + hh) % NS
                        gcol = GC[:, i, h:h + 1]
                        S.op("dve", lambda e, b=b, gcol=gcol: e.tensor_scalar_mul(out=diag[b][:], in0=self.ident[:], scalar1=gcol),
                             reads=[d_g, self.d_const], writes=[d_diag[b]])
                        pG, dG = slot()
                        S.op("pe", lambda e, pG=pG, b=b: e.matmul(pG, lhsT=self.ones[:], rhs=diag[b][:], start=True, stop=True),
                             reads=[d_diag[b], self.d_const], writes=[dG])
                        S.op("dve", lambda e, pG=pG, b=b, gcol=gcol: e.tensor_scalar(out=Dm[b][:], in0=pG, scalar1=gcol, scalar2=0.0,
                                                                                      op0=ALU.subtract, op1=ALU.max),
                             reads=[dG, d_g], writes=[d_Dm[b]])
                        S.op("act", lambda e, b=b: e.activation(out=Dm[b][:], in_=Dm[b][:], func=AF.Exp, scale=-1.0), reads=[d_Dm[b]], writes=[d_Dm[b]])
                        S.op("pool", lambda e, b=b: e.tensor_tensor(out=Dm[b][:], in0=Dm[b][:], in1=ML[:], op=ALU.mult), reads=[d_Dm[b], d_m], writes=[d_Dm[b]])
                        if self.dn_sub < 12:
                            continue
                        S.op("dve", lambda e, pG=pG, b=b, gcol=gcol: e.tensor_scalar(out=DTm[b][:], in0=pG, scalar1=gcol, scalar2=0.0,
                                                                                      op0=ALU.subtract, op1=ALU.min),
                             reads=[dG, d_g], writes=[d_DTm[b]])
                        S.op("act", lambda e, b=b: e.activation(out=DTm[b][:], in_=DTm[b][:], func=AF.Exp), reads=[d_DTm[b]], writes=[d_DTm[b]])
                        S.op("pool", lambda e, b=b: e.tensor_tensor(out=DTm[b][:], in0=DTm[b][:], in1=MIT[:], op=ALU.mult), reads=[d_DTm[b], d_m], writes=[d_DTm[b]])
                        S.op("act", lambda e, pG=pG, b=b: e.activation(out=EGR[b][:], in_=pG, func=AF.Exp), reads=[dG], writes=[d_EGR[b]])
                        if self.dn_sub < 13:
                            continue
                        S.op("dve", lambda e, b=b, i=i, h=h: e.scalar_tensor_tensor(out=Lm[b][:], in0=pKK, scalar=BETA[:, i, h:h + 1], in1=Dm[b][:],
                                                                                     op0=ALU.mult, op1=ALU.mult),
                             reads=[dKK, d_g, d_Dm[b]], writes=[d_Lm[b]])
                        S.op("dve", lambda e, b=b: e.tensor_tensor(out=qkT[b][:], in0=pQK, in1=DTm[b][:], op=ALU.mult),
                             reads=[dQK, d_DTm[b]], writes=[d_qkT[b]])
                        pN, dN = tslot()
                        S.op("pe", lambda e, pN=pN, b=b: e.transpose(pN, Lm[b][:], self.ident[:]), reads=[d_Lm[b], self.d_const], writes=[dN])
                        S.op("act", lambda e, pN=pN, b=b: e.activation(out=Nm[b][:], in_=pN, func=AF.Copy), reads=[dN], writes=[d_Nm[b]])
                        if self.dn_sub < 14:
                            continue
                        S.op("dve", lambda e, b=b: e.tensor_tensor(out=PT[b][:], in0=self.ident[:], in1=Nm[b][:], op=ALU.subtract),
                             reads=[d_Nm[b], self.d_const], writes=[d_PT[b]])
                        cl, dcl, cn, dcn = Lm[b], d_Lm[b], Nm[b], d_Nm[b]
                        cp, dcp, np_, dnp = PT[b], d_PT[b], PT2[b], d_PT2[b]
                        for sstep in range(1, 6):
                            pL2, dL2 = slot()
                            S.op("pe", lambda e, pL2=pL2, cl=cl, cn=cn: e.matmul(pL2, lhsT=cn[:], rhs=cl[:], start=True, stop=True),
                                 reads=[dcl, dcn], writes=[dL2])
                            if sstep < 5:
                                pN2, dN2 = slot()
                                S.op("pe", lambda e, pN2=pN2, cl=cl, cn=cn: e.matmul(pN2, lhsT=cl[:], rhs=cn[:], start=True, stop=True),
                                     reads=[dcl, dcn], writes=[dN2])
                            if sstep % 2 == 1:
                                nl, dnl, nn, dnn = AL[b], d_AL[b], AN[b], d_AN[b]
                            else:
                                nl, dnl, nn, dnn = Lm[b], d_Lm[b], Nm[b], d_Nm[b]
                            S.op("act", lambda e, pL2=pL2, nl=nl: e.activation(out=nl[:], in_=pL2, func=AF.Copy), reads=[dL2], writes=[dnl])
                            if sstep < 5:
                                S.op("dve", lambda e, pN2=pN2, nn=nn: e.tensor_copy(out=nn[:], in_=pN2), reads=[dN2], writes=[dnn])
                            DW = 7
                            pU, dU = slot()
                            if DW & 2:
                                S.op("pe", lambda e, pU=pU, nl=nl, cp=cp: e.matmul(pU, lhsT=nl[:], rhs=cp[:], start=True, stop=True),
                                     reads=[dnl, dcp], writes=[dU])
                            if DW & 4:
                                S.op("dve", lambda e, pU=pU, cp=cp, np_=np_: e.tensor_tensor(out=np_[:], in0=pU, in1=cp[:], op=ALU.add),
                                     reads=[dU, dcp], writes=[dnp])
                            cl, dcl, cn, dcn = nl, dnl, nn, dnn
                            cp, dcp, np_, dnp = np_, dnp, cp, dcp
                        TT, dTT = cp, dcp
                        if stop < 4:
                            continue
                        pV, dV = tslot()
                        S.op("pe", lambda e, pV=pV, hh=hh, ts_=ts_: e.transpose(pV, vT[hh][:, ts_], self.ident[:]),
                             reads=[d_v[hh], self.d_const], writes=[dV])
                        S.op("dve", lambda e, pV=pV, b=b, i=i, h=h: e.tensor_scalar_mul(out=vb[b][:], in0=pV, scalar1=BETA[:, i, h:h + 1]),
                             reads=[dV, d_g], writes=[d_vb[b]])
                        S.op("pool", lambda e, b=b, kb=kb, i=i, h=h: e.tensor_scalar_mul(out=kbe[b][:], in0=ktok[kb][:], scalar1=BEG[:, i, h:h + 1]),
                             reads=[d_ktok[kb], d_g], writes=[d_kbe[b]])
                        S.op("pool", lambda e, b=b, kb=kb, i=i, h=h: e.tensor_scalar_mul(out=kdec[b][:], in0=ktok[kb][:], scalar1=KD[:, i, h:h + 1]),
                             reads=[d_ktok[kb], d_g], writes=[d_kdec[b]])
                        S.op("pool", lambda e, b=b, ts_=ts_: e.tensor_tensor(out=qdT[b][:], in0=qn[:, ts_], in1=EGR[b][:], op=ALU.mult),
                             reads=[d_q, d_EGR[b]], writes=[d_qdT[b]])
                        pu, du = slot()
                        S.op("pe", lambda e, pu=pu, TT=TT, b=b: e.matmul(pu, lhsT=TT[:], rhs=vb[b][:], start=True, stop=True),
                             reads=[dTT, d_vb[b]], writes=[du])
                        S.op("act", lambda e, pu=pu, b=b: e.activation(out=uu[b][:], in_=pu, func=AF.Copy), reads=[du], writes=[d_uu[b]])
                        pw, dw = slot()
                        S.op("pe", lambda e, pw=pw, TT=TT, b=b: e.matmul(pw, lhsT=kbe[b][:], rhs=TT[:], start=True, stop=True),
                             reads=[dTT, d_kbe[b]], writes=[dw])
                        S.op("act", lambda e, pw=pw, b=b: e.activation(out=wT[b][:], in_=pw, func=AF.Copy), reads=[dw], writes=[d_wT[b]])
                        for c in range(2 if stop >= 5 else 0):
                            rs = slice(64 * c, 64 * c + 64)
                            Sc, dSc = Sst[hh][cur[hh]], d_Sst[hh][cur[hh]]
                            Sn, dSn = Sst[hh][1 - cur[hh]], d_Sst[hh][1 - cur[hh]]
                            p1, d1 = slot()
                            S.op("pe", lambda e, p1=p1, b=b, Sc=Sc: e.matmul(p1, lhsT=wT[b][:], rhs=Sc[:], start=True, stop=True),
                                 reads=[d_wT[b], dSc], writes=[d1])
                            S.op("dve", lambda e, p1=p1, b=b, rs=rs: e.tensor_tensor(out=vnew[b][rs, :], in0=uu[b][rs, :], in1=p1[rs, :], op=ALU.subtract),
                                 reads=[d1, d_uu[b]], writes=[d_vnew[b]])
                            p2, d2 = slot()
                            S.op("pe", lambda e, p2=p2, b=b, Sc=Sc: e.matmul(p2, lhsT=qdT[b][:], rhs=Sc[:], start=True, stop=False),
                                 reads=[d_qdT[b], dSc], writes=[d2])
                            S.op("pe", lambda e, p2=p2, b=b, rs=rs: e.matmul(p2, lhsT=qkT[b][rs, :], rhs=vnew[b][rs, :], start=False, stop=True),
                                 reads=[d_qkT[b], d_vnew[b]], writes=[d2])
                            S.op("act", lambda e, p2=p2, b=b, rs=rs: e.activation(out=otok[b][rs, :], in_=p2[rs, :], func=AF.Copy),
                                 reads=[d2], writes=[d_otok[b]])
                            p3, d3 = slot()
                            S.op("pe", lambda e, p3=p3, b=b, rs=rs: e.matmul(p3, lhsT=kdec[b][rs, :], rhs=vnew[b][rs, :], start=True, stop=True),
                                 reads=[d_kdec[b], d_vnew[b]], writes=[d3])
                            S.op("dve", lambda e, p3=p3, Sc=Sc, Sn=Sn, i=i, c=c, h=h: e.scalar_tensor_tensor(
                                out=Sn[:], in0=Sc[:], scalar=EGL[:, i, c, h:h + 1], in1=p3, op0=ALU.mult, op1=ALU.add),
                                reads=[d3, dSc, d_g], writes=[dSn])
                            cur[hh] = 1 - cur[hh]
                        S.op("act", lambda e, b=b: e.activation(out=ojunk[b][:], in_=otok[b][:], func=AF.Square, accum_out=ost[b][:, 0:1]),
                             reads=[d_otok[b]], writes=[d_ojunk[b], d_ost[b]])
                        S.op("act", lambda e, b=b: e.activation(out=ost[b][:, 1:2], in_=ost[b][:, 0:1], func=AF.Sqrt, scale=1.0 / DV_B,
                                                                 bias=self.epsc[:, 0:1]), reads=[d_ost[b], self.d_const], writes=[d_ost[b]])
                        S.op("dve", lambda e, b=b: e.reciprocal(out=ost[b][:, 2:3], in_=ost[b][:, 1:2]), reads=[d_ost[b]], writes=[d_ost[b]])
                        S.op("pool", lambda e, b=b: e.tensor_scalar_mul(out=otok[b][:], in0=otok[b][:], scalar1=ost[b][:, 2:3]),
                             reads=[d_ost[b], d_otok[b]], writes=[d_otok[b]])
                        pO, dO = tslot()
                        S.op("pe", lambda e, pO=pO, b=b: e.transpose(pO, otok[b][:], self.ident[:]), reads=[d_otok[b], self.d_const], writes=[dO])
                        S.op("dve", lambda e, pO=pO, hh=hh, h=h, ts_=ts_: e.scalar_tensor_tensor(
                            out=self.actT[:, 4 + h, ts_], in0=pO, scalar=ngc[:, 0:1], in1=zT[hh][:, ts_], op0=ALU.mult, op1=ALU.mult),
                            reads=[dO, d_par, d_z[hh]], writes=[self.d_act[i]])
                for hh in range(2):
                    h = 2 * kh + hh
                    S.op("pool", lambda e, hh=hh, h=h, cc=cur[hh]: e.tensor_copy(out=self.SCARRY[:, h, :], in_=Sst[hh][cc][:]),
                         reads=[d_Sst[hh][cur[hh]]], writes=[self.d_carry])
        S.barrier()

    def phase_outproj(self, l):
        S, I = self.S, self.I
        T, NT = self.T, self.NT
        wv = I["w_out"][l].rearrange("(kc p) n -> p kc n", p=128)
        with ExitStack() as ph:
            NB = 2
            W = [self.sb(ph, f"opW{i}", [128, KC, 512], BF16) for i in range(NB)]
            dW = S.deps(NB)
            Gp = [self.sb(ph, f"opG{i}", [128, 512], F32) for i in range(NB)]
            xt = [self.sb(ph, f"opx{i}", [128, 512], F32) for i in range(NB)]
            dx = S.deps(NB)
            tt_ = [self.sb(ph, f"opt{i}", [128, 512], F32) for i in range(NB)]
            dt_ = S.deps(NB)
            cnt = 0
            for cg in range(4):
                cs = slice(cg * 512, (cg + 1) * 512)
                wb = cg % NB
                S.dma("pool", W[wb][:], wv[:, :, cs], writes=[dW[wb]])
                S.dma("sp", Gp[wb][:], self.mod_d[self.bi, l, 2, :, cs], reads=[self.d_mod], writes=[dW[wb]])
                for i in range(NT):
                    b = cnt % NB
                    pb = cnt % 8
                    cnt += 1
                    rows = slice(self.r0 + i * 128, self.r0 + (i + 1) * 128)
                    for kc in range(KC):
                        S.op("pe", lambda e, kc=kc, i=i, wb=wb, pb=pb: e.matmul(self.ps[pb][:], lhsT=self.actT[:, kc, i * 128:(i + 1) * 128],
                                                                              rhs=W[wb][:, kc, :], start=(kc == 0), stop=(kc == KC - 1)),
                             reads=[dW[wb], self.d_act[i]], writes=[self.d_ps[pb]])
                    S.dma("sp", xt[b][:], self.x_d[rows, cs], reads=[self.d_xt[i]], writes=[dx[b]])
                    S.op("dve", lambda e, b=b, wb=wb, pb=pb: e.tensor_tensor(out=tt_[b][:], in0=self.ps[pb][:], in1=Gp[wb][:], op=ALU.mult),
                         reads=[self.d_ps[pb], dW[wb]], writes=[dt_[b]])
                    S.op("pool", lambda e, b=b: e.tensor_tensor(out=xt[b][:], in0=xt[b][:], in1=tt_[b][:], op=ALU.add),
                         reads=[dt_[b], dx[b]], writes=[dx[b]])
                    S.dma("sp", self.x_d[rows, cs], xt[b][:], reads=[dx[b]], writes=[self.d_xt[i]])
        S.barrier()

    def phase_moe(self, l):
        S, I, nc = self.S, self.I, self.nc
        T, NT, NBLK = self.T, self.NT, self.NBLK
        with ExitStack() as mo:
            router = {}
            RW = self.sb(mo, "RW", [128, KC, 36], F32)
            RB = self.sb(mo, "RB", [128, 36], F32)
            LG = self.sb(mo, "LG", [128, NT, 36], F32)
            router["RW"], router["RB"], router["LG"] = RW, RB, LG
            router["d_rw"], router["d_lg"] = S.dep(), S.dep()
            router["hbf"] = [self.sb(mo, f"hbf{i}", [128, D], BF16) for i in range(2)]
            router["d_hbf"] = S.deps(2)
            S.dma("sp", RW[:], I["rw"][l].rearrange("(kc p) n -> p kc n", p=128), writes=[router["d_rw"]])
            S.dma("sp", RB[:], I["rb"][l:l + 1, :].partition_broadcast(128), writes=[router["d_rw"]])
            self.phase_norm(l, 2, router=router)
            d_r = router["d_lg"]
            cnt = {"n": 0}

            def small(name, shape, dt=F32):
                return self.sb(mo, name, shape, dt)

            gmax = small("gmax", [128, NT])
            goh = small("goh", [128, NT, 4])
            gex = small("gex", [128, NT, 4])
            pg = small("pg", [128, NT])
            esel = small("esel", [128, NT, 8])
            etmp = small("etmp", [128, NT, 8])
            v1 = small("v1", [128, NT])
            v2 = small("v2", [128, NT])
            oh1 = small("oh1", [128, NT, 8])
            oh2 = small("oh2", [128, NT, 8])
            g0 = small("g0", [128, NT])
            g1 = small("g1", [128, NT])
            OH1 = small("OH1", [128, NT, 32])
            OH2 = small("OH2", [128, NT, 32])
            OH = small("OH", [128, NT, 32])
            glog = LG[:, :, 0:4]
            elog4 = LG[:, :, 4:36].rearrange("p n (g j) -> p n g j", g=4)

            def dv(fn, eng="dve"):
                S.op(eng, fn, reads=[d_r, self.d_const], writes=[d_r])

            dv(lambda e: e.tensor_reduce(out=gmax[:], in_=glog, axis=AX.X, op=ALU.max))
            dv(lambda e: e.tensor_tensor(out=goh[:], in0=glog, in1=gmax[:].unsqueeze(2).broadcast_to([128, NT, 4]), op=ALU.is_equal))
            dv(lambda e: e.tensor_tensor(out=gex[:], in0=glog, in1=gmax[:].unsqueeze(2).broadcast_to([128, NT, 4]), op=ALU.subtract))
            dv(lambda e: e.activation(out=gex[:], in_=gex[:], func=AF.Exp), "act")
            dv(lambda e: e.tensor_reduce(out=pg[:], in_=gex[:], axis=AX.X, op=ALU.add))
            dv(lambda e: e.reciprocal(out=pg[:], in_=pg[:]))
            for g in range(4):
                if g == 0:
                    dv(lambda e: e.tensor_tensor(out=esel[:], in0=elog4[:, :, 0, :], in1=goh[:, :, 0:1].broadcast_to([128, NT, 8]), op=ALU.mult))
                else:
                    dv(lambda e, g=g: e.tensor_tensor(out=etmp[:], in0=elog4[:, :, g, :], in1=goh[:, :, g:g + 1].broadcast_to([128, NT, 8]), op=ALU.mult))
                    dv(lambda e: e.tensor_tensor(out=esel[:], in0=esel[:], in1=etmp[:], op=ALU.add))
            dv(lambda e: e.tensor_reduce(out=v1[:], in_=esel[:], axis=AX.X, op=ALU.max))
            dv(lambda e: e.tensor_tensor(out=oh1[:], in0=esel[:], in1=v1[:].unsqueeze(2).broadcast_to([128, NT, 8]), op=ALU.is_equal))
            dv(lambda e: e.scalar_tensor_tensor(out=etmp[:], in0=oh1[:], scalar=NEG, in1=esel[:], op0=ALU.mult, op1=ALU.add))
            dv(lambda e: e.tensor_reduce(out=v2[:], in_=etmp[:], axis=AX.X, op=ALU.max))
            dv(lambda e: e.tensor_tensor(out=oh2[:], in0=etmp[:], in1=v2[:].unsqueeze(2).broadcast_to([128, NT, 8]), op=ALU.is_equal))
            dv(lambda e: e.tensor_tensor(out=g1[:], in0=v2[:], in1=v1[:], op=ALU.subtract))
            dv(lambda e: e.activation(out=g1[:], in_=g1[:], func=AF.Exp), "act")
            dv(lambda e: e.tensor_scalar_add(out=g0[:], in0=g1[:], scalar1=1.0))
            dv(lambda e: e.reciprocal(out=g0[:], in_=g0[:]))
            dv(lambda e: e.tensor_tensor(out=g1[:], in0=g1[:], in1=g0[:], op=ALU.mult))
            dv(lambda e: e.tensor_tensor(out=g0[:], in0=g0[:], in1=pg[:], op=ALU.mult))
            dv(lambda e: e.tensor_tensor(out=g1[:], in0=g1[:], in1=pg[:], op=ALU.mult))
            for (OHk, ohk) in ((OH1, oh1), (OH2, oh2)):
                for g in range(4):
                    dv(lambda e, OHk=OHk, ohk=ohk, g=g: e.tensor_tensor(out=OHk[:, :, g * 8:(g + 1) * 8], in0=ohk[:],
                                                                        in1=goh[:, :, g:g + 1].broadcast_to([128, NT, 8]), op=ALU.mult))
            dv(lambda e: e.tensor_tensor(out=OH[:], in0=OH1[:], in1=OH2[:], op=ALU.add))
            Ust = small("Ust", [128, 128])
            dv(lambda e: e.memset(Ust[:], 1.0), "pool")
            dv(lambda e: e.affine_select(out=Ust[:], in_=Ust[:], pattern=[[1, 128]], compare_op=ALU.is_ge,
                                         fill=self.reg_zero, base=-1, channel_multiplier=-1), "pool")
            TRIU = small("TRIU", [32, 32])
            dv(lambda e: e.memset(TRIU[:], 1.0), "pool")
            dv(lambda e: e.affine_select(out=TRIU[:], in_=TRIU[:], pattern=[[1, 32]], compare_op=ALU.is_ge,
                                         fill=self.reg_zero, base=0, channel_multiplier=-1), "pool")
            CUM = small("CUM", [128, NT + 1, 32])
            RANK = small("RANK", [128, NT, 32])
            dv(lambda e: e.memset(CUM[:, 0, :], 0.0), "pool")
            for i in range(NT):
                pb = 4 + (i % 2)
                S.op("pe", lambda e, i=i, pb=pb: e.matmul(self.ps[pb][:, 0:32], lhsT=self.ones[:], rhs=OH[:, i, :], start=True, stop=True),
                     reads=[d_r, self.d_const], writes=[self.d_ps[pb]])
                S.op("dve", lambda e, i=i, pb=pb: e.tensor_tensor(out=CUM[:, i + 1, :], in0=CUM[:, i, :], in1=self.ps[pb][:, 0:32], op=ALU.add),
                     reads=[self.d_ps[pb], d_r], writes=[d_r])
                pb2 = 6 + (i % 2)
                S.op("pe", lambda e, i=i, pb2=pb2: e.matmul(self.ps[pb2][:, 0:32], lhsT=Ust[:], rhs=OH[:, i, :], start=True, stop=True),
                     reads=[d_r], writes=[self.d_ps[pb2]])
                S.op("dve", lambda e, i=i, pb2=pb2: e.tensor_tensor(out=RANK[:, i, :], in0=CUM[:, i, :], in1=self.ps[pb2][:, 0:32], op=ALU.add),
                     reads=[self.d_ps[pb2], d_r], writes=[d_r])
            THRi = small("THRi", [128, 16], I32)
            THR = small("THR", [128, 16])
            dv(lambda e: e.iota(THRi[:], pattern=[[128, 16]], base=0, channel_multiplier=0), "pool")
            dv(lambda e: e.tensor_copy(out=THR[:], in_=THRi[:]), "pool")
            CMP = small("CMP", [128, 32, 16])
            PADD = small("PADD", [128, 32])
            dv(lambda e: e.tensor_tensor(out=CMP[:], in0=CUM[:, NT, :].unsqueeze(2).broadcast_to([128, 32, 16]),
                                         in1=THR[:].unsqueeze(1).broadcast_to([128, 32, 16]), op=ALU.is_gt))
            dv(lambda e: e.tensor_reduce(out=PADD[:], in_=CMP[:], axis=AX.X, op=ALU.add))
            dv(lambda e: e.tensor_scalar_mul(out=PADD[:], in0=PADD[:], scalar1=128.0))
            paddT = small("paddT", [32, 128])
            S.op("pe", lambda e: e.transpose(self.ps[0][0:32, 0:128], PADD[:], self.ident[:]), reads=[d_r, self.d_const], writes=[self.d_ps[0]])
            S.op("act", lambda e: e.activation(out=paddT[:], in_=self.ps[0][0:32, 0:128], func=AF.Copy), reads=[self.d_ps[0]], writes=[d_r])
            PEND = small("PEND", [128, 32])
            S.op("pe", lambda e: e.matmul(self.ps[4][:, 0:32], lhsT=paddT[:], rhs=TRIU[:], start=True, stop=True), reads=[d_r], writes=[self.d_ps[4]])
            S.op("dve", lambda e: e.tensor_copy(out=PEND[:], in_=self.ps[4][:, 0:32]), reads=[self.d_ps[4]], writes=[d_r])
            PST = small("PST", [128, 32])
            dv(lambda e: e.tensor_tensor(out=PST[:], in0=PEND[:], in1=PADD[:], op=ALU.subtract))
            dv(lambda e: e.tensor_tensor(out=RANK[:], in0=RANK[:], in1=PST[:].unsqueeze(1).broadcast_to([128, NT, 32]), op=ALU.add))
            DSTf = small("DSTf", [128, 2, NT])
            DSTi = small("DSTi", [128, 2, NT], I32)
            for k, OHk in enumerate((OH1, OH2)):
                dv(lambda e, OHk=OHk: e.tensor_tensor(out=OHk[:], in0=OHk[:], in1=RANK[:], op=ALU.mult))
                dv(lambda e, OHk=OHk, k=k: e.tensor_reduce(out=DSTf[:, k, :], in_=OHk[:], axis=AX.X, op=ALU.add))
            dv(lambda e: e.tensor_copy(out=DSTi[:], in_=DSTf[:]))
            pendc = small("pendc", [32, 1])
            S.op("pe", lambda e: e.matmul(self.ps[5][0:32, 0:1], lhsT=TRIU[:], rhs=paddT[:, 0:1], start=True, stop=True), reads=[d_r], writes=[self.d_ps[5]])
            S.op("dve", lambda e: e.tensor_copy(out=pendc[:], in_=self.ps[5][0:32, 0:1]), reads=[self.d_ps[5]], writes=[d_r])
            BVi = small("BVi", [32, NBLK], I32)
            BV = small("BV", [32, NBLK])
            dv(lambda e: e.iota(BVi[:], pattern=[[128, NBLK]], base=0, channel_multiplier=0), "pool")
            dv(lambda e: e.tensor_copy(out=BV[:], in_=BVi[:]), "pool")
            dv(lambda e: e.tensor_scalar(out=BV[:], in0=BV[:], scalar1=pendc[:, 0:1], scalar2=None, op0=ALU.is_ge))
            BEf = small("BEf", [1, NBLK])
            BEi = small("BEi", [1, NBLK], I32)
            S.op("pe", lambda e: e.matmul(self.ps[6][0:1, 0:NBLK], lhsT=self.ones[0:32, 0:1], rhs=BV[:], start=True, stop=True),
                 reads=[d_r, self.d_const], writes=[self.d_ps[6]])
            S.op("dve", lambda e: e.tensor_scalar_min(out=BEf[:], in0=self.ps[6][0:1, 0:NBLK], scalar1=float(N_EXP - 1)), reads=[self.d_ps[6]], writes=[d_r])
            dv(lambda e: e.tensor_copy(out=BEi[:], in_=BEf[:]))
            BEbc = small("BEbc", [128, NBLK])
            S.op("pe", lambda e: e.matmul(self.ps[7][:, 0:NBLK], lhsT=self.ones[0:32, :], rhs=BV[:], start=True, stop=True),
                 reads=[d_r, self.d_const], writes=[self.d_ps[7]])
            S.op("dve", lambda e: e.tensor_scalar_min(out=BEbc[:], in0=self.ps[7][:, 0:NBLK], scalar1=float(N_EXP - 1)), reads=[self.d_ps[7]], writes=[d_r])
            PKi = small("PKi", [128, KC], I32)
            PK = small("PK", [128, KC])
            dv(lambda e: e.iota(PKi[:], pattern=[[128, KC]], base=0, channel_multiplier=1), "pool")
            dv(lambda e: e.tensor_copy(out=PK[:], in_=PKi[:]), "pool")
            WIDXf = small("WIDXf", [128, NBLK, KC])
            WIDXg = small("WIDXg", [128, NBLK, KC], I32)
            WIDXd = small("WIDXd", [128, NBLK, 4], I32)
            dv(lambda e: e.tensor_scalar_add(out=BEbc[:], in0=BEbc[:], scalar1=float(l * N_EXP)))
            dv(lambda e: e.scalar_tensor_tensor(out=WIDXf[:], in0=BEbc[:].unsqueeze(2).broadcast_to([128, NBLK, KC]), scalar=float(D),
                                                in1=PK[:].unsqueeze(1).broadcast_to([128, NBLK, KC]), op0=ALU.mult, op1=ALU.add))
            dv(lambda e: e.tensor_copy(out=WIDXg[:], in_=WIDXf[:]))
            dv(lambda e: e.scalar_tensor_tensor(out=WIDXf[:, :, 0:4], in0=BEbc[:].unsqueeze(2).broadcast_to([128, NBLK, 4]), scalar=float(D_FF),
                                                in1=PK[:, 0:4].unsqueeze(1).broadcast_to([128, NBLK, 4]), op0=ALU.mult, op1=ALU.add))
            dv(lambda e: e.tensor_copy(out=WIDXd[:], in_=WIDXf[:, :, 0:4]))
            wg_rows = I["w_gate"].rearrange("l e k n -> (l e k) n")
            wu_rows = I["w_up"].rearrange("l e k n -> (l e k) n")
            wd_rows = I["w_down"].rearrange("l e k n -> (l e k) n")
            if "dbg_route" in self.phases:
                t1_ = self.dram("dbg_dst", [128, 2, NT], I32, kind="ExternalOutput").ap()
                t2_ = self.dram("dbg_be", [1, NBLK], I32, kind="ExternalOutput").ap()
                t3_ = self.dram("dbg_g", [128, 2, NT], F32, kind="ExternalOutput").ap()
                self.out_events.append(S.dma("sp", t1_, DSTi[:], reads=[d_r]))
                self.out_events.append(S.dma("sp", t2_, BEi[:], reads=[d_r]))
                gg_ = small("gg_", [128, 2, NT])
                dv(lambda e: e.tensor_copy(out=gg_[:, 0, :], in_=g0[:]))
                dv(lambda e: e.tensor_copy(out=gg_[:, 1, :], in_=g1[:]))
                self.out_events.append(S.dma("sp", t3_, gg_[:], reads=[d_r]))
            S.barrier()
            with ExitStack() as ph:
                hrow = [self.sb(ph, f"hrow{i}", [128, D], BF16) for i in range(2)]
                dhr = S.deps(2)
                for i in range(NT):
                    b = i % 2
                    S.dma("sp", hrow[b][:], self.h2_d[i * 128:(i + 1) * 128, :], reads=[self.d_h2], writes=[dhr[b]])
                    for k in range(2):
                        S.dma("pool", None, None, reads=[dhr[b], d_r], writes=[self.d_rows],
                              fn=lambda e, b=b, k=k, i=i: e.indirect_dma_start(
                                  out=self.rows_d[:, :], out_offset=bass.IndirectOffsetOnAxis(ap=DSTi[:, k, i:i + 1], axis=0),
                                  in_=hrow[b][:, :], in_offset=None))
            S.barrier()
            with ExitStack() as ph:
                identb = self.sb(ph, "identb2", [128, 128], BF16)
                d_ib = S.dep()
                S.op("dve", lambda e: e.tensor_copy(out=identb[:], in_=self.ident[:]), reads=[self.d_const], writes=[d_ib])
                NW = 2
                Wg = [self.sb(ph, f"Wg{i}", [128, KC, D_FF], BF16) for i in range(NW)]
                Wu = [self.sb(ph, f"Wu{i}", [128, KC, D_FF], BF16) for i in range(NW)]
                Wd = [self.sb(ph, f"Wd{i}", [128, 4, D], BF16) for i in range(NW)]
                dWt = S.deps(NW)
                rowsb = [self.sb(ph, f"rowsb{i}", [128, D], BF16) for i in range(2)]
                d_rb = S.deps(2)
                blkT = [self.sb(ph, f"blkT{i}", [128, KC, 128], BF16) for i in range(2)]
                d_bT = S.deps(2)
                sg = self.sb(ph, "sgate", [128, D_FF], F32)
                d_sg = S.dep()
                hid = self.sb(ph, "hid", [128, D_FF], BF16)
                d_hid = S.dep()
                hidT = self.sb(ph, "hidT", [128, 4, 128], BF16)
                d_hT = S.dep()
                yb = [self.sb(ph, f"yb{i}", [128, D], F32) for i in range(2)]
                d_yb = S.deps(2)
                psT = [self.ps[2].bitcast(BF16), self.ps[3].bitcast(BF16)]
                for blk in range(NBLK):
                    b = blk % 2
                    wb = blk % NW
                    S.dma("sp", rowsb[b][:], self.rows_d[blk * 128:(blk + 1) * 128, :], reads=[self.d_rows], writes=[d_rb[b]])
                    for kc in range(KC):
                        S.dma("pool", None, None, reads=[d_r], writes=[dWt[wb]],
                              fn=lambda e, wb=wb, kc=kc, blk=blk: e.indirect_dma_start(
                                  out=Wg[wb][:, kc, :], out_offset=None, in_=wg_rows,
                                  in_offset=bass.IndirectOffsetOnAxis(ap=WIDXg[:, blk, kc:kc + 1], axis=0)))
                        S.dma("pool", None, None, reads=[d_r], writes=[dWt[wb]],
                              fn=lambda e, wb=wb, kc=kc, blk=blk: e.indirect_dma_start(
                                  out=Wu[wb][:, kc, :], out_offset=None, in_=wu_rows,
                                  in_offset=bass.IndirectOffsetOnAxis(ap=WIDXg[:, blk, kc:kc + 1], axis=0)))
                    for f in range(4):
                        S.dma("pool", None, None, reads=[d_r], writes=[dWt[wb]],
                              fn=lambda e, wb=wb, f=f, blk=blk: e.indirect_dma_start(
                                  out=Wd[wb][:, f, :], out_offset=None, in_=wd_rows,
                                  in_offset=bass.IndirectOffsetOnAxis(ap=WIDXd[:, blk, f:f + 1], axis=0)))
                    for q4 in range(4):
                        pt = psT[q4 % 2]
                        dpt = self.d_ps[2 + q4 % 2]
                        for j in range(4):
                            kc = q4 * 4 + j
                            S.op("pe", lambda e, pt=pt, b=b, kc=kc, j=j: e.transpose(pt[:, j * 128:(j + 1) * 128], rowsb[b][:, kc * 128:(kc + 1) * 128], identb[:]),
                                 reads=[d_rb[b], d_ib], writes=[dpt])
                        if q4 % 2 == 0:
                            S.op("act", lambda e, pt=pt, b=b, q4=q4: e.activation(out=blkT[b][:, q4 * 4:(q4 + 1) * 4, :],
                                                                                   in_=pt[:, 0:512].rearrange("p (j n) -> p j n", j=4), func=AF.Copy),
                                 reads=[dpt], writes=[d_bT[b]])
                        else:
                            S.op("dve", lambda e, pt=pt, b=b, q4=q4: e.tensor_copy(out=blkT[b][:, q4 * 4:(q4 + 1) * 4, :],
                                                                                    in_=pt[:, 0:512].rearrange("p (j n) -> p j n", j=4)),
                                 reads=[dpt], writes=[d_bT[b]])
                    for (pb, Wt) in ((0, Wg[wb]), (1, Wu[wb])):
                        for kc in range(KC):
                            S.op("pe", lambda e, pb=pb, Wt=Wt, kc=kc, b=b: e.matmul(self.ps[pb][:], lhsT=blkT[b][:, kc, :], rhs=Wt[:, kc, :],
                                                                                     start=(kc == 0), stop=(kc == KC - 1)),
                                 reads=[d_bT[b], dWt[wb]], writes=[self.d_ps[pb]])
                    S.op("act", lambda e: e.activation(out=sg[:], in_=self.ps[0][:], func=AF.Silu), reads=[self.d_ps[0]], writes=[d_sg])
                    S.op("dve", lambda e: e.tensor_tensor(out=hid[:], in0=sg[:], in1=self.ps[1][:], op=ALU.mult),
                         reads=[d_sg, self.d_ps[1]], writes=[d_hid])
                    pt, dpt = psT[0], self.d_ps[2]
                    for f in range(4):
                        S.op("pe", lambda e, f=f, pt=pt: e.transpose(pt[:, f * 128:(f + 1) * 128], hid[:, f * 128:(f + 1) * 128], identb[:]),
                             reads=[d_hid, d_ib], writes=[dpt])
                    S.op("act", lambda e, pt=pt: e.activation(out=hidT[:], in_=pt[:, 0:512].rearrange("p (j n) -> p j n", j=4), func=AF.Copy),
                         reads=[dpt], writes=[d_hT])
                    for cgp in range(4):
                        pb = 4 + cgp
                        for f in range(4):
                            S.op("pe", lambda e, pb=pb, f=f, cgp=cgp, wb=wb: e.matmul(self.ps[pb][:], lhsT=hidT[:, f, :],
                                                                                       rhs=Wd[wb][:, f, cgp * 512:(cgp + 1) * 512],
                                                                                       start=(f == 0), stop=(f == 3)),
                                 reads=[d_hT, dWt[wb]], writes=[self.d_ps[pb]])
                        if cgp % 2 == 0:
                            S.op("act", lambda e, pb=pb, cgp=cgp, b=b: e.activation(out=yb[b][:, cgp * 512:(cgp + 1) * 512], in_=self.ps[pb][:], func=AF.Copy),
                                 reads=[self.d_ps[pb]], writes=[d_yb[b]])
                        else:
                            S.op("dve", lambda e, pb=pb, cgp=cgp, b=b: e.tensor_copy(out=yb[b][:, cgp * 512:(cgp + 1) * 512], in_=self.ps[pb][:]),
                                 reads=[self.d_ps[pb]], writes=[d_yb[b]])
                    S.dma("sp", self.yrows_d[blk * 128:(blk + 1) * 128, :], yb[b][:], reads=[d_yb[b]], writes=[self.d_yrows])
            S.barrier()
            with ExitStack() as ph:
                G2 = self.sb(ph, "G2bc", [128, D], F32)
                d_G2 = S.dep()
                S.dma("sp", G2[:], self.mod_d[self.bi, l, 5], reads=[self.d_mod], writes=[d_G2])
                y0 = [self.sb(ph, f"y0_{i}", [128, D], F32) for i in range(2)]
                y1 = [self.sb(ph, f"y1_{i}", [128, D], F32) for i in range(2)]
                xt = [self.sb(ph, f"cx{i}", [128, D], F32) for i in range(2)]
                d_y0, d_y1, d_cx = S.deps(2), S.deps(2), S.deps(2)
                for i in range(NT):
                    b = i % 2
                    rows = slice(self.r0 + i * 128, self.r0 + (i + 1) * 128)
                    for (yt, dy, k) in ((y0[b], d_y0[b], 0), (y1[b], d_y1[b], 1)):
                        S.dma("pool", None, None, reads=[self.d_yrows, d_r], writes=[dy],
                              fn=lambda e, yt=yt, k=k, i=i: e.indirect_dma_start(
                                  out=yt[:, :], out_offset=None, in_=self.yrows_d[:, :],
                                  in_offset=bass.IndirectOffsetOnAxis(ap=DSTi[:, k, i:i + 1], axis=0)))
                    S.dma("sp", xt[b][:], self.x_d[rows, :], reads=[self.d_xt[i]], writes=[d_cx[b]])
                    S.op("dve", lambda e, b=b, i=i: e.tensor_scalar_mul(out=y0[b][:], in0=y0[b][:], scalar1=g0[:, i:i + 1]),
                         reads=[d_y0[b], d_r], writes=[d_y0[b]])
                    S.op("dve", lambda e, b=b, i=i: e.scalar_tensor_tensor(out=y0[b][:], in0=y1[b][:], scalar=g1[:, i:i + 1], in1=y0[b][:],
                                                                           op0=ALU.mult, op1=ALU.add),
                         reads=[d_y0[b], d_y1[b], d_r], writes=[d_y0[b]])
                    S.op("pool", lambda e, b=b: e.tensor_tensor(out=y0[b][:], in0=y0[b][:], in1=G2[:], op=ALU.mult),
                         reads=[d_y0[b], d_G2], writes=[d_y0[b]])
                    S.op("pool", lambda e, b=b: e.tensor_tensor(out=xt[b][:], in0=xt[b][:], in1=y0[b][:], op=ALU.add),
                         reads=[d_y0[b], d_cx[b]], writes=[d_cx[b]])
                    S.dma("sp", self.x_d[rows, :], xt[b][:], reads=[d_cx[b]], writes=[self.d_xt[i]])
        S.barrier()

    def dump_act(self):
        S = self.S
        t = self.dram("act_dump", [128, KC, self.T], BF16, kind="ExternalOutput").ap()
        self.out_events.append(S.dma("sp", t, self.actT[:], reads=self.d_act))

    def build(self):
        self.declare()
        self.consts()
        self.phase_init()
        P = self.phases
        if "ada" in P:
            for b in range(self.NB):
                self.bi = b
                self.phase_ada()
        for l in range(self.L):
            for b in range(self.NB):
                for g in range(self.NSEG):
                    self.bi = b
                    self.r0 = (b * self.NSEG + g) * self.T
                    self.c0 = TP + g * self.T
                    self.seg_first = (g == 0)
                    with ExitStack() as mx:
                        self.actT = self.sb(mx, "actT", [128, KC, self.T], BF16)
                        if "norm1" in P:
                            self.phase_norm(l, 1)
                        if "proj" in P:
                            self.phase_proj(l)
                        if "attn_c" in P:
                            self.phase_attn_c(l)
                        if "attn_a" in P:
                            self.phase_attn_a(l)
                        if "dn" in P:
                            self.phase_dn(l)
                        if "dump_act" in P:
                            self.dump_act()
                        if "outproj" in P:
                            self.phase_outproj(l)
                        self.S.barrier()
                    if "moe" in P:
                        self.phase_moe(l)
        if "final" in P:
            for s_ in range(self.NB * self.NSEG):
                self.r0 = s_ * self.T
                self.phase_norm(0, "final")
        if "dump_x" in P:
            t = self.dram("x_dump", [self.NB * self.NSEG * self.T, D], F32, kind="ExternalOutput").ap()
            self.out_events.append(self.S.dma("sp", t, self.x_d, reads=[self.d_x] + self.d_xt))
        self.S.barrier()
        self.S.finish(self.out_events)
        self.st.close()


def host_inputs(inputs, batches, L, S_full=None):
    f = lambda a: np.ascontiguousarray(np.asarray(a, dtype=np.float32))
    m = {}
    xs = np.asarray(inputs["x"])
    m["x"] = f(np.concatenate([xs[b] for b in batches], axis=0))
    cc = np.asarray(inputs["c"])
    m["cT"] = f(np.stack([cc[b].reshape(KC, 128).T for b in batches], axis=0))
    m["norm1_g"] = f(inputs["norm1_g"][:L])
    m["norm2_g"] = f(inputs["norm2_g"][:L])
    m["ada_w"] = f(inputs["ada_w"][:L])
    m["ada_b"] = f(inputs["ada_b"][:L])
    m["w_in"] = f(inputs["w_in"][:L])
    cw = np.asarray(inputs["dn_conv_w"])[:L]
    m["conv_w"] = f(cw.transpose(2, 0, 1).reshape(16, 128, L, CONV_K).transpose(1, 2, 0, 3))
    m["a_log"] = f(np.asarray(inputs["dn_a_log"])[:L].reshape(1, L * 8))
    m["dt_bias"] = f(np.asarray(inputs["dn_dt_bias"])[:L].reshape(1, L * 8))
    m["dn_norm_g"] = f(np.asarray(inputs["dn_norm_g"])[:L].reshape(L, 128, 1))
    m["sinks"] = f(np.asarray(inputs["attn_sinks"])[:L].reshape(1, L * 8))
    m["w_out"] = f(inputs["w_out"][:L])
    m["rw"] = f(np.concatenate([np.asarray(inputs["router_group_w"])[:L], np.asarray(inputs["router_expert_w"])[:L]], axis=-1))
    m["rb"] = f(np.concatenate([np.asarray(inputs["router_group_b"])[:L], np.asarray(inputs["router_expert_b"])[:L]], axis=-1))
    if "expert_w_gate" in inputs:
        m["w_gate"] = f(inputs["expert_w_gate"][:L])
        m["w_up"] = f(inputs["expert_w_up"][:L])
        m["w_down"] = f(inputs["expert_w_down"][:L])
    m["final_g"] = f(np.asarray(inputs["final_norm_g"]).reshape(1, D))
    return m


ALL_PHASES = ("ada", "norm1", "proj", "attn_c", "attn_a", "dn", "outproj", "moe", "final")
N_CORES_USED = 2
SEG_T = 2048


def kernel(**inputs):
    x = np.asarray(inputs["x"])
    Bsz, S_full, _ = x.shape
    L = int(np.asarray(inputs["w_in"]).shape[0])
    nseg = S_full // SEG_T
    nb = Bsz // N_CORES_USED
    nc = bass.Bass("TRN2", target_bir_lowering=False)
    bld = Builder(nc, SEG_T, L, phases=ALL_PHASES, NB=nb, NSEG=nseg)
    bld.build()
    in_maps = []
    for c in range(N_CORES_USED):
        m = host_inputs(inputs, list(range(c * nb, (c + 1) * nb)), L)
        in_maps.append({k: v for k, v in m.items() if k in bld.I})
    res = run_bass_kernel_spmd(nc, in_maps, core_ids=list(range(N_CORES_USED)))
    outs = [np.asarray(res.results[c]["y_out"], dtype=np.float32).reshape(nb, S_full, D) for c in range(N_CORES_USED)]
    return np.concatenate(outs, axis=0)
```

```python
import numpy as np
import concourse.bass as bass
import concourse.mybir as mybir
from concourse.bass_utils import run_bass_kernel_spmd
from contextlib import ExitStack

F32 = mybir.dt.float32
BF16 = mybir.dt.bfloat16
I32 = mybir.dt.int32
U32 = mybir.dt.uint32
AF = mybir.ActivationFunctionType
ALU = mybir.AluOpType
AX = mybir.AxisListType
ds = bass.ds if hasattr(bass, "ds") else None

D = 2048
KC = D // 128
H_A, DH_A = 8, 64
PATTERNS = ((128, 1), (512, 4), (2048, 16))
H_K_B, H_V_B, DK_B, DV_B = 4, 8, 128, 128
CONV_K = 4
H_C, HKV_C, DH_C = 8, 2, 64
A_W = 512
BK_W = 512
BV_W = 1024
C_Q_W = 512
C_KV_W = 128
N_IN = 5392
MIX = 2048
N_EXP = 32
D_FF = 512
EPS = 1e-6
O_AQ, O_AK, O_AV = 0, 512, 1024
O_BQ, O_BK, O_BV, O_BZ = 1536, 2048, 2560, 3584
O_BB, O_BA = 4608, 4616
O_CQ, O_CK, O_CV = 4624, 5136, 5264
TP = 2048
NEG = -1.0e30

ENGS = ("pe", "act", "dve", "pool", "sp")
SAME_ENGINE_SYNC = True
NO_SELF_SYNC = ("pe",)
N_DMA_SEMS = 16
SEM_EPOCH = 30000


def sl(c0, n, step=1):
    return slice(c0, c0 + (n - 1) * step + 1, step)


class Dep:
    __slots__ = ("w", "r", "name")

    def __init__(self, name=""):
        self.w = None
        self.r = {}
        self.name = name


class Sched:
    def __init__(self, nc, stack):
        self.nc = nc
        self.stack = stack
        self.eng_obj = {"pe": nc.tensor, "act": nc.scalar, "dve": nc.vector,
                        "pool": nc.gpsimd, "sp": nc.sync}
        self.sems = {}
        self.nsem = 0
        self.ekey = {}
        self.ecount = {}
        self.allkeys = {e: [] for e in ENGS}
        for e in ENGS:
            self._new_epoch(e)
        self.waited = {e: {} for e in ENGS}
        self.dma_keys = {}
        self.dma_val = {}
        self.dma_rr = {}
        for q in ("sp", "pool"):
            ks = []
            for i in range(N_DMA_SEMS):
                k = self._newsem(f"d_{q}_{i}")
                ks.append(k)
                self.dma_val[k] = 0
            self.dma_keys[q] = ks
            self.dma_rr[q] = 0
        self.n_ins = 0

    def _newsem(self, name):
        k = self.nsem
        self.nsem += 1
        self.sems[k] = self.stack.enter_context(self.nc.semaphore(f"s{k}_{name}"))
        return k

    def _new_epoch(self, e):
        self.ekey[e] = self._newsem(f"e_{e}")
        self.ecount[e] = 0
        self.allkeys[e].append(self.ekey[e])

    def dep(self, name=""):
        return Dep(name)

    def deps(self, n, name=""):
        return [Dep(f"{name}{i}") for i in range(n)]

    def _wait(self, eng, ev):
        if ev is None:
            return
        k, v = ev
        if (not SAME_ENGINE_SYNC or eng in NO_SELF_SYNC) and k == self.ekey.get(eng):
            return
        if self.waited[eng].get(k, 0) >= v:
            return
        self.waited[eng][k] = v
        self.eng_obj[eng].wait_ge(self.sems[k], v)

    def _collect(self, eng, reads, writes):
        for d in reads:
            self._wait(eng, d.w)
        for d in writes:
            self._wait(eng, d.w)
            for k, v in d.r.items():
                self._wait(eng, (k, v))

    def _update(self, ev, reads, writes):
        k, v = ev
        for d in reads:
            if d.r.get(k, 0) < v:
                d.r[k] = v
        for d in writes:
            d.w = ev
            d.r = {}

    def op(self, eng, fn, reads=(), writes=()):
        if self.ecount[eng] >= SEM_EPOCH:
            self._new_epoch(eng)
        self._collect(eng, reads, writes)
        k = self.ekey[eng]
        self.ecount[eng] += 1
        ev = (k, self.ecount[eng])
        fn(self.eng_obj[eng]).then_inc(self.sems[k], 1)
        self._update(ev, reads, writes)
        self.n_ins += 1
        return ev

    def dma(self, q, out, in_, reads=(), writes=(), fn=None, **kw):
        ks = self.dma_keys[q]
        k = ks[self.dma_rr[q] % len(ks)]
        self.dma_rr[q] += 1
        if self.dma_val[k] > 0:
            self._wait(q, (k, self.dma_val[k]))
        self._collect(q, reads, writes)
        self.dma_val[k] += 16
        ev = (k, self.dma_val[k])
        if fn is not None:
            ins = fn(self.eng_obj[q])
        else:
            ins = self.eng_obj[q].dma_start(out=out, in_=in_, **kw)
        ins.then_inc(self.sems[k], 16)
        self._update(ev, reads, writes)
        self.n_ins += 1
        return ev

    def barrier(self):
        evs = []
        for e in ENGS:
            if self.ecount[e] > 0:
                evs.append((self.ekey[e], self.ecount[e]))
        for k, v in self.dma_val.items():
            if v > 0:
                evs.append((k, v))
        for e in ENGS:
            for ev in evs:
                if ev[0] == self.ekey[e]:
                    continue
                self._wait(e, ev)

    def finish(self, final_events=()):
        for ev in final_events:
            self._wait("sp", ev)


class Builder:
    def __init__(self, nc, T, L, n_cores=1, dbg=(), phases=("ada", "norm1", "proj"), NB=1, NSEG=1):
        self.nc = nc
        self.T = T
        self.L = L
        self.NB = NB
        self.NSEG = NSEG
        self.c0 = TP
        self.r0 = 0
        self.bi = 0
        self.seg_first = True
        self.NT = T // 128
        self.n_cores = n_cores
        self.dbg = set(dbg)
        self.phases = phases
        self.st = ExitStack()
        self.S = Sched(nc, self.st)
        self.out_events = []
        self.dn_stop = 99
        self.dn_sub = 99
        self.dn_fast = False

    def sb(self, stack, name, shape, dt):
        self._uid = getattr(self, "_uid", 0) + 1
        return stack.enter_context(self.nc.sbuf_tensor(f"{name}_u{self._uid}", list(shape), dt))

    def dram(self, name, shape, dt, kind=None):
        if kind is None:
            kind = "ExternalOutput" if name in self.dbg else "Internal"
        return self.nc.dram_tensor(name, list(shape), dt, kind=kind)

    def inp(self, name, shape, dt=F32):
        return self.nc.dram_tensor(name, list(shape), dt, kind="ExternalInput").ap()

    def declare(self):
        T, L = self.T, self.L
        I = {}
        NTOK = self.NB * self.NSEG * T
        I["x"] = self.inp("x", [NTOK, D])
        I["cT"] = self.inp("cT", [self.NB, 128, KC])
        I["norm1_g"] = self.inp("norm1_g", [L, D])
        I["norm2_g"] = self.inp("norm2_g", [L, D])
        I["ada_w"] = self.inp("ada_w", [L, D, 6 * D])
        I["ada_b"] = self.inp("ada_b", [L, 6 * D])
        I["w_in"] = self.inp("w_in", [L, D, N_IN])
        I["conv_w"] = self.inp("conv_w", [128, L, 16, CONV_K])
        I["a_log"] = self.inp("a_log", [1, L * 8])
        I["dt_bias"] = self.inp("dt_bias", [1, L * 8])
        I["dn_norm_g"] = self.inp("dn_norm_g", [L, 128, 1])
        I["sinks"] = self.inp("sinks", [1, L * 8])
        if "outproj" in self.phases:
            I["w_out"] = self.inp("w_out", [L, MIX, D])
        I["rw"] = self.inp("rw", [L, D, 36])
        I["rb"] = self.inp("rb", [L, 36])
        if "moe" in self.phases:
            I["w_gate"] = self.inp("w_gate", [L, N_EXP, D, D_FF])
            I["w_up"] = self.inp("w_up", [L, N_EXP, D, D_FF])
            I["w_down"] = self.inp("w_down", [L, N_EXP, D_FF, D])
        I["final_g"] = self.inp("final_g", [1, D])
        self.I = I
        self.y_out = self.nc.dram_tensor("y_out", [NTOK, D], F32, kind="ExternalOutput").ap()
        self.x_d = self.dram("x_d", [NTOK, D], F32).ap()
        self.mod_d = self.dram("mod_d", [self.NB, L, 6, 128, D], F32).ap()
        self.P_d = self.dram("P_d", [N_IN, TP + self.NSEG * T], F32).ap()
        self.NBLK = (2 * T) // 128 + N_EXP
        self.h2_d = self.dram("h2_d", [T, D], BF16).ap()
        self.rows_d = self.dram("rows_d", [self.NBLK * 128, D], BF16).ap()
        self.yrows_d = self.dram("yrows_d", [self.NBLK * 128, D], F32).ap()

    def consts(self):
        S, st = self.S, self.st
        self.reg_neg = self.nc.gpsimd.to_reg(NEG)
        self.reg_zero = self.nc.gpsimd.to_reg(0.0)
        self.ident = self.sb(st, "ident", [128, 128], F32)
        self.d_const = S.dep("const")
        d = self.d_const
        S.op("pool", lambda e: e.memset(self.ident[:], 1.0), writes=[d])
        S.op("pool", lambda e: e.affine_select(out=self.ident[:], in_=self.ident[:], pattern=[[-1, 128]],
                                                compare_op=ALU.is_equal, fill=self.reg_zero, base=0, channel_multiplier=1),
             reads=[d], writes=[d])
        self.ones = self.sb(st, "ones", [128, 128], F32)
        S.op("pool", lambda e: e.memset(self.ones[:], 1.0), writes=[d])
        self.epsc = self.sb(st, "epsc", [128, 1], F32)
        S.op("pool", lambda e: e.memset(self.epsc[:], EPS), writes=[d])
        self.zeros = self.sb(st, "zeros", [128, 2048], BF16)
        S.op("pool", lambda e: e.memset(self.zeros[:], 0.0), writes=[d])
        self.SCARRY = self.sb(st, "SCARRY", [128, 8, 128], F32)
        self.d_carry = S.dep("carry")
        self.d_act = S.deps(self.NT, "act")
        self.d_xt = S.deps(self.NT, "xt")
        self.d_h2 = S.dep("h2_d")
        self.d_rows = S.dep("rows_d")
        self.d_yrows = S.dep("yrows_d")
        self.ps = [st.enter_context(self.nc.psum_tensor(f"ps{i}", [128, 512], F32)) for i in range(8)]
        self.d_ps = S.deps(8, "ps")
        self.d_x = S.dep("x_d")
        self.d_mod = S.dep("mod_d")
        self.d_P = S.dep("P_d")

    def phase_init(self):
        S = self.S
        T = self.T
        S.dma("sp", self.x_d, self.I["x"], writes=[self.d_x])
        r = 0
        while r < N_IN:
            n = min(128, N_IN - r)
            S.dma("pool", self.P_d[r:r + n, 0:TP], self.zeros[:n, 0:TP], reads=[self.d_const], writes=[self.d_P])
            r += n
        for bb_ in range(self.NBLK):
            S.dma("sp", self.rows_d[bb_ * 128:(bb_ + 1) * 128, :], self.zeros[:, :], reads=[self.d_const], writes=[self.d_rows])
        S.barrier()

    def phase_ada(self):
        S, nc, I = self.S, self.nc, self.I
        with ExitStack() as ph:
            cT = self.sb(ph, "cT", [128, KC], F32)
            cact = self.sb(ph, "cact", [128, KC], F32)
            CB = self.sb(ph, "CB", [128, KC, 128], F32)
            d_c = S.dep()
            S.dma("sp", cT[:], I["cT"][self.bi], writes=[d_c])
            S.op("act", lambda e: e.activation(out=cact[:], in_=cT[:], func=AF.Silu), reads=[d_c], writes=[d_c])
            S.op("dve", lambda e: e.tensor_copy(out=CB[:], in_=cact[:].unsqueeze(2).broadcast_to([128, KC, 128])),
                 reads=[d_c], writes=[d_c])
            NB = 2
            W = [self.sb(ph, f"adaW{i}", [128, KC, 512], F32) for i in range(NB)]
            dW = S.deps(NB)
            bb = [self.sb(ph, f"adab{i}", [128, 512], F32) for i in range(NB)]
            gg = [self.sb(ph, f"adag{i}", [128, 512], F32) for i in range(NB)]
            dB = S.deps(NB)
            ot = [self.sb(ph, f"adao{i}", [128, 512], F32) for i in range(NB)]
            dO = S.deps(NB)
            it = 0
            for l in range(self.L):
                wv = I["ada_w"][l].rearrange("(kc p) n -> p kc n", p=128)
                for cg in range(24):
                    b = it % NB
                    it += 1
                    part, sub = cg // 4, cg % 4
                    cs = slice(cg * 512, (cg + 1) * 512)
                    fs = slice(sub * 512, (sub + 1) * 512)
                    S.dma("sp", W[b][:], wv[:, :, cs], writes=[dW[b]])
                    S.dma("sp", bb[b][:], I["ada_b"][l:l + 1, cs].partition_broadcast(128), writes=[dB[b]])
                    is_sc = part in (1, 4)
                    if is_sc:
                        gsrc = I["norm1_g"] if part == 1 else I["norm2_g"]
                        S.dma("sp", gg[b][:], gsrc[l:l + 1, fs].partition_broadcast(128), writes=[dB[b]])
                    pb = b
                    for kc in range(KC):
                        S.op("pe", lambda e, kc=kc, b=b, pb=pb: e.matmul(self.ps[pb][:], lhsT=CB[:, kc, :], rhs=W[b][:, kc, :],
                                                                           start=(kc == 0), stop=(kc == KC - 1)),
                             reads=[d_c, dW[b]], writes=[self.d_ps[pb]])
                    S.op("dve", lambda e, b=b, pb=pb: e.tensor_tensor(out=ot[b][:], in0=self.ps[pb][:], in1=bb[b][:], op=ALU.add),
                         reads=[self.d_ps[pb], dB[b]], writes=[dO[b]])
                    if is_sc:
                        S.op("dve", lambda e, b=b: e.scalar_tensor_tensor(out=ot[b][:], in0=ot[b][:], scalar=1.0, in1=gg[b][:],
                                                                           op0=ALU.add, op1=ALU.mult),
                             reads=[dB[b], dO[b]], writes=[dO[b]])
                    S.dma("sp", self.mod_d[self.bi, l, part, :, fs], ot[b][:], reads=[dO[b]], writes=[self.d_mod])
        S.barrier()

    def phase_norm(self, l, which, router=None):
        S, nc, I = self.S, self.nc, self.I
        T, NT = self.T, self.NT
        with ExitStack() as ph:
            Abc = self.sb(ph, "Abc", [128, D], F32)
            Bbc = self.sb(ph, "Bbc", [128, D], F32)
            d_ab = S.dep()
            if which == "final":
                S.dma("sp", Abc[:], I["final_g"][0:1, :].partition_broadcast(128), writes=[d_ab])
            else:
                pa, pb_ = (1, 0) if which == 1 else (4, 3)
                S.dma("sp", Abc[:], self.mod_d[self.bi, l, pa], reads=[self.d_mod], writes=[d_ab])
                S.dma("sp", Bbc[:], self.mod_d[self.bi, l, pb_], reads=[self.d_mod], writes=[d_ab])
            NB = 2
            xt = [self.sb(ph, f"xt{i}", [128, D], F32) for i in range(NB)]
            dx = S.deps(NB)
            hf = [self.sb(ph, f"hf{i}", [128, D], F32) for i in range(NB)]
            dh = S.deps(NB)
            junk = self.sb(ph, "junk", [128, D], BF16)
            d_junk = S.dep()
            st_ = [self.sb(ph, f"nst{i}", [128, 4], F32) for i in range(NB)]
            dst_ = S.deps(NB)
            if router is not None:
                hT32 = [self.sb(ph, f"hT32_{i}", [128, KC, 128], F32) for i in range(NB)]
                dhT = S.deps(NB)
            for i in range(NT):
                b = i % NB
                rows = slice(self.r0 + i * 128, self.r0 + (i + 1) * 128)
                S.dma("sp", xt[b][:], self.x_d[rows, :], reads=[self.d_x, self.d_xt[i]], writes=[dx[b]])
                S.op("act", lambda e, b=b: e.activation(out=junk[:], in_=xt[b][:], func=AF.Square, accum_out=st_[b][:, 0:1]),
                     reads=[dx[b]], writes=[d_junk, dst_[b]])
                S.op("act", lambda e, b=b: e.activation(out=st_[b][:, 1:2], in_=st_[b][:, 0:1], func=AF.Sqrt,
                                                        scale=1.0 / D, bias=self.epsc[:, 0:1]),
                     reads=[dst_[b], self.d_const], writes=[dst_[b]])
                S.op("dve", lambda e, b=b: e.reciprocal(out=st_[b][:, 2:3], in_=st_[b][:, 1:2]),
                     reads=[dst_[b]], writes=[dst_[b]])
                S.op("dve", lambda e, b=b: e.scalar_tensor_tensor(out=hf[b][:], in0=xt[b][:], scalar=st_[b][:, 2:3], in1=Abc[:],
                                                                   op0=ALU.mult, op1=ALU.mult),
                     reads=[dx[b], dst_[b], d_ab], writes=[dh[b]])
                if which == "final":
                    self.out_events.append(S.dma("sp", self.y_out[rows, :], hf[b][:], reads=[dh[b]]))
                    continue
                S.op("pool", lambda e, b=b: e.tensor_tensor(out=hf[b][:], in0=hf[b][:], in1=Bbc[:], op=ALU.add),
                     reads=[d_ab, dh[b]], writes=[dh[b]])
                if router is not None:
                    S.op("pool", lambda e, b=b: e.tensor_copy(out=router["hbf"][b][:], in_=hf[b][:]), reads=[dh[b]], writes=[router["d_hbf"][b]])
                    S.dma("sp", self.h2_d[i * 128:(i + 1) * 128, :], router["hbf"][b][:], reads=[router["d_hbf"][b]], writes=[self.d_h2])
                for q4 in range(4):
                    pb = (i * 4 + q4) % 4
                    for j in range(4):
                        kc = q4 * 4 + j
                        S.op("pe", lambda e, b=b, kc=kc, pb=pb, j=j: e.transpose(self.ps[pb][:, j * 128:(j + 1) * 128],
                                                                                   hf[b][:, kc * 128:(kc + 1) * 128], self.ident[:]),
                             reads=[dh[b], self.d_const], writes=[self.d_ps[pb]])
                    if router is None:
                        S.op("act", lambda e, pb=pb, q4=q4, i=i: e.activation(
                            out=self.actT[:, q4 * 4:(q4 + 1) * 4, i * 128:(i + 1) * 128],
                            in_=self.ps[pb][:].rearrange("p (j n) -> p j n", j=4), func=AF.Copy),
                            reads=[self.d_ps[pb]], writes=[self.d_act[i]])
                    if router is not None:
                        S.op("dve", lambda e, pb=pb, q4=q4, b=b: e.tensor_copy(
                            out=hT32[b][:, q4 * 4:(q4 + 1) * 4, :],
                            in_=self.ps[pb][:].rearrange("p (j n) -> p j n", j=4)),
                            reads=[self.d_ps[pb]], writes=[dhT[b]])
                if router is not None:
                    pr = 4 + (i % 2)
                    for kc in range(KC):
                        S.op("pe", lambda e, b=b, kc=kc, pr=pr: e.matmul(self.ps[pr][:, 0:36], lhsT=hT32[b][:, kc, :],
                                                                          rhs=router["RW"][:, kc, :],
                                                                          start=(kc == 0), stop=(kc == KC - 1)),
                             reads=[dhT[b], router["d_rw"]], writes=[self.d_ps[pr]])
                    S.op("dve", lambda e, pr=pr, i=i: e.tensor_tensor(out=router["LG"][:, i, :], in0=self.ps[pr][:, 0:36],
                                                                       in1=router["RB"][:], op=ALU.add),
                         reads=[self.d_ps[pr], router["d_rw"]], writes=[router["d_lg"]])
        S.barrier()

    def phase_proj(self, l):
        S, nc, I = self.S, self.nc, self.I
        T = self.T
        NTT = T // 512
        wv = I["w_in"][l].rearrange("(kc p) n -> p kc n", p=128)
        with ExitStack() as ph:
            NB = 2
            W = [self.sb(ph, f"pjW{i}", [128, KC, 512], BF16) for i in range(NB)]
            dW = S.deps(NB)
            stg = [self.sb(ph, f"pjS{i}", [128, T], F32) for i in range(NB)]
            dS_ = S.deps(NB)
            nsg = (N_IN + 511) // 512
            cnt = 0
            for sg in range(nsg):
                c0 = sg * 512
                cw = min(512, N_IN - c0)
                b = sg % NB
                S.dma("pool", W[b][:, :, 0:cw], wv[:, :, c0:c0 + cw], writes=[dW[b]])
                for j in range((cw + 127) // 128):
                    gs = min(128, cw - j * 128)
                    sb_ = cnt % NB
                    for tt in range(NTT):
                        pb = cnt % 8
                        cnt += 1
                        for kc in range(KC):
                            S.op("pe", lambda e, b=b, kc=kc, j=j, gs=gs, tt=tt, pb=pb: e.matmul(
                                self.ps[pb][0:gs, :], lhsT=W[b][:, kc, j * 128:j * 128 + gs],
                                rhs=self.actT[:, kc, tt * 512:(tt + 1) * 512], start=(kc == 0), stop=(kc == KC - 1)),
                                reads=[dW[b]] + self.d_act[tt * 4:(tt + 1) * 4], writes=[self.d_ps[pb]])
                        eng = "act" if (cnt % 2 == 0) else "dve"
                        if eng == "act":
                            S.op("act", lambda e, sb_=sb_, gs=gs, tt=tt, pb=pb: e.activation(
                                out=stg[sb_][0:gs, tt * 512:(tt + 1) * 512], in_=self.ps[pb][0:gs, :], func=AF.Copy),
                                reads=[self.d_ps[pb]], writes=[dS_[sb_]])
                        else:
                            S.op("dve", lambda e, sb_=sb_, gs=gs, tt=tt, pb=pb: e.tensor_copy(
                                out=stg[sb_][0:gs, tt * 512:(tt + 1) * 512], in_=self.ps[pb][0:gs, :]),
                                reads=[self.d_ps[pb]], writes=[dS_[sb_]])
                    r0 = c0 + j * 128
                    S.dma("sp", self.P_d[r0:r0 + gs, self.c0:self.c0 + T], stg[sb_][0:gs, :], reads=[dS_[sb_]], writes=[self.d_P])
        S.barrier()

    def attn_consts(self, ph):
        S = self.S
        d = S.dep("attnc")
        self.d_attnc = d
        reli = self.sb(ph, "reli", [128, 256], I32)
        self.REL = self.sb(ph, "REL", [128, 256], F32)
        S.op("pool", lambda e: e.iota(reli[:], pattern=[[-1, 256]], base=128, channel_multiplier=1), writes=[d])
        S.op("pool", lambda e: e.tensor_copy(out=self.REL[:], in_=reli[:]), reads=[d], writes=[d])
        self.HM = self.sb(ph, "HM", [128, 256], F32)
        S.op("pool", lambda e: e.memset(self.HM[:], 0.0), writes=[d])
        S.op("pool", lambda e: e.memset(self.HM[:, 0:128], NEG), reads=[d], writes=[d])
        self.E2 = self.sb(ph, "E2", [2, 128], F32)
        S.op("pool", lambda e: e.memset(self.E2[:], 1.0), writes=[d])
        S.op("pool", lambda e: e.affine_select(out=self.E2[:], in_=self.E2[:], pattern=[[1, 128]], compare_op=ALU.is_ge,
                                                fill=self.reg_zero, base=0, channel_multiplier=-64), reads=[d], writes=[d])
        S.op("pool", lambda e: e.affine_select(out=self.E2[:], in_=self.E2[:], pattern=[[-1, 128]], compare_op=ALU.is_ge,
                                                fill=self.reg_zero, base=63, channel_multiplier=64), reads=[d], writes=[d])

    def make_bias(self, tile, dep, coef, maxd):
        S = self.S
        S.op("dve", lambda e: e.tensor_scalar_mul(out=tile[:], in0=self.REL[:], scalar1=float(coef)),
             reads=[self.d_attnc], writes=[dep])
        S.op("pool", lambda e: e.affine_select(out=tile[:], in_=tile[:], pattern=[[-1, 256]], compare_op=ALU.is_ge,
                                                fill=self.reg_neg, base=128, channel_multiplier=1), reads=[dep], writes=[dep])
        S.op("pool", lambda e: e.affine_select(out=tile[:], in_=tile[:], pattern=[[1, 256]], compare_op=ALU.is_ge,
                                                fill=self.reg_neg, base=maxd - 128, channel_multiplier=-1), reads=[dep], writes=[dep])

    def attn_unit(self, ctx, q_ap, k_ap, bias, d_bias, first, Vb0, Vb1, d_V, half, out_ap, out_deps,
                  sink_ap=None, lse_ap=None, d_lse=None, in_deps=()):
        S = self.S
        u = ctx["u"]
        ctx["u"] += 1
        b = u % 2
        pS, dS = self.ps[b], self.d_ps[b]
        pT, dT = ctx["psT"][b], self.d_ps[2 + b]
        pO, dO = self.ps[4 + b], self.d_ps[4 + b]
        s32, d_s = ctx["s32"][b], ctx["d_s32"][b]
        pbf, d_p = ctx["pbf"][b], ctx["d_pbf"][b]
        pTs, d_pT = ctx["pTs"][b], ctx["d_pTs"][b]
        stt, d_st = ctx["stt"][b], ctx["d_stt"][b]
        S.op("pe", lambda e: e.matmul(pS[:, 0:256], lhsT=q_ap, rhs=k_ap, start=True, stop=True),
             reads=list(in_deps), writes=[dS])
        S.op("dve", lambda e: e.scalar_tensor_tensor(out=s32[:], in0=pS[:, 0:256], scalar=0.125, in1=bias[:],
                                                      op0=ALU.mult, op1=ALU.add), reads=[dS, d_bias], writes=[d_s])
        if first:
            S.op("pool", lambda e: e.tensor_tensor(out=s32[:], in0=s32[:], in1=self.HM[:], op=ALU.add),
                 reads=[d_s, self.d_attnc], writes=[d_s])
        S.op("dve", lambda e: e.tensor_reduce(out=stt[:, 0:1], in_=s32[:], axis=AX.X, op=ALU.max, negate=True),
             reads=[d_s], writes=[d_st])
        if sink_ap is not None:
            S.op("dve", lambda e: e.scalar_tensor_tensor(out=stt[:, 0:1], in0=sink_ap, scalar=-1.0, in1=stt[:, 0:1],
                                                          op0=ALU.mult, op1=ALU.min), reads=[d_st, ctx["d_sink"]], writes=[d_st])
        S.op("act", lambda e: e.activation(out=pbf[:], in_=s32[:], func=AF.Exp, bias=stt[:, 0:1], scale=1.0,
                                            accum_out=stt[:, 1:2]), reads=[d_s, d_st], writes=[d_p, d_st])
        if sink_ap is not None:
            S.op("act", lambda e: e.activation(out=stt[:, 3:4], in_=sink_ap, func=AF.Exp, bias=stt[:, 0:1], scale=1.0),
                 reads=[d_st, ctx["d_sink"]], writes=[d_st])
            S.op("dve", lambda e: e.tensor_tensor(out=stt[:, 1:2], in0=stt[:, 1:2], in1=stt[:, 3:4], op=ALU.add),
                 reads=[d_st], writes=[d_st])
        S.op("dve", lambda e: e.reciprocal(out=stt[:, 2:3], in_=stt[:, 1:2]), reads=[d_st], writes=[d_st])
        S.op("pool", lambda e: e.tensor_scalar_mul(out=pbf[:], in0=pbf[:], scalar1=stt[:, 2:3]),
             reads=[d_p, d_st], writes=[d_p])
        if lse_ap is not None:
            S.op("act", lambda e: e.activation(out=stt[:, 3:4], in_=stt[:, 1:2], func=AF.Ln), reads=[d_st], writes=[d_st])
            S.op("dve", lambda e: e.tensor_tensor(out=lse_ap, in0=stt[:, 3:4], in1=stt[:, 0:1], op=ALU.subtract),
                 reads=[d_st], writes=[d_lse])
        for j in range(2):
            S.op("pe", lambda e, j=j: e.transpose(pT[:, j * 128:(j + 1) * 128], pbf[:, j * 128:(j + 1) * 128], ctx["identb"][:]),
                 reads=[d_p, ctx["d_identb"]], writes=[dT])
        S.op("act", lambda e: e.activation(out=pTs[:], in_=pT[:, 0:256], func=AF.Copy), reads=[dT], writes=[d_pT])
        S.op("pe", lambda e: e.matmul(pO[:, 0:128], lhsT=Vb0, rhs=pTs[:, 0:128], start=True, stop=False),
             reads=[d_V, d_pT], writes=[dO])
        S.op("pe", lambda e: e.matmul(pO[:, 0:128], lhsT=Vb1, rhs=pTs[:, 128:256], start=False, stop=True),
             reads=[d_V, d_pT], writes=[dO])
        rs = slice(half * 64, half * 64 + 64)
        S.op("dve", lambda e: e.tensor_copy(out=out_ap, in_=pO[rs, 0:128]), reads=[dO], writes=list(out_deps))

    def attn_ctx(self, ph):
        S = self.S
        ctx = {"u": 0}
        ctx["psT"] = [self.ps[2].bitcast(BF16), self.ps[3].bitcast(BF16)]
        ctx["s32"] = [self.sb(ph, f"s32_{i}", [128, 256], F32) for i in range(2)]
        ctx["d_s32"] = S.deps(2)
        ctx["pbf"] = [self.sb(ph, f"pbf_{i}", [128, 256], BF16) for i in range(2)]
        ctx["d_pbf"] = S.deps(2)
        ctx["pTs"] = [self.sb(ph, f"pTs_{i}", [128, 256], BF16) for i in range(2)]
        ctx["d_pTs"] = S.deps(2)
        ctx["stt"] = [self.sb(ph, f"stt_{i}", [128, 8], F32) for i in range(2)]
        ctx["d_stt"] = S.deps(2)
        identb = self.sb(ph, "identb", [128, 128], BF16)
        ctx["identb"] = identb
        ctx["d_identb"] = S.dep()
        S.op("dve", lambda e: e.tensor_copy(out=identb[:], in_=self.ident[:]), reads=[self.d_const], writes=[ctx["d_identb"]])
        return ctx

    def build_vblocks(self, ctx, vT, d_vT, Vblk, d_Vblk, specs):
        S = self.S
        for n, (idx, c0, step) in enumerate(specs):
            b = n % 2
            pT, dT = ctx["psT"][b], self.d_ps[2 + b]
            src = vT[:, sl(c0, 128, step)]
            S.op("pe", lambda e, pT=pT, src=src: e.transpose(pT[:, 0:128], src, ctx["identb"][:]),
                 reads=[d_vT, ctx["d_identb"]], writes=[dT])
            eng = "act" if n % 2 == 0 else "dve"
            if eng == "act":
                S.op("act", lambda e, pT=pT, idx=idx: e.activation(out=Vblk[:, idx, :], in_=pT[:, 0:128], func=AF.Copy),
                     reads=[dT], writes=[d_Vblk])
            else:
                S.op("dve", lambda e, pT=pT, idx=idx: e.tensor_copy(out=Vblk[:, idx, :], in_=pT[:, 0:128]),
                     reads=[dT], writes=[d_Vblk])

    def phase_attn_c(self, l):
        S, I = self.S, self.I
        T, NT = self.T, self.NT
        with ExitStack() as ph:
            self.attn_consts(ph)
            ctx = self.attn_ctx(ph)
            sink = self.sb(ph, "sink", [128, 8], F32)
            ctx["d_sink"] = S.dep()
            S.dma("sp", sink[:], I["sinks"][0:1, l * 8:(l + 1) * 8].partition_broadcast(128), writes=[ctx["d_sink"]])
            qc = self.sb(ph, "qc", [128, 4, T], BF16)
            kc = self.sb(ph, "kc", [128, TP + T], BF16)
            vN = self.sb(ph, "vN", [128, TP + T], BF16)
            vS = self.sb(ph, "vS", [128, TP + T], BF16)
            d_q, d_k, d_vn, d_vs = S.deps(4)
            for g in range(2):
                S.dma("pool", qc[64 * g:64 * g + 64, :, :],
                      self.P_d[O_CQ + 256 * g:O_CQ + 256 * (g + 1), self.c0:self.c0 + T].rearrange("(j p) t -> p j t", p=64),
                      reads=[self.d_P], writes=[d_q])
            hs = slice(self.c0 - TP, self.c0 + T)
            S.dma("pool", kc[:], self.P_d[O_CK:O_CK + 128, hs], reads=[self.d_P], writes=[d_k])
            S.dma("pool", vN[:], self.P_d[O_CV:O_CV + 128, hs], reads=[self.d_P], writes=[d_vn])
            S.dma("pool", vS[0:64, :], self.P_d[O_CV + 64:O_CV + 128, hs], reads=[self.d_P], writes=[d_vs])
            S.dma("pool", vS[64:128, :], self.P_d[O_CV:O_CV + 64, hs], reads=[self.d_P], writes=[d_vs])
            VN = self.sb(ph, "VN", [128, NT + 1, 128], BF16)
            VS = self.sb(ph, "VS", [128, NT + 1, 128], BF16)
            d_VN, d_VS = S.deps(2)
            specs = [(j + 1, TP + 128 * j, 1) for j in range(-1, NT)]
            self.build_vblocks(ctx, vN, d_vn, VN, d_VN, specs)
            self.build_vblocks(ctx, vS, d_vs, VS, d_VS, specs)
            bias = [self.sb(ph, f"biasc{h}", [128, 256], F32) for h in range(8)]
            d_b = S.deps(8)
            for h in range(8):
                self.make_bias(bias[h], d_b[h], -(2.0 ** (-(h + 1))), 127)
            for h in range(8):
                g, hh = h // 4, h % 2
                Vb, dV = (VN, d_VN) if hh == g else (VS, d_VS)
                for j in range(NT):
                    self.attn_unit(ctx,
                                   q_ap=qc[64 * g:64 * g + 64, h % 4, 128 * j:128 * (j + 1)],
                                   k_ap=kc[64 * g:64 * g + 64, TP + 128 * (j - 1):TP + 128 * (j + 1)],
                                   bias=bias[h], d_bias=d_b[h], first=(j == 0 and self.seg_first),
                                   Vb0=Vb[:, j, :], Vb1=Vb[:, j + 1, :], d_V=dV, half=hh,
                                   out_ap=self.actT[64 * hh:64 * hh + 64, 12 + h // 2, 128 * j:128 * (j + 1)],
                                   out_deps=[self.d_act[j]], sink_ap=sink[:, h:h + 1], in_deps=[d_q, d_k])
        S.barrier()

    def phase_attn_a(self, l):
        S, I = self.S, self.I
        T, NT = self.T, self.NT
        with ExitStack() as ph:
            self.attn_consts(ph)
            ctx = self.attn_ctx(ph)
            qa = self.sb(ph, "qa", [128, T], BF16)
            ka = self.sb(ph, "ka", [128, TP + T], BF16)
            va = self.sb(ph, "va", [128, TP + T], BF16)
            d_q, d_k, d_v = S.deps(3)
            maxblk = max(d * (T // (128 * d) + 1) for _, d in PATTERNS)
            Vblk = self.sb(ph, "Vblk", [128, maxblk, 128], BF16)
            d_Vb = S.dep()
            opT = [self.sb(ph, f"opT{p}", [128, T], BF16) for p in range(3)]
            d_op = S.deps(3)
            STAT = [self.sb(ph, f"STAT{p}", [128, NT * 2], F32) for p in range(3)]
            d_stat = S.deps(3)
            R = self.sb(ph, "R", [2, 3, T], F32)
            d_R = S.dep()
            Mx = self.sb(ph, "Mx", [2, T], F32)
            d_M = S.dep()
            bias = [self.sb(ph, f"biasa{i}", [128, 256], F32) for i in range(6)]
            d_b = S.deps(6)
            acc = self.sb(ph, "acca", [128, 512], F32)
            d_acc = S.dep()
            tmp = self.sb(ph, "tmpa", [128, 512], F32)
            d_tmp = S.dep()
            for ch in range(4):
                hs = slice(self.c0 - TP, self.c0 + T)
                S.dma("pool", qa[:], self.P_d[O_AQ + 128 * ch:O_AQ + 128 * (ch + 1), self.c0:self.c0 + T], reads=[self.d_P], writes=[d_q])
                S.dma("pool", ka[:], self.P_d[O_AK + 128 * ch:O_AK + 128 * (ch + 1), hs], reads=[self.d_P], writes=[d_k])
                S.dma("pool", va[:], self.P_d[O_AV + 128 * ch:O_AV + 128 * (ch + 1), hs], reads=[self.d_P], writes=[d_v])
                for p, (w, d) in enumerate(PATTERNS):
                    for hh in range(2):
                        h = 2 * ch + hh
                        self.make_bias(bias[p * 2 + hh], d_b[p * 2 + hh], -(2.0 ** (-(h + 1))) * d, 128)
                for p, (w, d) in enumerate(PATTERNS):
                    nbq = T // (128 * d)
                    specs = []
                    for r in range(d):
                        for j in range(-1, nbq):
                            specs.append((r * (nbq + 1) + j + 1, TP + r + d * 128 * j, d))
                    self.build_vblocks(ctx, va, d_v, Vblk, d_Vb, specs)
                    for hh in range(2):
                        ps_ = slice(64 * hh, 64 * hh + 64)
                        for r in range(d):
                            for j in range(nbq):
                                blk = r * nbq + j
                                q0 = r + d * 128 * j
                                k0 = TP + r + d * 128 * (j - 1)
                                q_ap = qa[ps_, sl(q0, 128, d)]
                                k_ap = ka[ps_, sl(k0, 256, d)]
                                o_ap = opT[p][ps_, sl(q0, 128, d)]
                                vi = r * (nbq + 1) + j
                                self.attn_unit(ctx, q_ap=q_ap, k_ap=k_ap, bias=bias[p * 2 + hh], d_bias=d_b[p * 2 + hh],
                                               first=(j == 0 and self.seg_first), Vb0=Vblk[:, vi, :], Vb1=Vblk[:, vi + 1, :], d_V=d_Vb, half=hh,
                                               out_ap=o_ap, out_deps=[d_op[p]],
                                               lse_ap=STAT[p][:, blk * 2 + hh:blk * 2 + hh + 1], d_lse=d_stat[p],
                                               in_deps=[d_q, d_k])
                    for r in range(d):
                        for j in range(nbq):
                            blk = r * nbq + j
                            q0 = r + d * 128 * j
                            pb = 6 + (blk % 2)
                            S.op("pe", lambda e, pb=pb, p=p, blk=blk: e.transpose(self.ps[pb][0:2, 0:128], STAT[p][:, blk * 2:blk * 2 + 2],
                                                                                   self.ident[:]),
                                 reads=[d_stat[p], self.d_const], writes=[self.d_ps[pb]])
                            dst = R[0:2, p, sl(q0, 128, d)]
                            S.op("act", lambda e, pb=pb, dst=dst: e.activation(out=dst, in_=self.ps[pb][0:2, 0:128], func=AF.Copy),
                                 reads=[self.d_ps[pb]], writes=[d_R])
                S.op("dve", lambda e: e.tensor_tensor(out=Mx[:], in0=R[:, 0, :], in1=R[:, 1, :], op=ALU.max), reads=[d_R], writes=[d_M])
                S.op("dve", lambda e: e.tensor_tensor(out=Mx[:], in0=Mx[:], in1=R[:, 2, :], op=ALU.max), reads=[d_R, d_M], writes=[d_M])
                for p in range(3):
                    S.op("dve", lambda e, p=p: e.tensor_tensor(out=R[:, p, :], in0=R[:, p, :], in1=Mx[:], op=ALU.subtract),
                         reads=[d_M, d_R], writes=[d_R])
                S.op("act", lambda e: e.activation(out=R[:], in_=R[:], func=AF.Exp), reads=[d_R], writes=[d_R])
                S.op("dve", lambda e: e.tensor_tensor(out=Mx[:], in0=R[:, 0, :], in1=R[:, 1, :], op=ALU.add), reads=[d_R, d_M], writes=[d_M])
                S.op("dve", lambda e: e.tensor_tensor(out=Mx[:], in0=Mx[:], in1=R[:, 2, :], op=ALU.add), reads=[d_R, d_M], writes=[d_M])
                S.op("dve", lambda e: e.reciprocal(out=Mx[:], in_=Mx[:]), reads=[d_M], writes=[d_M])
                for p in range(3):
                    S.op("dve", lambda e, p=p: e.tensor_tensor(out=R[:, p, :], in0=R[:, p, :], in1=Mx[:], op=ALU.mult),
                         reads=[d_M, d_R], writes=[d_R])
                for tt in range(T // 512):
                    cs = slice(tt * 512, (tt + 1) * 512)
                    for p in range(3):
                        pb = 6 + (p % 2)
                        S.op("pe", lambda e, pb=pb, p=p, cs=cs: e.matmul(self.ps[pb][:], lhsT=self.E2[:], rhs=R[0:2, p, cs],
                                                                          start=True, stop=True),
                             reads=[d_R, self.d_attnc], writes=[self.d_ps[pb]])
                        if p == 0:
                            S.op("dve", lambda e, pb=pb, cs=cs: e.tensor_tensor(out=acc[:], in0=opT[0][:, cs], in1=self.ps[pb][:], op=ALU.mult),
                                 reads=[d_op[0], self.d_ps[pb]], writes=[d_acc])
                        else:
                            S.op("dve", lambda e, pb=pb, cs=cs, p=p: e.tensor_tensor(out=tmp[:], in0=opT[p][:, cs], in1=self.ps[pb][:], op=ALU.mult),
                                 reads=[d_op[p], self.d_ps[pb]], writes=[d_tmp])
                            if p == 1:
                                S.op("pool", lambda e: e.tensor_tensor(out=acc[:], in0=acc[:], in1=tmp[:], op=ALU.add),
                                     reads=[d_tmp, d_acc], writes=[d_acc])
                            else:
                                S.op("pool", lambda e, cs=cs, ch=ch: e.tensor_tensor(out=self.actT[:, ch, cs], in0=acc[:], in1=tmp[:], op=ALU.add),
                                     reads=[d_tmp, d_acc], writes=self.d_act[tt * 4:(tt + 1) * 4])
        S.barrier()

    def phase_dn(self, l):
        S, I = self.S, self.I
        T, NT = self.T, self.NT
        DKS = float(DK_B) ** -0.5
        with ExitStack() as ph:
            cnt = {"s": 0}

            cnt["t"] = 0
            d_bank = [S.dep() for _ in range(8)]
            d_slot = [d_bank[i // 4] for i in range(32)]

            def slot():
                bk = 2 + cnt["s"] % 6
                cnt["s"] += 1
                return self.ps[bk][:, 0:128], d_bank[bk]

            def tslot():
                bk = cnt["t"] % 2
                cnt["t"] += 1
                return self.ps[bk][:, 0:128], d_bank[bk]

            d_m = S.dep()
            ML = self.sb(ph, "ML", [128, 128], F32)
            MIT = self.sb(ph, "MIT", [128, 128], F32)
            CH0 = self.sb(ph, "CH0", [128, 128], F32)
            CH1 = self.sb(ph, "CH1", [128, 128], F32)
            S.op("pool", lambda e: e.memset(MIT[:], 1.0), writes=[d_m])
            S.op("pool", lambda e: e.affine_select(out=MIT[:], in_=MIT[:], pattern=[[1, 128]], compare_op=ALU.is_ge,
                                                    fill=self.reg_zero, base=0, channel_multiplier=-1), reads=[d_m], writes=[d_m])
            S.op("pool", lambda e: e.memset(MIT[0:64, 64:128], 0.0), reads=[d_m], writes=[d_m])
            S.op("pool", lambda e: e.memset(ML[:], 1.0), writes=[d_m])
            S.op("pool", lambda e: e.affine_select(out=ML[:], in_=ML[:], pattern=[[-1, 128]], compare_op=ALU.is_ge,
                                                    fill=self.reg_zero, base=-1, channel_multiplier=1), reads=[d_m], writes=[d_m])
            S.op("pool", lambda e: e.memset(ML[64:128, 0:64], 0.0), reads=[d_m], writes=[d_m])
            S.op("pool", lambda e: e.memset(CH0[:], 0.0), writes=[d_m])
            S.op("pool", lambda e: e.memset(CH0[0:64, :], 1.0), reads=[d_m], writes=[d_m])
            S.op("pool", lambda e: e.memset(CH1[:], 0.0), writes=[d_m])
            S.op("pool", lambda e: e.memset(CH1[64:128, :], 1.0), reads=[d_m], writes=[d_m])
            d_par = S.dep()
            cwt = self.sb(ph, "cwt", [128, 16, CONV_K], F32)
            S.dma("sp", cwt[:], I["conv_w"][:, l, :, :], writes=[d_par])
            ngc = self.sb(ph, "ngc", [128, 1], F32)
            S.dma("sp", ngc[:], I["dn_norm_g"][l], writes=[d_par])
            alog = self.sb(ph, "alog", [128, 8], F32)
            dtb = self.sb(ph, "dtb", [128, 8], F32)
            S.dma("sp", alog[:], I["a_log"][0:1, l * 8:(l + 1) * 8].partition_broadcast(128), writes=[d_par])
            S.dma("sp", dtb[:], I["dt_bias"][0:1, l * 8:(l + 1) * 8].partition_broadcast(128), writes=[d_par])
            nea = self.sb(ph, "nea", [128, 8], F32)
            S.op("act", lambda e: e.activation(out=nea[:], in_=alog[:], func=AF.Exp), reads=[d_par], writes=[d_par])
            S.op("dve", lambda e: e.tensor_scalar_mul(out=nea[:], in0=nea[:], scalar1=-1.0), reads=[d_par], writes=[d_par])
            d_g = S.dep()
            bbaT = self.sb(ph, "bbaT", [16, T], F32)
            S.dma("sp", bbaT[:], self.P_d[O_BB:O_BB + 16, self.c0:self.c0 + T], reads=[self.d_P], writes=[d_g])
            GB = self.sb(ph, "GB", [128, NT, 16], F32)
            for i in range(NT):
                p_, dp_ = tslot()
                S.op("pe", lambda e, p_=p_, i=i: e.transpose(p_[:, 0:16], bbaT[:, i * 128:(i + 1) * 128], self.ident[0:16, 0:16]),
                     reads=[d_g, self.d_const], writes=[dp_])
                S.op("act", lambda e, p_=p_, i=i: e.activation(out=GB[:, i, :], in_=p_[:, 0:16], func=AF.Copy),
                     reads=[dp_], writes=[d_g])
            BETA = self.sb(ph, "BETA", [128, NT, 8], F32)
            G = self.sb(ph, "G", [128, NT, 8], F32)
            t1 = self.sb(ph, "gt1", [128, NT, 8], F32)
            t2 = self.sb(ph, "gt2", [128, NT, 8], F32)
            S.op("act", lambda e: e.activation(out=BETA[:], in_=GB[:, :, 0:8], func=AF.Exp, scale=-1.0), reads=[d_g], writes=[d_g])
            S.op("dve", lambda e: e.tensor_scalar_add(out=BETA[:], in0=BETA[:], scalar1=1.0), reads=[d_g], writes=[d_g])
            S.op("dve", lambda e: e.reciprocal(out=BETA[:], in_=BETA[:]), reads=[d_g], writes=[d_g])
            S.op("dve", lambda e: e.tensor_tensor(out=G[:], in0=GB[:, :, 8:16], in1=dtb[:].unsqueeze(1).broadcast_to([128, NT, 8]), op=ALU.add),
                 reads=[d_g, d_par], writes=[d_g])
            S.op("dve", lambda e: e.tensor_scalar_mul(out=t1[:], in0=G[:], scalar1=-1.0), reads=[d_g], writes=[d_g])
            S.op("dve", lambda e: e.tensor_tensor(out=t1[:], in0=t1[:], in1=G[:], op=ALU.max), reads=[d_g], writes=[d_g])
            S.op("act", lambda e: e.activation(out=t1[:], in_=t1[:], func=AF.Exp, scale=-1.0), reads=[d_g], writes=[d_g])
            S.op("act", lambda e: e.activation(out=t1[:], in_=t1[:], func=AF.Ln, bias=self.ones[:, 0:1], scale=1.0),
                 reads=[d_g, self.d_const], writes=[d_g])
            S.op("dve", lambda e: e.tensor_scalar_max(out=t2[:], in0=G[:], scalar1=0.0), reads=[d_g], writes=[d_g])
            S.op("dve", lambda e: e.tensor_tensor(out=t2[:], in0=t2[:], in1=t1[:], op=ALU.add), reads=[d_g], writes=[d_g])
            S.op("dve", lambda e: e.tensor_tensor(out=G[:], in0=t2[:], in1=nea[:].unsqueeze(1).broadcast_to([128, NT, 8]), op=ALU.mult),
                 reads=[d_g, d_par], writes=[d_g])
            GC = self.sb(ph, "GC", [128, NT, 8], F32)
            GL = self.sb(ph, "GL", [128, NT, 2, 8], F32)
            for i in range(NT):
                p_, dp_ = slot()
                S.op("pe", lambda e, p_=p_, i=i: e.matmul(p_[:, 0:8], lhsT=MIT[:], rhs=G[:, i, :], start=True, stop=True),
                     reads=[d_g, d_m], writes=[dp_])
                S.op("pe", lambda e, p_=p_, i=i: e.matmul(p_[:, 8:16], lhsT=CH0[:], rhs=G[:, i, :], start=True, stop=True),
                     reads=[d_g, d_m], writes=[dp_])
                S.op("pe", lambda e, p_=p_, i=i: e.matmul(p_[:, 16:24], lhsT=CH1[:], rhs=G[:, i, :], start=True, stop=True),
                     reads=[d_g, d_m], writes=[dp_])
                S.op("act", lambda e, p_=p_, i=i: e.activation(out=GC[:, i, :], in_=p_[:, 0:8], func=AF.Copy), reads=[dp_], writes=[d_g])
                S.op("dve", lambda e, p_=p_, i=i: e.tensor_copy(out=GL[:, i, :, :], in_=p_[:, 8:24].rearrange("p (c h) -> p c h", c=2)),
                     reads=[dp_], writes=[d_g])
            EG = self.sb(ph, "EG", [128, NT, 8], F32)
            BEG = self.sb(ph, "BEG", [128, NT, 8], F32)
            KD = self.sb(ph, "KD", [128, NT, 8], F32)
            EGL = self.sb(ph, "EGL", [128, NT, 2, 8], F32)
            S.op("act", lambda e: e.activation(out=EG[:], in_=GC[:], func=AF.Exp), reads=[d_g], writes=[d_g])
            S.op("dve", lambda e: e.tensor_tensor(out=BEG[:], in0=EG[:], in1=BETA[:], op=ALU.mult), reads=[d_g], writes=[d_g])
            S.op("dve", lambda e: e.tensor_tensor(out=KD[0:64], in0=GL[0:64, :, 0, :], in1=GC[0:64], op=ALU.subtract), reads=[d_g], writes=[d_g])
            S.op("dve", lambda e: e.tensor_tensor(out=KD[64:128], in0=GL[64:128, :, 1, :], in1=GC[64:128], op=ALU.subtract), reads=[d_g], writes=[d_g])
            S.op("act", lambda e: e.activation(out=KD[:], in_=KD[:], func=AF.Exp), reads=[d_g], writes=[d_g])
            S.op("act", lambda e: e.activation(out=EGL[:], in_=GL[:], func=AF.Exp), reads=[d_g], writes=[d_g])

            stop = self.dn_stop
            qn = self.sb(ph, "qn", [128, T], F32)
            kn = self.sb(ph, "kn", [128, T], F32)
            vT = [self.sb(ph, f"vT{i}", [128, T], F32) for i in range(2)]
            zT = [self.sb(ph, f"zT{i}", [128, T], F32) for i in range(2)]
            X = self.sb(ph, "convX", [128, T + 3], F32)
            d_X = S.dep()
            d_q, d_k = S.dep(), S.dep()
            d_v = S.deps(2)
            d_z = S.deps(2)
            sq = self.sb(ph, "sqt", [128, 512], F32)
            rn = self.sb(ph, "rnt", [128, 512], F32)
            d_sq, d_rn = S.dep(), S.dep()
            NS = 2
            def mk(name):
                return [self.sb(ph, f"{name}{i}", [128, 128], F32) for i in range(NS)], S.deps(NS)
            ktok, d_ktok = mk("ktok")
            KKs, d_KKs = mk("KKs")
            QKs, d_QKs = mk("QKs")
            vtok, d_vtok = mk("vtok")
            diag, d_diag = mk("diag")
            Dm, d_Dm = mk("Dm")
            DTm, d_DTm = mk("DTm")
            EGR, d_EGR = mk("EGR")
            Lm, d_Lm = mk("Lm")
            Nm, d_Nm = mk("Nm")
            qkT, d_qkT = mk("qkT")
            AL, d_AL = mk("AL")
            AN, d_AN = mk("AN")
            PT, d_PT = mk("PT")
            PT2, d_PT2 = mk("PT2")
            vb, d_vb = mk("vb")
            kbe, d_kbe = mk("kbe")
            kdec, d_kdec = mk("kdec")
            uu, d_uu = mk("uu")
            wT, d_wT = mk("wT")
            qdT, d_qdT = mk("qdT")
            vnew, d_vnew = mk("vnew")
            otok, d_otok = mk("otok")
            ojunk, d_ojunk = mk("ojunk")
            ost = [self.sb(ph, f"ost{i}", [128, 4], F32) for i in range(NS)]
            d_ost = S.deps(NS)
            Sst = [[self.sb(ph, f"Sst{hh}_{i}", [128, 128], F32) for i in range(2)] for hh in range(2)]
            d_Sst = [S.deps(2) for _ in range(2)]

            def conv_load(dst, d_dst, row0, c16, l2=None):
                S.dma("sp", X[:], self.P_d[row0:row0 + 128, self.c0 - 3:self.c0 + T], reads=[self.d_P], writes=[d_X])
                S.op("dve", lambda e: e.tensor_scalar_mul(out=dst[:], in0=X[:, 0:T], scalar1=cwt[:, c16, 0:1]),
                     reads=[d_X, d_par], writes=[d_dst])
                for j in range(1, CONV_K):
                    S.op("dve", lambda e, j=j: e.scalar_tensor_tensor(out=dst[:], in0=X[:, j:j + T], scalar=cwt[:, c16, j:j + 1],
                                                                        in1=dst[:], op0=ALU.mult, op1=ALU.add),
                         reads=[d_X, d_par, d_dst], writes=[d_dst])
                if self.dn_sub >= 1:
                    S.op("act", lambda e: e.activation(out=dst[:], in_=dst[:], func=AF.Silu), reads=[d_dst], writes=[d_dst])
                if l2 is not None and self.dn_sub >= 2:
                    for tt in range(T // 512):
                        cs = slice(tt * 512, (tt + 1) * 512)
                        pb = 2 + tt % 2
                        S.op("act", lambda e, cs=cs: e.activation(out=sq[:], in_=dst[:, cs], func=AF.Square), reads=[d_dst], writes=[d_sq])
                        S.op("pe", lambda e, pb=pb: e.matmul(self.ps[pb][:], lhsT=self.ones[:], rhs=sq[:], start=True, stop=True),
                             reads=[d_sq, self.d_const], writes=[d_slot[pb * 4 + q] for q in range(4)])
                        if False:
                            S.op("dve", lambda e, pb=pb: e.tensor_scalar(out=rn[:], in0=self.ps[pb][:], scalar1=EPS, scalar2=-0.5, op0=ALU.add, op1=ALU.pow),
                                 reads=[d_slot[pb * 4 + q] for q in range(4)], writes=[d_rn])
                        else:
                            S.op("dve", lambda e, pb=pb: e.tensor_scalar_add(out=rn[:], in0=self.ps[pb][:], scalar1=EPS),
                                 reads=[d_slot[pb * 4 + q] for q in range(4)], writes=[d_rn])
                            S.op("act", lambda e: e.activation(out=rn[:], in_=rn[:], func=AF.Sqrt), reads=[d_rn], writes=[d_rn])
                            S.op("dve", lambda e: e.reciprocal(out=rn[:], in_=rn[:]), reads=[d_rn], writes=[d_rn])
                        S.op("dve", lambda e, cs=cs: e.scalar_tensor_tensor(out=dst[:, cs], in0=dst[:, cs], scalar=float(l2), in1=rn[:],
                                                                             op0=ALU.mult, op1=ALU.mult), reads=[d_rn, d_dst], writes=[d_dst])

            for kh in range((4 if not self.dn_fast else 1) if stop >= 2 else 0):
                conv_load(qn, d_q, O_BQ + 128 * kh, kh, l2=DKS)
                conv_load(kn, d_k, O_BK + 128 * kh, 4 + kh, l2=1.0)
                for hh in range(2):
                    h = 2 * kh + hh
                    conv_load(vT[hh], d_v[hh], O_BV + 128 * h, 8 + h)
                    S.dma("sp", zT[hh][:], self.P_d[O_BZ + 128 * h:O_BZ + 128 * (h + 1), self.c0:self.c0 + T], reads=[self.d_P], writes=[d_z[hh]])
                    S.op("act", lambda e, hh=hh: e.activation(out=zT[hh][:], in_=zT[hh][:], func=AF.Silu), reads=[d_z[hh]], writes=[d_z[hh]])
                    if self.seg_first:
                        S.op("pool", lambda e, hh=hh: e.memset(Sst[hh][0][:], 0.0), writes=[d_Sst[hh][0]])
                    else:
                        S.op("pool", lambda e, hh=hh, h=h: e.tensor_copy(out=Sst[hh][0][:], in_=self.SCARRY[:, h, :]),
                             reads=[self.d_carry], writes=[d_Sst[hh][0]])
                cur = [0, 0]
                for i in range((NT if not self.dn_fast else 2) if stop >= 3 else 0):
                    ts_ = slice(i * 128, (i + 1) * 128)
                    kb = i % NS
                    DV = 7
                    p_, dp_ = tslot()
                    if DV & 1:
                        S.op("pe", lambda e, p_=p_, ts_=ts_: e.transpose(p_, kn[:, ts_], self.ident[:]), reads=[d_k, self.d_const], writes=[dp_])
                        S.op("act", lambda e, p_=p_, kb=kb: e.activation(out=ktok[kb][:], in_=p_, func=AF.Copy), reads=[dp_], writes=[d_ktok[kb]])
                    pKK_, dKK_ = slot()
                    S.op("pe", lambda e, pKK_=pKK_, ts_=ts_: e.matmul(pKK_, lhsT=kn[:, ts_], rhs=kn[:, ts_], start=True, stop=True),
                         reads=[d_k], writes=[dKK_])
                    S.op("act", lambda e, pKK_=pKK_, kb=kb: e.activation(out=KKs[kb][:], in_=pKK_, func=AF.Copy), reads=[dKK_], writes=[d_KKs[kb]])
                    pQK_, dQK_ = slot()
                    S.op("pe", lambda e, pQK_=pQK_, ts_=ts_: e.matmul(pQK_, lhsT=kn[:, ts_], rhs=qn[:, ts_], start=True, stop=True),
                         reads=[d_k, d_q], writes=[dQK_])
                    S.op("dve", lambda e, pQK_=pQK_, kb=kb: e.tensor_copy(out=QKs[kb][:], in_=pQK_), reads=[dQK_], writes=[d_QKs[kb]])
                    pKK, dKK, pQK, dQK = KKs[kb][:], d_KKs[kb], QKs[kb][:], d_QKs[kb]
                    for hh in range(2 if self.dn_sub >= 11 else 0):
                        h = 2 * kh + hh
                        b = (i * 2 + hh) % NS
                        gcol = GC[:, i, h:h + 1]
                        S.op("dve", lambda e, b=b, gcol=gcol: e.tensor_scalar_mul(out=diag[b][:], in0=self.ident[:], scalar1=gcol),
                             reads=[d_g, self.d_const], writes=[d_diag[b]])
                        pG, dG = slot()
                        S.op("pe", lambda e, pG=pG, b=b: e.matmul(pG, lhsT=self.ones[:], rhs=diag[b][:], start=True, stop=True),
                             reads=[d_diag[b], self.d_const], writes=[dG])
                        S.op("dve", lambda e, pG=pG, b=b, gcol=gcol: e.tensor_scalar(out=Dm[b][:], in0=pG, scalar1=gcol, scalar2=0.0,
                                                                                      op0=ALU.subtract, op1=ALU.max),
                             reads=[dG, d_g], writes=[d_Dm[b]])
                        S.op("act", lambda e, b=b: e.activation(out=Dm[b][:], in_=Dm[b][:], func=AF.Exp, scale=-1.0), reads=[d_Dm[b]], writes=[d_Dm[b]])
                        S.op("pool", lambda e, b=b: e.tensor_tensor(out=Dm[b][:], in0=Dm[b][:], in1=ML[:], op=ALU.mult), reads=[d_Dm[b], d_m], writes=[d_Dm[b]])
                        if self.dn_sub < 12:
                            continue
                        S.op("dve", lambda e, pG=pG, b=b, gcol=gcol: e.tensor_scalar(out=DTm[b][:], in0=pG, scalar1=gcol, scalar2=0.0,
                                                                                      op0=ALU.subtract, op1=ALU.min),
                             reads=[dG, d_g], writes=[d_DTm[b]])
                        S.op("act", lambda e, b=b: e.activation(out=DTm[b][:], in_=DTm[b][:], func=AF.Exp), reads=[d_DTm[b]], writes=[d_DTm[b]])
                        S.op("pool", lambda e, b=b: e.tensor_tensor(out=DTm[b][:], in0=DTm[b][:], in1=MIT[:], op=ALU.mult), reads=[d_DTm[b], d_m], writes=[d_DTm[b]])
                        S.op("act", lambda e, pG=pG, b=b: e.activation(out=EGR[b][:], in_=pG, func=AF.Exp), reads=[dG], writes=[d_EGR[b]])
                        if self.dn_sub < 13:
                            continue
                        S.op("dve", lambda e, b=b, i=i, h=h: e.scalar_tensor_tensor(out=Lm[b][:], in0=pKK, scalar=BETA[:, i, h:h + 1], in1=Dm[b][:],
                                                                                     op0=ALU.mult, op1=ALU.mult),
                             reads=[dKK, d_g, d_Dm[b]], writes=[d_Lm[b]])
                        S.op("dve", lambda e, b=b: e.tensor_tensor(out=qkT[b][:], in0=pQK, in1=DTm[b][:], op=ALU.mult),
                             reads=[dQK, d_DTm[b]], writes=[d_qkT[b]])
                        pN, dN = tslot()
                        S.op("pe", lambda e, pN=pN, b=b: e.transpose(pN, Lm[b][:], self.ident[:]), reads=[d_Lm[b], self.d_const], writes=[dN])
                        S.op("act", lambda e, pN=pN, b=b: e.activation(out=Nm[b][:], in_=pN, func=AF.Copy), reads=[dN], writes=[d_Nm[b]])
                        if self.dn_sub < 14:
                            continue
                        S.op("dve", lambda e, b=b: e.tensor_tensor(out=PT[b][:], in0=self.ident[:], in1=Nm[b][:], op=ALU.subtract),
                             reads=[d_Nm[b], self.d_const], writes=[d_PT[b]])
                        cl, dcl, cn, dcn = Lm[b], d_Lm[b], Nm[b], d_Nm[b]
                        cp, dcp, np_, dnp = PT[b], d_PT[b], PT2[b], d_PT2[b]
                        for sstep in range(1, 6):
                            pL2, dL2 = slot()
                            S.op("pe", lambda e, pL2=pL2, cl=cl, cn=cn: e.matmul(pL2, lhsT=cn[:], rhs=cl[:], start=True, stop=True),
                                 reads=[dcl, dcn], writes=[dL2])
                            if sstep < 5:
                                pN2, dN2 = slot()
                                S.op("pe", lambda e, pN2=pN2, cl=cl, cn=cn: e.matmul(pN2, lhsT=cl[:], rhs=cn[:], start=True, stop=True),
                                     reads=[dcl, dcn], writes=[dN2])
                            if sstep % 2 == 1:
                                nl, dnl, nn, dnn = AL[b], d_AL[b], AN[b], d_AN[b]
                            else:
                                nl, dnl, nn, dnn = Lm[b], d_Lm[b], Nm[b], d_Nm[b]
                            S.op("act", lambda e, pL2=pL2, nl=nl: e.activation(out=nl[:], in_=pL2, func=AF.Copy), reads=[dL2], writes=[dnl])
                            if sstep < 5:
                                S.op("dve", lambda e, pN2=pN2, nn=nn: e.tensor_copy(out=nn[:], in_=pN2), reads=[dN2], writes=[dnn])
                            DW = 7
                            pU, dU = slot()
                            if DW & 2:
                                S.op("pe", lambda e, pU=pU, nl=nl, cp=cp: e.matmul(pU, lhsT=nl[:], rhs=cp[:], start=True, stop=True),
                                     reads=[dnl, dcp], writes=[dU])
                            if DW & 4:
                                S.op("dve", lambda e, pU=pU, cp=cp, np_=np_: e.tensor_tensor(out=np_[:], in0=pU, in1=cp[:], op=ALU.add),
                                     reads=[dU, dcp], writes=[dnp])
                            cl, dcl, cn, dcn = nl, dnl, nn, dnn
                            cp, dcp, np_, dnp = np_, dnp, cp, dcp
                        TT, dTT = cp, dcp
                        if stop < 4:
                            continue
                        pV, dV = tslot()
                        S.op("pe", lambda e, pV=pV, hh=hh, ts_=ts_: e.transpose(pV, vT[hh][:, ts_], self.ident[:]),
                             reads=[d_v[hh], self.d_const], writes=[dV])
                        S.op("dve", lambda e, pV=pV, b=b, i=i, h=h: e.tensor_scalar_mul(out=vb[b][:], in0=pV, scalar1=BETA[:, i, h:h + 1]),
                             reads=[dV, d_g], writes=[d_vb[b]])
                        S.op("pool", lambda e, b=b, kb=kb, i=i, h=h: e.tensor_scalar_mul(out=kbe[b][:], in0=ktok[kb][:], scalar1=BEG[:, i, h:h + 1]),
                             reads=[d_ktok[kb], d_g], writes=[d_kbe[b]])
                        S.op("pool", lambda e, b=b, kb=kb, i=i, h=h: e.tensor_scalar_mul(out=kdec[b][:], in0=ktok[kb][:], scalar1=KD[:, i, h:h + 1]),
                             reads=[d_ktok[kb], d_g], writes=[d_kdec[b]])
                        S.op("pool", lambda e, b=b, ts_=ts_: e.tensor_tensor(out=qdT[b][:], in0=qn[:, ts_], in1=EGR[b][:], op=ALU.mult),
                             reads=[d_q, d_EGR[b]], writes=[d_qdT[b]])
                        pu, du = slot()
                        S.op("pe", lambda e, pu=pu, TT=TT, b=b: e.matmul(pu, lhsT=TT[:], rhs=vb[b][:], start=True, stop=True),
                             reads=[dTT, d_vb[b]], writes=[du])
                        S.op("act", lambda e, pu=pu, b=b: e.activation(out=uu[b][:], in_=pu, func=AF.Copy), reads=[du], writes=[d_uu[b]])
                        pw, dw = slot()
                        S.op("pe", lambda e, pw=pw, TT=TT, b=b: e.matmul(pw, lhsT=kbe[b][:], rhs=TT[:], start=True, stop=True),
                             reads=[dTT, d_kbe[b]], writes=[dw])
                        S.op("act", lambda e, pw=pw, b=b: e.activation(out=wT[b][:], in_=pw, func=AF.Copy), reads=[dw], writes=[d_wT[b]])
                        for c in range(2 if stop >= 5 else 0):
                            rs = slice(64 * c, 64 * c + 64)
                            Sc, dSc = Sst[hh][cur[hh]], d_Sst[hh][cur[hh]]
                            Sn, dSn = Sst[hh][1 - cur[hh]], d_Sst[hh][1 - cur[hh]]
                            p1, d1 = slot()
                            S.op("pe", lambda e, p1=p1, b=b, Sc=Sc: e.matmul(p1, lhsT=wT[b][:], rhs=Sc[:], start=True, stop=True),
                                 reads=[d_wT[b], dSc], writes=[d1])
                            S.op("dve", lambda e, p1=p1, b=b, rs=rs: e.tensor_tensor(out=vnew[b][rs, :], in0=uu[b][rs, :], in1=p1[rs, :], op=ALU.subtract),
                                 reads=[d1, d_uu[b]], writes=[d_vnew[b]])
                            p2, d2 = slot()
                            S.op("pe", lambda e, p2=p2, b=b, Sc=Sc: e.matmul(p2, lhsT=qdT[b][:], rhs=Sc[:], start=True, stop=False),
                                 reads=[d_qdT[b], dSc], writes=[d2])
                            S.op("pe", lambda e, p2=p2, b=b, rs=rs: e.matmul(p2, lhsT=qkT[b][rs, :], rhs=vnew[b][rs, :], start=False, stop=True),
                                 reads=[d_qkT[b], d_vnew[b]], writes=[d2])
                            S.op("act", lambda e, p2=p2, b=b, rs=rs: e.activation(out=otok[b][rs, :], in_=p2[rs, :], func=AF.Copy),
                                 reads=[d2], writes=[d_otok[b]])
                            p3, d3 = slot()
                            S.op("pe", lambda e, p3=p3, b=b, rs=rs: e.matmul(p3, lhsT=kdec[b][rs, :], rhs=vnew[b][rs, :], start=True, stop=True),
                                 reads=[d_kdec[b], d_vnew[b]], writes=[d3])
                            S.op("dve", lambda e, p3=p3, Sc=Sc, Sn=Sn, i=i, c=c, h=h: e.scalar_tensor_tensor(
                                out=Sn[:], in0=Sc[:], scalar=EGL[:, i, c, h:h + 1], in1=p3, op0=ALU.mult, op1=ALU.add),
                                reads=[d3, dSc, d_g], writes=[dSn])
                            cur[hh] = 1 - cur[hh]
                        S.op("act", lambda e, b=b: e.activation(out=ojunk[b][:], in_=otok[b][:], func=AF.Square, accum_out=ost[b][:, 0:1]),
                             reads=[d_otok[b]], writes=[d_ojunk[b], d_ost[b]])
                        S.op("act", lambda e, b=b: e.activation(out=ost[b][:, 1:2], in_=ost[b][:, 0:1], func=AF.Sqrt, scale=1.0 / DV_B,
                                                                 bias=self.epsc[:, 0:1]), reads=[d_ost[b], self.d_const], writes=[d_ost[b]])
                        S.op("dve", lambda e, b=b: e.reciprocal(out=ost[b][:, 2:3], in_=ost[b][:, 1:2]), reads=[d_ost[b]], writes=[d_ost[b]])
                        S.op("pool", lambda e, b=b: e.tensor_scalar_mul(out=otok[b][:], in0=otok[b][:], scalar1=ost[b][:, 2:3]),
                             reads=[d_ost[b], d_otok[b]], writes=[d_otok[b]])
                        pO, dO = tslot()
                        S.op("pe", lambda e, pO=pO, b=b: e.transpose(pO, otok[b][:], self.ident[:]), reads=[d_otok[b], self.d_const], writes=[dO])
                        S.op("dve", lambda e, pO=pO, hh=hh, h=h, ts_=ts_: e.scalar_tensor_tensor(
                            out=self.actT[:, 4 + h, ts_], in0=pO, scalar=ngc[:, 0:1], in1=zT[hh][:, ts_], op0=ALU.mult, op1=ALU.mult),
                            reads=[dO, d_par, d_z[hh]], writes=[self.d_act[i]])
                for hh in range(2):
                    h = 2 * kh + hh
                    S.op("pool", lambda e, hh=hh, h=h, cc=cur[hh]: e.tensor_copy(out=self.SCARRY[:, h, :], in_=Sst[hh][cc][:]),
                         reads=[d_Sst[hh][cur[hh]]], writes=[self.d_carry])
        S.barrier()

    def phase_outproj(self, l):
        S, I = self.S, self.I
        T, NT = self.T, self.NT
        wv = I["w_out"][l].rearrange("(kc p) n -> p kc n", p=128)
        with ExitStack() as ph:
            NB = 2
            W = [self.sb(ph, f"opW{i}", [128, KC, 512], BF16) for i in range(NB)]
            dW = S.deps(NB)
            Gp = [self.sb(ph, f"opG{i}", [128, 512], F32) for i in range(NB)]
            xt = [self.sb(ph, f"opx{i}", [128, 512], F32) for i in range(NB)]
            dx = S.deps(NB)
            tt_ = [self.sb(ph, f"opt{i}", [128, 512], F32) for i in range(NB)]
            dt_ = S.deps(NB)
            cnt = 0
            for cg in range(4):
                cs = slice(cg * 512, (cg + 1) * 512)
                wb = cg % NB
                S.dma("pool", W[wb][:], wv[:, :, cs], writes=[dW[wb]])
                S.dma("sp", Gp[wb][:], self.mod_d[self.bi, l, 2, :, cs], reads=[self.d_mod], writes=[dW[wb]])
                for i in range(NT):
                    b = cnt % NB
                    pb = cnt % 8
                    cnt += 1
                    rows = slice(self.r0 + i * 128, self.r0 + (i + 1) * 128)
                    for kc in range(KC):
                        S.op("pe", lambda e, kc=kc, i=i, wb=wb, pb=pb: e.matmul(self.ps[pb][:], lhsT=self.actT[:, kc, i * 128:(i + 1) * 128],
                                                                              rhs=W[wb][:, kc, :], start=(kc == 0), stop=(kc == KC - 1)),
                             reads=[dW[wb], self.d_act[i]], writes=[self.d_ps[pb]])
                    S.dma("sp", xt[b][:], self.x_d[rows, cs], reads=[self.d_xt[i]], writes=[dx[b]])
                    S.op("dve", lambda e, b=b, wb=wb, pb=pb: e.tensor_tensor(out=tt_[b][:], in0=self.ps[pb][:], in1=Gp[wb][:], op=ALU.mult),
                         reads=[self.d_ps[pb], dW[wb]], writes=[dt_[b]])
                    S.op("pool", lambda e, b=b: e.tensor_tensor(out=xt[b][:], in0=xt[b][:], in1=tt_[b][:], op=ALU.add),
                         reads=[dt_[b], dx[b]], writes=[dx[b]])
                    S.dma("sp", self.x_d[rows, cs], xt[b][:], reads=[dx[b]], writes=[self.d_xt[i]])
        S.barrier()

    def phase_moe(self, l):
        S, I, nc = self.S, self.I, self.nc
        T, NT, NBLK = self.T, self.NT, self.NBLK
        with ExitStack() as mo:
            router = {}
            RW = self.sb(mo, "RW", [128, KC, 36], F32)
            RB = self.sb(mo, "RB", [128, 36], F32)
            LG = self.sb(mo, "LG", [128, NT, 36], F32)
            router["RW"], router["RB"], router["LG"] = RW, RB, LG
            router["d_rw"], router["d_lg"] = S.dep(), S.dep()
            router["hbf"] = [self.sb(mo, f"hbf{i}", [128, D], BF16) for i in range(2)]
            router["d_hbf"] = S.deps(2)
            S.dma("sp", RW[:], I["rw"][l].rearrange("(kc p) n -> p kc n", p=128), writes=[router["d_rw"]])
            S.dma("sp", RB[:], I["rb"][l:l + 1, :].partition_broadcast(128), writes=[router["d_rw"]])
            self.phase_norm(l, 2, router=router)
            d_r = router["d_lg"]
            cnt = {"n": 0}

            def small(name, shape, dt=F32):
                return self.sb(mo, name, shape, dt)

            gmax = small("gmax", [128, NT])
            goh = small("goh", [128, NT, 4])
            gex = small("gex", [128, NT, 4])
            pg = small("pg", [128, NT])
            esel = small("esel", [128, NT, 8])
            etmp = small("etmp", [128, NT, 8])
            v1 = small("v1", [128, NT])
            v2 = small("v2", [128, NT])
            oh1 = small("oh1", [128, NT, 8])
            oh2 = small("oh2", [128, NT, 8])
            g0 = small("g0", [128, NT])
            g1 = small("g1", [128, NT])
            OH1 = small("OH1", [128, NT, 32])
            OH2 = small("OH2", [128, NT, 32])
            OH = small("OH", [128, NT, 32])
            glog = LG[:, :, 0:4]
            elog4 = LG[:, :, 4:36].rearrange("p n (g j) -> p n g j", g=4)

            def dv(fn, eng="dve"):
                S.op(eng, fn, reads=[d_r, self.d_const], writes=[d_r])

            dv(lambda e: e.tensor_reduce(out=gmax[:], in_=glog, axis=AX.X, op=ALU.max))
            dv(lambda e: e.tensor_tensor(out=goh[:], in0=glog, in1=gmax[:].unsqueeze(2).broadcast_to([128, NT, 4]), op=ALU.is_equal))
            dv(lambda e: e.tensor_tensor(out=gex[:], in0=glog, in1=gmax[:].unsqueeze(2).broadcast_to([128, NT, 4]), op=ALU.subtract))
            dv(lambda e: e.activation(out=gex[:], in_=gex[:], func=AF.Exp), "act")
            dv(lambda e: e.tensor_reduce(out=pg[:], in_=gex[:], axis=AX.X, op=ALU.add))
            dv(lambda e: e.reciprocal(out=pg[:], in_=pg[:]))
            for g in range(4):
                if g == 0:
                    dv(lambda e: e.tensor_tensor(out=esel[:], in0=elog4[:, :, 0, :], in1=goh[:, :, 0:1].broadcast_to([128, NT, 8]), op=ALU.mult))
                else:
                    dv(lambda e, g=g: e.tensor_tensor(out=etmp[:], in0=elog4[:, :, g, :], in1=goh[:, :, g:g + 1].broadcast_to([128, NT, 8]), op=ALU.mult))
                    dv(lambda e: e.tensor_tensor(out=esel[:], in0=esel[:], in1=etmp[:], op=ALU.add))
            dv(lambda e: e.tensor_reduce(out=v1[:], in_=esel[:], axis=AX.X, op=ALU.max))
            dv(lambda e: e.tensor_tensor(out=oh1[:], in0=esel[:], in1=v1[:].unsqueeze(2).broadcast_to([128, NT, 8]), op=ALU.is_equal))
            dv(lambda e: e.scalar_tensor_tensor(out=etmp[:], in0=oh1[:], scalar=NEG, in1=esel[:], op0=ALU.mult, op1=ALU.add))
            dv(lambda e: e.tensor_reduce(out=v2[:], in_=etmp[:], axis=AX.X, op=ALU.max))
            dv(lambda e: e.tensor_tensor(out=oh2[:], in0=etmp[:], in1=v2[:].unsqueeze(2).broadcast_to([128, NT, 8]), op=ALU.is_equal))
            dv(lambda e: e.tensor_tensor(out=g1[:], in0=v2[:], in1=v1[:], op=ALU.subtract))
            dv(lambda e: e.activation(out=g1[:], in_=g1[:], func=AF.Exp), "act")
            dv(lambda e: e.tensor_scalar_add(out=g0[:], in0=g1[:], scalar1=1.0))
            dv(lambda e: e.reciprocal(out=g0[:], in_=g0[:]))
            dv(lambda e: e.tensor_tensor(out=g1[:], in0=g1[:], in1=g0[:], op=ALU.mult))
            dv(lambda e: e.tensor_tensor(out=g0[:], in0=g0[:], in1=pg[:], op=ALU.mult))
            dv(lambda e: e.tensor_tensor(out=g1[:], in0=g1[:], in1=pg[:], op=ALU.mult))
            for (OHk, ohk) in ((OH1, oh1), (OH2, oh2)):
                for g in range(4):
                    dv(lambda e, OHk=OHk, ohk=ohk, g=g: e.tensor_tensor(out=OHk[:, :, g * 8:(g + 1) * 8], in0=ohk[:],
                                                                        in1=goh[:, :, g:g + 1].broadcast_to([128, NT, 8]), op=ALU.mult))
            dv(lambda e: e.tensor_tensor(out=OH[:], in0=OH1[:], in1=OH2[:], op=ALU.add))
            Ust = small("Ust", [128, 128])
            dv(lambda e: e.memset(Ust[:], 1.0), "pool")
            dv(lambda e: e.affine_select(out=Ust[:], in_=Ust[:], pattern=[[1, 128]], compare_op=ALU.is_ge,
                                         fill=self.reg_zero, base=-1, channel_multiplier=-1), "pool")
            TRIU = small("TRIU", [32, 32])
            dv(lambda e: e.memset(TRIU[:], 1.0), "pool")
            dv(lambda e: e.affine_select(out=TRIU[:], in_=TRIU[:], pattern=[[1, 32]], compare_op=ALU.is_ge,
                                         fill=self.reg_zero, base=0, channel_multiplier=-1), "pool")
            CUM = small("CUM", [128, NT + 1, 32])
            RANK = small("RANK", [128, NT, 32])
            dv(lambda e: e.memset(CUM[:, 0, :], 0.0), "pool")
            for i in range(NT):
                pb = 4 + (i % 2)
                S.op("pe", lambda e, i=i, pb=pb: e.matmul(self.ps[pb][:, 0:32], lhsT=self.ones[:], rhs=OH[:, i, :], start=True, stop=True),
                     reads=[d_r, self.d_const], writes=[self.d_ps[pb]])
                S.op("dve", lambda e, i=i, pb=pb: e.tensor_tensor(out=CUM[:, i + 1, :], in0=CUM[:, i, :], in1=self.ps[pb][:, 0:32], op=ALU.add),
                     reads=[self.d_ps[pb], d_r], writes=[d_r])
                pb2 = 6 + (i % 2)
                S.op("pe", lambda e, i=i, pb2=pb2: e.matmul(self.ps[pb2][:, 0:32], lhsT=Ust[:], rhs=OH[:, i, :], start=True, stop=True),
                     reads=[d_r], writes=[self.d_ps[pb2]])
                S.op("dve", lambda e, i=i, pb2=pb2: e.tensor_tensor(out=RANK[:, i, :], in0=CUM[:, i, :], in1=self.ps[pb2][:, 0:32], op=ALU.add),
                     reads=[self.d_ps[pb2], d_r], writes=[d_r])
            THRi = small("THRi", [128, 16], I32)
            THR = small("THR", [128, 16])
            dv(lambda e: e.iota(THRi[:], pattern=[[128, 16]], base=0, channel_multiplier=0), "pool")
            dv(lambda e: e.tensor_copy(out=THR[:], in_=THRi[:]), "pool")
            CMP = small("CMP", [128, 32, 16])
            PADD = small("PADD", [128, 32])
            dv(lambda e: e.tensor_tensor(out=CMP[:], in0=CUM[:, NT, :].unsqueeze(2).broadcast_to([128, 32, 16]),
                                         in1=THR[:].unsqueeze(1).broadcast_to([128, 32, 16]), op=ALU.is_gt))
            dv(lambda e: e.tensor_reduce(out=PADD[:], in_=CMP[:], axis=AX.X, op=ALU.add))
            dv(lambda e: e.tensor_scalar_mul(out=PADD[:], in0=PADD[:], scalar1=128.0))
            paddT = small("paddT", [32, 128])
            S.op("pe", lambda e: e.transpose(self.ps[0][0:32, 0:128], PADD[:], self.ident[:]), reads=[d_r, self.d_const], writes=[self.d_ps[0]])
            S.op("act", lambda e: e.activation(out=paddT[:], in_=self.ps[0][0:32, 0:128], func=AF.Copy), reads=[self.d_ps[0]], writes=[d_r])
            PEND = small("PEND", [128, 32])
            S.op("pe", lambda e: e.matmul(self.ps[4][:, 0:32], lhsT=paddT[:], rhs=TRIU[:], start=True, stop=True), reads=[d_r], writes=[self.d_ps[4]])
            S.op("dve", lambda e: e.tensor_copy(out=PEND[:], in_=self.ps[4][:, 0:32]), reads=[self.d_ps[4]], writes=[d_r])
            PST = small("PST", [128, 32])
            dv(lambda e: e.tensor_tensor(out=PST[:], in0=PEND[:], in1=PADD[:], op=ALU.subtract))
            dv(lambda e: e.tensor_tensor(out=RANK[:], in0=RANK[:], in1=PST[:].unsqueeze(1).broadcast_to([128, NT, 32]), op=ALU.add))
            DSTf = small("DSTf", [128, 2, NT])
            DSTi = small("DSTi", [128, 2, NT], I32)
            for k, OHk in enumerate((OH1, OH2)):
                dv(lambda e, OHk=OHk: e.tensor_tensor(out=OHk[:], in0=OHk[:], in1=RANK[:], op=ALU.mult))
                dv(lambda e, OHk=OHk, k=k: e.tensor_reduce(out=DSTf[:, k, :], in_=OHk[:], axis=AX.X, op=ALU.add))
            dv(lambda e: e.tensor_copy(out=DSTi[:], in_=DSTf[:]))
            pendc = small("pendc", [32, 1])
            S.op("pe", lambda e: e.matmul(self.ps[5][0:32, 0:1], lhsT=TRIU[:], rhs=paddT[:, 0:1], start=True, stop=True), reads=[d_r], writes=[self.d_ps[5]])
            S.op("dve", lambda e: e.tensor_copy(out=pendc[:], in_=self.ps[5][0:32, 0:1]), reads=[self.d_ps[5]], writes=[d_r])
            BVi = small("BVi", [32, NBLK], I32)
            BV = small("BV", [32, NBLK])
            dv(lambda e: e.iota(BVi[:], pattern=[[128, NBLK]], base=0, channel_multiplier=0), "pool")
            dv(lambda e: e.tensor_copy(out=BV[:], in_=BVi[:]), "pool")
            dv(lambda e: e.tensor_scalar(out=BV[:], in0=BV[:], scalar1=pendc[:, 0:1], scalar2=None, op0=ALU.is_ge))
            BEf = small("BEf", [1, NBLK])
            BEi = small("BEi", [1, NBLK], I32)
            S.op("pe", lambda e: e.matmul(self.ps[6][0:1, 0:NBLK], lhsT=self.ones[0:32, 0:1], rhs=BV[:], start=True, stop=True),
                 reads=[d_r, self.d_const], writes=[self.d_ps[6]])
            S.op("dve", lambda e: e.tensor_scalar_min(out=BEf[:], in0=self.ps[6][0:1, 0:NBLK], scalar1=float(N_EXP - 1)), reads=[self.d_ps[6]], writes=[d_r])
            dv(lambda e: e.tensor_copy(out=BEi[:], in_=BEf[:]))
            BEbc = small("BEbc", [128, NBLK])
            S.op("pe", lambda e: e.matmul(self.ps[7][:, 0:NBLK], lhsT=self.ones[0:32, :], rhs=BV[:], start=True, stop=True),
                 reads=[d_r, self.d_const], writes=[self.d_ps[7]])
            S.op("dve", lambda e: e.tensor_scalar_min(out=BEbc[:], in0=self.ps[7][:, 0:NBLK], scalar1=float(N_EXP - 1)), reads=[self.d_ps[7]], writes=[d_r])
            PKi = small("PKi", [128, 1], I32)
            PK = small("PK", [128, 1])
            dv(lambda e: e.iota(PKi[:], pattern=[[0, 1]], base=0, channel_multiplier=1), "pool")
            dv(lambda e: e.tensor_copy(out=PK[:], in_=PKi[:]), "pool")
            WIDXf = small("WIDXf", [128, NBLK])
            WIDX = small("WIDX", [128, NBLK], I32)
            dv(lambda e: e.tensor_scalar_add(out=BEbc[:], in0=BEbc[:], scalar1=float(l * N_EXP)))
            dv(lambda e: e.tensor_scalar(out=WIDXf[:], in0=BEbc[:], scalar1=128.0, scalar2=PK[:, 0:1], op0=ALU.mult, op1=ALU.add))
            dv(lambda e: e.tensor_copy(out=WIDX[:], in_=WIDXf[:]))
            wg_rows = I["w_gate"].rearrange("l e (p j) n -> (l e p) (j n)", j=KC)
            wu_rows = I["w_up"].rearrange("l e (p j) n -> (l e p) (j n)", j=KC)
            wd_rows = I["w_down"].rearrange("l e (p j) n -> (l e p) (j n)", j=4)
            if "dbg_route" in self.phases:
                t1_ = self.dram("dbg_dst", [128, 2, NT], I32, kind="ExternalOutput").ap()
                t2_ = self.dram("dbg_be", [1, NBLK], I32, kind="ExternalOutput").ap()
                t3_ = self.dram("dbg_g", [128, 2, NT], F32, kind="ExternalOutput").ap()
                self.out_events.append(S.dma("sp", t1_, DSTi[:], reads=[d_r]))
                self.out_events.append(S.dma("sp", t2_, BEi[:], reads=[d_r]))
                gg_ = small("gg_", [128, 2, NT])
                dv(lambda e: e.tensor_copy(out=gg_[:, 0, :], in_=g0[:]))
                dv(lambda e: e.tensor_copy(out=gg_[:, 1, :], in_=g1[:]))
                self.out_events.append(S.dma("sp", t3_, gg_[:], reads=[d_r]))
            S.barrier()
            with ExitStack() as ph:
                hrow = [self.sb(ph, f"hrow{i}", [128, D], BF16) for i in range(2)]
                dhr = S.deps(2)
                for i in range(NT):
                    b = i % 2
                    S.dma("sp", hrow[b][:], self.h2_d[i * 128:(i + 1) * 128, :], reads=[self.d_h2], writes=[dhr[b]])
                    for k in range(2):
                        S.dma("pool", None, None, reads=[dhr[b], d_r], writes=[self.d_rows],
                              fn=lambda e, b=b, k=k, i=i: e.indirect_dma_start(
                                  out=self.rows_d[:, :], out_offset=bass.IndirectOffsetOnAxis(ap=DSTi[:, k, i:i + 1], axis=0),
                                  in_=hrow[b][:, :], in_offset=None))
            S.barrier()
            with ExitStack() as ph:
                identb = self.sb(ph, "identb2", [128, 128], BF16)
                d_ib = S.dep()
                S.op("dve", lambda e: e.tensor_copy(out=identb[:], in_=self.ident[:]), reads=[self.d_const], writes=[d_ib])
                NW = 2
                Wg = [self.sb(ph, f"Wg{i}", [128, KC, D_FF], BF16) for i in range(NW)]
                Wu = [self.sb(ph, f"Wu{i}", [128, KC, D_FF], BF16) for i in range(NW)]
                Wd = [self.sb(ph, f"Wd{i}", [128, 4, D], BF16) for i in range(NW)]
                dWt = S.deps(NW)
                rowsb = [self.sb(ph, f"rowsb{i}", [128, D], BF16) for i in range(2)]
                d_rb = S.deps(2)
                blkT = [self.sb(ph, f"blkT{i}", [128, KC, 128], BF16) for i in range(2)]
                d_bT = S.deps(2)
                sg = self.sb(ph, "sgate", [128, D_FF], F32)
                d_sg = S.dep()
                hid = self.sb(ph, "hid", [128, D_FF], BF16)
                d_hid = S.dep()
                hidT = self.sb(ph, "hidT", [128, 4, 128], BF16)
                d_hT = S.dep()
                yb = [self.sb(ph, f"yb{i}", [128, D], F32) for i in range(2)]
                d_yb = S.deps(2)
                psT = [self.ps[2].bitcast(BF16), self.ps[3].bitcast(BF16)]
                for blk in range(NBLK):
                    b = blk % 2
                    wb = blk % NW
                    S.dma("sp", rowsb[b][:], self.rows_d[blk * 128:(blk + 1) * 128, :], reads=[self.d_rows], writes=[d_rb[b]])
                    for (dst, srcv) in ((Wg[wb], wg_rows), (Wu[wb], wu_rows), (Wd[wb], wd_rows)):
                        S.dma("pool", None, None, reads=[d_r], writes=[dWt[wb]],
                              fn=lambda e, dst=dst, srcv=srcv, blk=blk: e.indirect_dma_start(
                                  out=dst[:].rearrange("p j n -> p (j n)"), out_offset=None, in_=srcv,
                                  in_offset=bass.IndirectOffsetOnAxis(ap=WIDX[:, blk:blk + 1], axis=0)))
                    for q4 in range(4):
                        pt = psT[q4 % 2]
                        dpt = self.d_ps[2 + q4 % 2]
                        for j in range(4):
                            kc = q4 * 4 + j
                            S.op("pe", lambda e, pt=pt, b=b, kc=kc, j=j: e.transpose(pt[:, j * 128:(j + 1) * 128], rowsb[b][:, sl(kc, 128, KC)], identb[:]),
                                 reads=[d_rb[b], d_ib], writes=[dpt])
                        if q4 % 2 == 0:
                            S.op("act", lambda e, pt=pt, b=b, q4=q4: e.activation(out=blkT[b][:, q4 * 4:(q4 + 1) * 4, :],
                                                                                   in_=pt[:, 0:512].rearrange("p (j n) -> p j n", j=4), func=AF.Copy),
                                 reads=[dpt], writes=[d_bT[b]])
                        else:
                            S.op("dve", lambda e, pt=pt, b=b, q4=q4: e.tensor_copy(out=blkT[b][:, q4 * 4:(q4 + 1) * 4, :],
                                                                                    in_=pt[:, 0:512].rearrange("p (j n) -> p j n", j=4)),
                                 reads=[dpt], writes=[d_bT[b]])
                    for (pb, Wt) in ((0, Wg[wb]), (1, Wu[wb])):
                        for kc in range(KC):
                            S.op("pe", lambda e, pb=pb, Wt=Wt, kc=kc, b=b: e.matmul(self.ps[pb][:], lhsT=blkT[b][:, kc, :], rhs=Wt[:, kc, :],
                                                                                     start=(kc == 0), stop=(kc == KC - 1)),
                                 reads=[d_bT[b], dWt[wb]], writes=[self.d_ps[pb]])
                    S.op("act", lambda e: e.activation(out=sg[:], in_=self.ps[0][:], func=AF.Silu), reads=[self.d_ps[0]], writes=[d_sg])
                    S.op("dve", lambda e: e.tensor_tensor(out=hid[:], in0=sg[:], in1=self.ps[1][:], op=ALU.mult),
                         reads=[d_sg, self.d_ps[1]], writes=[d_hid])
                    pt, dpt = psT[0], self.d_ps[2]
                    for f in range(4):
                        S.op("pe", lambda e, f=f, pt=pt: e.transpose(pt[:, f * 128:(f + 1) * 128], hid[:, sl(f, 128, 4)], identb[:]),
                             reads=[d_hid, d_ib], writes=[dpt])
                    S.op("act", lambda e, pt=pt: e.activation(out=hidT[:], in_=pt[:, 0:512].rearrange("p (j n) -> p j n", j=4), func=AF.Copy),
                         reads=[dpt], writes=[d_hT])
                    for cgp in range(4):
                        pb = 4 + cgp
                        for f in range(4):
                            S.op("pe", lambda e, pb=pb, f=f, cgp=cgp, wb=wb: e.matmul(self.ps[pb][:], lhsT=hidT[:, f, :],
                                                                                       rhs=Wd[wb][:, f, cgp * 512:(cgp + 1) * 512],
                                                                                       start=(f == 0), stop=(f == 3)),
                                 reads=[d_hT, dWt[wb]], writes=[self.d_ps[pb]])
                        if cgp % 2 == 0:
                            S.op("act", lambda e, pb=pb, cgp=cgp, b=b: e.activation(out=yb[b][:, cgp * 512:(cgp + 1) * 512], in_=self.ps[pb][:], func=AF.Copy),
                                 reads=[self.d_ps[pb]], writes=[d_yb[b]])
                        else:
                            S.op("dve", lambda e, pb=pb, cgp=cgp, b=b: e.tensor_copy(out=yb[b][:, cgp * 512:(cgp + 1) * 512], in_=self.ps[pb][:]),
                                 reads=[self.d_ps[pb]], writes=[d_yb[b]])
                    S.dma("sp", self.yrows_d[blk * 128:(blk + 1) * 128, :], yb[b][:], reads=[d_yb[b]], writes=[self.d_yrows])
            S.barrier()
            with ExitStack() as ph:
                G2 = self.sb(ph, "G2bc", [128, D], F32)
                d_G2 = S.dep()
                S.dma("sp", G2[:], self.mod_d[self.bi, l, 5], reads=[self.d_mod], writes=[d_G2])
                y0 = [self.sb(ph, f"y0_{i}", [128, D], F32) for i in range(2)]
                y1 = [self.sb(ph, f"y1_{i}", [128, D], F32) for i in range(2)]
                xt = [self.sb(ph, f"cx{i}", [128, D], F32) for i in range(2)]
                d_y0, d_y1, d_cx = S.deps(2), S.deps(2), S.deps(2)
                for i in range(NT):
                    b = i % 2
                    rows = slice(self.r0 + i * 128, self.r0 + (i + 1) * 128)
                    for (yt, dy, k) in ((y0[b], d_y0[b], 0), (y1[b], d_y1[b], 1)):
                        S.dma("pool", None, None, reads=[self.d_yrows, d_r], writes=[dy],
                              fn=lambda e, yt=yt, k=k, i=i: e.indirect_dma_start(
                                  out=yt[:, :], out_offset=None, in_=self.yrows_d[:, :],
                                  in_offset=bass.IndirectOffsetOnAxis(ap=DSTi[:, k, i:i + 1], axis=0)))
                    S.dma("sp", xt[b][:], self.x_d[rows, :], reads=[self.d_xt[i]], writes=[d_cx[b]])
                    S.op("dve", lambda e, b=b, i=i: e.tensor_scalar_mul(out=y0[b][:], in0=y0[b][:], scalar1=g0[:, i:i + 1]),
                         reads=[d_y0[b], d_r], writes=[d_y0[b]])
                    S.op("dve", lambda e, b=b, i=i: e.scalar_tensor_tensor(out=y0[b][:], in0=y1[b][:], scalar=g1[:, i:i + 1], in1=y0[b][:],
                                                                           op0=ALU.mult, op1=ALU.add),
                         reads=[d_y0[b], d_y1[b], d_r], writes=[d_y0[b]])
                    S.op("pool", lambda e, b=b: e.tensor_tensor(out=y0[b][:], in0=y0[b][:], in1=G2[:], op=ALU.mult),
                         reads=[d_y0[b], d_G2], writes=[d_y0[b]])
                    S.op("pool", lambda e, b=b: e.tensor_tensor(out=xt[b][:], in0=xt[b][:], in1=y0[b][:], op=ALU.add),
                         reads=[d_y0[b], d_cx[b]], writes=[d_cx[b]])
                    S.dma("sp", self.x_d[rows, :], xt[b][:], reads=[d_cx[b]], writes=[self.d_xt[i]])
        S.barrier()

    def dump_act(self):
        S = self.S
        t = self.dram("act_dump", [128, KC, self.T], BF16, kind="ExternalOutput").ap()
        self.out_events.append(S.dma("sp", t, self.actT[:], reads=self.d_act))

    def build(self):
        self.declare()
        self.consts()
        self.phase_init()
        P = self.phases
        if "ada" in P:
            for b in range(self.NB):
                self.bi = b
                self.phase_ada()
        for l in range(self.L):
            for b in range(self.NB):
                for g in range(self.NSEG):
                    self.bi = b
                    self.r0 = (b * self.NSEG + g) * self.T
                    self.c0 = TP + g * self.T
                    self.seg_first = (g == 0)
                    with ExitStack() as mx:
                        self.actT = self.sb(mx, "actT", [128, KC, self.T], BF16)
                        if "norm1" in P:
                            self.phase_norm(l, 1)
                        if "proj" in P:
                            self.phase_proj(l)
                        if "attn_c" in P:
                            self.phase_attn_c(l)
                        if "attn_a" in P:
                            self.phase_attn_a(l)
                        if "dn" in P:
                            self.phase_dn(l)
                        if "dump_act" in P:
                            self.dump_act()
                        if "outproj" in P:
                            self.phase_outproj(l)
                        self.S.barrier()
                    if "moe" in P:
                        self.phase_moe(l)
        if "final" in P:
            for s_ in range(self.NB * self.NSEG):
                self.r0 = s_ * self.T
                self.phase_norm(0, "final")
        if "dump_x" in P:
            t = self.dram("x_dump", [self.NB * self.NSEG * self.T, D], F32, kind="ExternalOutput").ap()
            self.out_events.append(self.S.dma("sp", t, self.x_d, reads=[self.d_x] + self.d_xt))
        self.S.barrier()
        self.S.finish(self.out_events)
        self.st.close()


def host_inputs(inputs, batches, L, S_full=None):
    f = lambda a: np.ascontiguousarray(np.asarray(a, dtype=np.float32))
    m = {}
    xs = np.asarray(inputs["x"])
    m["x"] = f(np.concatenate([xs[b] for b in batches], axis=0))
    cc = np.asarray(inputs["c"])
    m["cT"] = f(np.stack([cc[b].reshape(KC, 128).T for b in batches], axis=0))
    m["norm1_g"] = f(inputs["norm1_g"][:L])
    m["norm2_g"] = f(inputs["norm2_g"][:L])
    m["ada_w"] = f(inputs["ada_w"][:L])
    m["ada_b"] = f(inputs["ada_b"][:L])
    m["w_in"] = f(inputs["w_in"][:L])
    cw = np.asarray(inputs["dn_conv_w"])[:L]
    m["conv_w"] = f(cw.transpose(2, 0, 1).reshape(16, 128, L, CONV_K).transpose(1, 2, 0, 3))
    m["a_log"] = f(np.asarray(inputs["dn_a_log"])[:L].reshape(1, L * 8))
    m["dt_bias"] = f(np.asarray(inputs["dn_dt_bias"])[:L].reshape(1, L * 8))
    m["dn_norm_g"] = f(np.asarray(inputs["dn_norm_g"])[:L].reshape(L, 128, 1))
    m["sinks"] = f(np.asarray(inputs["attn_sinks"])[:L].reshape(1, L * 8))
    m["w_out"] = f(inputs["w_out"][:L])
    m["rw"] = f(np.concatenate([np.asarray(inputs["router_group_w"])[:L], np.asarray(inputs["router_expert_w"])[:L]], axis=-1))
    m["rb"] = f(np.concatenate([np.asarray(inputs["router_group_b"])[:L], np.asarray(inputs["router_expert_b"])[:L]], axis=-1))
    if "expert_w_gate" in inputs:
        m["w_gate"] = f(inputs["expert_w_gate"][:L])
        m["w_up"] = f(inputs["expert_w_up"][:L])
        m["w_down"] = f(inputs["expert_w_down"][:L])
    m["final_g"] = f(np.asarray(inputs["final_norm_g"]).reshape(1, D))
    return m


ALL_PHASES = ("ada", "norm1", "proj", "attn_c", "attn_a", "dn", "outproj", "moe", "final")
N_CORES_USED = 4
SEG_T = 2048


def kernel(**inputs):
    x = np.asarray(inputs["x"])
    Bsz, S_full, _ = x.shape
    L = int(np.asarray(inputs["w_in"]).shape[0])
    nseg = S_full // SEG_T
    nb = Bsz // N_CORES_USED
    nc = bass.Bass("TRN2", target_bir_lowering=False)
    bld = Builder(nc, SEG_T, L, phases=ALL_PHASES, NB=nb, NSEG=nseg)
    bld.build()
    in_maps = []
    for c in range(N_CORES_USED):
        m = host_inputs(inputs, list(range(c * nb, (c + 1) * nb)), L)
        in_maps.append({k: v for k, v in m.items() if k in bld.I})
    res = run_bass_kernel_spmd(nc, in_maps, core_ids=list(range(N_CORES_USED)))
    outs = [np.asarray(res.results[c]["y_out"], dtype=np.float32).reshape(nb, S_full, D) for c in range(N_CORES_USED)]
    return np.concatenate(outs, axis=0)
```

```python
import numpy as np
import concourse.bass as bass
import concourse.mybir as mybir
from concourse.bass_utils import run_bass_kernel_spmd
from contextlib import ExitStack

F32 = mybir.dt.float32
BF16 = mybir.dt.bfloat16
I32 = mybir.dt.int32
U32 = mybir.dt.uint32
AF = mybir.ActivationFunctionType
ALU = mybir.AluOpType
AX = mybir.AxisListType
ds = bass.ds if hasattr(bass, "ds") else None

D = 2048
KC = D // 128
H_A, DH_A = 8, 64
PATTERNS = ((128, 1), (512, 4), (2048, 16))
H_K_B, H_V_B, DK_B, DV_B = 4, 8, 128, 128
CONV_K = 4
H_C, HKV_C, DH_C = 8, 2, 64
A_W = 512
BK_W = 512
BV_W = 1024
C_Q_W = 512
C_KV_W = 128
N_IN = 5392
MIX = 2048
N_EXP = 32
D_FF = 512
EPS = 1e-6
O_AQ, O_AK, O_AV = 0, 512, 1024
O_BQ, O_BK, O_BV, O_BZ = 1536, 2048, 2560, 3584
O_BB, O_BA = 4608, 4616
O_CQ, O_CK, O_CV = 4624, 5136, 5264
TP = 2048
NEG = -1.0e30

ENGS = ("pe", "act", "dve", "pool", "sp")
SAME_ENGINE_SYNC = True
NO_SELF_SYNC = ("pe",)
N_DMA_SEMS = 16
SEM_EPOCH = 30000


def sl(c0, n, step=1):
    return slice(c0, c0 + (n - 1) * step + 1, step)


class Dep:
    __slots__ = ("w", "r", "name")

    def __init__(self, name=""):
        self.w = None
        self.r = {}
        self.name = name


class Sched:
    def __init__(self, nc, stack):
        self.nc = nc
        self.stack = stack
        self.eng_obj = {"pe": nc.tensor, "act": nc.scalar, "dve": nc.vector,
                        "pool": nc.gpsimd, "sp": nc.sync}
        self.sems = {}
        self.nsem = 0
        self.ekey = {}
        self.ecount = {}
        self.allkeys = {e: [] for e in ENGS}
        for e in ENGS:
            self._new_epoch(e)
        self.waited = {e: {} for e in ENGS}
        self.dma_keys = {}
        self.dma_val = {}
        self.dma_rr = {}
        for q in ("sp", "pool"):
            ks = []
            for i in range(N_DMA_SEMS):
                k = self._newsem(f"d_{q}_{i}")
                ks.append(k)
                self.dma_val[k] = 0
            self.dma_keys[q] = ks
            self.dma_rr[q] = 0
        self.n_ins = 0

    def _newsem(self, name):
        k = self.nsem
        self.nsem += 1
        self.sems[k] = self.stack.enter_context(self.nc.semaphore(f"s{k}_{name}"))
        return k

    def _new_epoch(self, e):
        self.ekey[e] = self._newsem(f"e_{e}")
        self.ecount[e] = 0
        self.allkeys[e].append(self.ekey[e])

    def dep(self, name=""):
        return Dep(name)

    def deps(self, n, name=""):
        return [Dep(f"{name}{i}") for i in range(n)]

    def _wait(self, eng, ev):
        if ev is None:
            return
        k, v = ev
        if (not SAME_ENGINE_SYNC or eng in NO_SELF_SYNC) and k == self.ekey.get(eng):
            return
        if self.waited[eng].get(k, 0) >= v:
            return
        self.waited[eng][k] = v
        self.eng_obj[eng].wait_ge(self.sems[k], v)

    def _collect(self, eng, reads, writes):
        for d in reads:
            self._wait(eng, d.w)
        for d in writes:
            self._wait(eng, d.w)
            for k, v in d.r.items():
                self._wait(eng, (k, v))

    def _update(self, ev, reads, writes):
        k, v = ev
        for d in reads:
            if d.r.get(k, 0) < v:
                d.r[k] = v
        for d in writes:
            d.w = ev
            d.r = {}

    def op(self, eng, fn, reads=(), writes=()):
        if self.ecount[eng] >= SEM_EPOCH:
            self._new_epoch(eng)
        self._collect(eng, reads, writes)
        k = self.ekey[eng]
        self.ecount[eng] += 1
        ev = (k, self.ecount[eng])
        fn(self.eng_obj[eng]).then_inc(self.sems[k], 1)
        self._update(ev, reads, writes)
        self.n_ins += 1
        return ev

    def dma(self, q, out, in_, reads=(), writes=(), fn=None, **kw):
        ks = self.dma_keys[q]
        k = ks[self.dma_rr[q] % len(ks)]
        self.dma_rr[q] += 1
        if self.dma_val[k] > 0:
            self._wait(q, (k, self.dma_val[k]))
        self._collect(q, reads, writes)
        self.dma_val[k] += 16
        ev = (k, self.dma_val[k])
        if fn is not None:
            ins = fn(self.eng_obj[q])
        else:
            ins = self.eng_obj[q].dma_start(out=out, in_=in_, **kw)
        ins.then_inc(self.sems[k], 16)
        self._update(ev, reads, writes)
        self.n_ins += 1
        return ev

    def barrier(self):
        evs = []
        for e in ENGS:
            if self.ecount[e] > 0:
                evs.append((self.ekey[e], self.ecount[e]))
        for k, v in self.dma_val.items():
            if v > 0:
                evs.append((k, v))
        for e in ENGS:
            for ev in evs:
                if ev[0] == self.ekey[e]:
                    continue
                self._wait(e, ev)

    def finish(self, final_events=()):
        for ev in final_events:
            self._wait("sp", ev)


class Builder:
    def __init__(self, nc, T, L, n_cores=1, dbg=(), phases=("ada", "norm1", "proj"), NB=1, NSEG=1):
        self.nc = nc
        self.T = T
        self.L = L
        self.NB = NB
        self.NSEG = NSEG
        self.c0 = TP
        self.r0 = 0
        self.bi = 0
        self.seg_first = True
        self.NT = T // 128
        self.n_cores = n_cores
        self.dbg = set(dbg)
        self.phases = phases
        self.st = ExitStack()
        self.S = Sched(nc, self.st)
        self.out_events = []
        self.dn_stop = 99
        self.dn_sub = 99
        self.dn_fast = False

    def sb(self, stack, name, shape, dt):
        self._uid = getattr(self, "_uid", 0) + 1
        return stack.enter_context(self.nc.sbuf_tensor(f"{name}_u{self._uid}", list(shape), dt))

    def dram(self, name, shape, dt, kind=None):
        if kind is None:
            kind = "ExternalOutput" if name in self.dbg else "Internal"
        return self.nc.dram_tensor(name, list(shape), dt, kind=kind)

    def inp(self, name, shape, dt=F32):
        return self.nc.dram_tensor(name, list(shape), dt, kind="ExternalInput").ap()

    def declare(self):
        T, L = self.T, self.L
        I = {}
        NTOK = self.NB * self.NSEG * T
        I["x"] = self.inp("x", [NTOK, D])
        I["cT"] = self.inp("cT", [self.NB, 128, KC])
        I["norm1_g"] = self.inp("norm1_g", [L, D])
        I["norm2_g"] = self.inp("norm2_g", [L, D])
        I["ada_w"] = self.inp("ada_w", [L, D, 6 * D])
        I["ada_b"] = self.inp("ada_b", [L, 6 * D])
        I["w_in"] = self.inp("w_in", [L, D, N_IN])
        I["conv_w"] = self.inp("conv_w", [128, L, 16, CONV_K])
        I["a_log"] = self.inp("a_log", [1, L * 8])
        I["dt_bias"] = self.inp("dt_bias", [1, L * 8])
        I["dn_norm_g"] = self.inp("dn_norm_g", [L, 128, 1])
        I["sinks"] = self.inp("sinks", [1, L * 8])
        if "outproj" in self.phases:
            I["w_out"] = self.inp("w_out", [L, MIX, D])
        I["rw"] = self.inp("rw", [L, D, 36])
        I["rb"] = self.inp("rb", [L, 36])
        if "moe" in self.phases:
            I["w_gate"] = self.inp("w_gate", [L, N_EXP, D, D_FF])
            I["w_up"] = self.inp("w_up", [L, N_EXP, D, D_FF])
            I["w_down"] = self.inp("w_down", [L, N_EXP, D_FF, D])
        I["final_g"] = self.inp("final_g", [1, D])
        self.I = I
        self.y_out = self.nc.dram_tensor("y_out", [NTOK, D], F32, kind="ExternalOutput").ap()
        self.x_d = self.dram("x_d", [NTOK, D], F32).ap()
        self.mod_d = self.dram("mod_d", [self.NB, L, 6, 128, D], F32).ap()
        self.P_d = self.dram("P_d", [N_IN, TP + self.NSEG * T], F32).ap()
        self.TM = self.NSEG * T
        self.NTM = self.TM // 128
        self.NBLK = (2 * self.TM) // 128 + N_EXP
        self.h2_d = self.dram("h2_d", [self.TM, D], BF16).ap()
        self.rows_d = self.dram("rows_d", [self.NBLK * 128, D], BF16).ap()
        self.yrows_d = self.dram("yrows_d", [self.NBLK * 128, D], F32).ap()

    def consts(self):
        S, st = self.S, self.st
        self.reg_neg = self.nc.gpsimd.to_reg(NEG)
        self.reg_zero = self.nc.gpsimd.to_reg(0.0)
        self.ident = self.sb(st, "ident", [128, 128], F32)
        self.d_const = S.dep("const")
        d = self.d_const
        S.op("pool", lambda e: e.memset(self.ident[:], 1.0), writes=[d])
        S.op("pool", lambda e: e.affine_select(out=self.ident[:], in_=self.ident[:], pattern=[[-1, 128]],
                                                compare_op=ALU.is_equal, fill=self.reg_zero, base=0, channel_multiplier=1),
             reads=[d], writes=[d])
        self.ones = self.sb(st, "ones", [128, 128], F32)
        S.op("pool", lambda e: e.memset(self.ones[:], 1.0), writes=[d])
        self.epsc = self.sb(st, "epsc", [128, 1], F32)
        S.op("pool", lambda e: e.memset(self.epsc[:], EPS), writes=[d])
        self.zeros = self.sb(st, "zeros", [128, 2048], BF16)
        S.op("pool", lambda e: e.memset(self.zeros[:], 0.0), writes=[d])
        self.SCARRY = self.sb(st, "SCARRY", [128, 8, 128], F32)
        self.d_carry = S.dep("carry")
        self.d_act = S.deps(self.NT, "act")
        self.d_xt = S.deps(self.NSEG * self.NT, "xt")
        self.d_h2 = S.dep("h2_d")
        self.d_rows = S.dep("rows_d")
        self.d_yrows = S.dep("yrows_d")
        self.ps = [st.enter_context(self.nc.psum_tensor(f"ps{i}", [128, 512], F32)) for i in range(8)]
        self.d_ps = S.deps(8, "ps")
        self.d_x = S.dep("x_d")
        self.d_mod = S.dep("mod_d")
        self.d_P = S.dep("P_d")

    def phase_init(self):
        S = self.S
        T = self.T
        S.dma("sp", self.x_d, self.I["x"], writes=[self.d_x])
        r = 0
        while r < N_IN:
            n = min(128, N_IN - r)
            S.dma("pool", self.P_d[r:r + n, 0:TP], self.zeros[:n, 0:TP], reads=[self.d_const], writes=[self.d_P])
            r += n
        for bb_ in range(self.NBLK):
            S.dma("sp", self.rows_d[bb_ * 128:(bb_ + 1) * 128, :], self.zeros[:, :], reads=[self.d_const], writes=[self.d_rows])
        S.barrier()

    def phase_ada(self):
        S, nc, I = self.S, self.nc, self.I
        with ExitStack() as ph:
            cT = self.sb(ph, "cT", [128, KC], F32)
            cact = self.sb(ph, "cact", [128, KC], F32)
            CB = self.sb(ph, "CB", [128, KC, 128], F32)
            d_c = S.dep()
            S.dma("sp", cT[:], I["cT"][self.bi], writes=[d_c])
            S.op("act", lambda e: e.activation(out=cact[:], in_=cT[:], func=AF.Silu), reads=[d_c], writes=[d_c])
            S.op("dve", lambda e: e.tensor_copy(out=CB[:], in_=cact[:].unsqueeze(2).broadcast_to([128, KC, 128])),
                 reads=[d_c], writes=[d_c])
            NB = 2
            W = [self.sb(ph, f"adaW{i}", [128, KC, 512], F32) for i in range(NB)]
            dW = S.deps(NB)
            bb = [self.sb(ph, f"adab{i}", [128, 512], F32) for i in range(NB)]
            gg = [self.sb(ph, f"adag{i}", [128, 512], F32) for i in range(NB)]
            dB = S.deps(NB)
            ot = [self.sb(ph, f"adao{i}", [128, 512], F32) for i in range(NB)]
            dO = S.deps(NB)
            it = 0
            for l in range(self.L):
                wv = I["ada_w"][l].rearrange("(kc p) n -> p kc n", p=128)
                for cg in range(24):
                    b = it % NB
                    it += 1
                    part, sub = cg // 4, cg % 4
                    cs = slice(cg * 512, (cg + 1) * 512)
                    fs = slice(sub * 512, (sub + 1) * 512)
                    S.dma("sp", W[b][:], wv[:, :, cs], writes=[dW[b]])
                    S.dma("sp", bb[b][:], I["ada_b"][l:l + 1, cs].partition_broadcast(128), writes=[dB[b]])
                    is_sc = part in (1, 4)
                    if is_sc:
                        gsrc = I["norm1_g"] if part == 1 else I["norm2_g"]
                        S.dma("sp", gg[b][:], gsrc[l:l + 1, fs].partition_broadcast(128), writes=[dB[b]])
                    pb = b
                    for kc in range(KC):
                        S.op("pe", lambda e, kc=kc, b=b, pb=pb: e.matmul(self.ps[pb][:], lhsT=CB[:, kc, :], rhs=W[b][:, kc, :],
                                                                           start=(kc == 0), stop=(kc == KC - 1)),
                             reads=[d_c, dW[b]], writes=[self.d_ps[pb]])
                    S.op("dve", lambda e, b=b, pb=pb: e.tensor_tensor(out=ot[b][:], in0=self.ps[pb][:], in1=bb[b][:], op=ALU.add),
                         reads=[self.d_ps[pb], dB[b]], writes=[dO[b]])
                    if is_sc:
                        S.op("dve", lambda e, b=b: e.scalar_tensor_tensor(out=ot[b][:], in0=ot[b][:], scalar=1.0, in1=gg[b][:],
                                                                           op0=ALU.add, op1=ALU.mult),
                             reads=[dB[b], dO[b]], writes=[dO[b]])
                    S.dma("sp", self.mod_d[self.bi, l, part, :, fs], ot[b][:], reads=[dO[b]], writes=[self.d_mod])
        S.barrier()

    def phase_norm(self, l, which, router=None):
        S, nc, I = self.S, self.nc, self.I
        T, NT = self.T, self.NT
        with ExitStack() as ph:
            Abc = self.sb(ph, "Abc", [128, D], F32)
            Bbc = self.sb(ph, "Bbc", [128, D], F32)
            d_ab = S.dep()
            if which == "final":
                S.dma("sp", Abc[:], I["final_g"][0:1, :].partition_broadcast(128), writes=[d_ab])
            else:
                pa, pb_ = (1, 0) if which == 1 else (4, 3)
                S.dma("sp", Abc[:], self.mod_d[self.bi, l, pa], reads=[self.d_mod], writes=[d_ab])
                S.dma("sp", Bbc[:], self.mod_d[self.bi, l, pb_], reads=[self.d_mod], writes=[d_ab])
            NB = 2
            xt = [self.sb(ph, f"xt{i}", [128, D], F32) for i in range(NB)]
            dx = S.deps(NB)
            hf = [self.sb(ph, f"hf{i}", [128, D], F32) for i in range(NB)]
            dh = S.deps(NB)
            junk = self.sb(ph, "junk", [128, D], BF16)
            d_junk = S.dep()
            st_ = [self.sb(ph, f"nst{i}", [128, 4], F32) for i in range(NB)]
            dst_ = S.deps(NB)
            if router is not None:
                hT32 = [self.sb(ph, f"hT32_{i}", [128, KC, 128], F32) for i in range(NB)]
                dhT = S.deps(NB)
            for i in range(NT):
                b = i % NB
                rows = slice(self.r0 + i * 128, self.r0 + (i + 1) * 128)
                S.dma("sp", xt[b][:], self.x_d[rows, :], reads=[self.d_x, self.d_xt[i]], writes=[dx[b]])
                S.op("act", lambda e, b=b: e.activation(out=junk[:], in_=xt[b][:], func=AF.Square, accum_out=st_[b][:, 0:1]),
                     reads=[dx[b]], writes=[d_junk, dst_[b]])
                S.op("act", lambda e, b=b: e.activation(out=st_[b][:, 1:2], in_=st_[b][:, 0:1], func=AF.Sqrt,
                                                        scale=1.0 / D, bias=self.epsc[:, 0:1]),
                     reads=[dst_[b], self.d_const], writes=[dst_[b]])
                S.op("dve", lambda e, b=b: e.reciprocal(out=st_[b][:, 2:3], in_=st_[b][:, 1:2]),
                     reads=[dst_[b]], writes=[dst_[b]])
                S.op("dve", lambda e, b=b: e.scalar_tensor_tensor(out=hf[b][:], in0=xt[b][:], scalar=st_[b][:, 2:3], in1=Abc[:],
                                                                   op0=ALU.mult, op1=ALU.mult),
                     reads=[dx[b], dst_[b], d_ab], writes=[dh[b]])
                if which == "final":
                    self.out_events.append(S.dma("sp", self.y_out[rows, :], hf[b][:], reads=[dh[b]]))
                    continue
                S.op("pool", lambda e, b=b: e.tensor_tensor(out=hf[b][:], in0=hf[b][:], in1=Bbc[:], op=ALU.add),
                     reads=[d_ab, dh[b]], writes=[dh[b]])
                if router is not None:
                    S.op("pool", lambda e, b=b: e.tensor_copy(out=router["hbf"][b][:], in_=hf[b][:]), reads=[dh[b]], writes=[router["d_hbf"][b]])
                    S.dma("sp", self.h2_d[i * 128:(i + 1) * 128, :], router["hbf"][b][:], reads=[router["d_hbf"][b]], writes=[self.d_h2])
                for q4 in range(4):
                    pb = (i * 4 + q4) % 4
                    for j in range(4):
                        kc = q4 * 4 + j
                        S.op("pe", lambda e, b=b, kc=kc, pb=pb, j=j: e.transpose(self.ps[pb][:, j * 128:(j + 1) * 128],
                                                                                   hf[b][:, kc * 128:(kc + 1) * 128], self.ident[:]),
                             reads=[dh[b], self.d_const], writes=[self.d_ps[pb]])
                    if router is None:
                        S.op("act", lambda e, pb=pb, q4=q4, i=i: e.activation(
                            out=self.actT[:, q4 * 4:(q4 + 1) * 4, i * 128:(i + 1) * 128],
                            in_=self.ps[pb][:].rearrange("p (j n) -> p j n", j=4), func=AF.Copy),
                            reads=[self.d_ps[pb]], writes=[self.d_act[i]])
                    if router is not None:
                        S.op("dve", lambda e, pb=pb, q4=q4, b=b: e.tensor_copy(
                            out=hT32[b][:, q4 * 4:(q4 + 1) * 4, :],
                            in_=self.ps[pb][:].rearrange("p (j n) -> p j n", j=4)),
                            reads=[self.d_ps[pb]], writes=[dhT[b]])
                if router is not None:
                    pr = 4 + (i % 2)
                    for kc in range(KC):
                        S.op("pe", lambda e, b=b, kc=kc, pr=pr: e.matmul(self.ps[pr][:, 0:36], lhsT=hT32[b][:, kc, :],
                                                                          rhs=router["RW"][:, kc, :],
                                                                          start=(kc == 0), stop=(kc == KC - 1)),
                             reads=[dhT[b], router["d_rw"]], writes=[self.d_ps[pr]])
                    S.op("dve", lambda e, pr=pr, i=i: e.tensor_tensor(out=router["LG"][:, i, :], in0=self.ps[pr][:, 0:36],
                                                                       in1=router["RB"][:], op=ALU.add),
                         reads=[self.d_ps[pr], router["d_rw"]], writes=[router["d_lg"]])
        S.barrier()

    def phase_proj(self, l):
        S, nc, I = self.S, self.nc, self.I
        T = self.T
        NTT = T // 512
        wv = I["w_in"][l].rearrange("(kc p) n -> p kc n", p=128)
        with ExitStack() as ph:
            NB = 2
            W = [self.sb(ph, f"pjW{i}", [128, KC, 512], BF16) for i in range(NB)]
            dW = S.deps(NB)
            stg = [self.sb(ph, f"pjS{i}", [128, T], F32) for i in range(NB)]
            dS_ = S.deps(NB)
            nsg = (N_IN + 511) // 512
            cnt = 0
            for sg in range(nsg):
                c0 = sg * 512
                cw = min(512, N_IN - c0)
                b = sg % NB
                S.dma("pool", W[b][:, :, 0:cw], wv[:, :, c0:c0 + cw], writes=[dW[b]])
                for j in range((cw + 127) // 128):
                    gs = min(128, cw - j * 128)
                    sb_ = cnt % NB
                    for tt in range(NTT):
                        pb = cnt % 8
                        cnt += 1
                        for kc in range(KC):
                            S.op("pe", lambda e, b=b, kc=kc, j=j, gs=gs, tt=tt, pb=pb: e.matmul(
                                self.ps[pb][0:gs, :], lhsT=W[b][:, kc, j * 128:j * 128 + gs],
                                rhs=self.actT[:, kc, tt * 512:(tt + 1) * 512], start=(kc == 0), stop=(kc == KC - 1)),
                                reads=[dW[b]] + self.d_act[tt * 4:(tt + 1) * 4], writes=[self.d_ps[pb]])
                        eng = "act" if (cnt % 2 == 0) else "dve"
                        if eng == "act":
                            S.op("act", lambda e, sb_=sb_, gs=gs, tt=tt, pb=pb: e.activation(
                                out=stg[sb_][0:gs, tt * 512:(tt + 1) * 512], in_=self.ps[pb][0:gs, :], func=AF.Copy),
                                reads=[self.d_ps[pb]], writes=[dS_[sb_]])
                        else:
                            S.op("dve", lambda e, sb_=sb_, gs=gs, tt=tt, pb=pb: e.tensor_copy(
                                out=stg[sb_][0:gs, tt * 512:(tt + 1) * 512], in_=self.ps[pb][0:gs, :]),
                                reads=[self.d_ps[pb]], writes=[dS_[sb_]])
                    r0 = c0 + j * 128
                    S.dma("sp", self.P_d[r0:r0 + gs, self.c0:self.c0 + T], stg[sb_][0:gs, :], reads=[dS_[sb_]], writes=[self.d_P])
        S.barrier()

    def attn_consts(self, ph):
        S = self.S
        d = S.dep("attnc")
        self.d_attnc = d
        reli = self.sb(ph, "reli", [128, 256], I32)
        self.REL = self.sb(ph, "REL", [128, 256], F32)
        S.op("pool", lambda e: e.iota(reli[:], pattern=[[-1, 256]], base=128, channel_multiplier=1), writes=[d])
        S.op("pool", lambda e: e.tensor_copy(out=self.REL[:], in_=reli[:]), reads=[d], writes=[d])
        self.HM = self.sb(ph, "HM", [128, 256], F32)
        S.op("pool", lambda e: e.memset(self.HM[:], 0.0), writes=[d])
        S.op("pool", lambda e: e.memset(self.HM[:, 0:128], NEG), reads=[d], writes=[d])
        self.E2 = self.sb(ph, "E2", [2, 128], F32)
        S.op("pool", lambda e: e.memset(self.E2[:], 1.0), writes=[d])
        S.op("pool", lambda e: e.affine_select(out=self.E2[:], in_=self.E2[:], pattern=[[1, 128]], compare_op=ALU.is_ge,
                                                fill=self.reg_zero, base=0, channel_multiplier=-64), reads=[d], writes=[d])
        S.op("pool", lambda e: e.affine_select(out=self.E2[:], in_=self.E2[:], pattern=[[-1, 128]], compare_op=ALU.is_ge,
                                                fill=self.reg_zero, base=63, channel_multiplier=64), reads=[d], writes=[d])

    def make_bias(self, tile, dep, coef, maxd):
        S = self.S
        S.op("dve", lambda e: e.tensor_scalar_mul(out=tile[:], in0=self.REL[:], scalar1=float(coef)),
             reads=[self.d_attnc], writes=[dep])
        S.op("pool", lambda e: e.affine_select(out=tile[:], in_=tile[:], pattern=[[-1, 256]], compare_op=ALU.is_ge,
                                                fill=self.reg_neg, base=128, channel_multiplier=1), reads=[dep], writes=[dep])
        S.op("pool", lambda e: e.affine_select(out=tile[:], in_=tile[:], pattern=[[1, 256]], compare_op=ALU.is_ge,
                                                fill=self.reg_neg, base=maxd - 128, channel_multiplier=-1), reads=[dep], writes=[dep])

    def attn_unit(self, ctx, q_ap, k_ap, bias, d_bias, first, Vb0, Vb1, d_V, half, out_ap, out_deps,
                  sink_ap=None, lse_ap=None, d_lse=None, in_deps=()):
        S = self.S
        u = ctx["u"]
        ctx["u"] += 1
        b = u % 2
        pS, dS = self.ps[b], self.d_ps[b]
        pT, dT = ctx["psT"][b], self.d_ps[2 + b]
        pO, dO = self.ps[4 + b], self.d_ps[4 + b]
        s32, d_s = ctx["s32"][b], ctx["d_s32"][b]
        pbf, d_p = ctx["pbf"][b], ctx["d_pbf"][b]
        pTs, d_pT = ctx["pTs"][b], ctx["d_pTs"][b]
        stt, d_st = ctx["stt"][b], ctx["d_stt"][b]
        S.op("pe", lambda e: e.matmul(pS[:, 0:256], lhsT=q_ap, rhs=k_ap, start=True, stop=True),
             reads=list(in_deps), writes=[dS])
        S.op("dve", lambda e: e.scalar_tensor_tensor(out=s32[:], in0=pS[:, 0:256], scalar=0.125, in1=bias[:],
                                                      op0=ALU.mult, op1=ALU.add), reads=[dS, d_bias], writes=[d_s])
        if first:
            S.op("pool", lambda e: e.tensor_tensor(out=s32[:], in0=s32[:], in1=self.HM[:], op=ALU.add),
                 reads=[d_s, self.d_attnc], writes=[d_s])
        S.op("dve", lambda e: e.tensor_reduce(out=stt[:, 0:1], in_=s32[:], axis=AX.X, op=ALU.max, negate=True),
             reads=[d_s], writes=[d_st])
        if sink_ap is not None:
            S.op("dve", lambda e: e.scalar_tensor_tensor(out=stt[:, 0:1], in0=sink_ap, scalar=-1.0, in1=stt[:, 0:1],
                                                          op0=ALU.mult, op1=ALU.min), reads=[d_st, ctx["d_sink"]], writes=[d_st])
        S.op("act", lambda e: e.activation(out=pbf[:], in_=s32[:], func=AF.Exp, bias=stt[:, 0:1], scale=1.0,
                                            accum_out=stt[:, 1:2]), reads=[d_s, d_st], writes=[d_p, d_st])
        if sink_ap is not None:
            S.op("act", lambda e: e.activation(out=stt[:, 3:4], in_=sink_ap, func=AF.Exp, bias=stt[:, 0:1], scale=1.0),
                 reads=[d_st, ctx["d_sink"]], writes=[d_st])
            S.op("dve", lambda e: e.tensor_tensor(out=stt[:, 1:2], in0=stt[:, 1:2], in1=stt[:, 3:4], op=ALU.add),
                 reads=[d_st], writes=[d_st])
        S.op("dve", lambda e: e.reciprocal(out=stt[:, 2:3], in_=stt[:, 1:2]), reads=[d_st], writes=[d_st])
        S.op("pool", lambda e: e.tensor_scalar_mul(out=pbf[:], in0=pbf[:], scalar1=stt[:, 2:3]),
             reads=[d_p, d_st], writes=[d_p])
        if lse_ap is not None:
            S.op("act", lambda e: e.activation(out=stt[:, 3:4], in_=stt[:, 1:2], func=AF.Ln), reads=[d_st], writes=[d_st])
            S.op("dve", lambda e: e.tensor_tensor(out=lse_ap, in0=stt[:, 3:4], in1=stt[:, 0:1], op=ALU.subtract),
                 reads=[d_st], writes=[d_lse])
        for j in range(2):
            S.op("pe", lambda e, j=j: e.transpose(pT[:, j * 128:(j + 1) * 128], pbf[:, j * 128:(j + 1) * 128], ctx["identb"][:]),
                 reads=[d_p, ctx["d_identb"]], writes=[dT])
        S.op("act", lambda e: e.activation(out=pTs[:], in_=pT[:, 0:256], func=AF.Copy), reads=[dT], writes=[d_pT])
        S.op("pe", lambda e: e.matmul(pO[:, 0:128], lhsT=Vb0, rhs=pTs[:, 0:128], start=True, stop=False),
             reads=[d_V, d_pT], writes=[dO])
        S.op("pe", lambda e: e.matmul(pO[:, 0:128], lhsT=Vb1, rhs=pTs[:, 128:256], start=False, stop=True),
             reads=[d_V, d_pT], writes=[dO])
        rs = slice(half * 64, half * 64 + 64)
        S.op("dve", lambda e: e.tensor_copy(out=out_ap, in_=pO[rs, 0:128]), reads=[dO], writes=list(out_deps))

    def attn_ctx(self, ph):
        S = self.S
        ctx = {"u": 0}
        ctx["psT"] = [self.ps[2].bitcast(BF16), self.ps[3].bitcast(BF16)]
        ctx["s32"] = [self.sb(ph, f"s32_{i}", [128, 256], F32) for i in range(2)]
        ctx["d_s32"] = S.deps(2)
        ctx["pbf"] = [self.sb(ph, f"pbf_{i}", [128, 256], BF16) for i in range(2)]
        ctx["d_pbf"] = S.deps(2)
        ctx["pTs"] = [self.sb(ph, f"pTs_{i}", [128, 256], BF16) for i in range(2)]
        ctx["d_pTs"] = S.deps(2)
        ctx["stt"] = [self.sb(ph, f"stt_{i}", [128, 8], F32) for i in range(2)]
        ctx["d_stt"] = S.deps(2)
        identb = self.sb(ph, "identb", [128, 128], BF16)
        ctx["identb"] = identb
        ctx["d_identb"] = S.dep()
        S.op("dve", lambda e: e.tensor_copy(out=identb[:], in_=self.ident[:]), reads=[self.d_const], writes=[ctx["d_identb"]])
        return ctx

    def build_vblocks(self, ctx, vT, d_vT, Vblk, d_Vblk, specs):
        S = self.S
        for n, (idx, c0, step) in enumerate(specs):
            b = n % 2
            pT, dT = ctx["psT"][b], self.d_ps[2 + b]
            src = vT[:, sl(c0, 128, step)]
            S.op("pe", lambda e, pT=pT, src=src: e.transpose(pT[:, 0:128], src, ctx["identb"][:]),
                 reads=[d_vT, ctx["d_identb"]], writes=[dT])
            eng = "act" if n % 2 == 0 else "dve"
            if eng == "act":
                S.op("act", lambda e, pT=pT, idx=idx: e.activation(out=Vblk[:, idx, :], in_=pT[:, 0:128], func=AF.Copy),
                     reads=[dT], writes=[d_Vblk])
            else:
                S.op("dve", lambda e, pT=pT, idx=idx: e.tensor_copy(out=Vblk[:, idx, :], in_=pT[:, 0:128]),
                     reads=[dT], writes=[d_Vblk])

    def phase_attn_c(self, l):
        S, I = self.S, self.I
        T, NT = self.T, self.NT
        with ExitStack() as ph:
            self.attn_consts(ph)
            ctx = self.attn_ctx(ph)
            sink = self.sb(ph, "sink", [128, 8], F32)
            ctx["d_sink"] = S.dep()
            S.dma("sp", sink[:], I["sinks"][0:1, l * 8:(l + 1) * 8].partition_broadcast(128), writes=[ctx["d_sink"]])
            qc = self.sb(ph, "qc", [128, 4, T], BF16)
            kc = self.sb(ph, "kc", [128, TP + T], BF16)
            vN = self.sb(ph, "vN", [128, TP + T], BF16)
            vS = self.sb(ph, "vS", [128, TP + T], BF16)
            d_q, d_k, d_vn, d_vs = S.deps(4)
            for g in range(2):
                S.dma("pool", qc[64 * g:64 * g + 64, :, :],
                      self.P_d[O_CQ + 256 * g:O_CQ + 256 * (g + 1), self.c0:self.c0 + T].rearrange("(j p) t -> p j t", p=64),
                      reads=[self.d_P], writes=[d_q])
            hs = slice(self.c0 - TP, self.c0 + T)
            S.dma("pool", kc[:], self.P_d[O_CK:O_CK + 128, hs], reads=[self.d_P], writes=[d_k])
            S.dma("pool", vN[:], self.P_d[O_CV:O_CV + 128, hs], reads=[self.d_P], writes=[d_vn])
            S.dma("pool", vS[0:64, :], self.P_d[O_CV + 64:O_CV + 128, hs], reads=[self.d_P], writes=[d_vs])
            S.dma("pool", vS[64:128, :], self.P_d[O_CV:O_CV + 64, hs], reads=[self.d_P], writes=[d_vs])
            VN = self.sb(ph, "VN", [128, NT + 1, 128], BF16)
            VS = self.sb(ph, "VS", [128, NT + 1, 128], BF16)
            d_VN, d_VS = S.deps(2)
            specs = [(j + 1, TP + 128 * j, 1) for j in range(-1, NT)]
            self.build_vblocks(ctx, vN, d_vn, VN, d_VN, specs)
            self.build_vblocks(ctx, vS, d_vs, VS, d_VS, specs)
            bias = [self.sb(ph, f"biasc{h}", [128, 256], F32) for h in range(8)]
            d_b = S.deps(8)
            for h in range(8):
                self.make_bias(bias[h], d_b[h], -(2.0 ** (-(h + 1))), 127)
            for h in range(8):
                g, hh = h // 4, h % 2
                Vb, dV = (VN, d_VN) if hh == g else (VS, d_VS)
                for j in range(NT):
                    self.attn_unit(ctx,
                                   q_ap=qc[64 * g:64 * g + 64, h % 4, 128 * j:128 * (j + 1)],
                                   k_ap=kc[64 * g:64 * g + 64, TP + 128 * (j - 1):TP + 128 * (j + 1)],
                                   bias=bias[h], d_bias=d_b[h], first=(j == 0 and self.seg_first),
                                   Vb0=Vb[:, j, :], Vb1=Vb[:, j + 1, :], d_V=dV, half=hh,
                                   out_ap=self.actT[64 * hh:64 * hh + 64, 12 + h // 2, 128 * j:128 * (j + 1)],
                                   out_deps=[self.d_act[j]], sink_ap=sink[:, h:h + 1], in_deps=[d_q, d_k])
        S.barrier()

    def phase_attn_a(self, l):
        S, I = self.S, self.I
        T, NT = self.T, self.NT
        with ExitStack() as ph:
            self.attn_consts(ph)
            ctx = self.attn_ctx(ph)
            qa = self.sb(ph, "qa", [128, T], BF16)
            ka = self.sb(ph, "ka", [128, TP + T], BF16)
            va = self.sb(ph, "va", [128, TP + T], BF16)
            d_q, d_k, d_v = S.deps(3)
            maxblk = max(d * (T // (128 * d) + 1) for _, d in PATTERNS)
            Vblk = self.sb(ph, "Vblk", [128, maxblk, 128], BF16)
            d_Vb = S.dep()
            opT = [self.sb(ph, f"opT{p}", [128, T], BF16) for p in range(3)]
            d_op = S.deps(3)
            STAT = [self.sb(ph, f"STAT{p}", [128, NT * 2], F32) for p in range(3)]
            d_stat = S.deps(3)
            R = self.sb(ph, "R", [2, 3, T], F32)
            d_R = S.dep()
            Mx = self.sb(ph, "Mx", [2, T], F32)
            d_M = S.dep()
            bias = [self.sb(ph, f"biasa{i}", [128, 256], F32) for i in range(6)]
            d_b = S.deps(6)
            acc = self.sb(ph, "acca", [128, 512], F32)
            d_acc = S.dep()
            tmp = self.sb(ph, "tmpa", [128, 512], F32)
            d_tmp = S.dep()
            for ch in range(4):
                hs = slice(self.c0 - TP, self.c0 + T)
                S.dma("pool", qa[:], self.P_d[O_AQ + 128 * ch:O_AQ + 128 * (ch + 1), self.c0:self.c0 + T], reads=[self.d_P], writes=[d_q])
                S.dma("pool", ka[:], self.P_d[O_AK + 128 * ch:O_AK + 128 * (ch + 1), hs], reads=[self.d_P], writes=[d_k])
                S.dma("pool", va[:], self.P_d[O_AV + 128 * ch:O_AV + 128 * (ch + 1), hs], reads=[self.d_P], writes=[d_v])
                for p, (w, d) in enumerate(PATTERNS):
                    for hh in range(2):
                        h = 2 * ch + hh
                        self.make_bias(bias[p * 2 + hh], d_b[p * 2 + hh], -(2.0 ** (-(h + 1))) * d, 128)
                for p, (w, d) in enumerate(PATTERNS):
                    nbq = T // (128 * d)
                    specs = []
                    for r in range(d):
                        for j in range(-1, nbq):
                            specs.append((r * (nbq + 1) + j + 1, TP + r + d * 128 * j, d))
                    self.build_vblocks(ctx, va, d_v, Vblk, d_Vb, specs)
                    for hh in range(2):
                        ps_ = slice(64 * hh, 64 * hh + 64)
                        for r in range(d):
                            for j in range(nbq):
                                blk = r * nbq + j
                                q0 = r + d * 128 * j
                                k0 = TP + r + d * 128 * (j - 1)
                                q_ap = qa[ps_, sl(q0, 128, d)]
                                k_ap = ka[ps_, sl(k0, 256, d)]
                                o_ap = opT[p][ps_, sl(q0, 128, d)]
                                vi = r * (nbq + 1) + j
                                self.attn_unit(ctx, q_ap=q_ap, k_ap=k_ap, bias=bias[p * 2 + hh], d_bias=d_b[p * 2 + hh],
                                               first=(j == 0 and self.seg_first), Vb0=Vblk[:, vi, :], Vb1=Vblk[:, vi + 1, :], d_V=d_Vb, half=hh,
                                               out_ap=o_ap, out_deps=[d_op[p]],
                                               lse_ap=STAT[p][:, blk * 2 + hh:blk * 2 + hh + 1], d_lse=d_stat[p],
                                               in_deps=[d_q, d_k])
                    for r in range(d):
                        for j in range(nbq):
                            blk = r * nbq + j
                            q0 = r + d * 128 * j
                            pb = 6 + (blk % 2)
                            S.op("pe", lambda e, pb=pb, p=p, blk=blk: e.transpose(self.ps[pb][0:2, 0:128], STAT[p][:, blk * 2:blk * 2 + 2],
                                                                                   self.ident[:]),
                                 reads=[d_stat[p], self.d_const], writes=[self.d_ps[pb]])
                            dst = R[0:2, p, sl(q0, 128, d)]
                            S.op("act", lambda e, pb=pb, dst=dst: e.activation(out=dst, in_=self.ps[pb][0:2, 0:128], func=AF.Copy),
                                 reads=[self.d_ps[pb]], writes=[d_R])
                S.op("dve", lambda e: e.tensor_tensor(out=Mx[:], in0=R[:, 0, :], in1=R[:, 1, :], op=ALU.max), reads=[d_R], writes=[d_M])
                S.op("dve", lambda e: e.tensor_tensor(out=Mx[:], in0=Mx[:], in1=R[:, 2, :], op=ALU.max), reads=[d_R, d_M], writes=[d_M])
                for p in range(3):
                    S.op("dve", lambda e, p=p: e.tensor_tensor(out=R[:, p, :], in0=R[:, p, :], in1=Mx[:], op=ALU.subtract),
                         reads=[d_M, d_R], writes=[d_R])
                S.op("act", lambda e: e.activation(out=R[:], in_=R[:], func=AF.Exp), reads=[d_R], writes=[d_R])
                S.op("dve", lambda e: e.tensor_tensor(out=Mx[:], in0=R[:, 0, :], in1=R[:, 1, :], op=ALU.add), reads=[d_R, d_M], writes=[d_M])
                S.op("dve", lambda e: e.tensor_tensor(out=Mx[:], in0=Mx[:], in1=R[:, 2, :], op=ALU.add), reads=[d_R, d_M], writes=[d_M])
                S.op("dve", lambda e: e.reciprocal(out=Mx[:], in_=Mx[:]), reads=[d_M], writes=[d_M])
                for p in range(3):
                    S.op("dve", lambda e, p=p: e.tensor_tensor(out=R[:, p, :], in0=R[:, p, :], in1=Mx[:], op=ALU.mult),
                         reads=[d_M, d_R], writes=[d_R])
                for tt in range(T // 512):
                    cs = slice(tt * 512, (tt + 1) * 512)
                    for p in range(3):
                        pb = 6 + (p % 2)
                        S.op("pe", lambda e, pb=pb, p=p, cs=cs: e.matmul(self.ps[pb][:], lhsT=self.E2[:], rhs=R[0:2, p, cs],
                                                                          start=True, stop=True),
                             reads=[d_R, self.d_attnc], writes=[self.d_ps[pb]])
                        if p == 0:
                            S.op("dve", lambda e, pb=pb, cs=cs: e.tensor_tensor(out=acc[:], in0=opT[0][:, cs], in1=self.ps[pb][:], op=ALU.mult),
                                 reads=[d_op[0], self.d_ps[pb]], writes=[d_acc])
                        else:
                            S.op("dve", lambda e, pb=pb, cs=cs, p=p: e.tensor_tensor(out=tmp[:], in0=opT[p][:, cs], in1=self.ps[pb][:], op=ALU.mult),
                                 reads=[d_op[p], self.d_ps[pb]], writes=[d_tmp])
                            if p == 1:
                                S.op("pool", lambda e: e.tensor_tensor(out=acc[:], in0=acc[:], in1=tmp[:], op=ALU.add),
                                     reads=[d_tmp, d_acc], writes=[d_acc])
                            else:
                                S.op("pool", lambda e, cs=cs, ch=ch: e.tensor_tensor(out=self.actT[:, ch, cs], in0=acc[:], in1=tmp[:], op=ALU.add),
                                     reads=[d_tmp, d_acc], writes=self.d_act[tt * 4:(tt + 1) * 4])
        S.barrier()

    def phase_dn(self, l):
        S, I = self.S, self.I
        T, NT = self.T, self.NT
        DKS = float(DK_B) ** -0.5
        with ExitStack() as ph:
            cnt = {"s": 0}

            cnt["t"] = 0
            d_bank = [S.dep() for _ in range(8)]
            d_slot = [d_bank[i // 4] for i in range(32)]

            def slot():
                bk = 2 + cnt["s"] % 6
                cnt["s"] += 1
                return self.ps[bk][:, 0:128], d_bank[bk]

            def tslot():
                bk = cnt["t"] % 2
                cnt["t"] += 1
                return self.ps[bk][:, 0:128], d_bank[bk]

            d_m = S.dep()
            ML = self.sb(ph, "ML", [128, 128], F32)
            MIT = self.sb(ph, "MIT", [128, 128], F32)
            CH0 = self.sb(ph, "CH0", [128, 128], F32)
            CH1 = self.sb(ph, "CH1", [128, 128], F32)
            S.op("pool", lambda e: e.memset(MIT[:], 1.0), writes=[d_m])
            S.op("pool", lambda e: e.affine_select(out=MIT[:], in_=MIT[:], pattern=[[1, 128]], compare_op=ALU.is_ge,
                                                    fill=self.reg_zero, base=0, channel_multiplier=-1), reads=[d_m], writes=[d_m])
            S.op("pool", lambda e: e.memset(MIT[0:64, 64:128], 0.0), reads=[d_m], writes=[d_m])
            S.op("pool", lambda e: e.memset(ML[:], 1.0), writes=[d_m])
            S.op("pool", lambda e: e.affine_select(out=ML[:], in_=ML[:], pattern=[[-1, 128]], compare_op=ALU.is_ge,
                                                    fill=self.reg_zero, base=-1, channel_multiplier=1), reads=[d_m], writes=[d_m])
            S.op("pool", lambda e: e.memset(ML[64:128, 0:64], 0.0), reads=[d_m], writes=[d_m])
            S.op("pool", lambda e: e.memset(CH0[:], 0.0), writes=[d_m])
            S.op("pool", lambda e: e.memset(CH0[0:64, :], 1.0), reads=[d_m], writes=[d_m])
            S.op("pool", lambda e: e.memset(CH1[:], 0.0), writes=[d_m])
            S.op("pool", lambda e: e.memset(CH1[64:128, :], 1.0), reads=[d_m], writes=[d_m])
            d_par = S.dep()
            cwt = self.sb(ph, "cwt", [128, 16, CONV_K], F32)
            S.dma("sp", cwt[:], I["conv_w"][:, l, :, :], writes=[d_par])
            ngc = self.sb(ph, "ngc", [128, 1], F32)
            S.dma("sp", ngc[:], I["dn_norm_g"][l], writes=[d_par])
            alog = self.sb(ph, "alog", [128, 8], F32)
            dtb = self.sb(ph, "dtb", [128, 8], F32)
            S.dma("sp", alog[:], I["a_log"][0:1, l * 8:(l + 1) * 8].partition_broadcast(128), writes=[d_par])
            S.dma("sp", dtb[:], I["dt_bias"][0:1, l * 8:(l + 1) * 8].partition_broadcast(128), writes=[d_par])
            nea = self.sb(ph, "nea", [128, 8], F32)
            S.op("act", lambda e: e.activation(out=nea[:], in_=alog[:], func=AF.Exp), reads=[d_par], writes=[d_par])
            S.op("dve", lambda e: e.tensor_scalar_mul(out=nea[:], in0=nea[:], scalar1=-1.0), reads=[d_par], writes=[d_par])
            d_g = S.dep()
            bbaT = self.sb(ph, "bbaT", [16, T], F32)
            S.dma("sp", bbaT[:], self.P_d[O_BB:O_BB + 16, self.c0:self.c0 + T], reads=[self.d_P], writes=[d_g])
            GB = self.sb(ph, "GB", [128, NT, 16], F32)
            for i in range(NT):
                p_, dp_ = tslot()
                S.op("pe", lambda e, p_=p_, i=i: e.transpose(p_[:, 0:16], bbaT[:, i * 128:(i + 1) * 128], self.ident[0:16, 0:16]),
                     reads=[d_g, self.d_const], writes=[dp_])
                S.op("act", lambda e, p_=p_, i=i: e.activation(out=GB[:, i, :], in_=p_[:, 0:16], func=AF.Copy),
                     reads=[dp_], writes=[d_g])
            BETA = self.sb(ph, "BETA", [128, NT, 8], F32)
            G = self.sb(ph, "G", [128, NT, 8], F32)
            t1 = self.sb(ph, "gt1", [128, NT, 8], F32)
            t2 = self.sb(ph, "gt2", [128, NT, 8], F32)
            S.op("act", lambda e: e.activation(out=BETA[:], in_=GB[:, :, 0:8], func=AF.Exp, scale=-1.0), reads=[d_g], writes=[d_g])
            S.op("dve", lambda e: e.tensor_scalar_add(out=BETA[:], in0=BETA[:], scalar1=1.0), reads=[d_g], writes=[d_g])
            S.op("dve", lambda e: e.reciprocal(out=BETA[:], in_=BETA[:]), reads=[d_g], writes=[d_g])
            S.op("dve", lambda e: e.tensor_tensor(out=G[:], in0=GB[:, :, 8:16], in1=dtb[:].unsqueeze(1).broadcast_to([128, NT, 8]), op=ALU.add),
                 reads=[d_g, d_par], writes=[d_g])
            S.op("dve", lambda e: e.tensor_scalar_mul(out=t1[:], in0=G[:], scalar1=-1.0), reads=[d_g], writes=[d_g])
            S.op("dve", lambda e: e.tensor_tensor(out=t1[:], in0=t1[:], in1=G[:], op=ALU.max), reads=[d_g], writes=[d_g])
            S.op("act", lambda e: e.activation(out=t1[:], in_=t1[:], func=AF.Exp, scale=-1.0), reads=[d_g], writes=[d_g])
            S.op("act", lambda e: e.activation(out=t1[:], in_=t1[:], func=AF.Ln, bias=self.ones[:, 0:1], scale=1.0),
                 reads=[d_g, self.d_const], writes=[d_g])
            S.op("dve", lambda e: e.tensor_scalar_max(out=t2[:], in0=G[:], scalar1=0.0), reads=[d_g], writes=[d_g])
            S.op("dve", lambda e: e.tensor_tensor(out=t2[:], in0=t2[:], in1=t1[:], op=ALU.add), reads=[d_g], writes=[d_g])
            S.op("dve", lambda e: e.tensor_tensor(out=G[:], in0=t2[:], in1=nea[:].unsqueeze(1).broadcast_to([128, NT, 8]), op=ALU.mult),
                 reads=[d_g, d_par], writes=[d_g])
            GC = self.sb(ph, "GC", [128, NT, 8], F32)
            GL = self.sb(ph, "GL", [128, NT, 2, 8], F32)
            for i in range(NT):
                p_, dp_ = slot()
                S.op("pe", lambda e, p_=p_, i=i: e.matmul(p_[:, 0:8], lhsT=MIT[:], rhs=G[:, i, :], start=True, stop=True),
                     reads=[d_g, d_m], writes=[dp_])
                S.op("pe", lambda e, p_=p_, i=i: e.matmul(p_[:, 8:16], lhsT=CH0[:], rhs=G[:, i, :], start=True, stop=True),
                     reads=[d_g, d_m], writes=[dp_])
                S.op("pe", lambda e, p_=p_, i=i: e.matmul(p_[:, 16:24], lhsT=CH1[:], rhs=G[:, i, :], start=True, stop=True),
                     reads=[d_g, d_m], writes=[dp_])
                S.op("act", lambda e, p_=p_, i=i: e.activation(out=GC[:, i, :], in_=p_[:, 0:8], func=AF.Copy), reads=[dp_], writes=[d_g])
                S.op("dve", lambda e, p_=p_, i=i: e.tensor_copy(out=GL[:, i, :, :], in_=p_[:, 8:24].rearrange("p (c h) -> p c h", c=2)),
                     reads=[dp_], writes=[d_g])
            EG = self.sb(ph, "EG", [128, NT, 8], F32)
            BEG = self.sb(ph, "BEG", [128, NT, 8], F32)
            KD = self.sb(ph, "KD", [128, NT, 8], F32)
            EGL = self.sb(ph, "EGL", [128, NT, 2, 8], F32)
            S.op("act", lambda e: e.activation(out=EG[:], in_=GC[:], func=AF.Exp), reads=[d_g], writes=[d_g])
            S.op("dve", lambda e: e.tensor_tensor(out=BEG[:], in0=EG[:], in1=BETA[:], op=ALU.mult), reads=[d_g], writes=[d_g])
            S.op("dve", lambda e: e.tensor_tensor(out=KD[0:64], in0=GL[0:64, :, 0, :], in1=GC[0:64], op=ALU.subtract), reads=[d_g], writes=[d_g])
            S.op("dve", lambda e: e.tensor_tensor(out=KD[64:128], in0=GL[64:128, :, 1, :], in1=GC[64:128], op=ALU.subtract), reads=[d_g], writes=[d_g])
            S.op("act", lambda e: e.activation(out=KD[:], in_=KD[:], func=AF.Exp), reads=[d_g], writes=[d_g])
            S.op("act", lambda e: e.activation(out=EGL[:], in_=GL[:], func=AF.Exp), reads=[d_g], writes=[d_g])

            stop = self.dn_stop
            qn = self.sb(ph, "qn", [128, T], F32)
            kn = self.sb(ph, "kn", [128, T], F32)
            vT = [self.sb(ph, f"vT{i}", [128, T], F32) for i in range(2)]
            zT = [self.sb(ph, f"zT{i}", [128, T], F32) for i in range(2)]
            X = self.sb(ph, "convX", [128, T + 3], F32)
            d_X = S.dep()
            d_q, d_k = S.dep(), S.dep()
            d_v = S.deps(2)
            d_z = S.deps(2)
            sq = self.sb(ph, "sqt", [128, 512], F32)
            rn = self.sb(ph, "rnt", [128, 512], F32)
            d_sq, d_rn = S.dep(), S.dep()
            NS = 2
            def mk(name):
                return [self.sb(ph, f"{name}{i}", [128, 128], F32) for i in range(NS)], S.deps(NS)
            ktok, d_ktok = mk("ktok")
            KKs, d_KKs = mk("KKs")
            QKs, d_QKs = mk("QKs")
            vtok, d_vtok = mk("vtok")
            diag, d_diag = mk("diag")
            Dm, d_Dm = mk("Dm")
            DTm, d_DTm = mk("DTm")
            EGR, d_EGR = mk("EGR")
            Lm, d_Lm = mk("Lm")
            Nm, d_Nm = mk("Nm")
            qkT, d_qkT = mk("qkT")
            AL, d_AL = mk("AL")
            AN, d_AN = mk("AN")
            PT, d_PT = mk("PT")
            PT2, d_PT2 = mk("PT2")
            vb, d_vb = mk("vb")
            kbe, d_kbe = mk("kbe")
            kdec, d_kdec = mk("kdec")
            uu, d_uu = mk("uu")
            wT, d_wT = mk("wT")
            qdT, d_qdT = mk("qdT")
            vnew, d_vnew = mk("vnew")
            otok, d_otok = mk("otok")
            ojunk, d_ojunk = mk("ojunk")
            ost = [self.sb(ph, f"ost{i}", [128, 4], F32) for i in range(NS)]
            d_ost = S.deps(NS)
            Sst = [[self.sb(ph, f"Sst{hh}_{i}", [128, 128], F32) for i in range(2)] for hh in range(2)]
            d_Sst = [S.deps(2) for _ in range(2)]

            def conv_load(dst, d_dst, row0, c16, l2=None):
                S.dma("sp", X[:], self.P_d[row0:row0 + 128, self.c0 - 3:self.c0 + T], reads=[self.d_P], writes=[d_X])
                S.op("dve", lambda e: e.tensor_scalar_mul(out=dst[:], in0=X[:, 0:T], scalar1=cwt[:, c16, 0:1]),
                     reads=[d_X, d_par], writes=[d_dst])
                for j in range(1, CONV_K):
                    S.op("dve", lambda e, j=j: e.scalar_tensor_tensor(out=dst[:], in0=X[:, j:j + T], scalar=cwt[:, c16, j:j + 1],
                                                                        in1=dst[:], op0=ALU.mult, op1=ALU.add),
                         reads=[d_X, d_par, d_dst], writes=[d_dst])
                if self.dn_sub >= 1:
                    S.op("act", lambda e: e.activation(out=dst[:], in_=dst[:], func=AF.Silu), reads=[d_dst], writes=[d_dst])
                if l2 is not None and self.dn_sub >= 2:
                    for tt in range(T // 512):
                        cs = slice(tt * 512, (tt + 1) * 512)
                        pb = 2 + tt % 2
                        S.op("act", lambda e, cs=cs: e.activation(out=sq[:], in_=dst[:, cs], func=AF.Square), reads=[d_dst], writes=[d_sq])
                        S.op("pe", lambda e, pb=pb: e.matmul(self.ps[pb][:], lhsT=self.ones[:], rhs=sq[:], start=True, stop=True),
                             reads=[d_sq, self.d_const], writes=[d_slot[pb * 4 + q] for q in range(4)])
                        if False:
                            S.op("dve", lambda e, pb=pb: e.tensor_scalar(out=rn[:], in0=self.ps[pb][:], scalar1=EPS, scalar2=-0.5, op0=ALU.add, op1=ALU.pow),
                                 reads=[d_slot[pb * 4 + q] for q in range(4)], writes=[d_rn])
                        else:
                            S.op("dve", lambda e, pb=pb: e.tensor_scalar_add(out=rn[:], in0=self.ps[pb][:], scalar1=EPS),
                                 reads=[d_slot[pb * 4 + q] for q in range(4)], writes=[d_rn])
                            S.op("act", lambda e: e.activation(out=rn[:], in_=rn[:], func=AF.Sqrt), reads=[d_rn], writes=[d_rn])
                            S.op("dve", lambda e: e.reciprocal(out=rn[:], in_=rn[:]), reads=[d_rn], writes=[d_rn])
                        S.op("dve", lambda e, cs=cs: e.scalar_tensor_tensor(out=dst[:, cs], in0=dst[:, cs], scalar=float(l2), in1=rn[:],
                                                                             op0=ALU.mult, op1=ALU.mult), reads=[d_rn, d_dst], writes=[d_dst])

            for kh in range((4 if not self.dn_fast else 1) if stop >= 2 else 0):
                conv_load(qn, d_q, O_BQ + 128 * kh, kh, l2=DKS)
                conv_load(kn, d_k, O_BK + 128 * kh, 4 + kh, l2=1.0)
                for hh in range(2):
                    h = 2 * kh + hh
                    conv_load(vT[hh], d_v[hh], O_BV + 128 * h, 8 + h)
                    S.dma("sp", zT[hh][:], self.P_d[O_BZ + 128 * h:O_BZ + 128 * (h + 1), self.c0:self.c0 + T], reads=[self.d_P], writes=[d_z[hh]])
                    S.op("act", lambda e, hh=hh: e.activation(out=zT[hh][:], in_=zT[hh][:], func=AF.Silu), reads=[d_z[hh]], writes=[d_z[hh]])
                    if self.seg_first:
                        S.op("pool", lambda e, hh=hh: e.memset(Sst[hh][0][:], 0.0), writes=[d_Sst[hh][0]])
                    else:
                        S.op("pool", lambda e, hh=hh, h=h: e.tensor_copy(out=Sst[hh][0][:], in_=self.SCARRY[:, h, :]),
                             reads=[self.d_carry], writes=[d_Sst[hh][0]])
                cur = [0, 0]
                for i in range((NT if not self.dn_fast else 2) if stop >= 3 else 0):
                    ts_ = slice(i * 128, (i + 1) * 128)
                    kb = i % NS
                    DV = 7
                    p_, dp_ = tslot()
                    if DV & 1:
                        S.op("pe", lambda e, p_=p_, ts_=ts_: e.transpose(p_, kn[:, ts_], self.ident[:]), reads=[d_k, self.d_const], writes=[dp_])
                        S.op("act", lambda e, p_=p_, kb=kb: e.activation(out=ktok[kb][:], in_=p_, func=AF.Copy), reads=[dp_], writes=[d_ktok[kb]])
                    pKK_, dKK_ = slot()
                    S.op("pe", lambda e, pKK_=pKK_, ts_=ts_: e.matmul(pKK_, lhsT=kn[:, ts_], rhs=kn[:, ts_], start=True, stop=True),
                         reads=[d_k], writes=[dKK_])
                    S.op("act", lambda e, pKK_=pKK_, kb=kb: e.activation(out=KKs[kb][:], in_=pKK_, func=AF.Copy), reads=[dKK_], writes=[d_KKs[kb]])
                    pQK_, dQK_ = slot()
                    S.op("pe", lambda e, pQK_=pQK_, ts_=ts_: e.matmul(pQK_, lhsT=kn[:, ts_], rhs=qn[:, ts_], start=True, stop=True),
                         reads=[d_k, d_q], writes=[dQK_])
                    S.op("dve", lambda e, pQK_=pQK_, kb=kb: e.tensor_copy(out=QKs[kb][:], in_=pQK_), reads=[dQK_], writes=[d_QKs[kb]])
                    pKK, dKK, pQK, dQK = KKs[kb][:], d_KKs[kb], QKs[kb][:], d_QKs[kb]
                    for hh in range(2 if self.dn_sub >= 11 else 0):
                        h = 2 * kh + hh
                        b = (i * 2 + hh) % NS
                        gcol = GC[:, i, h:h + 1]
                        S.op("dve", lambda e, b=b, gcol=gcol: e.tensor_scalar_mul(out=diag[b][:], in0=self.ident[:], scalar1=gcol),
                             reads=[d_g, self.d_const], writes=[d_diag[b]])
                        pG, dG = slot()
                        S.op("pe", lambda e, pG=pG, b=b: e.matmul(pG, lhsT=self.ones[:], rhs=diag[b][:], start=True, stop=True),
                             reads=[d_diag[b], self.d_const], writes=[dG])
                        S.op("dve", lambda e, pG=pG, b=b, gcol=gcol: e.tensor_scalar(out=Dm[b][:], in0=pG, scalar1=gcol, scalar2=0.0,
                                                                                      op0=ALU.subtract, op1=ALU.max),
                             reads=[dG, d_g], writes=[d_Dm[b]])
                        S.op("act", lambda e, b=b: e.activation(out=Dm[b][:], in_=Dm[b][:], func=AF.Exp, scale=-1.0), reads=[d_Dm[b]], writes=[d_Dm[b]])
                        S.op("pool", lambda e, b=b: e.tensor_tensor(out=Dm[b][:], in0=Dm[b][:], in1=ML[:], op=ALU.mult), reads=[d_Dm[b], d_m], writes=[d_Dm[b]])
                        if self.dn_sub < 12:
                            continue
                        S.op("dve", lambda e, pG=pG, b=b, gcol=gcol: e.tensor_scalar(out=DTm[b][:], in0=pG, scalar1=gcol, scalar2=0.0,
                                                                                      op0=ALU.subtract, op1=ALU.min),
                             reads=[dG, d_g], writes=[d_DTm[b]])
                        S.op("act", lambda e, b=b: e.activation(out=DTm[b][:], in_=DTm[b][:], func=AF.Exp), reads=[d_DTm[b]], writes=[d_DTm[b]])
                        S.op("pool", lambda e, b=b: e.tensor_tensor(out=DTm[b][:], in0=DTm[b][:], in1=MIT[:], op=ALU.mult), reads=[d_DTm[b], d_m], writes=[d_DTm[b]])
                        S.op("act", lambda e, pG=pG, b=b: e.activation(out=EGR[b][:], in_=pG, func=AF.Exp), reads=[dG], writes=[d_EGR[b]])
                        if self.dn_sub < 13:
                            continue
                        S.op("dve", lambda e, b=b, i=i, h=h: e.scalar_tensor_tensor(out=Lm[b][:], in0=pKK, scalar=BETA[:, i, h:h + 1], in1=Dm[b][:],
                                                                                     op0=ALU.mult, op1=ALU.mult),
                             reads=[dKK, d_g, d_Dm[b]], writes=[d_Lm[b]])
                        S.op("dve", lambda e, b=b: e.tensor_tensor(out=qkT[b][:], in0=pQK, in1=DTm[b][:], op=ALU.mult),
                             reads=[dQK, d_DTm[b]], writes=[d_qkT[b]])
                        pN, dN = tslot()
                        S.op("pe", lambda e, pN=pN, b=b: e.transpose(pN, Lm[b][:], self.ident[:]), reads=[d_Lm[b], self.d_const], writes=[dN])
                        S.op("act", lambda e, pN=pN, b=b: e.activation(out=Nm[b][:], in_=pN, func=AF.Copy), reads=[dN], writes=[d_Nm[b]])
                        if self.dn_sub < 14:
                            continue
                        S.op("dve", lambda e, b=b: e.tensor_tensor(out=PT[b][:], in0=self.ident[:], in1=Nm[b][:], op=ALU.subtract),
                             reads=[d_Nm[b], self.d_const], writes=[d_PT[b]])
                        cl, dcl, cn, dcn = Lm[b], d_Lm[b], Nm[b], d_Nm[b]
                        cp, dcp, np_, dnp = PT[b], d_PT[b], PT2[b], d_PT2[b]
                        for sstep in range(1, 6):
                            pL2, dL2 = slot()
                            S.op("pe", lambda e, pL2=pL2, cl=cl, cn=cn: e.matmul(pL2, lhsT=cn[:], rhs=cl[:], start=True, stop=True),
                                 reads=[dcl, dcn], writes=[dL2])
                            if sstep < 5:
                                pN2, dN2 = slot()
                                S.op("pe", lambda e, pN2=pN2, cl=cl, cn=cn: e.matmul(pN2, lhsT=cl[:], rhs=cn[:], start=True, stop=True),
                                     reads=[dcl, dcn], writes=[dN2])
                            if sstep % 2 == 1:
                                nl, dnl, nn, dnn = AL[b], d_AL[b], AN[b], d_AN[b]
                            else:
                                nl, dnl, nn, dnn = Lm[b], d_Lm[b], Nm[b], d_Nm[b]
                            S.op("act", lambda e, pL2=pL2, nl=nl: e.activation(out=nl[:], in_=pL2, func=AF.Copy), reads=[dL2], writes=[dnl])
                            if sstep < 5:
                                S.op("dve", lambda e, pN2=pN2, nn=nn: e.tensor_copy(out=nn[:], in_=pN2), reads=[dN2], writes=[dnn])
                            DW = 7
                            pU, dU = slot()
                            if DW & 2:
                                S.op("pe", lambda e, pU=pU, nl=nl, cp=cp: e.matmul(pU, lhsT=nl[:], rhs=cp[:], start=True, stop=True),
                                     reads=[dnl, dcp], writes=[dU])
                            if DW & 4:
                                S.op("dve", lambda e, pU=pU, cp=cp, np_=np_: e.tensor_tensor(out=np_[:], in0=pU, in1=cp[:], op=ALU.add),
                                     reads=[dU, dcp], writes=[dnp])
                            cl, dcl, cn, dcn = nl, dnl, nn, dnn
                            cp, dcp, np_, dnp = np_, dnp, cp, dcp
                        TT, dTT = cp, dcp
                        if stop < 4:
                            continue
                        pV, dV = tslot()
                        S.op("pe", lambda e, pV=pV, hh=hh, ts_=ts_: e.transpose(pV, vT[hh][:, ts_], self.ident[:]),
                             reads=[d_v[hh], self.d_const], writes=[dV])
                        S.op("dve", lambda e, pV=pV, b=b, i=i, h=h: e.tensor_scalar_mul(out=vb[b][:], in0=pV, scalar1=BETA[:, i, h:h + 1]),
                             reads=[dV, d_g], writes=[d_vb[b]])
                        S.op("pool", lambda e, b=b, kb=kb, i=i, h=h: e.tensor_scalar_mul(out=kbe[b][:], in0=ktok[kb][:], scalar1=BEG[:, i, h:h + 1]),
                             reads=[d_ktok[kb], d_g], writes=[d_kbe[b]])
                        S.op("pool", lambda e, b=b, kb=kb, i=i, h=h: e.tensor_scalar_mul(out=kdec[b][:], in0=ktok[kb][:], scalar1=KD[:, i, h:h + 1]),
                             reads=[d_ktok[kb], d_g], writes=[d_kdec[b]])
                        S.op("pool", lambda e, b=b, ts_=ts_: e.tensor_tensor(out=qdT[b][:], in0=qn[:, ts_], in1=EGR[b][:], op=ALU.mult),
                             reads=[d_q, d_EGR[b]], writes=[d_qdT[b]])
                        pu, du = slot()
                        S.op("pe", lambda e, pu=pu, TT=TT, b=b: e.matmul(pu, lhsT=TT[:], rhs=vb[b][:], start=True, stop=True),
                             reads=[dTT, d_vb[b]], writes=[du])
                        S.op("act", lambda e, pu=pu, b=b: e.activation(out=uu[b][:], in_=pu, func=AF.Copy), reads=[du], writes=[d_uu[b]])
                        pw, dw = slot()
                        S.op("pe", lambda e, pw=pw, TT=TT, b=b: e.matmul(pw, lhsT=kbe[b][:], rhs=TT[:], start=True, stop=True),
                             reads=[dTT, d_kbe[b]], writes=[dw])
                        S.op("act", lambda e, pw=pw, b=b: e.activation(out=wT[b][:], in_=pw, func=AF.Copy), reads=[dw], writes=[d_wT[b]])
                        for c in range(2 if stop >= 5 else 0):
                            rs = slice(64 * c, 64 * c + 64)
                            Sc, dSc = Sst[hh][cur[hh]], d_Sst[hh][cur[hh]]
                            Sn, dSn = Sst[hh][1 - cur[hh]], d_Sst[hh][1 - cur[hh]]
                            p1, d1 = slot()
                            S.op("pe", lambda e, p1=p1, b=b, Sc=Sc: e.matmul(p1, lhsT=wT[b][:], rhs=Sc[:], start=True, stop=True),
                                 reads=[d_wT[b], dSc], writes=[d1])
                            S.op("dve", lambda e, p1=p1, b=b, rs=rs: e.tensor_tensor(out=vnew[b][rs, :], in0=uu[b][rs, :], in1=p1[rs, :], op=ALU.subtract),
                                 reads=[d1, d_uu[b]], writes=[d_vnew[b]])
                            p2, d2 = slot()
                            S.op("pe", lambda e, p2=p2, b=b, Sc=Sc: e.matmul(p2, lhsT=qdT[b][:], rhs=Sc[:], start=True, stop=False),
                                 reads=[d_qdT[b], dSc], writes=[d2])
                            S.op("pe", lambda e, p2=p2, b=b, rs=rs: e.matmul(p2, lhsT=qkT[b][rs, :], rhs=vnew[b][rs, :], start=False, stop=True),
                                 reads=[d_qkT[b], d_vnew[b]], writes=[d2])
                            S.op("act", lambda e, p2=p2, b=b, rs=rs: e.activation(out=otok[b][rs, :], in_=p2[rs, :], func=AF.Copy),
                                 reads=[d2], writes=[d_otok[b]])
                            p3, d3 = slot()
                            S.op("pe", lambda e, p3=p3, b=b, rs=rs: e.matmul(p3, lhsT=kdec[b][rs, :], rhs=vnew[b][rs, :], start=True, stop=True),
                                 reads=[d_kdec[b], d_vnew[b]], writes=[d3])
                            S.op("dve", lambda e, p3=p3, Sc=Sc, Sn=Sn, i=i, c=c, h=h: e.scalar_tensor_tensor(
                                out=Sn[:], in0=Sc[:], scalar=EGL[:, i, c, h:h + 1], in1=p3, op0=ALU.mult, op1=ALU.add),
                                reads=[d3, dSc, d_g], writes=[dSn])
                            cur[hh] = 1 - cur[hh]
                        S.op("act", lambda e, b=b: e.activation(out=ojunk[b][:], in_=otok[b][:], func=AF.Square, accum_out=ost[b][:, 0:1]),
                             reads=[d_otok[b]], writes=[d_ojunk[b], d_ost[b]])
                        S.op("act", lambda e, b=b: e.activation(out=ost[b][:, 1:2], in_=ost[b][:, 0:1], func=AF.Sqrt, scale=1.0 / DV_B,
                                                                 bias=self.epsc[:, 0:1]), reads=[d_ost[b], self.d_const], writes=[d_ost[b]])
                        S.op("dve", lambda e, b=b: e.reciprocal(out=ost[b][:, 2:3], in_=ost[b][:, 1:2]), reads=[d_ost[b]], writes=[d_ost[b]])
                        S.op("pool", lambda e, b=b: e.tensor_scalar_mul(out=otok[b][:], in0=otok[b][:], scalar1=ost[b][:, 2:3]),
                             reads=[d_ost[b], d_otok[b]], writes=[d_otok[b]])
                        pO, dO = tslot()
                        S.op("pe", lambda e, pO=pO, b=b: e.transpose(pO, otok[b][:], self.ident[:]), reads=[d_otok[b], self.d_const], writes=[dO])
                        S.op("dve", lambda e, pO=pO, hh=hh, h=h, ts_=ts_: e.scalar_tensor_tensor(
                            out=self.actT[:, 4 + h, ts_], in0=pO, scalar=ngc[:, 0:1], in1=zT[hh][:, ts_], op0=ALU.mult, op1=ALU.mult),
                            reads=[dO, d_par, d_z[hh]], writes=[self.d_act[i]])
                for hh in range(2):
                    h = 2 * kh + hh
                    S.op("pool", lambda e, hh=hh, h=h, cc=cur[hh]: e.tensor_copy(out=self.SCARRY[:, h, :], in_=Sst[hh][cc][:]),
                         reads=[d_Sst[hh][cur[hh]]], writes=[self.d_carry])
        S.barrier()

    def phase_outproj(self, l):
        S, I = self.S, self.I
        T, NT = self.T, self.NT
        wv = I["w_out"][l].rearrange("(kc p) n -> p kc n", p=128)
        with ExitStack() as ph:
            NB = 2
            W = [self.sb(ph, f"opW{i}", [128, KC, 512], BF16) for i in range(NB)]
            dW = S.deps(NB)
            Gp = [self.sb(ph, f"opG{i}", [128, 512], F32) for i in range(NB)]
            xt = [self.sb(ph, f"opx{i}", [128, 512], F32) for i in range(NB)]
            dx = S.deps(NB)
            tt_ = [self.sb(ph, f"opt{i}", [128, 512], F32) for i in range(NB)]
            dt_ = S.deps(NB)
            cnt = 0
            for cg in range(4):
                cs = slice(cg * 512, (cg + 1) * 512)
                wb = cg % NB
                S.dma("pool", W[wb][:], wv[:, :, cs], writes=[dW[wb]])
                S.dma("sp", Gp[wb][:], self.mod_d[self.bi, l, 2, :, cs], reads=[self.d_mod], writes=[dW[wb]])
                for i in range(NT):
                    b = cnt % NB
                    pb = cnt % 8
                    cnt += 1
                    rows = slice(self.r0 + i * 128, self.r0 + (i + 1) * 128)
                    for kc in range(KC):
                        S.op("pe", lambda e, kc=kc, i=i, wb=wb, pb=pb: e.matmul(self.ps[pb][:], lhsT=self.actT[:, kc, i * 128:(i + 1) * 128],
                                                                              rhs=W[wb][:, kc, :], start=(kc == 0), stop=(kc == KC - 1)),
                             reads=[dW[wb], self.d_act[i]], writes=[self.d_ps[pb]])
                    S.dma("sp", xt[b][:], self.x_d[rows, cs], reads=[self.d_xt[i]], writes=[dx[b]])
                    S.op("dve", lambda e, b=b, wb=wb, pb=pb: e.tensor_tensor(out=tt_[b][:], in0=self.ps[pb][:], in1=Gp[wb][:], op=ALU.mult),
                         reads=[self.d_ps[pb], dW[wb]], writes=[dt_[b]])
                    S.op("pool", lambda e, b=b: e.tensor_tensor(out=xt[b][:], in0=xt[b][:], in1=tt_[b][:], op=ALU.add),
                         reads=[dt_[b], dx[b]], writes=[dx[b]])
                    S.dma("sp", self.x_d[rows, cs], xt[b][:], reads=[dx[b]], writes=[self.d_xt[i]])
        S.barrier()

    def phase_moe(self, l):
        S, I, nc = self.S, self.I, self.nc
        T, NT, NBLK = self.T, self.NT, self.NBLK
        with ExitStack() as mo:
            router = {}
            RW = self.sb(mo, "RW", [128, KC, 36], F32)
            RB = self.sb(mo, "RB", [128, 36], F32)
            LG = self.sb(mo, "LG", [128, NT, 36], F32)
            router["RW"], router["RB"], router["LG"] = RW, RB, LG
            router["d_rw"], router["d_lg"] = S.dep(), S.dep()
            router["hbf"] = [self.sb(mo, f"hbf{i}", [128, D], BF16) for i in range(2)]
            router["d_hbf"] = S.deps(2)
            S.dma("sp", RW[:], I["rw"][l].rearrange("(kc p) n -> p kc n", p=128), writes=[router["d_rw"]])
            S.dma("sp", RB[:], I["rb"][l:l + 1, :].partition_broadcast(128), writes=[router["d_rw"]])
            self.phase_norm(l, 2, router=router)
            d_r = router["d_lg"]
            cnt = {"n": 0}

            def small(name, shape, dt=F32):
                return self.sb(mo, name, shape, dt)

            gmax = small("gmax", [128, NT])
            goh = small("goh", [128, NT, 4])
            gex = small("gex", [128, NT, 4])
            pg = small("pg", [128, NT])
            esel = small("esel", [128, NT, 8])
            etmp = small("etmp", [128, NT, 8])
            v1 = small("v1", [128, NT])
            v2 = small("v2", [128, NT])
            oh1 = small("oh1", [128, NT, 8])
            oh2 = small("oh2", [128, NT, 8])
            g0 = small("g0", [128, NT])
            g1 = small("g1", [128, NT])
            OH1 = small("OH1", [128, NT, 32])
            OH2 = small("OH2", [128, NT, 32])
            OH = small("OH", [128, NT, 32])
            glog = LG[:, :, 0:4]
            elog4 = LG[:, :, 4:36].rearrange("p n (g j) -> p n g j", g=4)

            def dv(fn, eng="dve"):
                S.op(eng, fn, reads=[d_r, self.d_const], writes=[d_r])

            dv(lambda e: e.tensor_reduce(out=gmax[:], in_=glog, axis=AX.X, op=ALU.max))
            dv(lambda e: e.tensor_tensor(out=goh[:], in0=glog, in1=gmax[:].unsqueeze(2).broadcast_to([128, NT, 4]), op=ALU.is_equal))
            dv(lambda e: e.tensor_tensor(out=gex[:], in0=glog, in1=gmax[:].unsqueeze(2).broadcast_to([128, NT, 4]), op=ALU.subtract))
            dv(lambda e: e.activation(out=gex[:], in_=gex[:], func=AF.Exp), "act")
            dv(lambda e: e.tensor_reduce(out=pg[:], in_=gex[:], axis=AX.X, op=ALU.add))
            dv(lambda e: e.reciprocal(out=pg[:], in_=pg[:]))
            for g in range(4):
                if g == 0:
                    dv(lambda e: e.tensor_tensor(out=esel[:], in0=elog4[:, :, 0, :], in1=goh[:, :, 0:1].broadcast_to([128, NT, 8]), op=ALU.mult))
                else:
                    dv(lambda e, g=g: e.tensor_tensor(out=etmp[:], in0=elog4[:, :, g, :], in1=goh[:, :, g:g + 1].broadcast_to([128, NT, 8]), op=ALU.mult))
                    dv(lambda e: e.tensor_tensor(out=esel[:], in0=esel[:], in1=etmp[:], op=ALU.add))
            dv(lambda e: e.tensor_reduce(out=v1[:], in_=esel[:], axis=AX.X, op=ALU.max))
            dv(lambda e: e.tensor_tensor(out=oh1[:], in0=esel[:], in1=v1[:].unsqueeze(2).broadcast_to([128, NT, 8]), op=ALU.is_equal))
            dv(lambda e: e.scalar_tensor_tensor(out=etmp[:], in0=oh1[:], scalar=NEG, in1=esel[:], op0=ALU.mult, op1=ALU.add))
            dv(lambda e: e.tensor_reduce(out=v2[:], in_=etmp[:], axis=AX.X, op=ALU.max))
            dv(lambda e: e.tensor_tensor(out=oh2[:], in0=etmp[:], in1=v2[:].unsqueeze(2).broadcast_to([128, NT, 8]), op=ALU.is_equal))
            dv(lambda e: e.tensor_tensor(out=g1[:], in0=v2[:], in1=v1[:], op=ALU.subtract))
            dv(lambda e: e.activation(out=g1[:], in_=g1[:], func=AF.Exp), "act")
            dv(lambda e: e.tensor_scalar_add(out=g0[:], in0=g1[:], scalar1=1.0))
            dv(lambda e: e.reciprocal(out=g0[:], in_=g0[:]))
            dv(lambda e: e.tensor_tensor(out=g1[:], in0=g1[:], in1=g0[:], op=ALU.mult))
            dv(lambda e: e.tensor_tensor(out=g0[:], in0=g0[:], in1=pg[:], op=ALU.mult))
            dv(lambda e: e.tensor_tensor(out=g1[:], in0=g1[:], in1=pg[:], op=ALU.mult))
            for (OHk, ohk) in ((OH1, oh1), (OH2, oh2)):
                for g in range(4):
                    dv(lambda e, OHk=OHk, ohk=ohk, g=g: e.tensor_tensor(out=OHk[:, :, g * 8:(g + 1) * 8], in0=ohk[:],
                                                                        in1=goh[:, :, g:g + 1].broadcast_to([128, NT, 8]), op=ALU.mult))
            dv(lambda e: e.tensor_tensor(out=OH[:], in0=OH1[:], in1=OH2[:], op=ALU.add))
            Ust = small("Ust", [128, 128])
            dv(lambda e: e.memset(Ust[:], 1.0), "pool")
            dv(lambda e: e.affine_select(out=Ust[:], in_=Ust[:], pattern=[[1, 128]], compare_op=ALU.is_ge,
                                         fill=self.reg_zero, base=-1, channel_multiplier=-1), "pool")
            TRIU = small("TRIU", [32, 32])
            dv(lambda e: e.memset(TRIU[:], 1.0), "pool")
            dv(lambda e: e.affine_select(out=TRIU[:], in_=TRIU[:], pattern=[[1, 32]], compare_op=ALU.is_ge,
                                         fill=self.reg_zero, base=0, channel_multiplier=-1), "pool")
            CUM = small("CUM", [128, NT + 1, 32])
            RANK = small("RANK", [128, NT, 32])
            dv(lambda e: e.memset(CUM[:, 0, :], 0.0), "pool")
            for i in range(NT):
                pb = 4 + (i % 2)
                S.op("pe", lambda e, i=i, pb=pb: e.matmul(self.ps[pb][:, 0:32], lhsT=self.ones[:], rhs=OH[:, i, :], start=True, stop=True),
                     reads=[d_r, self.d_const], writes=[self.d_ps[pb]])
                S.op("dve", lambda e, i=i, pb=pb: e.tensor_tensor(out=CUM[:, i + 1, :], in0=CUM[:, i, :], in1=self.ps[pb][:, 0:32], op=ALU.add),
                     reads=[self.d_ps[pb], d_r], writes=[d_r])
                pb2 = 6 + (i % 2)
                S.op("pe", lambda e, i=i, pb2=pb2: e.matmul(self.ps[pb2][:, 0:32], lhsT=Ust[:], rhs=OH[:, i, :], start=True, stop=True),
                     reads=[d_r], writes=[self.d_ps[pb2]])
                S.op("dve", lambda e, i=i, pb2=pb2: e.tensor_tensor(out=RANK[:, i, :], in0=CUM[:, i, :], in1=self.ps[pb2][:, 0:32], op=ALU.add),
                     reads=[self.d_ps[pb2], d_r], writes=[d_r])
            THRi = small("THRi", [128, NT], I32)
            THR = small("THR", [128, NT])
            dv(lambda e: e.iota(THRi[:], pattern=[[128, NT]], base=0, channel_multiplier=0), "pool")
            dv(lambda e: e.tensor_copy(out=THR[:], in_=THRi[:]), "pool")
            CMP = small("CMP", [128, 32, NT])
            PADD = small("PADD", [128, 32])
            dv(lambda e: e.tensor_tensor(out=CMP[:], in0=CUM[:, NT, :].unsqueeze(2).broadcast_to([128, 32, NT]),
                                         in1=THR[:].unsqueeze(1).broadcast_to([128, 32, NT]), op=ALU.is_gt))
            dv(lambda e: e.tensor_reduce(out=PADD[:], in_=CMP[:], axis=AX.X, op=ALU.add))
            dv(lambda e: e.tensor_scalar_mul(out=PADD[:], in0=PADD[:], scalar1=128.0))
            paddT = small("paddT", [32, 128])
            S.op("pe", lambda e: e.transpose(self.ps[0][0:32, 0:128], PADD[:], self.ident[:]), reads=[d_r, self.d_const], writes=[self.d_ps[0]])
            S.op("act", lambda e: e.activation(out=paddT[:], in_=self.ps[0][0:32, 0:128], func=AF.Copy), reads=[self.d_ps[0]], writes=[d_r])
            PEND = small("PEND", [128, 32])
            S.op("pe", lambda e: e.matmul(self.ps[4][:, 0:32], lhsT=paddT[:], rhs=TRIU[:], start=True, stop=True), reads=[d_r], writes=[self.d_ps[4]])
            S.op("dve", lambda e: e.tensor_copy(out=PEND[:], in_=self.ps[4][:, 0:32]), reads=[self.d_ps[4]], writes=[d_r])
            PST = small("PST", [128, 32])
            dv(lambda e: e.tensor_tensor(out=PST[:], in0=PEND[:], in1=PADD[:], op=ALU.subtract))
            dv(lambda e: e.tensor_tensor(out=RANK[:], in0=RANK[:], in1=PST[:].unsqueeze(1).broadcast_to([128, NT, 32]), op=ALU.add))
            DSTf = small("DSTf", [128, 2, NT])
            DSTi = small("DSTi", [128, 2, NT], I32)
            for k, OHk in enumerate((OH1, OH2)):
                dv(lambda e, OHk=OHk: e.tensor_tensor(out=OHk[:], in0=OHk[:], in1=RANK[:], op=ALU.mult))
                dv(lambda e, OHk=OHk, k=k: e.tensor_reduce(out=DSTf[:, k, :], in_=OHk[:], axis=AX.X, op=ALU.add))
            dv(lambda e: e.tensor_copy(out=DSTi[:], in_=DSTf[:]))
            pendc = small("pendc", [32, 1])
            S.op("pe", lambda e: e.matmul(self.ps[5][0:32, 0:1], lhsT=TRIU[:], rhs=paddT[:, 0:1], start=True, stop=True), reads=[d_r], writes=[self.d_ps[5]])
            S.op("dve", lambda e: e.tensor_copy(out=pendc[:], in_=self.ps[5][0:32, 0:1]), reads=[self.d_ps[5]], writes=[d_r])
            BVi = small("BVi", [32, NBLK], I32)
            BV = small("BV", [32, NBLK])
            dv(lambda e: e.iota(BVi[:], pattern=[[128, NBLK]], base=0, channel_multiplier=0), "pool")
            dv(lambda e: e.tensor_copy(out=BV[:], in_=BVi[:]), "pool")
            dv(lambda e: e.tensor_scalar(out=BV[:], in0=BV[:], scalar1=pendc[:, 0:1], scalar2=None, op0=ALU.is_ge))
            BEf = small("BEf", [1, NBLK])
            BEi = small("BEi", [1, NBLK], I32)
            S.op("pe", lambda e: e.matmul(self.ps[6][0:1, 0:NBLK], lhsT=self.ones[0:32, 0:1], rhs=BV[:], start=True, stop=True),
                 reads=[d_r, self.d_const], writes=[self.d_ps[6]])
            S.op("dve", lambda e: e.tensor_scalar_min(out=BEf[:], in0=self.ps[6][0:1, 0:NBLK], scalar1=float(N_EXP - 1)), reads=[self.d_ps[6]], writes=[d_r])
            dv(lambda e: e.tensor_copy(out=BEi[:], in_=BEf[:]))
            BEbc = small("BEbc", [128, NBLK])
            S.op("pe", lambda e: e.matmul(self.ps[7][:, 0:NBLK], lhsT=self.ones[0:32, :], rhs=BV[:], start=True, stop=True),
                 reads=[d_r, self.d_const], writes=[self.d_ps[7]])
            S.op("dve", lambda e: e.tensor_scalar_min(out=BEbc[:], in0=self.ps[7][:, 0:NBLK], scalar1=float(N_EXP - 1)), reads=[self.d_ps[7]], writes=[d_r])
            PKi = small("PKi", [128, 1], I32)
            PK = small("PK", [128, 1])
            dv(lambda e: e.iota(PKi[:], pattern=[[0, 1]], base=0, channel_multiplier=1), "pool")
            dv(lambda e: e.tensor_copy(out=PK[:], in_=PKi[:]), "pool")
            WIDXf = small("WIDXf", [128, NBLK])
            WIDX = small("WIDX", [128, NBLK], I32)
            dv(lambda e: e.tensor_scalar_add(out=BEbc[:], in0=BEbc[:], scalar1=float(l * N_EXP)))
            dv(lambda e: e.tensor_scalar(out=WIDXf[:], in0=BEbc[:], scalar1=128.0, scalar2=PK[:, 0:1], op0=ALU.mult, op1=ALU.add))
            dv(lambda e: e.tensor_copy(out=WIDX[:], in_=WIDXf[:]))
            wg_rows = I["w_gate"].rearrange("l e (p j) n -> (l e p) (j n)", j=KC)
            wu_rows = I["w_up"].rearrange("l e (p j) n -> (l e p) (j n)", j=KC)
            wd_rows = I["w_down"].rearrange("l e (p j) n -> (l e p) (j n)", j=4)
            if "dbg_route" in self.phases:
                t1_ = self.dram("dbg_dst", [128, 2, NT], I32, kind="ExternalOutput").ap()
                t2_ = self.dram("dbg_be", [1, NBLK], I32, kind="ExternalOutput").ap()
                t3_ = self.dram("dbg_g", [128, 2, NT], F32, kind="ExternalOutput").ap()
                self.out_events.append(S.dma("sp", t1_, DSTi[:], reads=[d_r]))
                self.out_events.append(S.dma("sp", t2_, BEi[:], reads=[d_r]))
                gg_ = small("gg_", [128, 2, NT])
                dv(lambda e: e.tensor_copy(out=gg_[:, 0, :], in_=g0[:]))
                dv(lambda e: e.tensor_copy(out=gg_[:, 1, :], in_=g1[:]))
                self.out_events.append(S.dma("sp", t3_, gg_[:], reads=[d_r]))
            S.barrier()
            with ExitStack() as ph:
                hrow = [self.sb(ph, f"hrow{i}", [128, D], BF16) for i in range(2)]
                dhr = S.deps(2)
                for i in range(NT):
                    b = i % 2
                    S.dma("sp", hrow[b][:], self.h2_d[i * 128:(i + 1) * 128, :], reads=[self.d_h2], writes=[dhr[b]])
                    for k in range(2):
                        S.dma("pool", None, None, reads=[dhr[b], d_r], writes=[self.d_rows],
                              fn=lambda e, b=b, k=k, i=i: e.indirect_dma_start(
                                  out=self.rows_d[:, :], out_offset=bass.IndirectOffsetOnAxis(ap=DSTi[:, k, i:i + 1], axis=0),
                                  in_=hrow[b][:, :], in_offset=None))
            S.barrier()
            with ExitStack() as ph:
                identb = self.sb(ph, "identb2", [128, 128], BF16)
                d_ib = S.dep()
                S.op("dve", lambda e: e.tensor_copy(out=identb[:], in_=self.ident[:]), reads=[self.d_const], writes=[d_ib])
                NW = 2
                Wg = [self.sb(ph, f"Wg{i}", [128, KC, D_FF], BF16) for i in range(NW)]
                Wu = [self.sb(ph, f"Wu{i}", [128, KC, D_FF], BF16) for i in range(NW)]
                Wd = [self.sb(ph, f"Wd{i}", [128, 4, D], BF16) for i in range(NW)]
                dWt = S.deps(NW)
                rowsb = [self.sb(ph, f"rowsb{i}", [128, D], BF16) for i in range(2)]
                d_rb = S.deps(2)
                blkT = [self.sb(ph, f"blkT{i}", [128, KC, 128], BF16) for i in range(2)]
                d_bT = S.deps(2)
                sg = self.sb(ph, "sgate", [128, D_FF], F32)
                d_sg = S.dep()
                hid = self.sb(ph, "hid", [128, D_FF], BF16)
                d_hid = S.dep()
                hidT = self.sb(ph, "hidT", [128, 4, 128], BF16)
                d_hT = S.dep()
                yb = [self.sb(ph, f"yb{i}", [128, D], F32) for i in range(2)]
                d_yb = S.deps(2)
                psT = [self.ps[2].bitcast(BF16), self.ps[3].bitcast(BF16)]
                for blk in range(NBLK):
                    b = blk % 2
                    wb = blk % NW
                    S.dma("sp", rowsb[b][:], self.rows_d[blk * 128:(blk + 1) * 128, :], reads=[self.d_rows], writes=[d_rb[b]])
                    for (dst, srcv) in ((Wg[wb], wg_rows), (Wu[wb], wu_rows), (Wd[wb], wd_rows)):
                        S.dma("pool", None, None, reads=[d_r], writes=[dWt[wb]],
                              fn=lambda e, dst=dst, srcv=srcv, blk=blk: e.indirect_dma_start(
                                  out=dst[:].rearrange("p j n -> p (j n)"), out_offset=None, in_=srcv,
                                  in_offset=bass.IndirectOffsetOnAxis(ap=WIDX[:, blk:blk + 1], axis=0)))
                    for q4 in range(4):
                        pt = psT[q4 % 2]
                        dpt = self.d_ps[2 + q4 % 2]
                        for j in range(4):
                            kc = q4 * 4 + j
                            S.op("pe", lambda e, pt=pt, b=b, kc=kc, j=j: e.transpose(pt[:, j * 128:(j + 1) * 128], rowsb[b][:, sl(kc, 128, KC)], identb[:]),
                                 reads=[d_rb[b], d_ib], writes=[dpt])
                        if q4 % 2 == 0:
                            S.op("act", lambda e, pt=pt, b=b, q4=q4: e.activation(out=blkT[b][:, q4 * 4:(q4 + 1) * 4, :],
                                                                                   in_=pt[:, 0:512].rearrange("p (j n) -> p j n", j=4), func=AF.Copy),
                                 reads=[dpt], writes=[d_bT[b]])
                        else:
                            S.op("dve", lambda e, pt=pt, b=b, q4=q4: e.tensor_copy(out=blkT[b][:, q4 * 4:(q4 + 1) * 4, :],
                                                                                    in_=pt[:, 0:512].rearrange("p (j n) -> p j n", j=4)),
                                 reads=[dpt], writes=[d_bT[b]])
                    for (pb, Wt) in ((0, Wg[wb]), (1, Wu[wb])):
                        for kc in range(KC):
                            S.op("pe", lambda e, pb=pb, Wt=Wt, kc=kc, b=b: e.matmul(self.ps[pb][:], lhsT=blkT[b][:, kc, :], rhs=Wt[:, kc, :],
                                                                                     start=(kc == 0), stop=(kc == KC - 1)),
                                 reads=[d_bT[b], dWt[wb]], writes=[self.d_ps[pb]])
                    S.op("act", lambda e: e.activation(out=sg[:], in_=self.ps[0][:], func=AF.Silu), reads=[self.d_ps[0]], writes=[d_sg])
                    S.op("dve", lambda e: e.tensor_tensor(out=hid[:], in0=sg[:], in1=self.ps[1][:], op=ALU.mult),
                         reads=[d_sg, self.d_ps[1]], writes=[d_hid])
                    pt, dpt = psT[0], self.d_ps[2]
                    for f in range(4):
                        S.op("pe", lambda e, f=f, pt=pt: e.transpose(pt[:, f * 128:(f + 1) * 128], hid[:, sl(f, 128, 4)], identb[:]),
                             reads=[d_hid, d_ib], writes=[dpt])
                    S.op("act", lambda e, pt=pt: e.activation(out=hidT[:], in_=pt[:, 0:512].rearrange("p (j n) -> p j n", j=4), func=AF.Copy),
                         reads=[dpt], writes=[d_hT])
                    for cgp in range(4):
                        pb = 4 + cgp
                        for f in range(4):
                            S.op("pe", lambda e, pb=pb, f=f, cgp=cgp, wb=wb: e.matmul(self.ps[pb][:], lhsT=hidT[:, f, :],
                                                                                       rhs=Wd[wb][:, f, cgp * 512:(cgp + 1) * 512],
                                                                                       start=(f == 0), stop=(f == 3)),
                                 reads=[d_hT, dWt[wb]], writes=[self.d_ps[pb]])
                        if cgp % 2 == 0:
                            S.op("act", lambda e, pb=pb, cgp=cgp, b=b: e.activation(out=yb[b][:, cgp * 512:(cgp + 1) * 512], in_=self.ps[pb][:], func=AF.Copy),
                                 reads=[self.d_ps[pb]], writes=[d_yb[b]])
                        else:
                            S.op("dve", lambda e, pb=pb, cgp=cgp, b=b: e.tensor_copy(out=yb[b][:, cgp * 512:(cgp + 1) * 512], in_=self.ps[pb][:]),
                                 reads=[self.d_ps[pb]], writes=[d_yb[b]])
                    S.dma("sp", self.yrows_d[blk * 128:(blk + 1) * 128, :], yb[b][:], reads=[d_yb[b]], writes=[self.d_yrows])
            S.barrier()
            with ExitStack() as ph:
                G2 = self.sb(ph, "G2bc", [128, D], F32)
                d_G2 = S.dep()
                S.dma("sp", G2[:], self.mod_d[self.bi, l, 5], reads=[self.d_mod], writes=[d_G2])
                y0 = [self.sb(ph, f"y0_{i}", [128, D], F32) for i in range(2)]
                y1 = [self.sb(ph, f"y1_{i}", [128, D], F32) for i in range(2)]
                xt = [self.sb(ph, f"cx{i}", [128, D], F32) for i in range(2)]
                d_y0, d_y1, d_cx = S.deps(2), S.deps(2), S.deps(2)
                for i in range(NT):
                    b = i % 2
                    rows = slice(self.r0 + i * 128, self.r0 + (i + 1) * 128)
                    for (yt, dy, k) in ((y0[b], d_y0[b], 0), (y1[b], d_y1[b], 1)):
                        S.dma("pool", None, None, reads=[self.d_yrows, d_r], writes=[dy],
                              fn=lambda e, yt=yt, k=k, i=i: e.indirect_dma_start(
                                  out=yt[:, :], out_offset=None, in_=self.yrows_d[:, :],
                                  in_offset=bass.IndirectOffsetOnAxis(ap=DSTi[:, k, i:i + 1], axis=0)))
                    S.dma("sp", xt[b][:], self.x_d[rows, :], reads=[self.d_xt[i]], writes=[d_cx[b]])
                    S.op("dve", lambda e, b=b, i=i: e.tensor_scalar_mul(out=y0[b][:], in0=y0[b][:], scalar1=g0[:, i:i + 1]),
                         reads=[d_y0[b], d_r], writes=[d_y0[b]])
                    S.op("dve", lambda e, b=b, i=i: e.scalar_tensor_tensor(out=y0[b][:], in0=y1[b][:], scalar=g1[:, i:i + 1], in1=y0[b][:],
                                                                           op0=ALU.mult, op1=ALU.add),
                         reads=[d_y0[b], d_y1[b], d_r], writes=[d_y0[b]])
                    S.op("pool", lambda e, b=b: e.tensor_tensor(out=y0[b][:], in0=y0[b][:], in1=G2[:], op=ALU.mult),
                         reads=[d_y0[b], d_G2], writes=[d_y0[b]])
                    S.op("pool", lambda e, b=b: e.tensor_tensor(out=xt[b][:], in0=xt[b][:], in1=y0[b][:], op=ALU.add),
                         reads=[d_y0[b], d_cx[b]], writes=[d_cx[b]])
                    S.dma("sp", self.x_d[rows, :], xt[b][:], reads=[d_cx[b]], writes=[self.d_xt[i]])
        S.barrier()

    def dump_act(self):
        S = self.S
        t = self.dram("act_dump", [128, KC, self.T], BF16, kind="ExternalOutput").ap()
        self.out_events.append(S.dma("sp", t, self.actT[:], reads=self.d_act))

    def build(self):
        self.declare()
        self.consts()
        self.phase_init()
        P = self.phases
        if "ada" in P:
            for b in range(self.NB):
                self.bi = b
                self.phase_ada()
        for l in range(self.L):
            for b in range(self.NB):
                for g in range(self.NSEG):
                    self.bi = b
                    self.r0 = (b * self.NSEG + g) * self.T
                    self.c0 = TP + g * self.T
                    self.seg_first = (g == 0)
                    with ExitStack() as mx:
                        self.actT = self.sb(mx, "actT", [128, KC, self.T], BF16)
                        if "norm1" in P:
                            self.phase_norm(l, 1)
                        if "proj" in P:
                            self.phase_proj(l)
                        if "attn_c" in P:
                            self.phase_attn_c(l)
                        if "attn_a" in P:
                            self.phase_attn_a(l)
                        if "dn" in P:
                            self.phase_dn(l)
                        if "dump_act" in P:
                            self.dump_act()
                        if "outproj" in P:
                            self.phase_outproj(l)
                        self.S.barrier()
                if "moe" in P:
                    T_, NT_ = self.T, self.NT
                    self.T, self.NT = self.TM, self.NTM
                    self.r0 = b * self.NSEG * T_
                    self.phase_moe(l)
                    self.T, self.NT = T_, NT_
        if "final" in P:
            for s_ in range(self.NB * self.NSEG):
                self.r0 = s_ * self.T
                self.phase_norm(0, "final")
        if "dump_x" in P:
            t = self.dram("x_dump", [self.NB * self.NSEG * self.T, D], F32, kind="ExternalOutput").ap()
            self.out_events.append(self.S.dma("sp", t, self.x_d, reads=[self.d_x] + self.d_xt))
        self.S.barrier()
        self.S.finish(self.out_events)
        self.st.close()


def host_inputs(inputs, batches, L, S_full=None):
    f = lambda a: np.ascontiguousarray(np.asarray(a, dtype=np.float32))
    m = {}
    xs = np.asarray(inputs["x"])
    m["x"] = f(np.concatenate([xs[b] for b in batches], axis=0))
    cc = np.asarray(inputs["c"])
    m["cT"] = f(np.stack([cc[b].reshape(KC, 128).T for b in batches], axis=0))
    m["norm1_g"] = f(inputs["norm1_g"][:L])
    m["norm2_g"] = f(inputs["norm2_g"][:L])
    m["ada_w"] = f(inputs["ada_w"][:L])
    m["ada_b"] = f(inputs["ada_b"][:L])
    m["w_in"] = f(inputs["w_in"][:L])
    cw = np.asarray(inputs["dn_conv_w"])[:L]
    m["conv_w"] = f(cw.transpose(2, 0, 1).reshape(16, 128, L, CONV_K).transpose(1, 2, 0, 3))
    m["a_log"] = f(np.asarray(inputs["dn_a_log"])[:L].reshape(1, L * 8))
    m["dt_bias"] = f(np.asarray(inputs["dn_dt_bias"])[:L].reshape(1, L * 8))
    m["dn_norm_g"] = f(np.asarray(inputs["dn_norm_g"])[:L].reshape(L, 128, 1))
    m["sinks"] = f(np.asarray(inputs["attn_sinks"])[:L].reshape(1, L * 8))
    m["w_out"] = f(inputs["w_out"][:L])
    m["rw"] = f(np.concatenate([np.asarray(inputs["router_group_w"])[:L], np.asarray(inputs["router_expert_w"])[:L]], axis=-1))
    m["rb"] = f(np.concatenate([np.asarray(inputs["router_group_b"])[:L], np.asarray(inputs["router_expert_b"])[:L]], axis=-1))
    if "expert_w_gate" in inputs:
        m["w_gate"] = f(inputs["expert_w_gate"][:L])
        m["w_up"] = f(inputs["expert_w_up"][:L])
        m["w_down"] = f(inputs["expert_w_down"][:L])
    m["final_g"] = f(np.asarray(inputs["final_norm_g"]).reshape(1, D))
    return m


ALL_PHASES = ("ada", "norm1", "proj", "attn_c", "attn_a", "dn", "outproj", "moe", "final")
N_CORES_USED = 4
SEG_T = 2048


def kernel(**inputs):
    x = np.asarray(inputs["x"])
    Bsz, S_full, _ = x.shape
    L = int(np.asarray(inputs["w_in"]).shape[0])
    nseg = S_full // SEG_T
    nb = Bsz // N_CORES_USED
    nc = bass.Bass("TRN2", target_bir_lowering=False)
    bld = Builder(nc, SEG_T, L, phases=ALL_PHASES, NB=nb, NSEG=nseg)
    bld.build()
    in_maps = []
    for c in range(N_CORES_USED):
        m = host_inputs(inputs, list(range(c * nb, (c + 1) * nb)), L)
        in_maps.append({k: v for k, v in m.items() if k in bld.I})
    res = run_bass_kernel_spmd(nc, in_maps, core_ids=list(range(N_CORES_USED)))
    outs = [np.asarray(res.results[c]["y_out"], dtype=np.float32).reshape(nb, S_full, D) for c in range(N_CORES_USED)]
    return np.concatenate(outs, axis=0)
```

```python
import numpy as np
import concourse.bass as bass
import concourse.mybir as mybir
from concourse.bass_utils import run_bass_kernel_spmd
from contextlib import ExitStack

F32 = mybir.dt.float32
BF16 = mybir.dt.bfloat16
I32 = mybir.dt.int32
U32 = mybir.dt.uint32
AF = mybir.ActivationFunctionType
ALU = mybir.AluOpType
AX = mybir.AxisListType
ds = bass.ds if hasattr(bass, "ds") else None

D = 2048
KC = D // 128
H_A, DH_A = 8, 64
PATTERNS = ((128, 1), (512, 4), (2048, 16))
H_K_B, H_V_B, DK_B, DV_B = 4, 8, 128, 128
CONV_K = 4
H_C, HKV_C, DH_C = 8, 2, 64
A_W = 512
BK_W = 512
BV_W = 1024
C_Q_W = 512
C_KV_W = 128
N_IN = 5392
MIX = 2048
N_EXP = 32
D_FF = 512
EPS = 1e-6
O_AQ, O_AK, O_AV = 0, 512, 1024
O_BQ, O_BK, O_BV, O_BZ = 1536, 2048, 2560, 3584
O_BB, O_BA = 4608, 4616
O_CQ, O_CK, O_CV = 4624, 5136, 5264
TP = 2048
NEG = -1.0e30

ENGS = ("pe", "act", "dve", "pool", "sp")
SAME_ENGINE_SYNC = True
NO_SELF_SYNC = ("pe",)
N_DMA_SEMS = 16
SEM_EPOCH = 30000


def sl(c0, n, step=1):
    return slice(c0, c0 + (n - 1) * step + 1, step)


class Dep:
    __slots__ = ("w", "r", "name")

    def __init__(self, name=""):
        self.w = None
        self.r = {}
        self.name = name


class Sched:
    def __init__(self, nc, stack):
        self.nc = nc
        self.stack = stack
        self.eng_obj = {"pe": nc.tensor, "act": nc.scalar, "dve": nc.vector,
                        "pool": nc.gpsimd, "sp": nc.sync}
        self.sems = {}
        self.nsem = 0
        self.ekey = {}
        self.ecount = {}
        self.allkeys = {e: [] for e in ENGS}
        for e in ENGS:
            self._new_epoch(e)
        self.waited = {e: {} for e in ENGS}
        self.dma_keys = {}
        self.dma_val = {}
        self.dma_rr = {}
        for q in ("sp", "pool"):
            ks = []
            for i in range(N_DMA_SEMS):
                k = self._newsem(f"d_{q}_{i}")
                ks.append(k)
                self.dma_val[k] = 0
            self.dma_keys[q] = ks
            self.dma_rr[q] = 0
        self.n_ins = 0

    def _newsem(self, name):
        k = self.nsem
        self.nsem += 1
        self.sems[k] = self.stack.enter_context(self.nc.semaphore(f"s{k}_{name}"))
        return k

    def _new_epoch(self, e):
        self.ekey[e] = self._newsem(f"e_{e}")
        self.ecount[e] = 0
        self.allkeys[e].append(self.ekey[e])

    def dep(self, name=""):
        return Dep(name)

    def deps(self, n, name=""):
        return [Dep(f"{name}{i}") for i in range(n)]

    def _wait(self, eng, ev):
        if ev is None:
            return
        k, v = ev
        if (not SAME_ENGINE_SYNC or eng in NO_SELF_SYNC) and k == self.ekey.get(eng):
            return
        if self.waited[eng].get(k, 0) >= v:
            return
        self.waited[eng][k] = v
        self.eng_obj[eng].wait_ge(self.sems[k], v)

    def _collect(self, eng, reads, writes):
        for d in reads:
            self._wait(eng, d.w)
        for d in writes:
            self._wait(eng, d.w)
            for k, v in d.r.items():
                self._wait(eng, (k, v))

    def _update(self, ev, reads, writes):
        k, v = ev
        for d in reads:
            if d.r.get(k, 0) < v:
                d.r[k] = v
        for d in writes:
            d.w = ev
            d.r = {}

    def op(self, eng, fn, reads=(), writes=()):
        if self.ecount[eng] >= SEM_EPOCH:
            self._new_epoch(eng)
        self._collect(eng, reads, writes)
        k = self.ekey[eng]
        self.ecount[eng] += 1
        ev = (k, self.ecount[eng])
        fn(self.eng_obj[eng]).then_inc(self.sems[k], 1)
        self._update(ev, reads, writes)
        self.n_ins += 1
        return ev

    def dma(self, q, out, in_, reads=(), writes=(), fn=None, **kw):
        ks = self.dma_keys[q]
        k = ks[self.dma_rr[q] % len(ks)]
        self.dma_rr[q] += 1
        if self.dma_val[k] > 0:
            self._wait(q, (k, self.dma_val[k]))
        self._collect(q, reads, writes)
        self.dma_val[k] += 16
        ev = (k, self.dma_val[k])
        if fn is not None:
            ins = fn(self.eng_obj[q])
        else:
            ins = self.eng_obj[q].dma_start(out=out, in_=in_, **kw)
        ins.then_inc(self.sems[k], 16)
        self._update(ev, reads, writes)
        self.n_ins += 1
        return ev

    def barrier(self):
        evs = []
        for e in ENGS:
            if self.ecount[e] > 0:
                evs.append((self.ekey[e], self.ecount[e]))
        for k, v in self.dma_val.items():
            if v > 0:
                evs.append((k, v))
        for e in ENGS:
            for ev in evs:
                if ev[0] == self.ekey[e]:
                    continue
                self._wait(e, ev)

    def finish(self, final_events=()):
        for ev in final_events:
            self._wait("sp", ev)


class Builder:
    def __init__(self, nc, T, L, n_cores=1, dbg=(), phases=("ada", "norm1", "proj"), NB=1, NSEG=1):
        self.nc = nc
        self.T = T
        self.L = L
        self.NB = NB
        self.NSEG = NSEG
        self.c0 = TP
        self.r0 = 0
        self.bi = 0
        self.seg_first = True
        self.NT = T // 128
        self.n_cores = n_cores
        self.dbg = set(dbg)
        self.phases = phases
        self.st = ExitStack()
        self.S = Sched(nc, self.st)
        self.out_events = []
        self.dn_stop = 99
        self.dn_sub = 99
        self.dn_fast = False

    def sb(self, stack, name, shape, dt):
        self._uid = getattr(self, "_uid", 0) + 1
        return stack.enter_context(self.nc.sbuf_tensor(f"{name}_u{self._uid}", list(shape), dt))

    def dram(self, name, shape, dt, kind=None):
        if kind is None:
            kind = "ExternalOutput" if name in self.dbg else "Internal"
        return self.nc.dram_tensor(name, list(shape), dt, kind=kind)

    def inp(self, name, shape, dt=F32):
        return self.nc.dram_tensor(name, list(shape), dt, kind="ExternalInput").ap()

    def declare(self):
        T, L = self.T, self.L
        I = {}
        NTOK = self.NB * self.NSEG * T
        I["x"] = self.inp("x", [NTOK, D])
        I["cT"] = self.inp("cT", [self.NB, 128, KC])
        I["norm1_g"] = self.inp("norm1_g", [L, D])
        I["norm2_g"] = self.inp("norm2_g", [L, D])
        I["ada_w"] = self.inp("ada_w", [L, D, 6 * D])
        I["ada_b"] = self.inp("ada_b", [L, 6 * D])
        I["w_in"] = self.inp("w_in", [L, D, N_IN])
        I["conv_w"] = self.inp("conv_w", [128, L, 16, CONV_K])
        I["a_log"] = self.inp("a_log", [1, L * 8])
        I["dt_bias"] = self.inp("dt_bias", [1, L * 8])
        I["dn_norm_g"] = self.inp("dn_norm_g", [L, 128, 1])
        I["sinks"] = self.inp("sinks", [1, L * 8])
        if "outproj" in self.phases:
            I["w_out"] = self.inp("w_out", [L, MIX, D])
        I["rw"] = self.inp("rw", [L, D, 36])
        I["rb"] = self.inp("rb", [L, 36])
        if "moe" in self.phases:
            I["w_gate"] = self.inp("w_gate", [L, N_EXP, D, D_FF])
            I["w_up"] = self.inp("w_up", [L, N_EXP, D, D_FF])
            I["w_down"] = self.inp("w_down", [L, N_EXP, D_FF, D])
        I["final_g"] = self.inp("final_g", [1, D])
        self.I = I
        self.y_out = self.nc.dram_tensor("y_out", [NTOK, D], F32, kind="ExternalOutput").ap()
        self.x_d = self.dram("x_d", [NTOK, D], F32).ap()
        self.mod_d = self.dram("mod_d", [self.NB, L, 6, 128, D], F32).ap()
        self.P_d = self.dram("P_d", [N_IN, TP + self.NSEG * T], F32).ap()
        self.TM = self.NSEG * T
        self.NTM = self.TM // 128
        self.NBLK = (2 * self.TM) // 128 + N_EXP
        self.h2_d = self.dram("h2_d", [self.TM, D], BF16).ap()
        self.rows_d = self.dram("rows_d", [self.NBLK * 128, D], BF16).ap()
        self.yrows_d = self.dram("yrows_d", [self.NBLK * 128, D], F32).ap()

    def consts(self):
        S, st = self.S, self.st
        self.reg_neg = self.nc.gpsimd.to_reg(NEG)
        self.reg_zero = self.nc.gpsimd.to_reg(0.0)
        self.ident = self.sb(st, "ident", [128, 128], F32)
        self.d_const = S.dep("const")
        d = self.d_const
        S.op("pool", lambda e: e.memset(self.ident[:], 1.0), writes=[d])
        S.op("pool", lambda e: e.affine_select(out=self.ident[:], in_=self.ident[:], pattern=[[-1, 128]],
                                                compare_op=ALU.is_equal, fill=self.reg_zero, base=0, channel_multiplier=1),
             reads=[d], writes=[d])
        self.ones = self.sb(st, "ones", [128, 128], F32)
        S.op("pool", lambda e: e.memset(self.ones[:], 1.0), writes=[d])
        self.epsc = self.sb(st, "epsc", [128, 1], F32)
        S.op("pool", lambda e: e.memset(self.epsc[:], EPS), writes=[d])
        self.zeros = self.sb(st, "zeros", [128, 2048], BF16)
        S.op("pool", lambda e: e.memset(self.zeros[:], 0.0), writes=[d])
        self.SCARRY = self.sb(st, "SCARRY", [128, 8, 128], F32)
        self.d_carry = S.dep("carry")
        self.d_act = S.deps(self.NT, "act")
        self.d_xt = S.deps(self.NSEG * self.NT, "xt")
        self.d_h2 = S.dep("h2_d")
        self.d_rows = S.dep("rows_d")
        self.d_yrows = S.dep("yrows_d")
        self.ps = [st.enter_context(self.nc.psum_tensor(f"ps{i}", [128, 512], F32)) for i in range(8)]
        self.d_ps = S.deps(8, "ps")
        self.d_x = S.dep("x_d")
        self.d_mod = S.dep("mod_d")
        self.d_P = S.dep("P_d")

    def phase_init(self):
        S = self.S
        T = self.T
        S.dma("sp", self.x_d, self.I["x"], writes=[self.d_x])
        r = 0
        while r < N_IN:
            n = min(128, N_IN - r)
            S.dma("pool", self.P_d[r:r + n, 0:TP], self.zeros[:n, 0:TP], reads=[self.d_const], writes=[self.d_P])
            r += n
        for bb_ in range(self.NBLK):
            S.dma("sp", self.rows_d[bb_ * 128:(bb_ + 1) * 128, :], self.zeros[:, :], reads=[self.d_const], writes=[self.d_rows])
        S.barrier()

    def phase_ada(self):
        S, nc, I = self.S, self.nc, self.I
        with ExitStack() as ph:
            cT = self.sb(ph, "cT", [128, KC], F32)
            cact = self.sb(ph, "cact", [128, KC], F32)
            CB = self.sb(ph, "CB", [128, KC, 128], F32)
            d_c = S.dep()
            S.dma("sp", cT[:], I["cT"][self.bi], writes=[d_c])
            S.op("act", lambda e: e.activation(out=cact[:], in_=cT[:], func=AF.Silu), reads=[d_c], writes=[d_c])
            S.op("dve", lambda e: e.tensor_copy(out=CB[:], in_=cact[:].unsqueeze(2).broadcast_to([128, KC, 128])),
                 reads=[d_c], writes=[d_c])
            NB = 2
            W = [self.sb(ph, f"adaW{i}", [128, KC, 512], F32) for i in range(NB)]
            dW = S.deps(NB)
            bb = [self.sb(ph, f"adab{i}", [128, 512], F32) for i in range(NB)]
            gg = [self.sb(ph, f"adag{i}", [128, 512], F32) for i in range(NB)]
            dB = S.deps(NB)
            ot = [self.sb(ph, f"adao{i}", [128, 512], F32) for i in range(NB)]
            dO = S.deps(NB)
            it = 0
            for l in range(self.L):
                wv = I["ada_w"][l].rearrange("(kc p) n -> p kc n", p=128)
                for cg in range(24):
                    b = it % NB
                    it += 1
                    part, sub = cg // 4, cg % 4
                    cs = slice(cg * 512, (cg + 1) * 512)
                    fs = slice(sub * 512, (sub + 1) * 512)
                    S.dma("sp", W[b][:], wv[:, :, cs], writes=[dW[b]])
                    S.dma("sp", bb[b][:], I["ada_b"][l:l + 1, cs].partition_broadcast(128), writes=[dB[b]])
                    is_sc = part in (1, 4)
                    if is_sc:
                        gsrc = I["norm1_g"] if part == 1 else I["norm2_g"]
                        S.dma("sp", gg[b][:], gsrc[l:l + 1, fs].partition_broadcast(128), writes=[dB[b]])
                    pb = b
                    for kc in range(KC):
                        S.op("pe", lambda e, kc=kc, b=b, pb=pb: e.matmul(self.ps[pb][:], lhsT=CB[:, kc, :], rhs=W[b][:, kc, :],
                                                                           start=(kc == 0), stop=(kc == KC - 1)),
                             reads=[d_c, dW[b]], writes=[self.d_ps[pb]])
                    S.op("dve", lambda e, b=b, pb=pb: e.tensor_tensor(out=ot[b][:], in0=self.ps[pb][:], in1=bb[b][:], op=ALU.add),
                         reads=[self.d_ps[pb], dB[b]], writes=[dO[b]])
                    if is_sc:
                        S.op("dve", lambda e, b=b: e.scalar_tensor_tensor(out=ot[b][:], in0=ot[b][:], scalar=1.0, in1=gg[b][:],
                                                                           op0=ALU.add, op1=ALU.mult),
                             reads=[dB[b], dO[b]], writes=[dO[b]])
                    S.dma("sp", self.mod_d[self.bi, l, part, :, fs], ot[b][:], reads=[dO[b]], writes=[self.d_mod])
        S.barrier()

    def phase_norm(self, l, which, router=None):
        S, nc, I = self.S, self.nc, self.I
        T, NT = self.T, self.NT
        with ExitStack() as ph:
            Abc = self.sb(ph, "Abc", [128, D], F32)
            Bbc = self.sb(ph, "Bbc", [128, D], F32)
            d_ab = S.dep()
            if which == "final":
                S.dma("sp", Abc[:], I["final_g"][0:1, :].partition_broadcast(128), writes=[d_ab])
            else:
                pa, pb_ = (1, 0) if which == 1 else (4, 3)
                S.dma("sp", Abc[:], self.mod_d[self.bi, l, pa], reads=[self.d_mod], writes=[d_ab])
                S.dma("sp", Bbc[:], self.mod_d[self.bi, l, pb_], reads=[self.d_mod], writes=[d_ab])
            NB = 2
            xt = [self.sb(ph, f"xt{i}", [128, D], F32) for i in range(NB)]
            dx = S.deps(NB)
            hf = [self.sb(ph, f"hf{i}", [128, D], F32) for i in range(NB)]
            dh = S.deps(NB)
            junk = self.sb(ph, "junk", [128, D], BF16)
            d_junk = S.dep()
            st_ = [self.sb(ph, f"nst{i}", [128, 4], F32) for i in range(NB)]
            dst_ = S.deps(NB)
            if router is not None:
                hT32 = [self.sb(ph, f"hT32_{i}", [128, KC, 128], F32) for i in range(NB)]
                dhT = S.deps(NB)
            for i in range(NT):
                b = i % NB
                rows = slice(self.r0 + i * 128, self.r0 + (i + 1) * 128)
                S.dma("sp", xt[b][:], self.x_d[rows, :], reads=[self.d_x, self.d_xt[i]], writes=[dx[b]])
                S.op("act", lambda e, b=b: e.activation(out=junk[:], in_=xt[b][:], func=AF.Square, accum_out=st_[b][:, 0:1]),
                     reads=[dx[b]], writes=[d_junk, dst_[b]])
                S.op("act", lambda e, b=b: e.activation(out=st_[b][:, 1:2], in_=st_[b][:, 0:1], func=AF.Sqrt,
                                                        scale=1.0 / D, bias=self.epsc[:, 0:1]),
                     reads=[dst_[b], self.d_const], writes=[dst_[b]])
                S.op("dve", lambda e, b=b: e.reciprocal(out=st_[b][:, 2:3], in_=st_[b][:, 1:2]),
                     reads=[dst_[b]], writes=[dst_[b]])
                S.op("dve", lambda e, b=b: e.scalar_tensor_tensor(out=hf[b][:], in0=xt[b][:], scalar=st_[b][:, 2:3], in1=Abc[:],
                                                                   op0=ALU.mult, op1=ALU.mult),
                     reads=[dx[b], dst_[b], d_ab], writes=[dh[b]])
                if which == "final":
                    self.out_events.append(S.dma("sp", self.y_out[rows, :], hf[b][:], reads=[dh[b]]))
                    continue
                S.op("pool", lambda e, b=b: e.tensor_tensor(out=hf[b][:], in0=hf[b][:], in1=Bbc[:], op=ALU.add),
                     reads=[d_ab, dh[b]], writes=[dh[b]])
                if router is not None:
                    S.op("pool", lambda e, b=b: e.tensor_copy(out=router["hbf"][b][:], in_=hf[b][:]), reads=[dh[b]], writes=[router["d_hbf"][b]])
                    S.dma("sp", self.h2_d[i * 128:(i + 1) * 128, :], router["hbf"][b][:], reads=[router["d_hbf"][b]], writes=[self.d_h2])
                for q4 in range(4):
                    pb = (i * 4 + q4) % 4
                    for j in range(4):
                        kc = q4 * 4 + j
                        S.op("pe", lambda e, b=b, kc=kc, pb=pb, j=j: e.transpose(self.ps[pb][:, j * 128:(j + 1) * 128],
                                                                                   hf[b][:, kc * 128:(kc + 1) * 128], self.ident[:]),
                             reads=[dh[b], self.d_const], writes=[self.d_ps[pb]])
                    if router is None:
                        S.op("act", lambda e, pb=pb, q4=q4, i=i: e.activation(
                            out=self.actT[:, q4 * 4:(q4 + 1) * 4, i * 128:(i + 1) * 128],
                            in_=self.ps[pb][:].rearrange("p (j n) -> p j n", j=4), func=AF.Copy),
                            reads=[self.d_ps[pb]], writes=[self.d_act[i]])
                    if router is not None:
                        S.op("dve", lambda e, pb=pb, q4=q4, b=b: e.tensor_copy(
                            out=hT32[b][:, q4 * 4:(q4 + 1) * 4, :],
                            in_=self.ps[pb][:].rearrange("p (j n) -> p j n", j=4)),
                            reads=[self.d_ps[pb]], writes=[dhT[b]])
                if router is not None:
                    pr = 4 + (i % 2)
                    for kc in range(KC):
                        S.op("pe", lambda e, b=b, kc=kc, pr=pr: e.matmul(self.ps[pr][:, 0:36], lhsT=hT32[b][:, kc, :],
                                                                          rhs=router["RW"][:, kc, :],
                                                                          start=(kc == 0), stop=(kc == KC - 1)),
                             reads=[dhT[b], router["d_rw"]], writes=[self.d_ps[pr]])
                    S.op("dve", lambda e, pr=pr, i=i: e.tensor_tensor(out=router["LG"][:, i, :], in0=self.ps[pr][:, 0:36],
                                                                       in1=router["RB"][:], op=ALU.add),
                         reads=[self.d_ps[pr], router["d_rw"]], writes=[router["d_lg"]])
        S.barrier()

    def phase_proj(self, l):
        S, nc, I = self.S, self.nc, self.I
        T = self.T
        NTT = T // 512
        wv = I["w_in"][l].rearrange("(kc p) n -> p kc n", p=128)
        with ExitStack() as ph:
            NB = 2
            W = [self.sb(ph, f"pjW{i}", [128, KC, 512], BF16) for i in range(NB)]
            dW = S.deps(NB)
            stg = [self.sb(ph, f"pjS{i}", [128, T], F32) for i in range(NB)]
            dS_ = S.deps(NB)
            nsg = (N_IN + 511) // 512
            cnt = 0
            for sg in range(nsg):
                c0 = sg * 512
                cw = min(512, N_IN - c0)
                b = sg % NB
                S.dma("pool", W[b][:, :, 0:cw], wv[:, :, c0:c0 + cw], writes=[dW[b]])
                for j in range((cw + 127) // 128):
                    gs = min(128, cw - j * 128)
                    sb_ = cnt % NB
                    for tt in range(NTT):
                        pb = cnt % 8
                        cnt += 1
                        for kc in range(KC):
                            S.op("pe", lambda e, b=b, kc=kc, j=j, gs=gs, tt=tt, pb=pb: e.matmul(
                                self.ps[pb][0:gs, :], lhsT=W[b][:, kc, j * 128:j * 128 + gs],
                                rhs=self.actT[:, kc, tt * 512:(tt + 1) * 512], start=(kc == 0), stop=(kc == KC - 1)),
                                reads=[dW[b]] + self.d_act[tt * 4:(tt + 1) * 4], writes=[self.d_ps[pb]])
                        eng = "act" if (cnt % 2 == 0) else "dve"
                        if eng == "act":
                            S.op("act", lambda e, sb_=sb_, gs=gs, tt=tt, pb=pb: e.activation(
                                out=stg[sb_][0:gs, tt * 512:(tt + 1) * 512], in_=self.ps[pb][0:gs, :], func=AF.Copy),
                                reads=[self.d_ps[pb]], writes=[dS_[sb_]])
                        else:
                            S.op("dve", lambda e, sb_=sb_, gs=gs, tt=tt, pb=pb: e.tensor_copy(
                                out=stg[sb_][0:gs, tt * 512:(tt + 1) * 512], in_=self.ps[pb][0:gs, :]),
                                reads=[self.d_ps[pb]], writes=[dS_[sb_]])
                    r0 = c0 + j * 128
                    S.dma("sp", self.P_d[r0:r0 + gs, self.c0:self.c0 + T], stg[sb_][0:gs, :], reads=[dS_[sb_]], writes=[self.d_P])
        S.barrier()

    def attn_consts(self, ph):
        S = self.S
        d = S.dep("attnc")
        self.d_attnc = d
        reli = self.sb(ph, "reli", [128, 256], I32)
        self.REL = self.sb(ph, "REL", [128, 256], F32)
        S.op("pool", lambda e: e.iota(reli[:], pattern=[[-1, 256]], base=128, channel_multiplier=1), writes=[d])
        S.op("pool", lambda e: e.tensor_copy(out=self.REL[:], in_=reli[:]), reads=[d], writes=[d])
        self.HM = self.sb(ph, "HM", [128, 256], F32)
        S.op("pool", lambda e: e.memset(self.HM[:], 0.0), writes=[d])
        S.op("pool", lambda e: e.memset(self.HM[:, 0:128], NEG), reads=[d], writes=[d])
        self.E2 = self.sb(ph, "E2", [2, 128], F32)
        S.op("pool", lambda e: e.memset(self.E2[:], 1.0), writes=[d])
        S.op("pool", lambda e: e.affine_select(out=self.E2[:], in_=self.E2[:], pattern=[[1, 128]], compare_op=ALU.is_ge,
                                                fill=self.reg_zero, base=0, channel_multiplier=-64), reads=[d], writes=[d])
        S.op("pool", lambda e: e.affine_select(out=self.E2[:], in_=self.E2[:], pattern=[[-1, 128]], compare_op=ALU.is_ge,
                                                fill=self.reg_zero, base=63, channel_multiplier=64), reads=[d], writes=[d])

    def make_bias(self, tile, dep, coef, maxd):
        S = self.S
        S.op("dve", lambda e: e.tensor_scalar_mul(out=tile[:], in0=self.REL[:], scalar1=float(coef)),
             reads=[self.d_attnc], writes=[dep])
        S.op("pool", lambda e: e.affine_select(out=tile[:], in_=tile[:], pattern=[[-1, 256]], compare_op=ALU.is_ge,
                                                fill=self.reg_neg, base=128, channel_multiplier=1), reads=[dep], writes=[dep])
        S.op("pool", lambda e: e.affine_select(out=tile[:], in_=tile[:], pattern=[[1, 256]], compare_op=ALU.is_ge,
                                                fill=self.reg_neg, base=maxd - 128, channel_multiplier=-1), reads=[dep], writes=[dep])

    def attn_unit(self, ctx, q_ap, k_ap, bias, d_bias, first, Vb0, Vb1, d_V, half, out_ap, out_deps,
                  sink_ap=None, lse_ap=None, d_lse=None, in_deps=()):
        S = self.S
        u = ctx["u"]
        ctx["u"] += 1
        b = u % 2
        pS, dS = self.ps[b], self.d_ps[b]
        pT, dT = ctx["psT"][b], self.d_ps[2 + b]
        pO, dO = self.ps[4 + b], self.d_ps[4 + b]
        s32, d_s = ctx["s32"][b], ctx["d_s32"][b]
        pbf, d_p = ctx["pbf"][b], ctx["d_pbf"][b]
        pTs, d_pT = ctx["pTs"][b], ctx["d_pTs"][b]
        stt, d_st = ctx["stt"][b], ctx["d_stt"][b]
        S.op("pe", lambda e: e.matmul(pS[:, 0:256], lhsT=q_ap, rhs=k_ap, start=True, stop=True),
             reads=list(in_deps), writes=[dS])
        S.op("dve", lambda e: e.scalar_tensor_tensor(out=s32[:], in0=pS[:, 0:256], scalar=0.125, in1=bias[:],
                                                      op0=ALU.mult, op1=ALU.add), reads=[dS, d_bias], writes=[d_s])
        if first:
            S.op("pool", lambda e: e.tensor_tensor(out=s32[:], in0=s32[:], in1=self.HM[:], op=ALU.add),
                 reads=[d_s, self.d_attnc], writes=[d_s])
        S.op("dve", lambda e: e.tensor_reduce(out=stt[:, 0:1], in_=s32[:], axis=AX.X, op=ALU.max, negate=True),
             reads=[d_s], writes=[d_st])
        if sink_ap is not None:
            S.op("dve", lambda e: e.scalar_tensor_tensor(out=stt[:, 0:1], in0=sink_ap, scalar=-1.0, in1=stt[:, 0:1],
                                                          op0=ALU.mult, op1=ALU.min), reads=[d_st, ctx["d_sink"]], writes=[d_st])
        S.op("act", lambda e: e.activation(out=pbf[:], in_=s32[:], func=AF.Exp, bias=stt[:, 0:1], scale=1.0,
                                            accum_out=stt[:, 1:2]), reads=[d_s, d_st], writes=[d_p, d_st])
        if sink_ap is not None:
            S.op("act", lambda e: e.activation(out=stt[:, 3:4], in_=sink_ap, func=AF.Exp, bias=stt[:, 0:1], scale=1.0),
                 reads=[d_st, ctx["d_sink"]], writes=[d_st])
            S.op("dve", lambda e: e.tensor_tensor(out=stt[:, 1:2], in0=stt[:, 1:2], in1=stt[:, 3:4], op=ALU.add),
                 reads=[d_st], writes=[d_st])
        S.op("dve", lambda e: e.reciprocal(out=stt[:, 2:3], in_=stt[:, 1:2]), reads=[d_st], writes=[d_st])
        S.op("pool", lambda e: e.tensor_scalar_mul(out=pbf[:], in0=pbf[:], scalar1=stt[:, 2:3]),
             reads=[d_p, d_st], writes=[d_p])
        if lse_ap is not None:
            S.op("act", lambda e: e.activation(out=stt[:, 3:4], in_=stt[:, 1:2], func=AF.Ln), reads=[d_st], writes=[d_st])
            S.op("dve", lambda e: e.tensor_tensor(out=lse_ap, in0=stt[:, 3:4], in1=stt[:, 0:1], op=ALU.subtract),
                 reads=[d_st], writes=[d_lse])
        for j in range(2):
            S.op("pe", lambda e, j=j: e.transpose(pT[:, j * 128:(j + 1) * 128], pbf[:, j * 128:(j + 1) * 128], ctx["identb"][:]),
                 reads=[d_p, ctx["d_identb"]], writes=[dT])
        S.op("act", lambda e: e.activation(out=pTs[:], in_=pT[:, 0:256], func=AF.Copy), reads=[dT], writes=[d_pT])
        S.op("pe", lambda e: e.matmul(pO[:, 0:128], lhsT=Vb0, rhs=pTs[:, 0:128], start=True, stop=False),
             reads=[d_V, d_pT], writes=[dO])
        S.op("pe", lambda e: e.matmul(pO[:, 0:128], lhsT=Vb1, rhs=pTs[:, 128:256], start=False, stop=True),
             reads=[d_V, d_pT], writes=[dO])
        rs = slice(half * 64, half * 64 + 64)
        S.op("dve", lambda e: e.tensor_copy(out=out_ap, in_=pO[rs, 0:128]), reads=[dO], writes=list(out_deps))

    def attn_ctx(self, ph):
        S = self.S
        ctx = {"u": 0}
        ctx["psT"] = [self.ps[2].bitcast(BF16), self.ps[3].bitcast(BF16)]
        ctx["s32"] = [self.sb(ph, f"s32_{i}", [128, 256], F32) for i in range(2)]
        ctx["d_s32"] = S.deps(2)
        ctx["pbf"] = [self.sb(ph, f"pbf_{i}", [128, 256], BF16) for i in range(2)]
        ctx["d_pbf"] = S.deps(2)
        ctx["pTs"] = [self.sb(ph, f"pTs_{i}", [128, 256], BF16) for i in range(2)]
        ctx["d_pTs"] = S.deps(2)
        ctx["stt"] = [self.sb(ph, f"stt_{i}", [128, 8], F32) for i in range(2)]
        ctx["d_stt"] = S.deps(2)
        identb = self.sb(ph, "identb", [128, 128], BF16)
        ctx["identb"] = identb
        ctx["d_identb"] = S.dep()
        S.op("dve", lambda e: e.tensor_copy(out=identb[:], in_=self.ident[:]), reads=[self.d_const], writes=[ctx["d_identb"]])
        return ctx

    def build_vblocks(self, ctx, vT, d_vT, Vblk, d_Vblk, specs):
        S = self.S
        for n, (idx, c0, step) in enumerate(specs):
            b = n % 2
            pT, dT = ctx["psT"][b], self.d_ps[2 + b]
            src = vT[:, sl(c0, 128, step)]
            S.op("pe", lambda e, pT=pT, src=src: e.transpose(pT[:, 0:128], src, ctx["identb"][:]),
                 reads=[d_vT, ctx["d_identb"]], writes=[dT])
            eng = "act" if n % 2 == 0 else "dve"
            if eng == "act":
                S.op("act", lambda e, pT=pT, idx=idx: e.activation(out=Vblk[:, idx, :], in_=pT[:, 0:128], func=AF.Copy),
                     reads=[dT], writes=[d_Vblk])
            else:
                S.op("dve", lambda e, pT=pT, idx=idx: e.tensor_copy(out=Vblk[:, idx, :], in_=pT[:, 0:128]),
                     reads=[dT], writes=[d_Vblk])

    def phase_attn_c(self, l):
        S, I = self.S, self.I
        T, NT = self.T, self.NT
        with ExitStack() as ph:
            self.attn_consts(ph)
            ctx = self.attn_ctx(ph)
            sink = self.sb(ph, "sink", [128, 8], F32)
            ctx["d_sink"] = S.dep()
            S.dma("sp", sink[:], I["sinks"][0:1, l * 8:(l + 1) * 8].partition_broadcast(128), writes=[ctx["d_sink"]])
            qc = self.sb(ph, "qc", [128, 4, T], BF16)
            kc = self.sb(ph, "kc", [128, TP + T], BF16)
            vN = self.sb(ph, "vN", [128, TP + T], BF16)
            vS = self.sb(ph, "vS", [128, TP + T], BF16)
            d_q, d_k, d_vn, d_vs = S.deps(4)
            for g in range(2):
                S.dma("pool", qc[64 * g:64 * g + 64, :, :],
                      self.P_d[O_CQ + 256 * g:O_CQ + 256 * (g + 1), self.c0:self.c0 + T].rearrange("(j p) t -> p j t", p=64),
                      reads=[self.d_P], writes=[d_q])
            hs = slice(self.c0 - TP, self.c0 + T)
            S.dma("pool", kc[:], self.P_d[O_CK:O_CK + 128, hs], reads=[self.d_P], writes=[d_k])
            S.dma("pool", vN[:], self.P_d[O_CV:O_CV + 128, hs], reads=[self.d_P], writes=[d_vn])
            S.dma("pool", vS[0:64, :], self.P_d[O_CV + 64:O_CV + 128, hs], reads=[self.d_P], writes=[d_vs])
            S.dma("pool", vS[64:128, :], self.P_d[O_CV:O_CV + 64, hs], reads=[self.d_P], writes=[d_vs])
            VN = self.sb(ph, "VN", [128, NT + 1, 128], BF16)
            VS = self.sb(ph, "VS", [128, NT + 1, 128], BF16)
            d_VN, d_VS = S.deps(2)
            specs = [(j + 1, TP + 128 * j, 1) for j in range(-1, NT)]
            self.build_vblocks(ctx, vN, d_vn, VN, d_VN, specs)
            self.build_vblocks(ctx, vS, d_vs, VS, d_VS, specs)
            bias = [self.sb(ph, f"biasc{h}", [128, 256], F32) for h in range(8)]
            d_b = S.deps(8)
            for h in range(8):
                self.make_bias(bias[h], d_b[h], -(2.0 ** (-(h + 1))), 127)
            for h in range(8):
                g, hh = h // 4, h % 2
                Vb, dV = (VN, d_VN) if hh == g else (VS, d_VS)
                for j in range(NT):
                    self.attn_unit(ctx,
                                   q_ap=qc[64 * g:64 * g + 64, h % 4, 128 * j:128 * (j + 1)],
                                   k_ap=kc[64 * g:64 * g + 64, TP + 128 * (j - 1):TP + 128 * (j + 1)],
                                   bias=bias[h], d_bias=d_b[h], first=(j == 0 and self.seg_first),
                                   Vb0=Vb[:, j, :], Vb1=Vb[:, j + 1, :], d_V=dV, half=hh,
                                   out_ap=self.actT[64 * hh:64 * hh + 64, 12 + h // 2, 128 * j:128 * (j + 1)],
                                   out_deps=[self.d_act[j]], sink_ap=sink[:, h:h + 1], in_deps=[d_q, d_k])
        S.barrier()

    def phase_attn_a(self, l):
        S, I = self.S, self.I
        T, NT = self.T, self.NT
        with ExitStack() as ph:
            self.attn_consts(ph)
            ctx = self.attn_ctx(ph)
            qa = self.sb(ph, "qa", [128, T], BF16)
            ka = self.sb(ph, "ka", [128, TP + T], BF16)
            va = self.sb(ph, "va", [128, TP + T], BF16)
            d_q, d_k, d_v = S.deps(3)
            maxblk = max(d * (T // (128 * d) + 1) for _, d in PATTERNS)
            Vblk = self.sb(ph, "Vblk", [128, maxblk, 128], BF16)
            d_Vb = S.dep()
            opT = [self.sb(ph, f"opT{p}", [128, T], BF16) for p in range(3)]
            d_op = S.deps(3)
            STAT = [self.sb(ph, f"STAT{p}", [128, NT * 2], F32) for p in range(3)]
            d_stat = S.deps(3)
            R = self.sb(ph, "R", [2, 3, T], F32)
            d_R = S.dep()
            Mx = self.sb(ph, "Mx", [2, T], F32)
            d_M = S.dep()
            bias = [self.sb(ph, f"biasa{i}", [128, 256], F32) for i in range(6)]
            d_b = S.deps(6)
            acc = self.sb(ph, "acca", [128, 512], F32)
            d_acc = S.dep()
            tmp = self.sb(ph, "tmpa", [128, 512], F32)
            d_tmp = S.dep()
            for ch in range(4):
                hs = slice(self.c0 - TP, self.c0 + T)
                S.dma("pool", qa[:], self.P_d[O_AQ + 128 * ch:O_AQ + 128 * (ch + 1), self.c0:self.c0 + T], reads=[self.d_P], writes=[d_q])
                S.dma("pool", ka[:], self.P_d[O_AK + 128 * ch:O_AK + 128 * (ch + 1), hs], reads=[self.d_P], writes=[d_k])
                S.dma("pool", va[:], self.P_d[O_AV + 128 * ch:O_AV + 128 * (ch + 1), hs], reads=[self.d_P], writes=[d_v])
                for p, (w, d) in enumerate(PATTERNS):
                    for hh in range(2):
                        h = 2 * ch + hh
                        self.make_bias(bias[p * 2 + hh], d_b[p * 2 + hh], -(2.0 ** (-(h + 1))) * d, 128)
                for p, (w, d) in enumerate(PATTERNS):
                    nbq = T // (128 * d)
                    specs = []
                    for r in range(d):
                        for j in range(-1, nbq):
                            specs.append((r * (nbq + 1) + j + 1, TP + r + d * 128 * j, d))
                    self.build_vblocks(ctx, va, d_v, Vblk, d_Vb, specs)
                    for hh in range(2):
                        ps_ = slice(64 * hh, 64 * hh + 64)
                        for r in range(d):
                            for j in range(nbq):
                                blk = r * nbq + j
                                q0 = r + d * 128 * j
                                k0 = TP + r + d * 128 * (j - 1)
                                q_ap = qa[ps_, sl(q0, 128, d)]
                                k_ap = ka[ps_, sl(k0, 256, d)]
                                o_ap = opT[p][ps_, sl(q0, 128, d)]
                                vi = r * (nbq + 1) + j
                                self.attn_unit(ctx, q_ap=q_ap, k_ap=k_ap, bias=bias[p * 2 + hh], d_bias=d_b[p * 2 + hh],
                                               first=(j == 0 and self.seg_first), Vb0=Vblk[:, vi, :], Vb1=Vblk[:, vi + 1, :], d_V=d_Vb, half=hh,
                                               out_ap=o_ap, out_deps=[d_op[p]],
                                               lse_ap=STAT[p][:, blk * 2 + hh:blk * 2 + hh + 1], d_lse=d_stat[p],
                                               in_deps=[d_q, d_k])
                    for r in range(d):
                        for j in range(nbq):
                            blk = r * nbq + j
                            q0 = r + d * 128 * j
                            pb = 6 + (blk % 2)
                            S.op("pe", lambda e, pb=pb, p=p, blk=blk: e.transpose(self.ps[pb][0:2, 0:128], STAT[p][:, blk * 2:blk * 2 + 2],
                                                                                   self.ident[:]),
                                 reads=[d_stat[p], self.d_const], writes=[self.d_ps[pb]])
                            dst = R[0:2, p, sl(q0, 128, d)]
                            S.op("act", lambda e, pb=pb, dst=dst: e.activation(out=dst, in_=self.ps[pb][0:2, 0:128], func=AF.Copy),
                                 reads=[self.d_ps[pb]], writes=[d_R])
                S.op("dve", lambda e: e.tensor_tensor(out=Mx[:], in0=R[:, 0, :], in1=R[:, 1, :], op=ALU.max), reads=[d_R], writes=[d_M])
                S.op("dve", lambda e: e.tensor_tensor(out=Mx[:], in0=Mx[:], in1=R[:, 2, :], op=ALU.max), reads=[d_R, d_M], writes=[d_M])
                for p in range(3):
                    S.op("dve", lambda e, p=p: e.tensor_tensor(out=R[:, p, :], in0=R[:, p, :], in1=Mx[:], op=ALU.subtract),
                         reads=[d_M, d_R], writes=[d_R])
                S.op("act", lambda e: e.activation(out=R[:], in_=R[:], func=AF.Exp), reads=[d_R], writes=[d_R])
                S.op("dve", lambda e: e.tensor_tensor(out=Mx[:], in0=R[:, 0, :], in1=R[:, 1, :], op=ALU.add), reads=[d_R, d_M], writes=[d_M])
                S.op("dve", lambda e: e.tensor_tensor(out=Mx[:], in0=Mx[:], in1=R[:, 2, :], op=ALU.add), reads=[d_R, d_M], writes=[d_M])
                S.op("dve", lambda e: e.reciprocal(out=Mx[:], in_=Mx[:]), reads=[d_M], writes=[d_M])
                for p in range(3):
                    S.op("dve", lambda e, p=p: e.tensor_tensor(out=R[:, p, :], in0=R[:, p, :], in1=Mx[:], op=ALU.mult),
                         reads=[d_M, d_R], writes=[d_R])
                for tt in range(T // 512):
                    cs = slice(tt * 512, (tt + 1) * 512)
                    for p in range(3):
                        pb = 6 + (p % 2)
                        S.op("pe", lambda e, pb=pb, p=p, cs=cs: e.matmul(self.ps[pb][:], lhsT=self.E2[:], rhs=R[0:2, p, cs],
                                                                          start=True, stop=True),
                             reads=[d_R, self.d_attnc], writes=[self.d_ps[pb]])
                        if p == 0:
                            S.op("dve", lambda e, pb=pb, cs=cs: e.tensor_tensor(out=acc[:], in0=opT[0][:, cs], in1=self.ps[pb][:], op=ALU.mult),
                                 reads=[d_op[0], self.d_ps[pb]], writes=[d_acc])
                        else:
                            S.op("dve", lambda e, pb=pb, cs=cs, p=p: e.tensor_tensor(out=tmp[:], in0=opT[p][:, cs], in1=self.ps[pb][:], op=ALU.mult),
                                 reads=[d_op[p], self.d_ps[pb]], writes=[d_tmp])
                            if p == 1:
                                S.op("pool", lambda e: e.tensor_tensor(out=acc[:], in0=acc[:], in1=tmp[:], op=ALU.add),
                                     reads=[d_tmp, d_acc], writes=[d_acc])
                            else:
                                S.op("pool", lambda e, cs=cs, ch=ch: e.tensor_tensor(out=self.actT[:, ch, cs], in0=acc[:], in1=tmp[:], op=ALU.add),
                                     reads=[d_tmp, d_acc], writes=self.d_act[tt * 4:(tt + 1) * 4])
        S.barrier()

    def phase_dn(self, l):
        S, I = self.S, self.I
        T, NT = self.T, self.NT
        DKS = float(DK_B) ** -0.5
        with ExitStack() as ph:
            cnt = {"s": 0}

            cnt["t"] = 0
            d_bank = [S.dep() for _ in range(8)]
            d_slot = [d_bank[i // 4] for i in range(32)]

            def slot():
                bk = 2 + cnt["s"] % 6
                cnt["s"] += 1
                return self.ps[bk][:, 0:128], d_bank[bk]

            def tslot():
                bk = cnt["t"] % 2
                cnt["t"] += 1
                return self.ps[bk][:, 0:128], d_bank[bk]

            d_m = S.dep()
            ML = self.sb(ph, "ML", [128, 128], F32)
            MIT = self.sb(ph, "MIT", [128, 128], F32)
            CH0 = self.sb(ph, "CH0", [128, 128], F32)
            CH1 = self.sb(ph, "CH1", [128, 128], F32)
            S.op("pool", lambda e: e.memset(MIT[:], 1.0), writes=[d_m])
            S.op("pool", lambda e: e.affine_select(out=MIT[:], in_=MIT[:], pattern=[[1, 128]], compare_op=ALU.is_ge,
                                                    fill=self.reg_zero, base=0, channel_multiplier=-1), reads=[d_m], writes=[d_m])
            S.op("pool", lambda e: e.memset(MIT[0:64, 64:128], 0.0), reads=[d_m], writes=[d_m])
            S.op("pool", lambda e: e.memset(ML[:], 1.0), writes=[d_m])
            S.op("pool", lambda e: e.affine_select(out=ML[:], in_=ML[:], pattern=[[-1, 128]], compare_op=ALU.is_ge,
                                                    fill=self.reg_zero, base=-1, channel_multiplier=1), reads=[d_m], writes=[d_m])
            S.op("pool", lambda e: e.memset(ML[64:128, 0:64], 0.0), reads=[d_m], writes=[d_m])
            S.op("pool", lambda e: e.memset(CH0[:], 0.0), writes=[d_m])
            S.op("pool", lambda e: e.memset(CH0[0:64, :], 1.0), reads=[d_m], writes=[d_m])
            S.op("pool", lambda e: e.memset(CH1[:], 0.0), writes=[d_m])
            S.op("pool", lambda e: e.memset(CH1[64:128, :], 1.0), reads=[d_m], writes=[d_m])
            d_par = S.dep()
            cwt = self.sb(ph, "cwt", [128, 16, CONV_K], F32)
            S.dma("sp", cwt[:], I["conv_w"][:, l, :, :], writes=[d_par])
            ngc = self.sb(ph, "ngc", [128, 1], F32)
            S.dma("sp", ngc[:], I["dn_norm_g"][l], writes=[d_par])
            alog = self.sb(ph, "alog", [128, 8], F32)
            dtb = self.sb(ph, "dtb", [128, 8], F32)
            S.dma("sp", alog[:], I["a_log"][0:1, l * 8:(l + 1) * 8].partition_broadcast(128), writes=[d_par])
            S.dma("sp", dtb[:], I["dt_bias"][0:1, l * 8:(l + 1) * 8].partition_broadcast(128), writes=[d_par])
            nea = self.sb(ph, "nea", [128, 8], F32)
            S.op("act", lambda e: e.activation(out=nea[:], in_=alog[:], func=AF.Exp), reads=[d_par], writes=[d_par])
            S.op("dve", lambda e: e.tensor_scalar_mul(out=nea[:], in0=nea[:], scalar1=-1.0), reads=[d_par], writes=[d_par])
            d_g = S.dep()
            bbaT = self.sb(ph, "bbaT", [16, T], F32)
            S.dma("sp", bbaT[:], self.P_d[O_BB:O_BB + 16, self.c0:self.c0 + T], reads=[self.d_P], writes=[d_g])
            GB = self.sb(ph, "GB", [128, NT, 16], F32)
            for i in range(NT):
                p_, dp_ = tslot()
                S.op("pe", lambda e, p_=p_, i=i: e.transpose(p_[:, 0:16], bbaT[:, i * 128:(i + 1) * 128], self.ident[0:16, 0:16]),
                     reads=[d_g, self.d_const], writes=[dp_])
                S.op("act", lambda e, p_=p_, i=i: e.activation(out=GB[:, i, :], in_=p_[:, 0:16], func=AF.Copy),
                     reads=[dp_], writes=[d_g])
            BETA = self.sb(ph, "BETA", [128, NT, 8], F32)
            G = self.sb(ph, "G", [128, NT, 8], F32)
            t1 = self.sb(ph, "gt1", [128, NT, 8], F32)
            t2 = self.sb(ph, "gt2", [128, NT, 8], F32)
            S.op("act", lambda e: e.activation(out=BETA[:], in_=GB[:, :, 0:8], func=AF.Exp, scale=-1.0), reads=[d_g], writes=[d_g])
            S.op("dve", lambda e: e.tensor_scalar_add(out=BETA[:], in0=BETA[:], scalar1=1.0), reads=[d_g], writes=[d_g])
            S.op("dve", lambda e: e.reciprocal(out=BETA[:], in_=BETA[:]), reads=[d_g], writes=[d_g])
            S.op("dve", lambda e: e.tensor_tensor(out=G[:], in0=GB[:, :, 8:16], in1=dtb[:].unsqueeze(1).broadcast_to([128, NT, 8]), op=ALU.add),
                 reads=[d_g, d_par], writes=[d_g])
            S.op("dve", lambda e: e.tensor_scalar_mul(out=t1[:], in0=G[:], scalar1=-1.0), reads=[d_g], writes=[d_g])
            S.op("dve", lambda e: e.tensor_tensor(out=t1[:], in0=t1[:], in1=G[:], op=ALU.max), reads=[d_g], writes=[d_g])
            S.op("act", lambda e: e.activation(out=t1[:], in_=t1[:], func=AF.Exp, scale=-1.0), reads=[d_g], writes=[d_g])
            S.op("act", lambda e: e.activation(out=t1[:], in_=t1[:], func=AF.Ln, bias=self.ones[:, 0:1], scale=1.0),
                 reads=[d_g, self.d_const], writes=[d_g])
            S.op("dve", lambda e: e.tensor_scalar_max(out=t2[:], in0=G[:], scalar1=0.0), reads=[d_g], writes=[d_g])
            S.op("dve", lambda e: e.tensor_tensor(out=t2[:], in0=t2[:], in1=t1[:], op=ALU.add), reads=[d_g], writes=[d_g])
            S.op("dve", lambda e: e.tensor_tensor(out=G[:], in0=t2[:], in1=nea[:].unsqueeze(1).broadcast_to([128, NT, 8]), op=ALU.mult),
                 reads=[d_g, d_par], writes=[d_g])
            GC = self.sb(ph, "GC", [128, NT, 8], F32)
            GL = self.sb(ph, "GL", [128, NT, 2, 8], F32)
            for i in range(NT):
                p_, dp_ = slot()
                S.op("pe", lambda e, p_=p_, i=i: e.matmul(p_[:, 0:8], lhsT=MIT[:], rhs=G[:, i, :], start=True, stop=True),
                     reads=[d_g, d_m], writes=[dp_])
                S.op("pe", lambda e, p_=p_, i=i: e.matmul(p_[:, 8:16], lhsT=CH0[:], rhs=G[:, i, :], start=True, stop=True),
                     reads=[d_g, d_m], writes=[dp_])
                S.op("pe", lambda e, p_=p_, i=i: e.matmul(p_[:, 16:24], lhsT=CH1[:], rhs=G[:, i, :], start=True, stop=True),
                     reads=[d_g, d_m], writes=[dp_])
                S.op("act", lambda e, p_=p_, i=i: e.activation(out=GC[:, i, :], in_=p_[:, 0:8], func=AF.Copy), reads=[dp_], writes=[d_g])
                S.op("dve", lambda e, p_=p_, i=i: e.tensor_copy(out=GL[:, i, :, :], in_=p_[:, 8:24].rearrange("p (c h) -> p c h", c=2)),
                     reads=[dp_], writes=[d_g])
            EG = self.sb(ph, "EG", [128, NT, 8], F32)
            BEG = self.sb(ph, "BEG", [128, NT, 8], F32)
            KD = self.sb(ph, "KD", [128, NT, 8], F32)
            EGL = self.sb(ph, "EGL", [128, NT, 2, 8], F32)
            S.op("act", lambda e: e.activation(out=EG[:], in_=GC[:], func=AF.Exp), reads=[d_g], writes=[d_g])
            S.op("dve", lambda e: e.tensor_tensor(out=BEG[:], in0=EG[:], in1=BETA[:], op=ALU.mult), reads=[d_g], writes=[d_g])
            S.op("dve", lambda e: e.tensor_tensor(out=KD[0:64], in0=GL[0:64, :, 0, :], in1=GC[0:64], op=ALU.subtract), reads=[d_g], writes=[d_g])
            S.op("dve", lambda e: e.tensor_tensor(out=KD[64:128], in0=GL[64:128, :, 1, :], in1=GC[64:128], op=ALU.subtract), reads=[d_g], writes=[d_g])
            S.op("act", lambda e: e.activation(out=KD[:], in_=KD[:], func=AF.Exp), reads=[d_g], writes=[d_g])
            S.op("act", lambda e: e.activation(out=EGL[:], in_=GL[:], func=AF.Exp), reads=[d_g], writes=[d_g])

            stop = self.dn_stop
            qn = self.sb(ph, "qn", [128, T], F32)
            kn = self.sb(ph, "kn", [128, T], F32)
            vT = [self.sb(ph, f"vT{i}", [128, T], F32) for i in range(2)]
            zT = [self.sb(ph, f"zT{i}", [128, T], F32) for i in range(2)]
            X = self.sb(ph, "convX", [128, T + 3], F32)
            d_X = S.dep()
            d_q, d_k = S.dep(), S.dep()
            d_v = S.deps(2)
            d_z = S.deps(2)
            sq = self.sb(ph, "sqt", [128, 512], F32)
            rn = self.sb(ph, "rnt", [128, 512], F32)
            d_sq, d_rn = S.dep(), S.dep()
            NS = 4
            def mk(name):
                return [self.sb(ph, f"{name}{i}", [128, 128], F32) for i in range(NS)], S.deps(NS)
            ktok, d_ktok = mk("ktok")
            KKs, d_KKs = mk("KKs")
            QKs, d_QKs = mk("QKs")
            vtok, d_vtok = mk("vtok")
            diag, d_diag = mk("diag")
            Dm, d_Dm = mk("Dm")
            DTm, d_DTm = mk("DTm")
            EGR, d_EGR = mk("EGR")
            Lm, d_Lm = mk("Lm")
            Nm, d_Nm = mk("Nm")
            qkT, d_qkT = mk("qkT")
            AL, d_AL = mk("AL")
            AN, d_AN = mk("AN")
            PT, d_PT = mk("PT")
            PT2, d_PT2 = mk("PT2")
            vb, d_vb = mk("vb")
            kbe, d_kbe = mk("kbe")
            kdec, d_kdec = mk("kdec")
            uu, d_uu = mk("uu")
            wT, d_wT = mk("wT")
            qdT, d_qdT = mk("qdT")
            vnew, d_vnew = mk("vnew")
            otok, d_otok = mk("otok")
            ojunk, d_ojunk = mk("ojunk")
            ost = [self.sb(ph, f"ost{i}", [128, 4], F32) for i in range(NS)]
            d_ost = S.deps(NS)
            Sst = [[self.sb(ph, f"Sst{hh}_{i}", [128, 128], F32) for i in range(2)] for hh in range(2)]
            d_Sst = [S.deps(2) for _ in range(2)]

            def conv_load(dst, d_dst, row0, c16, l2=None):
                S.dma("sp", X[:], self.P_d[row0:row0 + 128, self.c0 - 3:self.c0 + T], reads=[self.d_P], writes=[d_X])
                S.op("dve", lambda e: e.tensor_scalar_mul(out=dst[:], in0=X[:, 0:T], scalar1=cwt[:, c16, 0:1]),
                     reads=[d_X, d_par], writes=[d_dst])
                for j in range(1, CONV_K):
                    S.op("dve", lambda e, j=j: e.scalar_tensor_tensor(out=dst[:], in0=X[:, j:j + T], scalar=cwt[:, c16, j:j + 1],
                                                                        in1=dst[:], op0=ALU.mult, op1=ALU.add),
                         reads=[d_X, d_par, d_dst], writes=[d_dst])
                if self.dn_sub >= 1:
                    S.op("act", lambda e: e.activation(out=dst[:], in_=dst[:], func=AF.Silu), reads=[d_dst], writes=[d_dst])
                if l2 is not None and self.dn_sub >= 2:
                    for tt in range(T // 512):
                        cs = slice(tt * 512, (tt + 1) * 512)
                        pb = 2 + tt % 2
                        S.op("act", lambda e, cs=cs: e.activation(out=sq[:], in_=dst[:, cs], func=AF.Square), reads=[d_dst], writes=[d_sq])
                        S.op("pe", lambda e, pb=pb: e.matmul(self.ps[pb][:], lhsT=self.ones[:], rhs=sq[:], start=True, stop=True),
                             reads=[d_sq, self.d_const], writes=[d_slot[pb * 4 + q] for q in range(4)])
                        if False:
                            S.op("dve", lambda e, pb=pb: e.tensor_scalar(out=rn[:], in0=self.ps[pb][:], scalar1=EPS, scalar2=-0.5, op0=ALU.add, op1=ALU.pow),
                                 reads=[d_slot[pb * 4 + q] for q in range(4)], writes=[d_rn])
                        else:
                            S.op("dve", lambda e, pb=pb: e.tensor_scalar_add(out=rn[:], in0=self.ps[pb][:], scalar1=EPS),
                                 reads=[d_slot[pb * 4 + q] for q in range(4)], writes=[d_rn])
                            S.op("act", lambda e: e.activation(out=rn[:], in_=rn[:], func=AF.Sqrt), reads=[d_rn], writes=[d_rn])
                            S.op("dve", lambda e: e.reciprocal(out=rn[:], in_=rn[:]), reads=[d_rn], writes=[d_rn])
                        S.op("dve", lambda e, cs=cs: e.scalar_tensor_tensor(out=dst[:, cs], in0=dst[:, cs], scalar=float(l2), in1=rn[:],
                                                                             op0=ALU.mult, op1=ALU.mult), reads=[d_rn, d_dst], writes=[d_dst])

            for kh in range((4 if not self.dn_fast else 1) if stop >= 2 else 0):
                conv_load(qn, d_q, O_BQ + 128 * kh, kh, l2=DKS)
                conv_load(kn, d_k, O_BK + 128 * kh, 4 + kh, l2=1.0)
                for hh in range(2):
                    h = 2 * kh + hh
                    conv_load(vT[hh], d_v[hh], O_BV + 128 * h, 8 + h)
                    S.dma("sp", zT[hh][:], self.P_d[O_BZ + 128 * h:O_BZ + 128 * (h + 1), self.c0:self.c0 + T], reads=[self.d_P], writes=[d_z[hh]])
                    S.op("act", lambda e, hh=hh: e.activation(out=zT[hh][:], in_=zT[hh][:], func=AF.Silu), reads=[d_z[hh]], writes=[d_z[hh]])
                    if self.seg_first:
                        S.op("pool", lambda e, hh=hh: e.memset(Sst[hh][0][:], 0.0), writes=[d_Sst[hh][0]])
                    else:
                        S.op("pool", lambda e, hh=hh, h=h: e.tensor_copy(out=Sst[hh][0][:], in_=self.SCARRY[:, h, :]),
                             reads=[self.d_carry], writes=[d_Sst[hh][0]])
                cur = [0, 0]
                def prep(i):
                    ts_ = slice(i * 128, (i + 1) * 128)
                    kb = i % NS
                    DV = 7
                    p_, dp_ = tslot()
                    if DV & 1:
                        S.op("pe", lambda e, p_=p_, ts_=ts_: e.transpose(p_, kn[:, ts_], self.ident[:]), reads=[d_k, self.d_const], writes=[dp_])
                        S.op("act", lambda e, p_=p_, kb=kb: e.activation(out=ktok[kb][:], in_=p_, func=AF.Copy), reads=[dp_], writes=[d_ktok[kb]])
                    pKK_, dKK_ = slot()
                    S.op("pe", lambda e, pKK_=pKK_, ts_=ts_: e.matmul(pKK_, lhsT=kn[:, ts_], rhs=kn[:, ts_], start=True, stop=True),
                         reads=[d_k], writes=[dKK_])
                    S.op("act", lambda e, pKK_=pKK_, kb=kb: e.activation(out=KKs[kb][:], in_=pKK_, func=AF.Copy), reads=[dKK_], writes=[d_KKs[kb]])
                    pQK_, dQK_ = slot()
                    S.op("pe", lambda e, pQK_=pQK_, ts_=ts_: e.matmul(pQK_, lhsT=kn[:, ts_], rhs=qn[:, ts_], start=True, stop=True),
                         reads=[d_k, d_q], writes=[dQK_])
                    S.op("dve", lambda e, pQK_=pQK_, kb=kb: e.tensor_copy(out=QKs[kb][:], in_=pQK_), reads=[dQK_], writes=[d_QKs[kb]])
                    pKK, dKK, pQK, dQK = KKs[kb][:], d_KKs[kb], QKs[kb][:], d_QKs[kb]
                    for hh in range(2 if self.dn_sub >= 11 else 0):
                        h = 2 * kh + hh
                        b = (i * 2 + hh) % NS
                        gcol = GC[:, i, h:h + 1]
                        S.op("dve", lambda e, b=b, gcol=gcol: e.tensor_scalar_mul(out=diag[b][:], in0=self.ident[:], scalar1=gcol),
                             reads=[d_g, self.d_const], writes=[d_diag[b]])
                        pG, dG = slot()
                        S.op("pe", lambda e, pG=pG, b=b: e.matmul(pG, lhsT=self.ones[:], rhs=diag[b][:], start=True, stop=True),
                             reads=[d_diag[b], self.d_const], writes=[dG])
                        S.op("dve", lambda e, pG=pG, b=b, gcol=gcol: e.tensor_scalar(out=Dm[b][:], in0=pG, scalar1=gcol, scalar2=0.0,
                                                                                      op0=ALU.subtract, op1=ALU.max),
                             reads=[dG, d_g], writes=[d_Dm[b]])
                        S.op("act", lambda e, b=b: e.activation(out=Dm[b][:], in_=Dm[b][:], func=AF.Exp, scale=-1.0), reads=[d_Dm[b]], writes=[d_Dm[b]])
                        S.op("pool", lambda e, b=b: e.tensor_tensor(out=Dm[b][:], in0=Dm[b][:], in1=ML[:], op=ALU.mult), reads=[d_Dm[b], d_m], writes=[d_Dm[b]])
                        if self.dn_sub < 12:
                            continue
                        S.op("dve", lambda e, pG=pG, b=b, gcol=gcol: e.tensor_scalar(out=DTm[b][:], in0=pG, scalar1=gcol, scalar2=0.0,
                                                                                      op0=ALU.subtract, op1=ALU.min),
                             reads=[dG, d_g], writes=[d_DTm[b]])
                        S.op("act", lambda e, b=b: e.activation(out=DTm[b][:], in_=DTm[b][:], func=AF.Exp), reads=[d_DTm[b]], writes=[d_DTm[b]])
                        S.op("pool", lambda e, b=b: e.tensor_tensor(out=DTm[b][:], in0=DTm[b][:], in1=MIT[:], op=ALU.mult), reads=[d_DTm[b], d_m], writes=[d_DTm[b]])
                        S.op("act", lambda e, pG=pG, b=b: e.activation(out=EGR[b][:], in_=pG, func=AF.Exp), reads=[dG], writes=[d_EGR[b]])
                        if self.dn_sub < 13:
                            continue
                        S.op("dve", lambda e, b=b, i=i, h=h: e.scalar_tensor_tensor(out=Lm[b][:], in0=pKK, scalar=BETA[:, i, h:h + 1], in1=Dm[b][:],
                                                                                     op0=ALU.mult, op1=ALU.mult),
                             reads=[dKK, d_g, d_Dm[b]], writes=[d_Lm[b]])
                        S.op("dve", lambda e, b=b: e.tensor_tensor(out=qkT[b][:], in0=pQK, in1=DTm[b][:], op=ALU.mult),
                             reads=[dQK, d_DTm[b]], writes=[d_qkT[b]])
                        pN, dN = tslot()
                        S.op("pe", lambda e, pN=pN, b=b: e.transpose(pN, Lm[b][:], self.ident[:]), reads=[d_Lm[b], self.d_const], writes=[dN])
                        S.op("act", lambda e, pN=pN, b=b: e.activation(out=Nm[b][:], in_=pN, func=AF.Copy), reads=[dN], writes=[d_Nm[b]])
                        if self.dn_sub < 14:
                            continue
                        S.op("dve", lambda e, b=b: e.tensor_tensor(out=PT[b][:], in0=self.ident[:], in1=Nm[b][:], op=ALU.subtract),
                             reads=[d_Nm[b], self.d_const], writes=[d_PT[b]])
                        cl, dcl, cn, dcn = Lm[b], d_Lm[b], Nm[b], d_Nm[b]
                        cp, dcp, np_, dnp = PT[b], d_PT[b], PT2[b], d_PT2[b]
                        for sstep in range(1, 6):
                            pL2, dL2 = slot()
                            S.op("pe", lambda e, pL2=pL2, cl=cl, cn=cn: e.matmul(pL2, lhsT=cn[:], rhs=cl[:], start=True, stop=True),
                                 reads=[dcl, dcn], writes=[dL2])
                            if sstep < 5:
                                pN2, dN2 = slot()
                                S.op("pe", lambda e, pN2=pN2, cl=cl, cn=cn: e.matmul(pN2, lhsT=cl[:], rhs=cn[:], start=True, stop=True),
                                     reads=[dcl, dcn], writes=[dN2])
                            if sstep % 2 == 1:
                                nl, dnl, nn, dnn = AL[b], d_AL[b], AN[b], d_AN[b]
                            else:
                                nl, dnl, nn, dnn = Lm[b], d_Lm[b], Nm[b], d_Nm[b]
                            S.op("act", lambda e, pL2=pL2, nl=nl: e.activation(out=nl[:], in_=pL2, func=AF.Copy), reads=[dL2], writes=[dnl])
                            if sstep < 5:
                                S.op("dve", lambda e, pN2=pN2, nn=nn: e.tensor_copy(out=nn[:], in_=pN2), reads=[dN2], writes=[dnn])
                            DW = 7
                            pU, dU = slot()
                            if DW & 2:
                                S.op("pe", lambda e, pU=pU, nl=nl, cp=cp: e.matmul(pU, lhsT=nl[:], rhs=cp[:], start=True, stop=True),
                                     reads=[dnl, dcp], writes=[dU])
                            if DW & 4:
                                S.op("dve", lambda e, pU=pU, cp=cp, np_=np_: e.tensor_tensor(out=np_[:], in0=pU, in1=cp[:], op=ALU.add),
                                     reads=[dU, dcp], writes=[dnp])
                            cl, dcl, cn, dcn = nl, dnl, nn, dnn
                            cp, dcp, np_, dnp = np_, dnp, cp, dcp
                        TT, dTT = cp, dcp
                        if stop < 4:
                            continue
                        pV, dV = tslot()
                        S.op("pe", lambda e, pV=pV, hh=hh, ts_=ts_: e.transpose(pV, vT[hh][:, ts_], self.ident[:]),
                             reads=[d_v[hh], self.d_const], writes=[dV])
                        S.op("dve", lambda e, pV=pV, b=b, i=i, h=h: e.tensor_scalar_mul(out=vb[b][:], in0=pV, scalar1=BETA[:, i, h:h + 1]),
                             reads=[dV, d_g], writes=[d_vb[b]])
                        S.op("pool", lambda e, b=b, kb=kb, i=i, h=h: e.tensor_scalar_mul(out=kbe[b][:], in0=ktok[kb][:], scalar1=BEG[:, i, h:h + 1]),
                             reads=[d_ktok[kb], d_g], writes=[d_kbe[b]])
                        S.op("pool", lambda e, b=b, kb=kb, i=i, h=h: e.tensor_scalar_mul(out=kdec[b][:], in0=ktok[kb][:], scalar1=KD[:, i, h:h + 1]),
                             reads=[d_ktok[kb], d_g], writes=[d_kdec[b]])
                        S.op("pool", lambda e, b=b, ts_=ts_: e.tensor_tensor(out=qdT[b][:], in0=qn[:, ts_], in1=EGR[b][:], op=ALU.mult),
                             reads=[d_q, d_EGR[b]], writes=[d_qdT[b]])
                        pu, du = slot()
                        S.op("pe", lambda e, pu=pu, TT=TT, b=b: e.matmul(pu, lhsT=TT[:], rhs=vb[b][:], start=True, stop=True),
                             reads=[dTT, d_vb[b]], writes=[du])
                        S.op("act", lambda e, pu=pu, b=b: e.activation(out=uu[b][:], in_=pu, func=AF.Copy), reads=[du], writes=[d_uu[b]])
                        pw, dw = slot()
                        S.op("pe", lambda e, pw=pw, TT=TT, b=b: e.matmul(pw, lhsT=kbe[b][:], rhs=TT[:], start=True, stop=True),
                             reads=[dTT, d_kbe[b]], writes=[dw])
                        S.op("act", lambda e, pw=pw, b=b: e.activation(out=wT[b][:], in_=pw, func=AF.Copy), reads=[dw], writes=[d_wT[b]])

                def scan(i):
                  ts_ = slice(i * 128, (i + 1) * 128)
                  for hh in range(2):
                    if True:
                        h = 2 * kh + hh
                        b = (i * 2 + hh) % NS
                        for c in range(2 if stop >= 5 else 0):
                            rs = slice(64 * c, 64 * c + 64)
                            Sc, dSc = Sst[hh][cur[hh]], d_Sst[hh][cur[hh]]
                            Sn, dSn = Sst[hh][1 - cur[hh]], d_Sst[hh][1 - cur[hh]]
                            p1, d1 = slot()
                            S.op("pe", lambda e, p1=p1, b=b, Sc=Sc: e.matmul(p1, lhsT=wT[b][:], rhs=Sc[:], start=True, stop=True),
                                 reads=[d_wT[b], dSc], writes=[d1])
                            S.op("dve", lambda e, p1=p1, b=b, rs=rs: e.tensor_tensor(out=vnew[b][rs, :], in0=uu[b][rs, :], in1=p1[rs, :], op=ALU.subtract),
                                 reads=[d1, d_uu[b]], writes=[d_vnew[b]])
                            p2, d2 = slot()
                            S.op("pe", lambda e, p2=p2, b=b, Sc=Sc: e.matmul(p2, lhsT=qdT[b][:], rhs=Sc[:], start=True, stop=False),
                                 reads=[d_qdT[b], dSc], writes=[d2])
                            S.op("pe", lambda e, p2=p2, b=b, rs=rs: e.matmul(p2, lhsT=qkT[b][rs, :], rhs=vnew[b][rs, :], start=False, stop=True),
                                 reads=[d_qkT[b], d_vnew[b]], writes=[d2])
                            S.op("act", lambda e, p2=p2, b=b, rs=rs: e.activation(out=otok[b][rs, :], in_=p2[rs, :], func=AF.Copy),
                                 reads=[d2], writes=[d_otok[b]])
                            p3, d3 = slot()
                            S.op("pe", lambda e, p3=p3, b=b, rs=rs: e.matmul(p3, lhsT=kdec[b][rs, :], rhs=vnew[b][rs, :], start=True, stop=True),
                                 reads=[d_kdec[b], d_vnew[b]], writes=[d3])
                            S.op("dve", lambda e, p3=p3, Sc=Sc, Sn=Sn, i=i, c=c, h=h: e.scalar_tensor_tensor(
                                out=Sn[:], in0=Sc[:], scalar=EGL[:, i, c, h:h + 1], in1=p3, op0=ALU.mult, op1=ALU.add),
                                reads=[d3, dSc, d_g], writes=[dSn])
                            cur[hh] = 1 - cur[hh]
                        S.op("act", lambda e, b=b: e.activation(out=ojunk[b][:], in_=otok[b][:], func=AF.Square, accum_out=ost[b][:, 0:1]),
                             reads=[d_otok[b]], writes=[d_ojunk[b], d_ost[b]])
                        S.op("act", lambda e, b=b: e.activation(out=ost[b][:, 1:2], in_=ost[b][:, 0:1], func=AF.Sqrt, scale=1.0 / DV_B,
                                                                 bias=self.epsc[:, 0:1]), reads=[d_ost[b], self.d_const], writes=[d_ost[b]])
                        S.op("dve", lambda e, b=b: e.reciprocal(out=ost[b][:, 2:3], in_=ost[b][:, 1:2]), reads=[d_ost[b]], writes=[d_ost[b]])
                        S.op("pool", lambda e, b=b: e.tensor_scalar_mul(out=otok[b][:], in0=otok[b][:], scalar1=ost[b][:, 2:3]),
                             reads=[d_ost[b], d_otok[b]], writes=[d_otok[b]])
                        pO, dO = tslot()
                        S.op("pe", lambda e, pO=pO, b=b: e.transpose(pO, otok[b][:], self.ident[:]), reads=[d_otok[b], self.d_const], writes=[dO])
                        S.op("dve", lambda e, pO=pO, hh=hh, h=h, ts_=ts_: e.scalar_tensor_tensor(
                            out=self.actT[:, 4 + h, ts_], in0=pO, scalar=ngc[:, 0:1], in1=zT[hh][:, ts_], op0=ALU.mult, op1=ALU.mult),
                            reads=[dO, d_par, d_z[hh]], writes=[self.d_act[i]])
                ntl = (NT if not self.dn_fast else 2) if stop >= 3 else 0
                if ntl > 0:
                    prep(0)
                for i in range(ntl):
                    if i + 1 < ntl:
                        prep(i + 1)
                    if stop >= 4 and self.dn_sub >= 14:
                        scan(i)
                for hh in range(2):
                    h = 2 * kh + hh
                    S.op("pool", lambda e, hh=hh, h=h, cc=cur[hh]: e.tensor_copy(out=self.SCARRY[:, h, :], in_=Sst[hh][cc][:]),
                         reads=[d_Sst[hh][cur[hh]]], writes=[self.d_carry])
        S.barrier()

    def phase_outproj(self, l):
        S, I = self.S, self.I
        T, NT = self.T, self.NT
        wv = I["w_out"][l].rearrange("(kc p) n -> p kc n", p=128)
        with ExitStack() as ph:
            NB = 2
            W = [self.sb(ph, f"opW{i}", [128, KC, 512], BF16) for i in range(NB)]
            dW = S.deps(NB)
            Gp = [self.sb(ph, f"opG{i}", [128, 512], F32) for i in range(NB)]
            xt = [self.sb(ph, f"opx{i}", [128, 512], F32) for i in range(NB)]
            dx = S.deps(NB)
            tt_ = [self.sb(ph, f"opt{i}", [128, 512], F32) for i in range(NB)]
            dt_ = S.deps(NB)
            cnt = 0
            for cg in range(4):
                cs = slice(cg * 512, (cg + 1) * 512)
                wb = cg % NB
                S.dma("pool", W[wb][:], wv[:, :, cs], writes=[dW[wb]])
                S.dma("sp", Gp[wb][:], self.mod_d[self.bi, l, 2, :, cs], reads=[self.d_mod], writes=[dW[wb]])
                for i in range(NT):
                    b = cnt % NB
                    pb = cnt % 8
                    cnt += 1
                    rows = slice(self.r0 + i * 128, self.r0 + (i + 1) * 128)
                    for kc in range(KC):
                        S.op("pe", lambda e, kc=kc, i=i, wb=wb, pb=pb: e.matmul(self.ps[pb][:], lhsT=self.actT[:, kc, i * 128:(i + 1) * 128],
                                                                              rhs=W[wb][:, kc, :], start=(kc == 0), stop=(kc == KC - 1)),
                             reads=[dW[wb], self.d_act[i]], writes=[self.d_ps[pb]])
                    S.dma("sp", xt[b][:], self.x_d[rows, cs], reads=[self.d_xt[i]], writes=[dx[b]])
                    S.op("dve", lambda e, b=b, wb=wb, pb=pb: e.tensor_tensor(out=tt_[b][:], in0=self.ps[pb][:], in1=Gp[wb][:], op=ALU.mult),
                         reads=[self.d_ps[pb], dW[wb]], writes=[dt_[b]])
                    S.op("pool", lambda e, b=b: e.tensor_tensor(out=xt[b][:], in0=xt[b][:], in1=tt_[b][:], op=ALU.add),
                         reads=[dt_[b], dx[b]], writes=[dx[b]])
                    S.dma("sp", self.x_d[rows, cs], xt[b][:], reads=[dx[b]], writes=[self.d_xt[i]])
        S.barrier()

    def phase_moe(self, l):
        S, I, nc = self.S, self.I, self.nc
        T, NT, NBLK = self.T, self.NT, self.NBLK
        with ExitStack() as mo:
            router = {}
            RW = self.sb(mo, "RW", [128, KC, 36], F32)
            RB = self.sb(mo, "RB", [128, 36], F32)
            LG = self.sb(mo, "LG", [128, NT, 36], F32)
            router["RW"], router["RB"], router["LG"] = RW, RB, LG
            router["d_rw"], router["d_lg"] = S.dep(), S.dep()
            router["hbf"] = [self.sb(mo, f"hbf{i}", [128, D], BF16) for i in range(2)]
            router["d_hbf"] = S.deps(2)
            S.dma("sp", RW[:], I["rw"][l].rearrange("(kc p) n -> p kc n", p=128), writes=[router["d_rw"]])
            S.dma("sp", RB[:], I["rb"][l:l + 1, :].partition_broadcast(128), writes=[router["d_rw"]])
            self.phase_norm(l, 2, router=router)
            d_r = router["d_lg"]
            cnt = {"n": 0}

            def small(name, shape, dt=F32):
                return self.sb(mo, name, shape, dt)

            gmax = small("gmax", [128, NT])
            goh = small("goh", [128, NT, 4])
            gex = small("gex", [128, NT, 4])
            pg = small("pg", [128, NT])
            esel = small("esel", [128, NT, 8])
            etmp = small("etmp", [128, NT, 8])
            v1 = small("v1", [128, NT])
            v2 = small("v2", [128, NT])
            oh1 = small("oh1", [128, NT, 8])
            oh2 = small("oh2", [128, NT, 8])
            g0 = small("g0", [128, NT])
            g1 = small("g1", [128, NT])
            OH1 = small("OH1", [128, NT, 32])
            OH2 = small("OH2", [128, NT, 32])
            OH = small("OH", [128, NT, 32])
            glog = LG[:, :, 0:4]
            elog4 = LG[:, :, 4:36].rearrange("p n (g j) -> p n g j", g=4)

            def dv(fn, eng="dve"):
                S.op(eng, fn, reads=[d_r, self.d_const], writes=[d_r])

            dv(lambda e: e.tensor_reduce(out=gmax[:], in_=glog, axis=AX.X, op=ALU.max))
            dv(lambda e: e.tensor_tensor(out=goh[:], in0=glog, in1=gmax[:].unsqueeze(2).broadcast_to([128, NT, 4]), op=ALU.is_equal))
            dv(lambda e: e.tensor_tensor(out=gex[:], in0=glog, in1=gmax[:].unsqueeze(2).broadcast_to([128, NT, 4]), op=ALU.subtract))
            dv(lambda e: e.activation(out=gex[:], in_=gex[:], func=AF.Exp), "act")
            dv(lambda e: e.tensor_reduce(out=pg[:], in_=gex[:], axis=AX.X, op=ALU.add))
            dv(lambda e: e.reciprocal(out=pg[:], in_=pg[:]))
            for g in range(4):
                if g == 0:
                    dv(lambda e: e.tensor_tensor(out=esel[:], in0=elog4[:, :, 0, :], in1=goh[:, :, 0:1].broadcast_to([128, NT, 8]), op=ALU.mult))
                else:
                    dv(lambda e, g=g: e.tensor_tensor(out=etmp[:], in0=elog4[:, :, g, :], in1=goh[:, :, g:g + 1].broadcast_to([128, NT, 8]), op=ALU.mult))
                    dv(lambda e: e.tensor_tensor(out=esel[:], in0=esel[:], in1=etmp[:], op=ALU.add))
            dv(lambda e: e.tensor_reduce(out=v1[:], in_=esel[:], axis=AX.X, op=ALU.max))
            dv(lambda e: e.tensor_tensor(out=oh1[:], in0=esel[:], in1=v1[:].unsqueeze(2).broadcast_to([128, NT, 8]), op=ALU.is_equal))
            dv(lambda e: e.scalar_tensor_tensor(out=etmp[:], in0=oh1[:], scalar=NEG, in1=esel[:], op0=ALU.mult, op1=ALU.add))
            dv(lambda e: e.tensor_reduce(out=v2[:], in_=etmp[:], axis=AX.X, op=ALU.max))
            dv(lambda e: e.tensor_tensor(out=oh2[:], in0=etmp[:], in1=v2[:].unsqueeze(2).broadcast_to([128, NT, 8]), op=ALU.is_equal))
            dv(lambda e: e.tensor_tensor(out=g1[:], in0=v2[:], in1=v1[:], op=ALU.subtract))
            dv(lambda e: e.activation(out=g1[:], in_=g1[:], func=AF.Exp), "act")
            dv(lambda e: e.tensor_scalar_add(out=g0[:], in0=g1[:], scalar1=1.0))
            dv(lambda e: e.reciprocal(out=g0[:], in_=g0[:]))
            dv(lambda e: e.tensor_tensor(out=g1[:], in0=g1[:], in1=g0[:], op=ALU.mult))
            dv(lambda e: e.tensor_tensor(out=g0[:], in0=g0[:], in1=pg[:], op=ALU.mult))
            dv(lambda e: e.tensor_tensor(out=g1[:], in0=g1[:], in1=pg[:], op=ALU.mult))
            for (OHk, ohk) in ((OH1, oh1), (OH2, oh2)):
                for g in range(4):
                    dv(lambda e, OHk=OHk, ohk=ohk, g=g: e.tensor_tensor(out=OHk[:, :, g * 8:(g + 1) * 8], in0=ohk[:],
                                                                        in1=goh[:, :, g:g + 1].broadcast_to([128, NT, 8]), op=ALU.mult))
            dv(lambda e: e.tensor_tensor(out=OH[:], in0=OH1[:], in1=OH2[:], op=ALU.add))
            Ust = small("Ust", [128, 128])
            dv(lambda e: e.memset(Ust[:], 1.0), "pool")
            dv(lambda e: e.affine_select(out=Ust[:], in_=Ust[:], pattern=[[1, 128]], compare_op=ALU.is_ge,
                                         fill=self.reg_zero, base=-1, channel_multiplier=-1), "pool")
            TRIU = small("TRIU", [32, 32])
            dv(lambda e: e.memset(TRIU[:], 1.0), "pool")
            dv(lambda e: e.affine_select(out=TRIU[:], in_=TRIU[:], pattern=[[1, 32]], compare_op=ALU.is_ge,
                                         fill=self.reg_zero, base=0, channel_multiplier=-1), "pool")
            CUM = small("CUM", [128, NT + 1, 32])
            RANK = small("RANK", [128, NT, 32])
            dv(lambda e: e.memset(CUM[:, 0, :], 0.0), "pool")
            for i in range(NT):
                pb = 4 + (i % 2)
                S.op("pe", lambda e, i=i, pb=pb: e.matmul(self.ps[pb][:, 0:32], lhsT=self.ones[:], rhs=OH[:, i, :], start=True, stop=True),
                     reads=[d_r, self.d_const], writes=[self.d_ps[pb]])
                S.op("dve", lambda e, i=i, pb=pb: e.tensor_tensor(out=CUM[:, i + 1, :], in0=CUM[:, i, :], in1=self.ps[pb][:, 0:32], op=ALU.add),
                     reads=[self.d_ps[pb], d_r], writes=[d_r])
                pb2 = 6 + (i % 2)
                S.op("pe", lambda e, i=i, pb2=pb2: e.matmul(self.ps[pb2][:, 0:32], lhsT=Ust[:], rhs=OH[:, i, :], start=True, stop=True),
                     reads=[d_r], writes=[self.d_ps[pb2]])
                S.op("dve", lambda e, i=i, pb2=pb2: e.tensor_tensor(out=RANK[:, i, :], in0=CUM[:, i, :], in1=self.ps[pb2][:, 0:32], op=ALU.add),
                     reads=[self.d_ps[pb2], d_r], writes=[d_r])
            THRi = small("THRi", [128, NT], I32)
            THR = small("THR", [128, NT])
            dv(lambda e: e.iota(THRi[:], pattern=[[128, NT]], base=0, channel_multiplier=0), "pool")
            dv(lambda e: e.tensor_copy(out=THR[:], in_=THRi[:]), "pool")
            CMP = small("CMP", [128, 32, NT])
            PADD = small("PADD", [128, 32])
            dv(lambda e: e.tensor_tensor(out=CMP[:], in0=CUM[:, NT, :].unsqueeze(2).broadcast_to([128, 32, NT]),
                                         in1=THR[:].unsqueeze(1).broadcast_to([128, 32, NT]), op=ALU.is_gt))
            dv(lambda e: e.tensor_reduce(out=PADD[:], in_=CMP[:], axis=AX.X, op=ALU.add))
            dv(lambda e: e.tensor_scalar_mul(out=PADD[:], in0=PADD[:], scalar1=128.0))
            paddT = small("paddT", [32, 128])
            S.op("pe", lambda e: e.transpose(self.ps[0][0:32, 0:128], PADD[:], self.ident[:]), reads=[d_r, self.d_const], writes=[self.d_ps[0]])
            S.op("act", lambda e: e.activation(out=paddT[:], in_=self.ps[0][0:32, 0:128], func=AF.Copy), reads=[self.d_ps[0]], writes=[d_r])
            PEND = small("PEND", [128, 32])
            S.op("pe", lambda e: e.matmul(self.ps[4][:, 0:32], lhsT=paddT[:], rhs=TRIU[:], start=True, stop=True), reads=[d_r], writes=[self.d_ps[4]])
            S.op("dve", lambda e: e.tensor_copy(out=PEND[:], in_=self.ps[4][:, 0:32]), reads=[self.d_ps[4]], writes=[d_r])
            PST = small("PST", [128, 32])
            dv(lambda e: e.tensor_tensor(out=PST[:], in0=PEND[:], in1=PADD[:], op=ALU.subtract))
            dv(lambda e: e.tensor_tensor(out=RANK[:], in0=RANK[:], in1=PST[:].unsqueeze(1).broadcast_to([128, NT, 32]), op=ALU.add))
            DSTf = small("DSTf", [128, 2, NT])
            DSTi = small("DSTi", [128, 2, NT], I32)
            for k, OHk in enumerate((OH1, OH2)):
                dv(lambda e, OHk=OHk: e.tensor_tensor(out=OHk[:], in0=OHk[:], in1=RANK[:], op=ALU.mult))
                dv(lambda e, OHk=OHk, k=k: e.tensor_reduce(out=DSTf[:, k, :], in_=OHk[:], axis=AX.X, op=ALU.add))
            dv(lambda e: e.tensor_copy(out=DSTi[:], in_=DSTf[:]))
            pendc = small("pendc", [32, 1])
            S.op("pe", lambda e: e.matmul(self.ps[5][0:32, 0:1], lhsT=TRIU[:], rhs=paddT[:, 0:1], start=True, stop=True), reads=[d_r], writes=[self.d_ps[5]])
            S.op("dve", lambda e: e.tensor_copy(out=pendc[:], in_=self.ps[5][0:32, 0:1]), reads=[self.d_ps[5]], writes=[d_r])
            BVi = small("BVi", [32, NBLK], I32)
            BV = small("BV", [32, NBLK])
            dv(lambda e: e.iota(BVi[:], pattern=[[128, NBLK]], base=0, channel_multiplier=0), "pool")
            dv(lambda e: e.tensor_copy(out=BV[:], in_=BVi[:]), "pool")
            dv(lambda e: e.tensor_scalar(out=BV[:], in0=BV[:], scalar1=pendc[:, 0:1], scalar2=None, op0=ALU.is_ge))
            BEf = small("BEf", [1, NBLK])
            BEi = small("BEi", [1, NBLK], I32)
            S.op("pe", lambda e: e.matmul(self.ps[6][0:1, 0:NBLK], lhsT=self.ones[0:32, 0:1], rhs=BV[:], start=True, stop=True),
                 reads=[d_r, self.d_const], writes=[self.d_ps[6]])
            S.op("dve", lambda e: e.tensor_scalar_min(out=BEf[:], in0=self.ps[6][0:1, 0:NBLK], scalar1=float(N_EXP - 1)), reads=[self.d_ps[6]], writes=[d_r])
            dv(lambda e: e.tensor_copy(out=BEi[:], in_=BEf[:]))
            BEbc = small("BEbc", [128, NBLK])
            S.op("pe", lambda e: e.matmul(self.ps[7][:, 0:NBLK], lhsT=self.ones[0:32, :], rhs=BV[:], start=True, stop=True),
                 reads=[d_r, self.d_const], writes=[self.d_ps[7]])
            S.op("dve", lambda e: e.tensor_scalar_min(out=BEbc[:], in0=self.ps[7][:, 0:NBLK], scalar1=float(N_EXP - 1)), reads=[self.d_ps[7]], writes=[d_r])
            PKi = small("PKi", [128, 1], I32)
            PK = small("PK", [128, 1])
            dv(lambda e: e.iota(PKi[:], pattern=[[0, 1]], base=0, channel_multiplier=1), "pool")
            dv(lambda e: e.tensor_copy(out=PK[:], in_=PKi[:]), "pool")
            WIDXf = small("WIDXf", [128, NBLK])
            WIDX = small("WIDX", [128, NBLK], I32)
            dv(lambda e: e.tensor_scalar_add(out=BEbc[:], in0=BEbc[:], scalar1=float(l * N_EXP)))
            dv(lambda e: e.tensor_scalar(out=WIDXf[:], in0=BEbc[:], scalar1=128.0, scalar2=PK[:, 0:1], op0=ALU.mult, op1=ALU.add))
            dv(lambda e: e.tensor_copy(out=WIDX[:], in_=WIDXf[:]))
            wg_rows = I["w_gate"].rearrange("l e (p j) n -> (l e p) (j n)", j=KC)
            wu_rows = I["w_up"].rearrange("l e (p j) n -> (l e p) (j n)", j=KC)
            wd_rows = I["w_down"].rearrange("l e (p j) n -> (l e p) (j n)", j=4)
            if "dbg_route" in self.phases:
                t1_ = self.dram("dbg_dst", [128, 2, NT], I32, kind="ExternalOutput").ap()
                t2_ = self.dram("dbg_be", [1, NBLK], I32, kind="ExternalOutput").ap()
                t3_ = self.dram("dbg_g", [128, 2, NT], F32, kind="ExternalOutput").ap()
                self.out_events.append(S.dma("sp", t1_, DSTi[:], reads=[d_r]))
                self.out_events.append(S.dma("sp", t2_, BEi[:], reads=[d_r]))
                gg_ = small("gg_", [128, 2, NT])
                dv(lambda e: e.tensor_copy(out=gg_[:, 0, :], in_=g0[:]))
                dv(lambda e: e.tensor_copy(out=gg_[:, 1, :], in_=g1[:]))
                self.out_events.append(S.dma("sp", t3_, gg_[:], reads=[d_r]))
            S.barrier()
            with ExitStack() as ph:
                hrow = [self.sb(ph, f"hrow{i}", [128, D], BF16) for i in range(2)]
                dhr = S.deps(2)
                for i in range(NT):
                    b = i % 2
                    S.dma("sp", hrow[b][:], self.h2_d[i * 128:(i + 1) * 128, :], reads=[self.d_h2], writes=[dhr[b]])
                    for k in range(2):
                        S.dma("pool", None, None, reads=[dhr[b], d_r], writes=[self.d_rows],
                              fn=lambda e, b=b, k=k, i=i: e.indirect_dma_start(
                                  out=self.rows_d[:, :], out_offset=bass.IndirectOffsetOnAxis(ap=DSTi[:, k, i:i + 1], axis=0),
                                  in_=hrow[b][:, :], in_offset=None))
            S.barrier()
            with ExitStack() as ph:
                identb = self.sb(ph, "identb2", [128, 128], BF16)
                d_ib = S.dep()
                S.op("dve", lambda e: e.tensor_copy(out=identb[:], in_=self.ident[:]), reads=[self.d_const], writes=[d_ib])
                NW = 2
                Wg = [self.sb(ph, f"Wg{i}", [128, KC, D_FF], BF16) for i in range(NW)]
                Wu = [self.sb(ph, f"Wu{i}", [128, KC, D_FF], BF16) for i in range(NW)]
                Wd = [self.sb(ph, f"Wd{i}", [128, 4, D], BF16) for i in range(NW)]
                dWt = S.deps(NW)
                rowsb = [self.sb(ph, f"rowsb{i}", [128, D], BF16) for i in range(2)]
                d_rb = S.deps(2)
                blkT = [self.sb(ph, f"blkT{i}", [128, KC, 128], BF16) for i in range(2)]
                d_bT = S.deps(2)
                sg = self.sb(ph, "sgate", [128, D_FF], F32)
                d_sg = S.dep()
                hid = self.sb(ph, "hid", [128, D_FF], BF16)
                d_hid = S.dep()
                hidT = self.sb(ph, "hidT", [128, 4, 128], BF16)
                d_hT = S.dep()
                yb = [self.sb(ph, f"yb{i}", [128, D], F32) for i in range(2)]
                d_yb = S.deps(2)
                psT = [self.ps[2].bitcast(BF16), self.ps[3].bitcast(BF16)]
                for blk in range(NBLK):
                    b = blk % 2
                    wb = blk % NW
                    S.dma("sp", rowsb[b][:], self.rows_d[blk * 128:(blk + 1) * 128, :], reads=[self.d_rows], writes=[d_rb[b]])
                    for (dst, srcv) in ((Wg[wb], wg_rows), (Wu[wb], wu_rows), (Wd[wb], wd_rows)):
                        S.dma("pool", None, None, reads=[d_r], writes=[dWt[wb]],
                              fn=lambda e, dst=dst, srcv=srcv, blk=blk: e.indirect_dma_start(
                                  out=dst[:].rearrange("p j n -> p (j n)"), out_offset=None, in_=srcv,
                                  in_offset=bass.IndirectOffsetOnAxis(ap=WIDX[:, blk:blk + 1], axis=0)))
                    for q4 in range(4):
                        pt = psT[q4 % 2]
                        dpt = self.d_ps[2 + q4 % 2]
                        for j in range(4):
                            kc = q4 * 4 + j
                            S.op("pe", lambda e, pt=pt, b=b, kc=kc, j=j: e.transpose(pt[:, j * 128:(j + 1) * 128], rowsb[b][:, sl(kc, 128, KC)], identb[:]),
                                 reads=[d_rb[b], d_ib], writes=[dpt])
                        if q4 % 2 == 0:
                            S.op("act", lambda e, pt=pt, b=b, q4=q4: e.activation(out=blkT[b][:, q4 * 4:(q4 + 1) * 4, :],
                                                                                   in_=pt[:, 0:512].rearrange("p (j n) -> p j n", j=4), func=AF.Copy),
                                 reads=[dpt], writes=[d_bT[b]])
                        else:
                            S.op("dve", lambda e, pt=pt, b=b, q4=q4: e.tensor_copy(out=blkT[b][:, q4 * 4:(q4 + 1) * 4, :],
                                                                                    in_=pt[:, 0:512].rearrange("p (j n) -> p j n", j=4)),
                                 reads=[dpt], writes=[d_bT[b]])
                    for (pb, Wt) in ((0, Wg[wb]), (1, Wu[wb])):
                        for kc in range(KC):
                            S.op("pe", lambda e, pb=pb, Wt=Wt, kc=kc, b=b: e.matmul(self.ps[pb][:], lhsT=blkT[b][:, kc, :], rhs=Wt[:, kc, :],
                                                                                     start=(kc == 0), stop=(kc == KC - 1)),
                                 reads=[d_bT[b], dWt[wb]], writes=[self.d_ps[pb]])
                    S.op("act", lambda e: e.activation(out=sg[:], in_=self.ps[0][:], func=AF.Silu), reads=[self.d_ps[0]], writes=[d_sg])
                    S.op("dve", lambda e: e.tensor_tensor(out=hid[:], in0=sg[:], in1=self.ps[1][:], op=ALU.mult),
                         reads=[d_sg, self.d_ps[1]], writes=[d_hid])
                    pt, dpt = psT[0], self.d_ps[2]
                    for f in range(4):
                        S.op("pe", lambda e, f=f, pt=pt: e.transpose(pt[:, f * 128:(f + 1) * 128], hid[:, sl(f, 128, 4)], identb[:]),
                             reads=[d_hid, d_ib], writes=[dpt])
                    S.op("act", lambda e, pt=pt: e.activation(out=hidT[:], in_=pt[:, 0:512].rearrange("p (j n) -> p j n", j=4), func=AF.Copy),
                         reads=[dpt], writes=[d_hT])
                    for cgp in range(4):
                        pb = 4 + cgp
                        for f in range(4):
                            S.op("pe", lambda e, pb=pb, f=f, cgp=cgp, wb=wb: e.matmul(self.ps[pb][:], lhsT=hidT[:, f, :],
                                                                                       rhs=Wd[wb][:, f, cgp * 512:(cgp + 1) * 512],
                                                                                       start=(f == 0), stop=(f == 3)),
                                 reads=[d_hT, dWt[wb]], writes=[self.d_ps[pb]])
                        if cgp % 2 == 0:
                            S.op("act", lambda e, pb=pb, cgp=cgp, b=b: e.activation(out=yb[b][:, cgp * 512:(cgp + 1) * 512], in_=self.ps[pb][:], func=AF.Copy),
                                 reads=[self.d_ps[pb]], writes=[d_yb[b]])
                        else:
                            S.op("dve", lambda e, pb=pb, cgp=cgp, b=b: e.tensor_copy(out=yb[b][:, cgp * 512:(cgp + 1) * 512], in_=self.ps[pb][:]),
                                 reads=[self.d_ps[pb]], writes=[d_yb[b]])
                    S.dma("sp", self.yrows_d[blk * 128:(blk + 1) * 128, :], yb[b][:], reads=[d_yb[b]], writes=[self.d_yrows])
            S.barrier()
            with ExitStack() as ph:
                G2 = self.sb(ph, "G2bc", [128, D], F32)
                d_G2 = S.dep()
                S.dma("sp", G2[:], self.mod_d[self.bi, l, 5], reads=[self.d_mod], writes=[d_G2])
                y0 = [self.sb(ph, f"y0_{i}", [128, D], F32) for i in range(2)]
                y1 = [self.sb(ph, f"y1_{i}", [128, D], F32) for i in range(2)]
                xt = [self.sb(ph, f"cx{i}", [128, D], F32) for i in range(2)]
                d_y0, d_y1, d_cx = S.deps(2), S.deps(2), S.deps(2)
                for i in range(NT):
                    b = i % 2
                    rows = slice(self.r0 + i * 128, self.r0 + (i + 1) * 128)
                    for (yt, dy, k) in ((y0[b], d_y0[b], 0), (y1[b], d_y1[b], 1)):
                        S.dma("pool", None, None, reads=[self.d_yrows, d_r], writes=[dy],
                              fn=lambda e, yt=yt, k=k, i=i: e.indirect_dma_start(
                                  out=yt[:, :], out_offset=None, in_=self.yrows_d[:, :],
                                  in_offset=bass.IndirectOffsetOnAxis(ap=DSTi[:, k, i:i + 1], axis=0)))
                    S.dma("sp", xt[b][:], self.x_d[rows, :], reads=[self.d_xt[i]], writes=[d_cx[b]])
                    S.op("dve", lambda e, b=b, i=i: e.tensor_scalar_mul(out=y0[b][:], in0=y0[b][:], scalar1=g0[:, i:i + 1]),
                         reads=[d_y0[b], d_r], writes=[d_y0[b]])
                    S.op("dve", lambda e, b=b, i=i: e.scalar_tensor_tensor(out=y0[b][:], in0=y1[b][:], scalar=g1[:, i:i + 1], in1=y0[b][:],
                                                                           op0=ALU.mult, op1=ALU.add),
                         reads=[d_y0[b], d_y1[b], d_r], writes=[d_y0[b]])
                    S.op("pool", lambda e, b=b: e.tensor_tensor(out=y0[b][:], in0=y0[b][:], in1=G2[:], op=ALU.mult),
                         reads=[d_y0[b], d_G2], writes=[d_y0[b]])
                    S.op("pool", lambda e, b=b: e.tensor_tensor(out=xt[b][:], in0=xt[b][:], in1=y0[b][:], op=ALU.add),
                         reads=[d_y0[b], d_cx[b]], writes=[d_cx[b]])
                    S.dma("sp", self.x_d[rows, :], xt[b][:], reads=[d_cx[b]], writes=[self.d_xt[i]])
        S.barrier()

    def dump_act(self):
        S = self.S
        t = self.dram("act_dump", [128, KC, self.T], BF16, kind="ExternalOutput").ap()
        self.out_events.append(S.dma("sp", t, self.actT[:], reads=self.d_act))

    def build(self):
        self.declare()
        self.consts()
        self.phase_init()
        P = self.phases
        if "ada" in P:
            for b in range(self.NB):
                self.bi = b
                self.phase_ada()
        for l in range(self.L):
            for b in range(self.NB):
                for g in range(self.NSEG):
                    self.bi = b
                    self.r0 = (b * self.NSEG + g) * self.T
                    self.c0 = TP + g * self.T
                    self.seg_first = (g == 0)
                    with ExitStack() as mx:
                        self.actT = self.sb(mx, "actT", [128, KC, self.T], BF16)
                        if "norm1" in P:
                            self.phase_norm(l, 1)
                        if "proj" in P:
                            self.phase_proj(l)
                        if "attn_c" in P:
                            self.phase_attn_c(l)
                        if "attn_a" in P:
                            self.phase_attn_a(l)
                        if "dn" in P:
                            self.phase_dn(l)
                        if "dump_act" in P:
                            self.dump_act()
                        if "outproj" in P:
                            self.phase_outproj(l)
                        self.S.barrier()
                if "moe" in P:
                    T_, NT_ = self.T, self.NT
                    self.T, self.NT = self.TM, self.NTM
                    self.r0 = b * self.NSEG * T_
                    self.phase_moe(l)
                    self.T, self.NT = T_, NT_
        if "final" in P:
            for s_ in range(self.NB * self.NSEG):
                self.r0 = s_ * self.T
                self.phase_norm(0, "final")
        if "dump_x" in P:
            t = self.dram("x_dump", [self.NB * self.NSEG * self.T, D], F32, kind="ExternalOutput").ap()
            self.out_events.append(self.S.dma("sp", t, self.x_d, reads=[self.d_x] + self.d_xt))
        self.S.barrier()
        self.S.finish(self.out_events)
        self.st.close()


def host_inputs(inputs, batches, L, S_full=None):
    f = lambda a: np.ascontiguousarray(np.asarray(a, dtype=np.float32))
    m = {}
    xs = np.asarray(inputs["x"])
    m["x"] = f(np.concatenate([xs[b] for b in batches], axis=0))
    cc = np.asarray(inputs["c"])
    m["cT"] = f(np.stack([cc[b].reshape(KC, 128).T for b in batches], axis=0))
    m["norm1_g"] = f(inputs["norm1_g"][:L])
    m["norm2_g"] = f(inputs["norm2_g"][:L])
    m["ada_w"] = f(inputs["ada_w"][:L])
    m["ada_b"] = f(inputs["ada_b"][:L])
    m["w_in"] = f(inputs["w_in"][:L])
    cw = np.asarray(inputs["dn_conv_w"])[:L]
    m["conv_w"] = f(cw.transpose(2, 0, 1).reshape(16, 128, L, CONV_K).transpose(1, 2, 0, 3))
    m["a_log"] = f(np.asarray(inputs["dn_a_log"])[:L].reshape(1, L * 8))
    m["dt_bias"] = f(np.asarray(inputs["dn_dt_bias"])[:L].reshape(1, L * 8))
    m["dn_norm_g"] = f(np.asarray(inputs["dn_norm_g"])[:L].reshape(L, 128, 1))
    m["sinks"] = f(np.asarray(inputs["attn_sinks"])[:L].reshape(1, L * 8))
    m["w_out"] = f(inputs["w_out"][:L])
    m["rw"] = f(np.concatenate([np.asarray(inputs["router_group_w"])[:L], np.asarray(inputs["router_expert_w"])[:L]], axis=-1))
    m["rb"] = f(np.concatenate([np.asarray(inputs["router_group_b"])[:L], np.asarray(inputs["router_expert_b"])[:L]], axis=-1))
    if "expert_w_gate" in inputs:
        m["w_gate"] = f(inputs["expert_w_gate"][:L])
        m["w_up"] = f(inputs["expert_w_up"][:L])
        m["w_down"] = f(inputs["expert_w_down"][:L])
    m["final_g"] = f(np.asarray(inputs["final_norm_g"]).reshape(1, D))
    return m


ALL_PHASES = ("ada", "norm1", "proj", "attn_c", "attn_a", "dn", "outproj", "moe", "final")
N_CORES_USED = 4
SEG_T = 2048


def kernel(**inputs):
    x = np.asarray(inputs["x"])
    Bsz, S_full, _ = x.shape
    L = int(np.asarray(inputs["w_in"]).shape[0])
    nseg = S_full // SEG_T
    nb = Bsz // N_CORES_USED
    nc = bass.Bass("TRN2", target_bir_lowering=False)
    bld = Builder(nc, SEG_T, L, phases=ALL_PHASES, NB=nb, NSEG=nseg)
    bld.build()
    in_maps = []
    for c in range(N_CORES_USED):
        m = host_inputs(inputs, list(range(c * nb, (c + 1) * nb)), L)
        in_maps.append({k: v for k, v in m.items() if k in bld.I})
    res = run_bass_kernel_spmd(nc, in_maps, core_ids=list(range(N_CORES_USED)))
    outs = [np.asarray(res.results[c]["y_out"], dtype=np.float32).reshape(nb, S_full, D) for c in range(N_CORES_USED)]
    return np.concatenate(outs, axis=0)
```
